# Optimizing a Trainium2 kernel written in Bass

```python
import jax
import jax.numpy as jnp
from jax import lax
import numpy as np

D_MODEL = 2048
BATCH = 8
SEQ = 2048
DEPTH = 1

NSA_HEADS = 16
NSA_KV_GROUPS = 4
NSA_HPG = NSA_HEADS // NSA_KV_GROUPS
NSA_DH = 64
NSA_Q = NSA_HEADS * NSA_DH
NSA_KV = NSA_KV_GROUPS * NSA_DH
CMP_STRIDE = 16
CMP_LEN = 2 * CMP_STRIDE
CMP_HID = 256
SEL_LEN = 64
SEL_TOP = 16
WIN = 512
WIN_QBLK = 128
SEL_QBLK = 32
FORCE = 1e3
NEG = -1e9
M_HEADS = 4
M_DQK = 128
M_DV = 256
M_QK = M_HEADS * M_DQK
M_V = M_HEADS * M_DV
M_CHUNK = 64
CONV_W = 4
N_EXPERTS = 64
TOP_K = 8
N_GROUPS = 8
TOP_GROUPS = 4
EXPERT_FF = 512
SHARED_FF = 512
ROUTE_SCALE = 2.5
MOE_BLK = 128
EPS = 1e-6

IN_SIZES = (NSA_Q, 6 * NSA_KV, 3 * NSA_HEADS, 2 * M_QK, M_V, 2 * M_HEADS, M_V, D_MODEL, D_MODEL)
D_IN = sum(IN_SIZES)

kernel_name = 'hybrid_nsa_mlstm_moe_layer'


def rmsnorm(x, g):
    xf = x.astype(jnp.float32)
    y = xf * lax.rsqrt(jnp.mean(xf * xf, axis=-1, keepdims=True) + EPS)
    return (y * g.astype(jnp.float32)).astype(x.dtype)


def masked_softmax(s, mask):
    s = jnp.where(mask, s.astype(jnp.float32), NEG)
    return jax.nn.softmax(s, axis=-1) * mask


def swiglu(x, wg, wu, wd):
    return (jax.nn.silu(x @ wg) * (x @ wu)) @ wd


def causal_conv(x, w, b):
    T = x.shape[1]
    xp = jnp.pad(x, ((0, 0), (CONV_W - 1, 0), (0, 0)))
    y = b
    for i in range(CONV_W):
        y = y + xp[:, i:i + T] * w[i]
    return y


def compress_blocks(kv, pe, w1, w2):
    B, T, G, DH = kv.shape
    s = kv.reshape(B, T // CMP_STRIDE, CMP_STRIDE, G, DH)
    blocks = jnp.concatenate([s[:, :-1], s[:, 1:]], axis=2)
    blocks = blocks + pe[None, None, :, None, :]
    n_cmp = blocks.shape[1]
    flat = blocks.transpose(0, 1, 3, 2, 4).reshape(B, n_cmp, G, CMP_LEN * DH)
    return jax.nn.gelu(flat @ w1) @ w2


def nsa_mixer(q_a, kv_a, g_a, cmp_pe, cmp_w1, cmp_w2):
    B, T, _ = q_a.shape
    G, HPG, DH = NSA_KV_GROUPS, NSA_HPG, NSA_DH
    scale = DH ** -0.5
    q5 = q_a.reshape(B, T, G, HPG, DH)
    kv = kv_a.reshape(B, T, 6, G, DH)
    k_c, v_c, k_s, v_s, k_w, v_w = (kv[:, :, i] for i in range(6))
    t = jnp.arange(T)

    kc = compress_blocks(k_c, cmp_pe[0], cmp_w1[0], cmp_w2[0])
    vc = compress_blocks(v_c, cmp_pe[1], cmp_w1[1], cmp_w2[1])
    n_cmp = kc.shape[1]
    cmp_end = jnp.arange(n_cmp) * CMP_STRIDE + CMP_LEN - 1
    p_cmp = masked_softmax(jnp.einsum('btghd,bngd->bghtn', q5, kc) * scale,
                           cmp_end[None, :] <= t[:, None])
    o_cmp = jnp.einsum('bghtn,bngd->btghd', p_cmp.astype(vc.dtype), vc)

    n_sel = T // SEL_LEN
    top = min(SEL_TOP, n_sel)
    ci = jnp.arange(n_cmp)[:, None]
    sj = jnp.arange(n_sel)[None, :]
    overlap = (jnp.minimum(ci * CMP_STRIDE + CMP_LEN, (sj + 1) * SEL_LEN)
               - jnp.maximum(ci * CMP_STRIDE, sj * SEL_LEN))
    cmp_to_sel = jnp.clip(overlap, 0, None).astype(jnp.float32) / CMP_LEN
    imp = jnp.einsum('bgtn,nj->bgtj', p_cmp.sum(axis=2), cmp_to_sel)
    cur = (t // SEL_LEN)[:, None]
    forced = (sj == 0) | (sj == cur) | (sj == cur - 1)
    score = jnp.where(sj <= cur, imp + jnp.where(forced, FORCE, 0.0), NEG)
    top_s, top_idx = lax.top_k(score, top)
    top_ok = top_s > 0.5 * NEG
    ks_blk = k_s.reshape(B, n_sel, SEL_LEN, G, DH).transpose(0, 3, 1, 2, 4)
    vs_blk = v_s.reshape(B, n_sel, SEL_LEN, G, DH).transpose(0, 3, 1, 2, 4)
    nqs = T // SEL_QBLK
    bi = jnp.arange(B)[:, None, None, None]
    gi = jnp.arange(G)[None, :, None, None]

    def sel_block(args):
        qb, q_blk, idx, ok = args
        k_g = ks_blk[bi, gi, idx]
        v_g = vs_blk[bi, gi, idx].reshape(B, G, SEL_QBLK, top * SEL_LEN, DH)
        s = jnp.einsum('bqghd,bgqjld->bghqjl', q_blk, k_g) * scale
        tq = qb * SEL_QBLK + jnp.arange(SEL_QBLK)
        kpos = idx[..., None] * SEL_LEN + jnp.arange(SEL_LEN)
        mask = ok[..., None] & (kpos <= tq[None, None, :, None, None])
        p = masked_softmax(s.reshape(B, G, HPG, SEL_QBLK, top * SEL_LEN),
                           mask.reshape(B, G, 1, SEL_QBLK, top * SEL_LEN))
        return jnp.einsum('bghqn,bgqnd->bqghd', p.astype(v_g.dtype), v_g)

    o_slc = lax.map(sel_block, (jnp.arange(nqs),
                                jnp.moveaxis(q5.reshape(B, nqs, SEL_QBLK, G, HPG, DH), 1, 0),
                                jnp.moveaxis(top_idx.reshape(B, G, nqs, SEL_QBLK, top), 2, 0),
                                jnp.moveaxis(top_ok.reshape(B, G, nqs, SEL_QBLK, top), 2, 0)))
    o_slc = jnp.moveaxis(o_slc, 0, 1).reshape(B, T, G, HPG, DH)

    kw_pad = jnp.pad(k_w, ((0, 0), (WIN, 0), (0, 0), (0, 0)))
    vw_pad = jnp.pad(v_w, ((0, 0), (WIN, 0), (0, 0), (0, 0)))
    nqw = T // WIN_QBLK

    def win_block(args):
        qb, q_blk = args
        start = qb * WIN_QBLK
        k_b = lax.dynamic_slice_in_dim(kw_pad, start, WIN + WIN_QBLK, axis=1)
        v_b = lax.dynamic_slice_in_dim(vw_pad, start, WIN + WIN_QBLK, axis=1)
        s = jnp.einsum('bqghd,bsgd->bghqs', q_blk, k_b) * scale
        tq = start + jnp.arange(WIN_QBLK)
        kp = start - WIN + jnp.arange(WIN + WIN_QBLK)
        diff = tq[:, None] - kp[None, :]
        p = masked_softmax(s, (kp[None, :] >= 0) & (diff >= 0) & (diff < WIN))
        return jnp.einsum('bghqs,bsgd->bqghd', p.astype(v_b.dtype), v_b)

    o_win = lax.map(win_block, (jnp.arange(nqw),
                                jnp.moveaxis(q5.reshape(B, nqw, WIN_QBLK, G, HPG, DH), 1, 0)))
    o_win = jnp.moveaxis(o_win, 0, 1).reshape(B, T, G, HPG, DH)

    g = jax.nn.sigmoid(g_a).reshape(B, T, G, HPG, 3)
    o = g[..., 0:1] * o_cmp + g[..., 1:2] * o_slc + g[..., 2:3] * o_win
    return o.reshape(B, T, NSA_Q)


def mlstm_chunkwise(q, k, v, logi, logf):
    B, H, T, DQK = q.shape
    DV = v.shape[-1]
    L = M_CHUNK
    nc = T // L

    def chunk(z):
        return jnp.moveaxis(z.reshape((B, H, nc, L) + z.shape[3:]), 2, 0)

    tri = jnp.tril(jnp.ones((L, L), dtype=bool))

    def step(carry, xs):
        C, n, m = carry
        qc, kc, vc, li, lf = xs
        a = jnp.cumsum(lf, axis=-1)
        dlog = jnp.where(tri, a[..., :, None] - a[..., None, :] + li[..., None, :], -jnp.inf)
        inter = a + m[..., None]
        mt = jnp.maximum(inter, dlog.max(axis=-1))
        dmat = jnp.exp(dlog - mt[..., None])
        iw = jnp.exp(inter - mt)
        s = jnp.einsum('bhtd,bhsd->bhts', qc, kc) * dmat
        num = iw[..., None] * jnp.einsum('bhtd,bhde->bhte', qc, C) + jnp.einsum('bhts,bhse->bhte', s, vc)
        den = iw * jnp.einsum('bhtd,bhd->bht', qc, n) + s.sum(axis=-1)
        hc = num / jnp.maximum(jnp.abs(den), jnp.exp(-mt))[..., None]
        a_end = a[..., -1]
        elog = a_end[..., None] - a + li
        m_new = jnp.maximum(a_end + m, elog.max(axis=-1))
        ew = jnp.exp(elog - m_new[..., None])
        decay = jnp.exp(a_end + m - m_new)
        C_new = decay[..., None, None] * C + jnp.einsum('bhs,bhsd,bhse->bhde', ew, kc, vc)
        n_new = decay[..., None] * n + jnp.einsum('bhs,bhsd->bhd', ew, kc)
        return (C_new, n_new, m_new), hc

    init = (jnp.zeros((B, H, DQK, DV), jnp.float32), jnp.zeros((B, H, DQK), jnp.float32),
            jnp.zeros((B, H), jnp.float32))
    _, hs = lax.scan(step, init, (chunk(q), chunk(k), chunk(v), chunk(logi), chunk(logf)))
    return jnp.moveaxis(hs, 0, 2).reshape(B, H, T, DV)


def mlstm_mixer(qk_m, v_m, if_m, o_m, conv_w, conv_b, b_gates_m, mh_norm_g):
    B, T, _ = qk_m.shape
    dt = qk_m.dtype
    qk = jax.nn.silu(causal_conv(qk_m, conv_w, conv_b))
    q, k = jnp.split(qk, 2, axis=-1)

    def heads(z, d):
        return z.reshape(B, T, M_HEADS, d).transpose(0, 2, 1, 3).astype(jnp.float32)

    q = heads(q, M_DQK)
    k = heads(k, M_DQK) * (M_DQK ** -0.5)
    v = heads(v_m, M_DV)
    gates = (if_m + b_gates_m).astype(jnp.float32).transpose(0, 2, 1)
    logi = gates[:, :M_HEADS]
    logf = jax.nn.log_sigmoid(gates[:, M_HEADS:])
    hs = mlstm_chunkwise(q, k, v, logi, logf).transpose(0, 2, 1, 3)
    hn = rmsnorm(hs, mh_norm_g.reshape(M_HEADS, M_DV)).astype(dt)
    return (jax.nn.sigmoid(o_m).reshape(B, T, M_HEADS, M_DV) * hn).reshape(B, T, M_V)


def hybrid_mixer(h, w_in, cmp_pe, cmp_w1, cmp_w2, conv_w, conv_b, b_gates_m, mh_norm_g,
                 w_up_nsa, w_up_mlstm, w_out):
    proj = h @ w_in
    pts = np.cumsum(IN_SIZES)[:-1].tolist()
    q_a, kv_a, g_a, qk_m, v_m, if_m, o_m, gate_a, gate_b = jnp.split(proj, pts, axis=-1)
    o_nsa = nsa_mixer(q_a, kv_a, g_a, cmp_pe, cmp_w1, cmp_w2)
    o_mlstm = mlstm_mixer(qk_m, v_m, if_m, o_m, conv_w, conv_b, b_gates_m, mh_norm_g)
    merged = (jax.nn.sigmoid(gate_a) * (o_nsa @ w_up_nsa)
              + jax.nn.sigmoid(gate_b) * (o_mlstm @ w_up_mlstm))
    return merged @ w_out


def moe_ffn(h, w_router, b_router, w_e_gate, w_e_up, w_e_down, w_sh_gate, w_sh_up, w_sh_down):
    B, T, D = h.shape
    N = B * T
    hf = h.reshape(N, D)
    s = jax.nn.sigmoid((hf @ w_router).astype(jnp.float32))
    sb = s + b_router.astype(jnp.float32)
    gscore = lax.top_k(sb.reshape(N, N_GROUPS, N_EXPERTS // N_GROUPS), 2)[0].sum(axis=-1)
    _, gidx = lax.top_k(gscore, TOP_GROUPS)
    gmask = jax.nn.one_hot(gidx, N_GROUPS, dtype=jnp.float32).sum(axis=-2) > 0
    emask = jnp.repeat(gmask, N_EXPERTS // N_GROUPS, axis=-1)
    _, eidx = lax.top_k(jnp.where(emask, sb, NEG), TOP_K)
    w = jnp.take_along_axis(s, eidx, axis=-1)
    w = (w / w.sum(axis=-1, keepdims=True) * ROUTE_SCALE).astype(h.dtype)

    A = N * TOP_K
    e_flat = eidx.reshape(A)
    tok = jnp.repeat(jnp.arange(N, dtype=jnp.int32), TOP_K)
    w_flat = w.reshape(A)
    order = jnp.argsort(e_flat)
    e_s, tok_s, w_s = e_flat[order], tok[order], w_flat[order]
    counts = jnp.bincount(e_flat, length=N_EXPERTS)
    padded = (counts + MOE_BLK - 1) // MOE_BLK * MOE_BLK
    pad_end = jnp.cumsum(padded)
    pad_start = pad_end - padded
    raw_start = jnp.cumsum(counts) - counts
    dest = pad_start[e_s] + (jnp.arange(A) - raw_start[e_s])
    n_blk = -(-A // MOE_BLK) + N_EXPERTS
    P = n_blk * MOE_BLK
    slot_tok = jnp.full((P,), N, jnp.int32).at[dest].set(tok_s)
    slot_w = jnp.zeros((P,), h.dtype).at[dest].set(w_s)
    blk_exp = jnp.minimum(jnp.searchsorted(pad_end, jnp.arange(n_blk) * MOE_BLK, side='right'),
                          N_EXPERTS - 1)
    hp = jnp.concatenate([hf, jnp.zeros((1, D), hf.dtype)], axis=0)

    def run_block(args):
        e, toks, ws = args
        return swiglu(hp[toks], w_e_gate[e], w_e_up[e], w_e_down[e]) * ws[:, None]

    y = lax.map(run_block, (blk_exp, slot_tok.reshape(n_blk, MOE_BLK), slot_w.reshape(n_blk, MOE_BLK)))
    routed = jnp.zeros((N + 1, D), hf.dtype).at[slot_tok].add(y.reshape(P, D))[:N]
    shared = swiglu(hf, w_sh_gate, w_sh_up, w_sh_down)
    return (routed + shared).reshape(B, T, D)


def setup_inputs(seed: int = 0) -> dict:
    key = jax.random.key(seed)
    ks = iter(jax.random.split(key, 32))

    def nrm(shape, scale):
        return jax.random.normal(next(ks), shape, jnp.float32) * scale

    def gain(shape):
        return 1.0 + nrm(shape, 0.02)

    L = DEPTH
    return {
        'x': nrm((BATCH, SEQ, D_MODEL), 1.0),
        'c': nrm((BATCH, D_MODEL), 1.0),
        'w_ada': nrm((L, D_MODEL, 6 * D_MODEL), 0.2 * D_MODEL ** -0.5),
        'b_ada': nrm((L, 6 * D_MODEL), 0.01),
        'g_pre_mix': gain((L, D_MODEL)),
        'g_post_mix': gain((L, D_MODEL)),
        'w_in': nrm((L, D_MODEL, D_IN), D_MODEL ** -0.5),
        'cmp_pe': nrm((L, 2, CMP_LEN, NSA_DH), 0.02),
        'cmp_w1': nrm((L, 2, CMP_LEN * NSA_DH, CMP_HID), (CMP_LEN * NSA_DH) ** -0.5),
        'cmp_w2': nrm((L, 2, CMP_HID, NSA_DH), CMP_HID ** -0.5),
        'conv_w': nrm((L, CONV_W, 2 * M_QK), CONV_W ** -0.5),
        'conv_b': nrm((L, 2 * M_QK), 0.01),
        'b_gates_m': jnp.concatenate([nrm((L, M_HEADS), 0.1),
                                      jnp.linspace(3.0, 6.0, M_HEADS)[None, :] + nrm((L, M_HEADS), 0.1)],
                                     axis=-1),
        'mh_norm_g': gain((L, M_V)),
        'w_up_nsa': nrm((L, NSA_Q, D_MODEL), NSA_Q ** -0.5),
        'w_up_mlstm': nrm((L, M_V, D_MODEL), M_V ** -0.5),
        'w_out': nrm((L, D_MODEL, D_MODEL), D_MODEL ** -0.5),
        'g_pre_ffn': gain((L, D_MODEL)),
        'g_post_ffn': gain((L, D_MODEL)),
        'w_router': nrm((L, D_MODEL, N_EXPERTS), D_MODEL ** -0.5),
        'b_router': nrm((L, N_EXPERTS), 0.01),
        'w_e_gate': nrm((L, N_EXPERTS, D_MODEL, EXPERT_FF), D_MODEL ** -0.5),
        'w_e_up': nrm((L, N_EXPERTS, D_MODEL, EXPERT_FF), D_MODEL ** -0.5),
        'w_e_down': nrm((L, N_EXPERTS, EXPERT_FF, D_MODEL), EXPERT_FF ** -0.5),
        'w_sh_gate': nrm((L, D_MODEL, SHARED_FF), D_MODEL ** -0.5),
        'w_sh_up': nrm((L, D_MODEL, SHARED_FF), D_MODEL ** -0.5),
        'w_sh_down': nrm((L, SHARED_FF, D_MODEL), SHARED_FF ** -0.5),
    }


def reference(x, c, w_ada, b_ada, g_pre_mix, g_post_mix, w_in, cmp_pe, cmp_w1, cmp_w2, conv_w, conv_b,
              b_gates_m, mh_norm_g, w_up_nsa, w_up_mlstm, w_out, g_pre_ffn, g_post_ffn, w_router, b_router,
              w_e_gate, w_e_up, w_e_down, w_sh_gate, w_sh_up, w_sh_down):
    for l in range(DEPTH):
        mod = jax.nn.silu(c) @ w_ada[l] + b_ada[l]
        sh_m, sc_m, gt_m, sh_f, sc_f, gt_f = (m[:, None, :] for m in jnp.split(mod, 6, axis=-1))
        h = rmsnorm(x, g_pre_mix[l]) * (1.0 + sc_m) + sh_m
        y = hybrid_mixer(h, w_in[l], cmp_pe[l], cmp_w1[l], cmp_w2[l], conv_w[l], conv_b[l], b_gates_m[l],
                         mh_norm_g[l], w_up_nsa[l], w_up_mlstm[l], w_out[l])
        x = x + gt_m * rmsnorm(y, g_post_mix[l])
        h = rmsnorm(x, g_pre_ffn[l]) * (1.0 + sc_f) + sh_f
        y = moe_ffn(h, w_router[l], b_router[l], w_e_gate[l], w_e_up[l], w_e_down[l],
                    w_sh_gate[l], w_sh_up[l], w_sh_down[l])
        x = x + gt_f * rmsnorm(y, g_post_ffn[l])
    return x
```

```python
import numpy as np
import concourse.bass as bass
import concourse.mybir as mybir

F32 = mybir.dt.float32
BF16 = mybir.dt.bfloat16
U32 = mybir.dt.uint32
I32 = mybir.dt.int32
AF = mybir.ActivationFunctionType
ALU = mybir.AluOpType
AX = mybir.AxisListType

ENGS = ("pe", "act", "dve", "pool", "sp")


class Buf:
    __slots__ = ("name", "w", "r")

    def __init__(self, name=""):
        self.name = name
        self.w = {}
        self.r = {}


class FW:
    def __init__(self, nc, dma_ring=8):
        self.nc = nc
        self.prog = {e: [] for e in ENGS}
        self.sem = {e: nc.alloc_semaphore("c_" + e) for e in ENGS}
        self.cnt = {e: 0 for e in ENGS}
        self.known = {e: {} for e in ENGS}
        self.R = dma_ring
        self.dsem = {q: [nc.alloc_semaphore("d_%s%d" % (q, i)) for i in range(dma_ring)] for q in ("sp", "pool", "act")}
        self.dn = {q: 0 for q in ("sp", "pool", "act")}

    def _need(self, e, tickets):
        need = {}
        for (s, v) in tickets:
            if v > need.get(s, 0):
                need[s] = v
        out = []
        kn = self.known[e]
        own = self.sem[e] if e == "pe" else None
        for s, v in need.items():
            if s is own:
                continue
            if kn.get(s, 0) < v:
                kn[s] = v
                out.append((s, v))
        return out

    def _deps(self, reads, writes, partial=False):
        t = []
        for b in reads:
            t.extend(b.w.items())
        for b in writes:
            if not partial:
                t.extend(b.w.items())
            t.extend(b.r.items())
        return t

    def _commit(self, ticket, reads, writes, partial=False):
        s, v = ticket
        for b in reads:
            if b.r.get(s, 0) < v:
                b.r[s] = v
        for b in writes:
            if partial:
                if b.w.get(s, 0) < v:
                    b.w[s] = v
            else:
                b.w = {s: v}
                b.r = {}

    @staticmethod
    def _compact(ts):
        m = {}
        so = {}
        for (s, v) in ts:
            k = id(s)
            so[k] = s
            if v > m.get(k, 0):
                m[k] = v
        return [(so[k], v) for k, v in m.items()]

    def op(self, e, fn, reads=(), writes=(), partial=False):
        deps = self._deps(reads, writes, partial)
        waits = self._need(e, deps)
        self.cnt[e] += 1
        tk = (self.sem[e], self.cnt[e])
        sem = self.sem[e]

        def emit(eng, waits=waits, fn=fn, sem=sem):
            for (s, v) in waits:
                eng.wait_ge(s, v)
            fn(eng).then_inc(sem, 1)
        self.prog[e].append(emit)
        self._commit(tk, reads, writes, partial)
        return tk

    def barrier(self):
        ts = [(self.sem[e], self.cnt[e]) for e in ENGS if self.cnt[e] > 0]
        for q in self.dsem:
            n = self.dn[q]
            for i, s in enumerate(self.dsem[q]):
                k = (n - 1 - i) // self.R + 1 if n > i else 0
                if k > 0:
                    ts.append((s, 16 * k))
        for e in ENGS:
            waits = self._need(e, ts)

            def emit(eng, waits=waits):
                for (s, v) in waits:
                    eng.wait_ge(s, v)
            self.prog[e].append(emit)

    def dma(self, q, fn, reads=(), writes=(), partial=False):
        n = self.dn[q]
        self.dn[q] += 1
        s = self.dsem[q][n % self.R]
        prev = 16 * (n // self.R)
        deps = self._deps(reads, writes, partial)
        if prev > 0:
            deps = deps + [(s, prev)]
        waits = self._need(q, deps)
        tk = (s, prev + 16)

        def emit(eng, waits=waits, fn=fn, s=s):
            for (ss, v) in waits:
                eng.wait_ge(ss, v)
            fn(eng).then_inc(s, 16)
        self.prog[q].append(emit)
        self._commit(tk, reads, writes, partial)
        return tk

    def finish(self, final_bufs):
        deps = []
        for b in final_bufs:
            deps.extend(b.w.items())
        waits = self._need("sp", deps)

        def emit(eng, waits=waits):
            for (s, v) in waits:
                eng.wait_ge(s, v)
        self.prog["sp"].append(emit)
        nc = self.nc
        prog = self.prog
        with nc.Block() as block:
            @block.tensor
            def _(e):
                for f in prog["pe"]:
                    f(e)

            @block.scalar
            def _(e):
                for f in prog["act"]:
                    f(e)

            @block.vector
            def _(e):
                for f in prog["dve"]:
                    f(e)

            @block.gpsimd
            def _(e):
                for f in prog["pool"]:
                    f(e)

            @block.sync
            def _(e):
                for f in prog["sp"]:
                    f(e)


T = 2048
D = 2048
KC = 16
D_IN = 9784
C_Q, C_KV, C_GA, C_QK, C_VM, C_IF, C_OM, C_GTA, C_GTB = 0, 1024, 2560, 2608, 3632, 4656, 4664, 5688, 7736
EPS = 1e-6


class Ctx:
    pass


def win_chunks():
    groups = [(C_Q, 1024), (C_KV, 1536), (C_KV + 768, 256), (C_KV + 1280, 256), (C_GA, 48), (C_QK, 1024), (C_VM, 1024),
              (C_IF, 8), (C_OM, 1024), (C_GTA, 2048), (C_GTB, 2048)]
    out = []
    for c0, n in groups:
        for cc in range(0, n, 512):
            out.append((c0 + cc, min(512, n - cc)))
    return out


def pack_cols(w, chunks):
    import numpy as np
    K = w.shape[0] // 128
    parts, offs, off = [], {}, 0
    for (c0, n) in chunks:
        offs[(c0, n)] = off
        for h0 in range(0, n, 256):
            nn = min(256, n - h0)
            blk = w[:, c0 + h0:c0 + h0 + nn].reshape(K, 128, nn).transpose(1, 0, 2)
            parts.append(np.ascontiguousarray(blk).reshape(-1))
            off += 128 * K * nn
    return np.concatenate(parts), offs, off


def declare_io_a(nc, cx, dbg=False):
    cx.dbg = dbg
    ch = win_chunks()
    off = 0
    cx.win_off = {}
    for (c0, n) in ch:
        cx.win_off[(c0, n)] = off
        off += 128 * KC * n
    cx.win_total = off
    def din(name, shape, dt=F32):
        t = nc.dram_tensor(name, list(shape), dt, kind="ExternalInput").ap()
        setattr(cx, name, t)
        return t

    def dscr(name, shape, dt):
        t = nc.dram_tensor(name, list(shape), dt, kind="ExternalOutput" if dbg else "Internal").ap()
        setattr(cx, name, t)
        return t
    din("x", [T, D])
    din("cT", [128, KC])
    din("w_ada_p", [D * 6 * D])
    din("b_ada", [1, 6 * D])
    din("g4", [128, 4 * KC])
    din("gpost", [2, D])
    din("w_in_p", [cx.win_total])
    din("ident", [128, 128])
    if dbg:
        dscr("dbg_modc", [128, 4 * KC], F32)
        dscr("dbg_G", [128, D], F32)
    dscr("qT_d", [1024, T], BF16)
    dscr("kvT_d", [1536, T], BF16)
    dscr("qkT_d", [1024, T], F32)
    dscr("ifT_d", [8, T], F32)
    dscr("gaT_d", [D, T], BF16)
    dscr("gbT_d", [D, T], BF16)
    dscr("vs_d", [T, 256], BF16)
    dscr("vw_d", [T, 256], BF16)
    dscr("ga_d", [T, 48], F32)
    dscr("vm_d", [T, 1024], BF16)
    dscr("om_d", [T, 1024], BF16)
    dscr("if_d", [T, 8], F32)


class Ring:
    def __init__(self, tiles):
        self.tiles = tiles
        self.bufs = [Buf() for _ in tiles]
        self.i = 0

    def next(self):
        k = self.i % len(self.tiles)
        self.i += 1
        return self.tiles[k], self.bufs[k]


def stage_abc(nc, fw, cx, es, stop=None, gs=None):
    def sb(name, shape, dt):
        return es.enter_context(nc.sbuf_tensor(name, list(shape), dt))

    def ps(name, shape, dt):
        return es.enter_context(nc.psum_tensor(name, list(shape), dt))

    def gsb(name, shape, dt):
        return (gs or es).enter_context(nc.sbuf_tensor(name, list(shape), dt))

    ident_f = gsb("ident_f", [128, 128], F32)
    ident_b = gsb("ident_b", [128, 128], BF16)
    modc = gsb("modc", [128, 4 * KC], F32)
    Grow = [gsb("Grow%d" % i, [128, D], F32) for i in range(2)]
    cT = sb("cT_s", [128, KC], F32)
    sc = sb("sc_s", [128, KC], F32)
    S_b = sb("S_b", [128, KC, 128], BF16)
    g4 = sb("g4_s", [128, 4 * KC], F32)
    modraw = sb("modraw", [128, 4 * KC], F32)
    b_Grow = [Buf(), Buf()]
    b_id, b_idb, b_cT, b_sc, b_Sb, b_g4, b_modraw, b_modc = (Buf() for _ in range(8))
    fw.dma("sp", lambda e: e.dma_start(out=ident_f[:], in_=cx.ident[:, :]), writes=[b_id])
    fw.dma("sp", lambda e: e.dma_start(out=cT[:], in_=cx.cT[:, :]), writes=[b_cT])
    fw.dma("sp", lambda e: e.dma_start(out=g4[:], in_=cx.g4[:, :]), writes=[b_g4])
    fw.op("dve", lambda e: e.tensor_copy(out=ident_b[:], in_=ident_f[:]), reads=[b_id], writes=[b_idb])
    fw.op("act", lambda e: e.activation(out=sc[:], in_=cT[:], func=AF.Silu), reads=[b_cT], writes=[b_sc])
    for kc in range(KC):
        fw.op("dve", lambda e, kc=kc: e.tensor_copy(out=S_b[:, kc, :], in_=sc[:, kc:kc + 1].to_broadcast([128, 128])),
              reads=[b_sc], writes=[b_Sb], partial=True)

    wf = Ring([sb("wf%d" % i, [128, KC, 256], F32) for i in range(2)])
    wb = Ring([sb("wb%d" % i, [128, KC, 512], BF16) for i in range(2)])
    pacc = Ring([ps("pacc%d" % i, [128, 512], F32) for i in range(4)])
    ptr = Ring([ps("ptr%d" % i, [128, 1024], BF16) for i in range(2)])
    bada_r = Ring([sb("bada%d" % i, [128, 512], F32) for i in range(2)])
    mrow_r = Ring([sb("mrow%d" % i, [128, 512], F32) for i in range(2)])
    junkf = sb("junkf", [128, 128], F32)
    b_junkf = Buf()
    cast_tog = [0]

    def load_w(src_flat, off, ncols):
        wbt, wbb = wb.next()
        for h0 in range(0, ncols, 256):
            n = min(256, ncols - h0)
            wt, wtb = wf.next()
            o = off + h0 * 128 * KC
            fw.dma("sp", lambda e, wt=wt, o=o, n=n: e.dma_start(
                out=wt[:, :, 0:n], in_=src_flat[o:o + 128 * KC * n].rearrange("(p k n) -> p k n", p=128, k=KC)), writes=[wtb])
            eng = "pool" if cast_tog[0] % 2 == 0 else "dve"
            cast_tog[0] += 1
            fw.op(eng, lambda e, wt=wt, wbt=wbt, h0=h0, n=n: e.tensor_copy(out=wbt[:, :, h0:h0 + n], in_=wt[:, :, 0:n]),
                  reads=[wtb], writes=[wbb], partial=True)
        return wbt, wbb

    for ch in range(24):
        mi, q = ch // 4, ch % 4
        pt, pb = pacc.next()
        bt, btb = bada_r.next()
        fw.dma("sp", lambda e, bt=bt, ch=ch: e.dma_start(out=bt[:], in_=cx.b_ada[0, ch * 512:(ch + 1) * 512].partition_broadcast(128)), writes=[btb])
        wbt, wbb = load_w(cx.w_ada_p, ch * 512 * 128 * KC, 512)
        for kc in range(KC):
            fw.op("pe", lambda e, pt=pt, wbt=wbt, kc=kc: e.matmul(pt[:, :], lhsT=S_b[:, kc, :], rhs=wbt[:, kc, :],
                  start=(kc == 0), stop=(kc == KC - 1)), reads=[b_Sb, wbb], writes=[pb])
        mr, mrb = mrow_r.next()
        fw.op("dve", lambda e, pt=pt, mr=mr, bt=bt: e.tensor_tensor(out=mr[:], in0=pt[:, :], in1=bt[:], op=ALU.add),
              reads=[pb, btb], writes=[mrb])
        if mi in (2, 5):
            gi = 0 if mi == 2 else 1
            gt, gtb = bada_r.next()
            fw.dma("sp", lambda e, gt=gt, gi=gi, q=q: e.dma_start(out=gt[:], in_=cx.gpost[gi, q * 512:(q + 1) * 512].partition_broadcast(128)), writes=[gtb])
            fw.op("pool", lambda e, mr=mr, gt=gt, gi=gi, q=q: e.tensor_tensor(out=Grow[gi][:, q * 512:(q + 1) * 512], in0=mr[:], in1=gt[:], op=ALU.mult),
                  reads=[mrb, gtb], writes=[b_Grow[gi]], partial=True)
        else:
            vi = {1: 0, 0: 1, 4: 2, 3: 3}[mi]
            for jj in range(4):
                j = q * 4 + jj
                fw.op("dve", lambda e, mr=mr, jj=jj, vi=vi, j=j: e.scalar_tensor_tensor(
                    out=junkf[:], in0=mr[:, jj * 128:(jj + 1) * 128], scalar=1.0, in1=ident_f[:],
                    op0=ALU.mult, op1=ALU.mult, accum_out=modraw[:, vi * KC + j:vi * KC + j + 1]),
                    reads=[mrb, b_id], writes=[b_junkf, b_modraw])
    for a_i, g_i in ((0, 0), (2, 1)):
        fw.op("dve", lambda e, a_i=a_i, g_i=g_i: e.scalar_tensor_tensor(
            out=modc[:, a_i * KC:(a_i + 1) * KC], in0=modraw[:, a_i * KC:(a_i + 1) * KC], scalar=1.0,
            in1=g4[:, g_i * KC:(g_i + 1) * KC], op0=ALU.add, op1=ALU.mult), reads=[b_modraw, b_g4], writes=[b_modc], partial=True)
        fw.op("dve", lambda e, a_i=a_i: e.tensor_copy(
            out=modc[:, (a_i + 1) * KC:(a_i + 2) * KC], in_=modraw[:, (a_i + 1) * KC:(a_i + 2) * KC]),
            reads=[b_modraw], writes=[b_modc], partial=True)
    cx.modc, cx.b_modc, cx.Grow, cx.b_Grow = modc, b_modc, Grow, b_Grow
    cx.ident_b, cx.b_idb, cx.ident_f, cx.b_id = ident_b, b_idb, ident_f, b_id
    if cx.dbg:
        fw.dma("sp", lambda e: e.dma_start(out=cx.dbg_modc[:, :], in_=modc[:]), reads=[b_modc], writes=[Buf()])
        fw.dma("sp", lambda e: e.dma_start(out=cx.dbg_G[:, :], in_=Grow[0][:]), reads=[b_Grow[0]], writes=[Buf()])
    if stop == 'A':
        return
    hT = sb("hT", [128, KC, T], BF16)
    b_hT = [Buf() for _ in range(16)]
    xt_r = Ring([sb("xt%d" % i, [128, D], F32) for i in range(2)])
    xn_r = Ring([sb("xn%d" % i, [128, D], BF16) for i in range(2)])
    junk = sb("junk", [128, D], BF16)
    b_junk = Buf()
    st_r = Ring([sb("stat%d" % i, [128, 4], F32) for i in range(2)])
    norm_pre(nc, fw, cx, cx.x, hT, b_hT, xt_r, xn_r, junk, b_junk, st_r, ptr, 0)
    cx.hT, cx.b_hT = hT, b_hT

    if stop == 'B':
        return
    stg_b = Ring([sb("stgb%d" % i, [128, 512], BF16) for i in range(4)])
    stg_f = Ring([sb("stgf%d" % i, [128, 512], F32) for i in range(2)])
    def evac(pt, pb, rows, ncols, func, dt, dst_ap):
        st, stb = (stg_b if dt == BF16 else stg_f).next()
        fw.op("act", lambda e: e.activation(out=st[0:rows, 0:ncols], in_=pt[0:rows, 0:ncols], func=func),
              reads=[pb], writes=[stb])
        fw.dma("sp", lambda e: e.dma_start(out=dst_ap, in_=st[0:rows, 0:ncols]), reads=[stb], writes=[Buf()])

    def proj_F(c0, ncols, func, dt, dst):
        for cc in range(0, ncols, 512):
            ncc = min(512, ncols - cc)
            wbt, wbb = load_w(cx.w_in_p, cx.win_off[(c0 + cc, ncc)], ncc)
            for m0 in range(0, ncc, 128):
                m = min(128, ncc - m0)
                for tch in range(4):
                    pt, pb = pacc.next()
                    for kc in range(KC):
                        fw.op("pe", lambda e, pt=pt, wbt=wbt, kc=kc, m0=m0, m=m, tch=tch: e.matmul(
                            pt[0:m, :], lhsT=wbt[:, kc, m0:m0 + m], rhs=hT[:, kc, tch * 512:(tch + 1) * 512],
                            start=(kc == 0), stop=(kc == KC - 1)), reads=[wbb] + b_hT[tch * 4:tch * 4 + 4], writes=[pb])
                    evac(pt, pb, m, 512, func, dt, dst[cc + m0:cc + m0 + m, tch * 512:(tch + 1) * 512])

    def proj_T(c0, ncols, func, dt, dst):
        for cc in range(0, ncols, 512):
            ncc = min(512, ncols - cc)
            wbt, wbb = load_w(cx.w_in_p, cx.win_off[(c0 + cc, ncc)], ncc)
            for ti in range(16):
                pt, pb = pacc.next()
                for kc in range(KC):
                    fw.op("pe", lambda e, pt=pt, wbt=wbt, kc=kc, ti=ti, ncc=ncc: e.matmul(
                        pt[:, 0:ncc], lhsT=hT[:, kc, ti * 128:(ti + 1) * 128], rhs=wbt[:, kc, 0:ncc],
                        start=(kc == 0), stop=(kc == KC - 1)), reads=[wbb, b_hT[ti]], writes=[pb])
                evac(pt, pb, 128, ncc, func, dt, dst[ti * 128:(ti + 1) * 128, cc:cc + ncc])

    ID = AF.Identity
    proj_F(C_Q, 1024, ID, BF16, cx.qT_d)
    proj_F(C_KV, 1536, ID, BF16, cx.kvT_d)
    proj_T(C_KV + 3 * 256, 256, ID, BF16, cx.vs_d)
    proj_T(C_KV + 5 * 256, 256, ID, BF16, cx.vw_d)
    proj_T(C_GA, 48, AF.Sigmoid, F32, cx.ga_d)
    proj_F(C_QK, 1024, ID, F32, cx.qkT_d)
    proj_T(C_VM, 1024, ID, BF16, cx.vm_d)
    proj_F(C_IF, 8, ID, F32, cx.ifT_d)
    proj_T(C_IF, 8, ID, F32, cx.if_d)
    proj_T(C_OM, 1024, AF.Sigmoid, BF16, cx.om_d)
    proj_F(C_GTA, 2048, AF.Sigmoid, BF16, cx.gaT_d)
    proj_F(C_GTB, 2048, AF.Sigmoid, BF16, cx.gbT_d)


def norm_pre(nc, fw, cx, x_ap, hT, b_hT, xt_r, xn_r, junk, b_junk, st_r, ptr, which, ntiles=16, x_dep=None):
    modc, b_modc = cx.modc, cx.b_modc
    a_off = which * 2 * KC
    for i in range(ntiles):
        xt, xtb = xt_r.next()
        fw.dma("sp", lambda e, xt=xt, i=i: e.dma_start(out=xt[:], in_=x_ap[i * 128:(i + 1) * 128, :]), writes=[xtb])
        st, stb = st_r.next()
        fw.op("dve", lambda e, xt=xt, st=st: e.scalar_tensor_tensor(out=junk[:], in0=xt[:], scalar=1.0 / D, in1=xt[:],
              op0=ALU.mult, op1=ALU.mult, accum_out=st[:, 0:1]), reads=[xtb], writes=[b_junk, stb])
        fw.op("dve", lambda e, st=st: e.tensor_scalar(out=st[:, 1:2], in0=st[:, 0:1], scalar1=EPS, scalar2=None, op0=ALU.add),
              reads=[stb], writes=[stb])
        fw.op("act", lambda e, st=st: e.activation(out=st[:, 3:4], in_=st[:, 1:2], func=AF.Sqrt), reads=[stb], writes=[stb])
        fw.op("dve", lambda e, st=st: e.reciprocal(out=st[:, 2:3], in_=st[:, 3:4]), reads=[stb], writes=[stb])
        xn, xnb = xn_r.next()
        fw.op("dve", lambda e, xn=xn, xt=xt, st=st: e.tensor_scalar(out=xn[:], in0=xt[:], scalar1=st[:, 2:3], scalar2=None, op0=ALU.mult),
              reads=[xtb, stb], writes=[xnb])
        import os
        LV = int(os.environ.get("LV", "4"))
        if LV <= 2:
            fw.op("dve", lambda e, xn=xn, i=i: e.tensor_copy(out=hT[:, :, i * 128:(i + 1) * 128], in_=xn[:].rearrange("p (k n) -> p k n", k=16)), reads=[xnb], writes=[b_hT[i]])
            continue
        for half in range(2):
            pt, pb = ptr.next()
            for k8 in range(8):
                kc = half * 8 + k8
                fw.op("pe", lambda e, pt=pt, xn=xn, kc=kc, k8=k8: e.transpose(
                    out=pt[:, k8 * 128:(k8 + 1) * 128], in_=xn[:, kc * 128:(kc + 1) * 128], identity=cx.ident_b[:]),
                    reads=[xnb, cx.b_idb], writes=[pb])
            for k8 in range(8):
                kc = half * 8 + k8
                if LV == 3:
                    fw.op("dve", lambda e, pt=pt, kc=kc, k8=k8, i=i: e.tensor_copy(out=hT[:, kc, i * 128:(i + 1) * 128], in_=pt[:, k8 * 128:(k8 + 1) * 128]), reads=[pb], writes=[b_hT[i]], partial=True)
                elif k8 % 2 == 0 or LV == 4:
                    fw.op("dve", lambda e, pt=pt, kc=kc, k8=k8, i=i: e.tensor_scalar(
                        out=hT[:, kc, i * 128:(i + 1) * 128], in0=pt[:, k8 * 128:(k8 + 1) * 128],
                        scalar1=modc[:, a_off + kc:a_off + kc + 1], scalar2=modc[:, a_off + KC + kc:a_off + KC + kc + 1],
                        op0=ALU.mult, op1=ALU.add), reads=[pb, b_modc], writes=[b_hT[i]], partial=True)
                else:
                    fw.op("act", lambda e, pt=pt, kc=kc, k8=k8, i=i: e.activation(
                        out=hT[:, kc, i * 128:(i + 1) * 128], in_=pt[:, k8 * 128:(k8 + 1) * 128], func=AF.Identity,
                        scale=modc[:, a_off + kc:a_off + kc + 1], bias=modc[:, a_off + KC + kc:a_off + KC + kc + 1]),
                        reads=[pb, b_modc], writes=[b_hT[i]], partial=True)


SCALE = 0.125


def nsa_consts():
    import numpy as np
    import ml_dtypes
    bf = ml_dtypes.bfloat16
    t = np.arange(T)
    n = np.arange(127)
    cmaskT = ((16 * n[:, None] + 31) <= t[None, :]).astype(bf)
    ci = n[:, None]
    sj = np.arange(32)[None, :]
    ov = np.minimum(ci * 16 + 32, (sj + 1) * 64) - np.maximum(ci * 16, sj * 64)
    c2s = (np.clip(ov, 0, None).astype(np.float32) / 32).astype(bf)
    cur = (t // 64)[:, None]
    forced = (sj == 0) | (sj == cur) | (sj == cur - 1)
    addc = np.where(sj <= cur, np.where(forced, 1e3, 0.0), -1e9).astype(np.float32)
    addc = np.ascontiguousarray(addc.reshape(16, 128, 32).transpose(1, 0, 2))
    Xall = (np.arange(T)[None, :] // 64 == np.arange(32)[:, None]).astype(bf)
    a = np.arange(128)
    tri = (a[:, None] <= a[None, :]).astype(bf)
    tri2 = (a[:, None] > a[None, :]).astype(bf)
    return {"cmaskT": cmaskT, "c2s": c2s, "addc": addc, "Xall": Xall, "tri": tri, "tri2": tri2}


def declare_io_b(nc, cx, dbg=False):
    def din(name, shape, dt=F32):
        setattr(cx, name, nc.dram_tensor(name, list(shape), dt, kind="ExternalInput").ap())

    def dscr(name, shape, dt):
        setattr(cx, name, nc.dram_tensor(name, list(shape), dt, kind="ExternalOutput" if dbg else "Internal").ap())
    din("cmaskT", [127, T], BF16)
    din("c2s", [127, 32], BF16)
    din("addc", [128, 16, 32], F32)
    din("Xall", [32, T], BF16)
    din("tri", [128, 128], BF16)
    din("tri2", [128, 128], BF16)
    din("cmp_w1", [2, 2048, 256])
    din("cmp_w2", [2, 256, 64])
    din("peT", [2, 64, 32])
    dscr("onT_d", [1024, T], BF16)
    if dbg:
        dscr("dbg_kcc", [64, 4, 127], BF16)
        dscr("dbg_vcc", [127, 4, 97], BF16)
        dscr("dbg_sel", [T, 32], BF16)
        dscr("dbg_sm", [T, 256], F32)


def stage_nsa(nc, fw, cx, es):
    def sb(name, shape, dt):
        return es.enter_context(nc.sbuf_tensor(name, list(shape), dt))

    def ps(name, shape, dt):
        return es.enter_context(nc.psum_tensor(name, list(shape), dt))

    def load(name, shape, dt, src, q="sp", n_split=1):
        t = sb(name, shape, dt)
        b = Buf()
        if n_split == 1:
            fw.dma(q, lambda e: e.dma_start(out=t[:], in_=src), writes=[b])
        return t, b

    ident_b, b_idb = cx.ident_b, cx.b_idb
    cmaskT, b_cm = load("cmaskT_s", [127, T], BF16, cx.cmaskT[:, :])
    addc, b_addc = load("addc_s", [128, 16, 32], F32, cx.addc[:, :, :])
    Xall, b_X = load("Xall_s", [32, T], BF16, cx.Xall[:, :])
    tri, b_tri = load("tri_s", [128, 128], BF16, cx.tri[:, :])
    tri2, b_tri2 = load("tri2_s", [128, 128], BF16, cx.tri2[:, :])
    kT = {}
    for nm, r0 in (("ks", 512), ("kw", 1024)):
        kT[nm] = load("kT_" + nm, [64, 4, T], BF16, cx.kvT_d[r0:r0 + 256, :].rearrange("(g d) t -> d g t", d=64))
    v1 = {}
    for nm, src in (("vs", cx.vs_d), ("vw", cx.vw_d)):
        t_ = sb("v1_" + nm, [128, 16, 4, 65], BF16)
        b_ = Buf()
        fw.op("pool", lambda e, t_=t_: e.memset(t_[:], 1.0), writes=[b_])
        for i in range(16):
            fw.dma("sp", lambda e, t_=t_, i=i, src=src: e.dma_start(
                out=t_[:, i, :, 0:64], in_=src[i * 128:(i + 1) * 128, :].rearrange("p (g d) -> p g d", g=4)),
                reads=[b_], writes=[b_], partial=True)
        v1[nm] = (t_, b_)
    ga, b_ga = sb("ga_s", [128, 16, 48], F32), Buf()
    for i4 in range(4):
        fw.dma("sp", lambda e, i4=i4: e.dma_start(
            out=ga[:, i4 * 4:(i4 + 1) * 4, :], in_=cx.ga_d[i4 * 512:(i4 + 1) * 512, :].rearrange("(i p) c -> p i c", p=128)),
            writes=[b_ga], partial=True)

    pS = Ring([ps("pS%d" % i, [128, 512], F32) for i in range(2)])
    pM = Ring([ps("pM%d" % i, [128, 512], F32) for i in range(1)])
    pc_t, b_pc = ps("pc", [128, 512], F32)[:, 0:388].rearrange("p (h c) -> p h c", h=4), Buf()
    pss_t, b_pss = ps("pss", [128, 512], F32)[:, 0:260].rearrange("p (h c) -> p h c", h=4), Buf()
    psw_t, b_psw = ps("psw", [128, 512], F32)[:, 0:260].rearrange("p (h c) -> p h c", h=4), Buf()
    pT = Ring([ps("pT%d" % i, [128, 1024], BF16) for i in range(2)])

    kccT, b_kcc = sb("kccT", [64, 4, 127], BF16), Buf()
    vcc, b_vcc = sb("vcc", [127, 4, 97], BF16), Buf()
    from contextlib import ExitStack
    es_outer = es
    es = ExitStack()
    es.__enter__()
    w1f, b_w1f = sb("w1f", [64, 16, 256], F32), Buf()
    w1b, b_w1b = sb("w1b", [64, 32, 256], BF16), Buf()
    w2f, b_w2f = sb("w2f", [128, 2, 64], F32), Buf()
    w2b, b_w2b = sb("w2b", [128, 2, 64], BF16), Buf()
    pef, b_pef = sb("pef", [64, 32], F32), Buf()
    peb, b_peb = sb("peb", [64, 32], BF16), Buf()
    pbias, b_pbias = sb("pbias", [128, 2], F32), Buf()
    hidT, b_hid = sb("hidT", [128, 2, 508], BF16), Buf()
    gz = [sb("gz%d" % i, [128, 508], F32) for i in range(3)]
    b_gz = [Buf() for _ in range(3)]
    fw.op("pool", lambda e: e.memset(vcc[:], 1.0), writes=[b_vcc])
    for g in range(4):
        fw.dma("sp", lambda e, g=g: e.dma_start(out=vcc[:, g, 65:97], in_=cx.c2s[:, :]), reads=[b_vcc], writes=[b_vcc], partial=True)
    xT, b_xT = sb("xT_c", [64, 4, T], BF16), Buf()
    for ci, nm in ((0, "kc"), (1, "vc")):
        fw.dma("sp", lambda e, ci=ci: e.dma_start(out=xT[:], in_=cx.kvT_d[ci * 256:(ci + 1) * 256, :].rearrange("(g d) t -> d g t", d=64)), writes=[b_xT])
        for lh in range(2):
            fw.dma("sp", lambda e, ci=ci, lh=lh: e.dma_start(out=w1f[:], in_=cx.cmp_w1[ci, lh * 1024:(lh + 1) * 1024, :].rearrange("(l d) c -> d l c", d=64)), writes=[b_w1f])
            fw.op("pool", lambda e, lh=lh: e.tensor_copy(out=w1b[:, lh * 16:(lh + 1) * 16, :], in_=w1f[:]), reads=[b_w1f], writes=[b_w1b], partial=(lh == 1))
        fw.dma("sp", lambda e, ci=ci: e.dma_start(out=w2f[:], in_=cx.cmp_w2[ci].rearrange("(cc p) d -> p cc d", p=128)), writes=[b_w2f])
        fw.dma("sp", lambda e, ci=ci: e.dma_start(out=pef[:], in_=cx.peT[ci]), writes=[b_pef])
        fw.op("dve", lambda e: e.tensor_copy(out=w2b[:], in_=w2f[:]), reads=[b_w2f], writes=[b_w2b])
        fw.op("dve", lambda e: e.tensor_copy(out=peb[:], in_=pef[:]), reads=[b_pef], writes=[b_peb])
        for cc in range(2):
            pm, pmb = pM.next()
            for l in range(32):
                fw.op("pe", lambda e, pm=pm, l=l, cc=cc: e.matmul(pm[:, 0:1], lhsT=w1b[:, l, cc * 128:(cc + 1) * 128], rhs=peb[:, l:l + 1],
                      start=(l == 0), stop=(l == 31)), reads=[b_w1b, b_peb], writes=[pmb])
            fw.op("dve", lambda e, pm=pm, cc=cc: e.tensor_copy(out=pbias[:, cc:cc + 1], in_=pm[:, 0:1]), reads=[pmb], writes=[b_pbias])
            ph, phb = pS.next()
            for l in range(32):
                fw.op("pe", lambda e, ph=ph, l=l, cc=cc, xT=xT: e.matmul(
                    ph[:, 0:508].rearrange("p (g n) -> p g n", g=4), lhsT=w1b[:, l, cc * 128:(cc + 1) * 128],
                    rhs=xT[:, :, l:l + 2017:16], start=(l == 0), stop=(l == 31)), reads=[b_w1b, b_xT], writes=[phb])
            fw.op("dve", lambda e, ph=ph, cc=cc: e.tensor_scalar(out=gz[0][:], in0=ph[:, 0:508], scalar1=pbias[:, cc:cc + 1], scalar2=None, op0=ALU.add),
                  reads=[phb, b_pbias], writes=[b_gz[0]])
            fw.op("dve", lambda e: e.tensor_tensor(out=gz[1][:], in0=gz[0][:], in1=gz[0][:], op=ALU.mult), reads=[b_gz[0]], writes=[b_gz[1]])
            fw.op("dve", lambda e: e.tensor_scalar(out=gz[1][:], in0=gz[1][:], scalar1=0.044715, scalar2=1.0, op0=ALU.mult, op1=ALU.add),
                  reads=[b_gz[1]], writes=[b_gz[1]])
            fw.op("dve", lambda e: e.tensor_tensor(out=gz[1][:], in0=gz[1][:], in1=gz[0][:], op=ALU.mult), reads=[b_gz[0], b_gz[1]], writes=[b_gz[1]])
            fw.op("act", lambda e: e.activation(out=gz[2][:], in_=gz[1][:], func=AF.Tanh, scale=0.7978845608), reads=[b_gz[1]], writes=[b_gz[2]])
            fw.op("dve", lambda e: e.scalar_tensor_tensor(out=gz[2][:], in0=gz[2][:], scalar=1.0, in1=gz[0][:], op0=ALU.add, op1=ALU.mult),
                  reads=[b_gz[2], b_gz[0]], writes=[b_gz[2]])
            fw.op("dve", lambda e, cc=cc: e.tensor_scalar(out=hidT[:, cc, :], in0=gz[2][:], scalar1=0.5, scalar2=None, op0=ALU.mult),
                  reads=[b_gz[2]], writes=[b_hid], partial=(cc == 1))
        if ci == 0:
            pm, pmb = pM.next()
            for cc in range(2):
                fw.op("pe", lambda e, pm=pm, cc=cc: e.matmul(pm[0:64, 0:508], lhsT=w2b[:, cc, :], rhs=hidT[:, cc, :], start=(cc == 0), stop=(cc == 1)),
                      reads=[b_w2b, b_hid], writes=[pmb])
            fw.op("dve", lambda e, pm=pm: e.tensor_copy(out=kccT[:].rearrange("p g n -> p (g n)"), in_=pm[0:64, 0:508]), reads=[pmb], writes=[b_kcc])
        else:
            for g in range(4):
                pm, pmb = pM.next()
                for cc in range(2):
                    fw.op("pe", lambda e, pm=pm, cc=cc, g=g: e.matmul(pm[0:127, 0:64], lhsT=hidT[:, cc, g * 127:(g + 1) * 127], rhs=w2b[:, cc, :],
                          start=(cc == 0), stop=(cc == 1)), reads=[b_w2b, b_hid], writes=[pmb])
                fw.op("dve", lambda e, pm=pm, g=g: e.tensor_copy(out=vcc[:, g, 0:64], in_=pm[0:127, 0:64]), reads=[pmb, b_vcc], writes=[b_vcc], partial=True)
    if cx.dbg:
        fw.dma("sp", lambda e: e.dma_start(out=cx.dbg_kcc[:, :, :], in_=kccT[:]), reads=[b_kcc], writes=[Buf()])
        fw.dma("sp", lambda e: e.dma_start(out=cx.dbg_vcc[:, :, :], in_=vcc[:]), reads=[b_vcc], writes=[Buf()])

    es.__exit__(None, None, None)
    es = es_outer
    fw.barrier()
    qT, b_qT = sb("qT_s", [64, 16, T], BF16), Buf()
    for h4 in range(4):
        fw.dma("sp", lambda e, h4=h4: e.dma_start(
            out=qT[:, h4 * 4:(h4 + 1) * 4, :], in_=cx.qT_d[h4 * 256:(h4 + 1) * 256, :].rearrange("(h d) t -> d h t", d=64)),
            writes=[b_qT], partial=True)
    E_r = Ring([sb("E%d" % i, [128, 4, 128], BF16) for i in range(3)])
    Em_r = Ring([sb("Em%d" % i, [128, 4, 128], BF16) for i in range(3)])
    Mb_r = Ring([sb("Mb%d" % i, [128, 128], BF16) for i in range(2)])
    sm = Ring([sb("sm%d" % i, [128, 256], F32) for i in range(2)])
    selb_r = Ring([sb("selb%d" % i, [128, 32], BF16) for i in range(2)])
    selT_r = Ring([sb("selT%d" % i, [32, 128], BF16) for i in range(2)])
    o_r = Ring([sb("onsa%d" % i, [128, 1024], BF16) for i in range(2)])
    oT_r = Ring([sb("onsaT%d" % i, [128, 8, 128], BF16) for i in range(2)])
    acc_r = Ring([sb("oacc%d" % i, [128, 64], F32) for i in range(2)])
    kccT_, vs1, vw1 = kccT, v1["vs"], v1["vw"]
    ksT, b_ks = kT["ks"]
    kwT, b_kw = kT["kw"]

    def attn_branch(i, g, kts, kTt, b_k, v1t, pacc, b_pacc, mask_for):
        first = True
        for kt in kts:
            pst, psb = pS.next()
            fw.op("pe", lambda e, pst=pst, kt=kt: e.matmul(pst[:, :].rearrange("p (h t) -> p h t", h=4), lhsT=kTt[:, g, kt * 128:(kt + 1) * 128],
                  rhs=qT[:, 4 * g:4 * g + 4, i * 128:(i + 1) * 128], start=True, stop=True), reads=[b_k, b_qT], writes=[psb])
            Et, Eb = E_r.next()
            fw.op("act", lambda e, pst=pst, Et=Et: e.activation(out=Et[:].rearrange("p h t -> p (h t)"), in_=pst[:, :], func=AF.Exp, scale=SCALE),
                  reads=[psb], writes=[Eb])
            mk = mask_for(kt)
            if mk is None:
                lt, lb = Et, Eb
            else:
                mt, mb = mk
                lt, lb = Em_r.next()
                fw.op("dve", lambda e, lt=lt, Et=Et, mt=mt: e.tensor_tensor(out=lt[:], in0=Et[:], in1=mt[:].unsqueeze(1).to_broadcast([128, 4, 128]), op=ALU.mult),
                      reads=[Eb, mb], writes=[lb])
            for h in range(4):
                fw.op("pe", lambda e, lt=lt, h=h, kt=kt, first=first: e.matmul(pacc[:, h, :], lhsT=lt[:, h, :], rhs=v1t[0][:, kt, g, :],
                      start=(first and h == 0), stop=False, skip_group_check=True), reads=[lb, v1t[1]], writes=[b_pacc], partial=not (first and h == 0))
            first = False

    def do_group(i, g, ot, ob):
        pst, psb = pS.next()
        fw.op("pe", lambda e, pst=pst: e.matmul(pst[0:127, :].rearrange("p (h t) -> p h t", h=4), lhsT=kccT_[:, g, :],
              rhs=qT[:, 4 * g:4 * g + 4, i * 128:(i + 1) * 128], start=True, stop=True), reads=[b_kcc, b_qT], writes=[psb])
        Et, Eb = E_r.next()
        fw.op("act", lambda e, pst=pst, Et=Et: e.activation(out=Et[0:127].rearrange("p h t -> p (h t)"), in_=pst[0:127, :], func=AF.Exp, scale=SCALE),
              reads=[psb], writes=[Eb])
        Emt, Emb = Em_r.next()
        fw.op("dve", lambda e, Emt=Emt, Et=Et: e.tensor_tensor(out=Emt[0:127], in0=Et[0:127],
              in1=cmaskT[:, i * 128:(i + 1) * 128].unsqueeze(1).to_broadcast([127, 4, 128]), op=ALU.mult), reads=[Eb, b_cm], writes=[Emb])
        for h in range(4):
            fw.op("pe", lambda e, Emt=Emt, h=h: e.matmul(pc_t[:, h, :], lhsT=Emt[0:127, h, :], rhs=vcc[:, g, :], start=True, stop=True,
                  skip_group_check=True), reads=[Emb, b_vcc], writes=[b_pc], partial=(h > 0))
        s_, sb_ = sm.next()
        fw.op("dve", lambda e, s_=s_: e.tensor_scalar(out=s_[:, 0:4], in0=pc_t[:, :, 64], scalar1=1e-30, scalar2=None, op0=ALU.max), reads=[b_pc], writes=[sb_])
        fw.op("dve", lambda e, s_=s_: e.reciprocal(out=s_[:, 0:4], in_=s_[:, 0:4]), reads=[sb_], writes=[sb_])
        fw.op("dve", lambda e, s_=s_: e.scalar_tensor_tensor(out=s_[:, 32:64], in0=pc_t[:, 0, 65:97], scalar=s_[:, 0:1], in1=addc[:, i, :], op0=ALU.mult, op1=ALU.add),
              reads=[b_pc, sb_, b_addc], writes=[sb_])
        for h in range(1, 4):
            fw.op("dve", lambda e, s_=s_, h=h: e.scalar_tensor_tensor(out=s_[:, 32:64], in0=pc_t[:, h, 65:97], scalar=s_[:, h:h + 1], in1=s_[:, 32:64], op0=ALU.mult, op1=ALU.add),
                  reads=[b_pc, sb_], writes=[sb_])
        fw.op("dve", lambda e, s_=s_: e.max(out=s_[:, 96:104], in_=s_[:, 32:64]), reads=[sb_], writes=[sb_])
        fw.op("dve", lambda e, s_=s_: e.match_replace(out=s_[:, 64:96], in_to_replace=s_[:, 96:104], in_values=s_[:, 32:64], imm_value=-3.0e38), reads=[sb_], writes=[sb_])
        fw.op("dve", lambda e, s_=s_: e.max(out=s_[:, 104:112], in_=s_[:, 64:96]), reads=[sb_], writes=[sb_])
        fw.op("dve", lambda e, s_=s_: e.tensor_scalar(out=s_[:, 112:113], in0=s_[:, 111:112], scalar1=-1.0e8, scalar2=None, op0=ALU.max), reads=[sb_], writes=[sb_])
        selb, selbb = selb_r.next()
        fw.op("dve", lambda e, s_=s_, selb=selb: e.tensor_scalar(out=selb[:], in0=s_[:, 32:64], scalar1=s_[:, 112:113], scalar2=None, op0=ALU.is_ge), reads=[sb_], writes=[selbb])
        ptt, ptb = pT.next()
        fw.op("pe", lambda e, ptt=ptt, selb=selb: e.transpose(out=ptt[0:32, 0:128], in_=selb[:], identity=ident_b[:]), reads=[selbb, b_idb], writes=[ptb])
        selT, selTb = selT_r.next()
        fw.op("dve", lambda e, ptt=ptt, selT=selT: e.tensor_copy(out=selT[:], in_=ptt[0:32, 0:128]), reads=[ptb], writes=[selTb])
        if cx.dbg and g == 0:
            fw.dma("sp", lambda e, selb=selb: e.dma_start(out=cx.dbg_sel[i * 128:(i + 1) * 128, :], in_=selb[:]), reads=[selbb], writes=[Buf()])
            fw.dma("sp", lambda e, s_=s_: e.dma_start(out=cx.dbg_sm[i * 128:(i + 1) * 128, :], in_=s_[:]), reads=[sb_], writes=[Buf()])

        def sel_mask(kt, selT=selT, selTb=selTb):
            pm, pmb = pM.next()
            fw.op("pe", lambda e, pm=pm: e.matmul(pm[:, 0:128], lhsT=Xall[:, kt * 128:(kt + 1) * 128], rhs=selT[:], start=True, stop=True),
                  reads=[b_X, selTb], writes=[pmb])
            mt, mb = Mb_r.next()
            if kt == i:
                fw.op("dve", lambda e, pm=pm, mt=mt: e.tensor_tensor(out=mt[:], in0=pm[:, 0:128], in1=tri[:], op=ALU.mult), reads=[pmb, b_tri], writes=[mb])
            else:
                fw.op("dve", lambda e, pm=pm, mt=mt: e.tensor_copy(out=mt[:], in_=pm[:, 0:128]), reads=[pmb], writes=[mb])
            return mt, mb
        attn_branch(i, g, list(range(0, i + 1)), ksT, b_ks, vs1, pss_t, b_pss, sel_mask)

        def win_mask(kt):
            if kt == i:
                return tri, b_tri
            if kt == i - 4:
                return tri2, b_tri2
            return None
        attn_branch(i, g, list(range(max(0, i - 4), i + 1)), kwT, b_kw, vw1, psw_t, b_psw, win_mask)

        fw.op("dve", lambda e, s_=s_: e.reciprocal(out=s_[:, 4:8], in_=pss_t[:, :, 64]), reads=[b_pss, sb_], writes=[sb_])
        fw.op("dve", lambda e, s_=s_: e.reciprocal(out=s_[:, 8:12], in_=psw_t[:, :, 64]), reads=[b_psw, sb_], writes=[sb_])
        gav = ga[:, i, g * 12:(g + 1) * 12].rearrange("p (h b) -> p b h", b=3)
        fw.op("dve", lambda e, s_=s_, gav=gav: e.tensor_tensor(out=s_[:, 12:24].rearrange("p (b h) -> p b h", b=3), in0=gav,
              in1=s_[:, 0:12].rearrange("p (b h) -> p b h", b=3), op=ALU.mult), reads=[sb_, b_ga], writes=[sb_])
        for h in range(4):
            at, ab = acc_r.next()
            fw.op("dve", lambda e, s_=s_, at=at, h=h: e.tensor_scalar(out=at[:], in0=pc_t[:, h, 0:64], scalar1=s_[:, 12 + h:13 + h], scalar2=None, op0=ALU.mult),
                  reads=[b_pc, sb_], writes=[ab])
            fw.op("dve", lambda e, s_=s_, at=at, h=h: e.scalar_tensor_tensor(out=at[:], in0=pss_t[:, h, 0:64], scalar=s_[:, 16 + h:17 + h], in1=at[:], op0=ALU.mult, op1=ALU.add),
                  reads=[b_pss, sb_, ab], writes=[ab])
            c0 = (4 * g + h) * 64
            fw.op("dve", lambda e, s_=s_, at=at, h=h, ot=ot, c0=c0: e.scalar_tensor_tensor(out=ot[:, c0:c0 + 64], in0=psw_t[:, h, 0:64], scalar=s_[:, 20 + h:21 + h], in1=at[:], op0=ALU.mult, op1=ALU.add),
                  reads=[b_psw, sb_, ab], writes=[ob], partial=True)

    def finish_tile(i, ot, ob):
        ptt, ptb = pT.next()
        for j in range(8):
            fw.op("pe", lambda e, ptt=ptt, ot=ot, j=j: e.transpose(out=ptt[:, j * 128:(j + 1) * 128], in_=ot[:, j * 128:(j + 1) * 128], identity=ident_b[:]),
                  reads=[ob, b_idb], writes=[ptb], partial=(j > 0))
        oT, oTb = oT_r.next()
        fw.op("dve", lambda e, ptt=ptt, oT=oT: e.tensor_copy(out=oT[:].rearrange("p j t -> p (j t)"), in_=ptt[:, :]), reads=[ptb], writes=[oTb])
        fw.dma("sp", lambda e, oT=oT, i=i: e.dma_start(out=cx.onT_d[:, i * 128:(i + 1) * 128].rearrange("(j p) t -> p j t", p=128), in_=oT[:]),
               reads=[oTb], writes=[Buf()])

    for i in range(16):
        ot, ob = o_r.next()
        for g in range(4):
            do_group(i, g, ot, ob)
        finish_tile(i, ot, ob)


def mlstm_consts():
    import numpy as np
    import ml_dtypes
    a = np.arange(128)
    tri = (a[:, None] <= a[None, :])
    ms = np.zeros((4, 128, 512), np.float32)
    for r in range(4):
        for j in range(4):
            if j == r:
                ms[r][:, j * 128:(j + 1) * 128] = tri
            elif j > r:
                ms[r][:, j * 128:(j + 1) * 128] = 1.0
    return {"mmask": ms.astype(ml_dtypes.bfloat16)}


def declare_io_c(nc, cx, dbg=False):
    def din(name, shape, dt=F32):
        setattr(cx, name, nc.dram_tensor(name, list(shape), dt, kind="ExternalInput").ap())

    def dscr(name, shape, dt, force_out=False):
        setattr(cx, name, nc.dram_tensor(name, list(shape), dt, kind="ExternalOutput" if (dbg or force_out) else "Internal").ap())
    din("mmask", [4, 128, 512], BF16)
    din("convp", [128, 8, 5])
    din("bgate", [4, 2])
    din("mhg", [1, 1024])
    din("w_up_p", [2 * 1024 * D])
    din("w_out_p", [D * D])
    dscr("omT_d", [1024, T], BF16)
    dscr("ac_d", [8, T], F32)
    dscr("x1_d", [T, D], F32)


def stage_mlstm(nc, fw, cx, es):
    def sb(name, shape, dt):
        return es.enter_context(nc.sbuf_tensor(name, list(shape), dt))

    def ps(name, shape, dt):
        return es.enter_context(nc.psum_tensor(name, list(shape), dt))
    ident_b, b_idb, ident_f, b_id = cx.ident_b, cx.b_idb, cx.ident_f, cx.b_id

    mmask, b_mm = sb("mmask_s", [128, 4, 512], BF16), Buf()
    fw.dma("sp", lambda e: e.dma_start(out=mmask[:], in_=cx.mmask.rearrange("r p t -> p r t")), writes=[b_mm])
    convp, b_cp = sb("convp_s", [128, 8, 5], F32), Buf()
    fw.dma("sp", lambda e: e.dma_start(out=convp[:], in_=cx.convp[:, :, :]), writes=[b_cp])
    bg, b_bg = sb("bg_s", [4, 2], F32), Buf()
    fw.dma("sp", lambda e: e.dma_start(out=bg[:], in_=cx.bgate[:, :]), writes=[b_bg])
    mhg, b_mhg = sb("mhg_s", [128, 1024], F32), Buf()
    fw.dma("sp", lambda e: e.dma_start(out=mhg[:], in_=cx.mhg[0, :].partition_broadcast(128)), writes=[b_mhg])
    vm1, b_vm = sb("vm1", [128, 16, 4, 257], BF16), Buf()
    fw.op("pool", lambda e: e.memset(vm1[:], 1.0), writes=[b_vm])
    for i in range(16):
        fw.dma("sp", lambda e, i=i: e.dma_start(out=vm1[:, i, :, 0:256], in_=cx.vm_d[i * 128:(i + 1) * 128, :].rearrange("p (h d) -> p h d", h=4)),
               reads=[b_vm], writes=[b_vm], partial=True)

    qkb, b_qkb = sb("qkb", [128, 8, T], BF16), [Buf() for _ in range(8)]
    xin = Ring([sb("cx%d" % i, [128, T + 3], F32) for i in range(2)])
    yv = Ring([sb("cy%d" % i, [128, T], F32) for i in range(2)])
    for c in range(8):
        xt, xb = xin.next()
        fw.op("pool", lambda e, xt=xt: e.memset(xt[:, 0:3], 0.0), writes=[xb])
        fw.dma("sp", lambda e, xt=xt, c=c: e.dma_start(out=xt[:, 3:T + 3], in_=cx.qkT_d[c * 128:(c + 1) * 128, :]), reads=[xb], writes=[xb], partial=True)
        yt, yb = yv.next()
        fw.op("dve", lambda e, xt=xt, yt=yt, c=c: e.tensor_scalar(out=yt[:], in0=xt[:, 0:T], scalar1=convp[:, c, 0:1], scalar2=convp[:, c, 4:5], op0=ALU.mult, op1=ALU.add),
              reads=[xb, b_cp], writes=[yb])
        for k in range(1, 4):
            fw.op("dve", lambda e, xt=xt, yt=yt, c=c, k=k: e.scalar_tensor_tensor(out=yt[:], in0=xt[:, k:k + T], scalar=convp[:, c, k:k + 1], in1=yt[:], op0=ALU.mult, op1=ALU.add),
                  reads=[xb, b_cp, yb], writes=[yb])
        if c < 4:
            fw.op("act", lambda e, yt=yt, c=c: e.activation(out=qkb[:, c, :], in_=yt[:], func=AF.Silu), reads=[yb], writes=[b_qkb[c]])
        else:
            fw.op("act", lambda e, yt=yt: e.activation(out=yt[:], in_=yt[:], func=AF.Silu), reads=[yb], writes=[yb])
            fw.op("dve", lambda e, yt=yt, c=c: e.tensor_scalar(out=qkb[:, c, :], in0=yt[:], scalar1=128 ** -0.5, scalar2=None, op0=ALU.mult), reads=[yb], writes=[b_qkb[c]])

    gi, b_gi = sb("gi", [4, T], F32), Buf()
    gf, b_gf = sb("gf", [4, T], F32), Buf()
    ga_, b_ga = sb("ga_", [4, T], F32), Buf()
    ones4, b_o4 = sb("ones4", [4, T], F32), Buf()
    fw.dma("sp", lambda e: e.dma_start(out=gi[:], in_=cx.ifT_d[0:4, :]), writes=[b_gi])
    fw.dma("sp", lambda e: e.dma_start(out=gf[:], in_=cx.ifT_d[4:8, :]), writes=[b_gf])
    fw.op("pool", lambda e: e.memset(ones4[:], 1.0), writes=[b_o4])
    fw.op("dve", lambda e: e.tensor_scalar(out=gf[:], in0=gf[:], scalar1=bg[:, 1:2], scalar2=None, op0=ALU.add), reads=[b_gf, b_bg], writes=[b_gf])
    fw.op("act", lambda e: e.activation(out=gf[:], in_=gf[:], func=AF.Exp, scale=-1.0), reads=[b_gf], writes=[b_gf])
    fw.op("dve", lambda e: e.tensor_scalar(out=gf[:], in0=gf[:], scalar1=1.0, scalar2=None, op0=ALU.add), reads=[b_gf], writes=[b_gf])
    fw.op("act", lambda e: e.activation(out=gf[:], in_=gf[:], func=AF.Ln), reads=[b_gf], writes=[b_gf])
    fw.op("dve", lambda e: e.tensor_scalar(out=gf[:], in0=gf[:], scalar1=-1.0, scalar2=None, op0=ALU.mult), reads=[b_gf], writes=[b_gf])
    fw.op("dve", lambda e: e.tensor_tensor_scan(out=ga_[:], data0=ones4[:], data1=gf[:], initial=0.0, op0=ALU.mult, op1=ALU.add),
          reads=[b_gf, b_o4], writes=[b_ga])
    fw.op("dve", lambda e: e.scalar_tensor_tensor(out=gi[:], in0=gi[:], scalar=bg[:, 0:1], in1=ga_[:], op0=ALU.add, op1=ALU.subtract),
          reads=[b_gi, b_bg, b_ga], writes=[b_gi])
    b_acd = Buf()
    fw.dma("sp", lambda e: e.dma_start(out=cx.ac_d[0:4, :], in_=ga_[:]), reads=[b_ga], writes=[b_acd])
    pmisc, b_pmisc = ps("pmisc_m", [128, 512], F32), Buf()
    for i in range(16):
        fw.op("pe", lambda e, i=i: e.transpose(out=pmisc[:, i * 4:(i + 1) * 4], in_=gi[:, i * 128:(i + 1) * 128], identity=ident_f[0:4, 0:4]),
              reads=[b_gi, b_id], writes=[b_pmisc], partial=(i > 0))
    cT, b_cT = sb("cT_m", [128, 16, 4], F32), Buf()
    fw.op("dve", lambda e: e.tensor_copy(out=cT[:].rearrange("p i h -> p (i h)"), in_=pmisc[:, 0:64]), reads=[b_pmisc], writes=[b_cT])

    pS = Ring([ps("pSm%d" % i, [128, 512], F32) for i in range(2)])
    pacc = [ps("paccm%d" % i, [128, 512], F32) for i in range(4)]
    b_pacc = [Buf() for _ in range(4)]
    pT = Ring([ps("pTm%d" % i, [128, 1024], BF16) for i in range(1)])
    Abc_r = Ring([sb("Abc%d" % i, [128, T], F32) for i in range(2)])
    D_r = Ring([sb("Dm%d" % i, [128, 512], F32) for i in range(3)])
    W_r = Ring([sb("Wm%d" % i, [128, 512], BF16) for i in range(3)])
    om_r = Ring([sb("omt%d" % i, [128, 256], BF16) for i in range(2)])
    hc_r = Ring([sb("hc%d" % i, [128, 256], F32) for i in range(2)])
    hj_r = Ring([sb("hj%d" % i, [128, 256], F32) for i in range(2)])
    ho_r = Ring([sb("ho%d" % i, [128, 256], BF16) for i in range(2)])
    hT_r = Ring([sb("hoT%d" % i, [128, 2, 128], BF16) for i in range(2)])
    st_r = Ring([sb("stm%d" % i, [128, 8], F32) for i in range(2)])

    def head_chunk(h, c, Abc, Abcb):
        for kt in range(4 * c + 4):
            pst, psb = pS.next()
            fw.op("pe", lambda e, pst=pst, kt=kt: e.matmul(pst[:, :], lhsT=qkb[:, 4 + h, kt * 128:(kt + 1) * 128], rhs=qkb[:, h, c * 512:(c + 1) * 512],
                  start=True, stop=True), reads=[b_qkb[4 + h], b_qkb[h]], writes=[psb])
            Dt, Db = D_r.next()
            fw.op("act", lambda e, Dt=Dt, kt=kt: e.activation(out=Dt[:], in_=Abc[:, c * 512:(c + 1) * 512], func=AF.Exp, bias=cT[:, kt, h:h + 1]),
                  reads=[Abcb, b_cT], writes=[Db])
            Wt, Wb = W_r.next()
            if kt // 4 == c:
                fw.op("dve", lambda e, Dt=Dt, kt=kt: e.tensor_tensor(out=Dt[:], in0=Dt[:], in1=mmask[:, kt % 4, :], op=ALU.mult), reads=[Db, b_mm], writes=[Db])
            fw.op("dve", lambda e, Dt=Dt, Wt=Wt, pst=pst: e.tensor_tensor(out=Wt[:], in0=pst[:, :], in1=Dt[:], op=ALU.mult), reads=[psb, Db], writes=[Wb])
            for j in range(4):
                tj = 4 * c + j
                if tj < kt:
                    continue
                fw.op("pe", lambda e, Wt=Wt, j=j, kt=kt, tj=tj: e.matmul(pacc[j][:, 0:257], lhsT=Wt[:, j * 128:(j + 1) * 128], rhs=vm1[:, kt, h, :],
                      start=(kt == 0), stop=(kt == tj)), reads=[Wb, b_vm], writes=[b_pacc[j]])
        for j in range(4):
            tj = 4 * c + j
            st, stb = st_r.next()
            fw.op("dve", lambda e, st=st, j=j: e.tensor_scalar(out=st[:, 6:7], in0=pacc[j][:, 256:257], scalar1=-1.0, scalar2=None, op0=ALU.mult),
                  reads=[b_pacc[j]], writes=[stb])
            fw.op("dve", lambda e, st=st, j=j: e.tensor_tensor(out=st[:, 7:8], in0=st[:, 6:7], in1=pacc[j][:, 256:257], op=ALU.max),
                  reads=[b_pacc[j], stb], writes=[stb])
            fw.op("dve", lambda e, st=st, j=j: e.tensor_scalar(out=st[:, 0:1], in0=st[:, 7:8], scalar1=1.0, scalar2=None, op0=ALU.max),
                  reads=[stb], writes=[stb])
            fw.op("dve", lambda e, st=st: e.reciprocal(out=st[:, 1:2], in_=st[:, 0:1]), reads=[stb], writes=[stb])
            hc, hcb = hc_r.next()
            fw.op("dve", lambda e, st=st, hc=hc, j=j: e.tensor_scalar(out=hc[:], in0=pacc[j][:, 0:256], scalar1=st[:, 1:2], scalar2=None, op0=ALU.mult),
                  reads=[b_pacc[j], stb], writes=[hcb])
            hj, hjb = hj_r.next()
            fw.op("dve", lambda e, st=st, hc=hc, hj=hj: e.scalar_tensor_tensor(out=hj[:], in0=hc[:], scalar=1.0 / 256, in1=hc[:], op0=ALU.mult, op1=ALU.mult, accum_out=st[:, 2:3]),
                  reads=[hcb], writes=[hjb, stb])
            fw.op("dve", lambda e, st=st: e.tensor_scalar(out=st[:, 3:4], in0=st[:, 2:3], scalar1=EPS, scalar2=None, op0=ALU.add), reads=[stb], writes=[stb])
            fw.op("act", lambda e, st=st: e.activation(out=st[:, 4:5], in_=st[:, 3:4], func=AF.Sqrt), reads=[stb], writes=[stb])
            fw.op("dve", lambda e, st=st: e.reciprocal(out=st[:, 5:6], in_=st[:, 4:5]), reads=[stb], writes=[stb])
            omt, omb = om_r.next()
            fw.dma("sp", lambda e, omt=omt, tj=tj: e.dma_start(out=omt[:], in_=cx.om_d[tj * 128:(tj + 1) * 128, h * 256:(h + 1) * 256]), writes=[omb])
            fw.op("dve", lambda e, st=st, hc=hc, hj=hj: e.scalar_tensor_tensor(out=hj[:], in0=hc[:], scalar=st[:, 5:6], in1=mhg[:, h * 256:(h + 1) * 256], op0=ALU.mult, op1=ALU.mult),
                  reads=[hcb, stb, b_mhg], writes=[hjb])
            ho, hob = ho_r.next()
            fw.op("dve", lambda e, hj=hj, ho=ho, omt=omt: e.tensor_tensor(out=ho[:], in0=hj[:], in1=omt[:], op=ALU.mult), reads=[hjb, omb], writes=[hob])
            ptt, ptb = pT.next()
            for bk in range(2):
                fw.op("pe", lambda e, ptt=ptt, ho=ho, bk=bk: e.transpose(out=ptt[:, bk * 128:(bk + 1) * 128], in_=ho[:, bk * 128:(bk + 1) * 128], identity=ident_b[:]),
                      reads=[hob, b_idb], writes=[ptb], partial=(bk > 0))
            hT, hTb = hT_r.next()
            fw.op("dve", lambda e, ptt=ptt, hT=hT: e.tensor_copy(out=hT[:].rearrange("p b t -> p (b t)"), in_=ptt[:, 0:256]), reads=[ptb], writes=[hTb])
            fw.dma("sp", lambda e, hT=hT, tj=tj: e.dma_start(out=cx.omT_d[h * 256:(h + 1) * 256, tj * 128:(tj + 1) * 128].rearrange("(b p) t -> p b t", p=128), in_=hT[:]),
                   reads=[hTb], writes=[Buf()])

    for h in range(4):
        Abc, Abcb = Abc_r.next()
        fw.dma("sp", lambda e, Abc=Abc, h=h: e.dma_start(out=Abc[:], in_=cx.ac_d[h, :].partition_broadcast(128)), reads=[b_acd], writes=[Abcb])
        for c in range(4):
            head_chunk(h, c, Abc, Abcb)


def stage_merge(nc, fw, cx, es):
    from contextlib import ExitStack
    stack = [es]

    def sb(name, shape, dt):
        return stack[-1].enter_context(nc.sbuf_tensor(name, list(shape), dt))

    def ps(name, shape, dt):
        return stack[-1].enter_context(nc.psum_tensor(name, list(shape), dt))
    Grow, b_Grow = cx.Grow, cx.b_Grow
    mT = sb("mT", [128, KC, T], BF16)
    b_mT = [Buf() for _ in range(4)]
    pA = Ring([ps("pA%d" % i, [128, 512], F32) for i in range(2)])
    pB = Ring([ps("pB%d" % i, [128, 512], F32) for i in range(2)])
    pY = [ps("pY%d" % i, [128, 512], F32) for i in range(4)]
    b_pY = [Buf() for _ in range(4)]
    cast_tog = [0]

    stack.append(ExitStack())
    oT = [sb("oTa", [128, 8, T], BF16), sb("oTb", [128, 8, T], BF16)]
    b_oT = [Buf(), Buf()]
    for k, src in enumerate((cx.onT_d, cx.omT_d)):
        for j2 in range(2):
            fw.dma("sp", lambda e, k=k, src=src, j2=j2: e.dma_start(out=oT[k][:, j2 * 4:(j2 + 1) * 4, :],
                   in_=src[j2 * 512:(j2 + 1) * 512, :].rearrange("(j p) t -> p j t", p=128)), writes=[b_oT[k]], partial=True)
    wf = Ring([sb("wuf%d" % i, [128, 8, 256], F32) for i in range(2)])
    wb = Ring([sb("wub%d" % i, [128, 8, 512], BF16) for i in range(4)])
    gt_r = Ring([sb("gtile%d" % i, [128, 512], BF16) for i in range(4)])
    m1_r = Ring([sb("m1_%d" % i, [128, 512], F32) for i in range(2)])

    def load_up(which, cc):
        wbt, wbb = wb.next()
        for h0 in range(2):
            wt, wtb = wf.next()
            o = which * 1024 * D + (cc * 512 + h0 * 256) * 128 * 8
            fw.dma("sp", lambda e, wt=wt, o=o: e.dma_start(out=wt[:], in_=cx.w_up_p[o:o + 128 * 8 * 256].rearrange("(p k n) -> p k n", p=128, k=8)), writes=[wtb])
            eng = "pool" if cast_tog[0] % 2 == 0 else "dve"
            cast_tog[0] += 1
            fw.op(eng, lambda e, wt=wt, wbt=wbt, h0=h0: e.tensor_copy(out=wbt[:, :, h0 * 256:(h0 + 1) * 256], in_=wt[:]), reads=[wtb], writes=[wbb], partial=True)
        return wbt, wbb

    for cc in range(4):
        wa, wab = load_up(0, cc)
        wbm, wbmb = load_up(1, cc)
        for m in range(4):
            fc = cc * 4 + m
            for tc in range(4):
                pa, pab = pA.next()
                pb_, pbb = pB.next()
                for (pt, ptb, w, wbuf, k) in ((pa, pab, wa, wab, 0), (pb_, pbb, wbm, wbmb, 1)):
                    for kc in range(8):
                        fw.op("pe", lambda e, pt=pt, w=w, kc=kc, m=m, tc=tc, k=k: e.matmul(pt[:, :], lhsT=w[:, kc, m * 128:(m + 1) * 128],
                              rhs=oT[k][:, kc, tc * 512:(tc + 1) * 512], start=(kc == 0), stop=(kc == 7)), reads=[wbuf, b_oT[k]], writes=[ptb])
                g1, g1b = gt_r.next()
                g2, g2b = gt_r.next()
                fw.dma("sp", lambda e, g1=g1, fc=fc, tc=tc: e.dma_start(out=g1[:], in_=cx.gaT_d[fc * 128:(fc + 1) * 128, tc * 512:(tc + 1) * 512]), writes=[g1b])
                fw.dma("sp", lambda e, g2=g2, fc=fc, tc=tc: e.dma_start(out=g2[:], in_=cx.gbT_d[fc * 128:(fc + 1) * 128, tc * 512:(tc + 1) * 512]), writes=[g2b])
                m1, m1b = m1_r.next()
                m2, m2b = m1_r.next()
                fw.op("dve", lambda e, m1=m1, pa=pa, g1=g1: e.tensor_tensor(out=m1[:], in0=pa[:, :], in1=g1[:], op=ALU.mult), reads=[pab, g1b], writes=[m1b])
                fw.op("dve", lambda e, m2=m2, pb_=pb_, g2=g2: e.tensor_tensor(out=m2[:], in0=pb_[:, :], in1=g2[:], op=ALU.mult), reads=[pbb, g2b], writes=[m2b])
                fw.op("pool", lambda e, m1=m1, m2=m2, fc=fc, tc=tc: e.tensor_tensor(out=mT[:, fc, tc * 512:(tc + 1) * 512], in0=m1[:], in1=m2[:], op=ALU.add),
                      reads=[m1b, m2b], writes=[b_mT[tc]], partial=True)
    stack.pop().close()
    fw.barrier()

    stack.append(ExitStack())
    wo = sb("wo_b", [128, KC, D], BF16)
    b_wo = [Buf() for _ in range(4)]
    wof = Ring([sb("wof%d" % i, [128, KC, 128], F32) for i in range(2)])
    cx.junk_f, cx.b_junk_f, cx.b_x1d = sb("junk_f", [128, D], BF16), Buf(), Buf()
    for c16 in range(16):
        wt, wtb = wof.next()
        o = c16 * 128 * 128 * KC
        fw.dma("sp", lambda e, wt=wt, o=o: e.dma_start(out=wt[:], in_=cx.w_out_p[o:o + 128 * KC * 128].rearrange("(p k n) -> p k n", p=128, k=KC)), writes=[wtb])
        eng = "pool" if c16 % 2 == 0 else "dve"
        fw.op(eng, lambda e, wt=wt, c16=c16: e.tensor_copy(out=wo[:, :, c16 * 128:(c16 + 1) * 128], in_=wt[:]), reads=[wtb], writes=[b_wo[c16 // 4]], partial=True)
    xt_r = Ring([sb("xm%d" % i, [128, D], F32) for i in range(2)])
    y_r = Ring([sb("ym%d" % i, [128, D], F32) for i in range(2)])
    st_r = Ring([sb("stz%d" % i, [128, 8], F32) for i in range(2)])
    for i in range(16):
        for n in range(4):
            for fc in range(KC):
                fw.op("pe", lambda e, i=i, n=n, fc=fc: e.matmul(pY[n][:, :], lhsT=mT[:, fc, i * 128:(i + 1) * 128], rhs=wo[:, fc, n * 512:(n + 1) * 512],
                      start=(fc == 0), stop=(fc == KC - 1)), reads=[b_mT[i // 4], b_wo[n]], writes=[b_pY[n]])
        yt, yb = y_r.next()
        for n in range(4):
            if n % 2 == 0:
                fw.op("act", lambda e, yt=yt, n=n: e.activation(out=yt[:, n * 512:(n + 1) * 512], in_=pY[n][:, :], func=AF.Identity), reads=[b_pY[n]], writes=[yb], partial=True)
            else:
                fw.op("dve", lambda e, yt=yt, n=n: e.tensor_copy(out=yt[:, n * 512:(n + 1) * 512], in_=pY[n][:, :]), reads=[b_pY[n]], writes=[yb], partial=True)
        post_norm_residual(fw, cx, i, yt, yb, xt_r, st_r, cx.x, Grow[0], b_Grow[0], cx.x1_d)
    stack.pop().close()


def post_norm_residual(fw, cx, i, yt, yb, xt_r, st_r, x_src, G, b_G, dst, x_dep=None):
    xt, xb = xt_r.next()
    fw.dma("sp", lambda e: e.dma_start(out=xt[:], in_=x_src[i * 128:(i + 1) * 128, :]), writes=[xb])
    st, stb = st_r.next()
    fw.op("dve", lambda e: e.scalar_tensor_tensor(out=cx.junk_f[:], in0=yt[:], scalar=1.0 / D, in1=yt[:], op0=ALU.mult, op1=ALU.mult, accum_out=st[:, 0:1]),
          reads=[yb], writes=[cx.b_junk_f, stb])
    fw.op("dve", lambda e: e.tensor_scalar(out=st[:, 1:2], in0=st[:, 0:1], scalar1=EPS, scalar2=None, op0=ALU.add), reads=[stb], writes=[stb])
    fw.op("act", lambda e: e.activation(out=st[:, 2:3], in_=st[:, 1:2], func=AF.Sqrt), reads=[stb], writes=[stb])
    fw.op("dve", lambda e: e.reciprocal(out=st[:, 3:4], in_=st[:, 2:3]), reads=[stb], writes=[stb])
    fw.op("dve", lambda e: e.scalar_tensor_tensor(out=yt[:], in0=yt[:], scalar=st[:, 3:4], in1=G[:], op0=ALU.mult, op1=ALU.mult), reads=[yb, stb, b_G], writes=[yb])
    fw.op("pool", lambda e: e.tensor_tensor(out=xt[:], in0=xt[:], in1=yt[:], op=ALU.add), reads=[xb, yb], writes=[xb])
    fw.dma("sp", lambda e: e.dma_start(out=dst[i * 128:(i + 1) * 128, :], in_=xt[:]), reads=[xb], writes=[cx.b_x1d], partial=True)


NE = 65


def pack_gu(w):
    return pack_cols(w, [(0, 512)])[0]


def pack_dn(w):
    import numpy as np
    return np.concatenate([np.ascontiguousarray(w[:, h * 1024:(h + 1) * 1024].reshape(4, 128, 1024).transpose(1, 0, 2)).reshape(-1) for h in range(2)])


def declare_io_d(nc, cx, dbg=False):
    def din(name, shape, dt=F32):
        setattr(cx, name, nc.dram_tensor(name, list(shape), dt, kind="ExternalInput").ap())

    def dscr(name, shape, dt, out=False):
        setattr(cx, name, nc.dram_tensor(name, list(shape), dt, kind="ExternalOutput" if (dbg or out) else "Internal").ap())
    din("w_router", [D, 64])
    din("b_router", [1, 64])
    din("w_eg_p", [NE * D * 512])
    din("w_eu_p", [NE * D * 512])
    din("w_ed_p", [NE * 512 * D])
    dscr("h2T_d", [128, KC * T], BF16)
    dscr("out", [T, D], F32, out=True)
    if dbg:
        dscr("dbg_wr", [128, 16, NE], F32)


def stage_router(nc, fw, cx, es):
    def sb(name, shape, dt):
        return es.enter_context(nc.sbuf_tensor(name, list(shape), dt))

    def ps(name, shape, dt):
        return es.enter_context(nc.psum_tensor(name, list(shape), dt))
    Wr, b_Wr = cx.Wr, cx.b_Wr
    h2T = sb("h2T", [128, KC, T], BF16)
    b_h2T = [Buf() for _ in range(16)]
    xt_r = Ring([sb("xt2_%d" % i, [128, D], F32) for i in range(2)])
    xn_r = Ring([sb("xn2_%d" % i, [128, D], BF16) for i in range(2)])
    junk = sb("junk2", [128, D], BF16)
    st_r = Ring([sb("stat2_%d" % i, [128, 4], F32) for i in range(2)])
    ptr = Ring([ps("ptr2_%d" % i, [128, 1024], BF16) for i in range(2)])
    norm_pre(nc, fw, cx, cx.x1_d, h2T, b_h2T, xt_r, xn_r, junk, Buf(), st_r, ptr, 1, x_dep=[cx.b_x1d])
    for i4 in range(4):
        fw.dma("sp", lambda e, i4=i4: e.dma_start(out=cx.h2T_d.rearrange("p (k t) -> p k t", k=KC)[:, :, i4 * 512:(i4 + 1) * 512], in_=h2T[:, :, i4 * 512:(i4 + 1) * 512]),
               reads=b_h2T[i4 * 4:(i4 + 1) * 4], writes=[cx.b_h2Td], partial=True)
    wrf, b_wrf = sb("wrf", [128, KC, 64], F32), Buf()
    wrb, b_wrb = sb("wrb", [128, KC, 64], BF16), Buf()
    brt, b_brt = sb("brt", [128, 64], F32), Buf()
    fw.dma("sp", lambda e: e.dma_start(out=wrf[:], in_=cx.w_router.rearrange("(k p) n -> p k n", p=128)), writes=[b_wrf])
    fw.dma("sp", lambda e: e.dma_start(out=brt[:], in_=cx.b_router[0, :].partition_broadcast(128)), writes=[b_brt])
    fw.op("dve", lambda e: e.tensor_copy(out=wrb[:], in_=wrf[:]), reads=[b_wrf], writes=[b_wrb])
    fw.op("pool", lambda e: e.memset(Wr[:], 1.0), writes=[b_Wr])
    pl = Ring([ps("plog%d" % i, [128, 512], F32) for i in range(2)])
    r_r = Ring([sb("rt%d" % i, [128, 512], F32) for i in range(2)])

    def route_tile(i):
        pt, pb = pl.next()
        for kc in range(KC):
            fw.op("pe", lambda e, kc=kc: e.matmul(pt[:, 0:64], lhsT=h2T[:, kc, i * 128:(i + 1) * 128], rhs=wrb[:, kc, :], start=(kc == 0), stop=(kc == KC - 1)),
                  reads=[b_h2T[i], b_wrb], writes=[pb])
        r, rb = r_r.next()
        S, SB, M8, GS, M, GM, T1, SBM, M8B, SEL = (r[:, 0:64], r[:, 64:128], r[:, 128:192], r[:, 192:200], r[:, 200:208], r[:, 208:216],
                                                  r[:, 216:224], r[:, 224:288], r[:, 288:296], r[:, 296:360])
        ops = []
        fw.op("act", lambda e: e.activation(out=S, in_=pt[:, 0:64], func=AF.Sigmoid), reads=[pb], writes=[rb])
        dv = lambda f, extra=(): fw.op("dve", f, reads=[rb] + list(extra), writes=[rb])
        dv(lambda e: e.tensor_tensor(out=SB, in0=S, in1=brt[:], op=ALU.add), [b_brt])
        for g in range(8):
            dv(lambda e, g=g: e.max(out=r[:, 128 + g * 8:136 + g * 8], in_=r[:, 64 + g * 8:72 + g * 8]))
        m8v = M8.rearrange("p (g k) -> p g k", k=8)
        dv(lambda e: e.tensor_tensor(out=GS, in0=m8v[:, :, 0], in1=m8v[:, :, 1], op=ALU.add))
        dv(lambda e: e.max(out=M, in_=GS))
        dv(lambda e: e.tensor_scalar(out=GM, in0=GS, scalar1=r[:, 203:204], scalar2=None, op0=ALU.is_ge))
        dv(lambda e: e.tensor_scalar(out=T1, in0=GM, scalar1=1.0e9, scalar2=-1.0e9, op0=ALU.mult, op1=ALU.add))
        dv(lambda e: e.tensor_tensor(out=SBM.rearrange("p (g k) -> p g k", k=8), in0=SB.rearrange("p (g k) -> p g k", k=8),
                                     in1=GM.unsqueeze(2).to_broadcast([128, 8, 8]), op=ALU.mult))
        dv(lambda e: e.tensor_tensor(out=SBM.rearrange("p (g k) -> p g k", k=8), in0=SBM.rearrange("p (g k) -> p g k", k=8),
                                     in1=T1.unsqueeze(2).to_broadcast([128, 8, 8]), op=ALU.add))
        dv(lambda e: e.max(out=M8B, in_=SBM))
        dv(lambda e: e.tensor_scalar(out=SEL, in0=SBM, scalar1=r[:, 295:296], scalar2=None, op0=ALU.is_ge))
        dv(lambda e: e.tensor_tensor(out=SEL, in0=SEL, in1=S, op=ALU.mult))
        dv(lambda e: e.tensor_reduce(out=r[:, 360:361], in_=SEL, axis=AX.X, op=ALU.add))
        dv(lambda e: e.reciprocal(out=r[:, 361:362], in_=r[:, 360:361]))
        fw.op("dve", lambda e: e.tensor_scalar(out=Wr[:, i, 0:64], in0=SEL, scalar1=r[:, 361:362], scalar2=2.5, op0=ALU.mult, op1=ALU.mult),
              reads=[rb, b_Wr], writes=[b_Wr], partial=True)

    for i in range(16):
        route_tile(i)
    if cx.dbg:
        fw.dma("sp", lambda e: e.dma_start(out=cx.dbg_wr[:, :, :], in_=Wr[:]), reads=[b_Wr], writes=[Buf()])


def stage_moe(nc, fw, cx, es, n_exp=NE, groups=(0, 1, 2, 3)):
    def sb(name, shape, dt):
        return es.enter_context(nc.sbuf_tensor(name, list(shape), dt))

    def ps(name, shape, dt):
        return es.enter_context(nc.psum_tensor(name, list(shape), dt))
    Wr, b_Wr = cx.Wr, cx.b_Wr
    Grow, b_Grow = cx.Grow, cx.b_Grow
    yacc = sb("yacc", [128, 4, D], F32)
    b_y = [Buf() for _ in range(4)]
    h2g, b_h2g = sb("h2g", [128, KC, 512], BF16), Buf()
    wgu = Ring([sb("wgu%d" % i, [128, KC, 512], BF16) for i in range(3)])
    wdr = Ring([sb("wdn%d" % i, [128, 4, D], BF16) for i in range(2)])
    wst = Ring([sb("wst%d" % i, [128, 4096], F32) for i in range(2)])
    sg_r = Ring([sb("sg%d" % i, [128, 512], F32) for i in range(2)])
    hT_r = Ring([sb("hTe%d" % i, [128, 4, 512], BF16) for i in range(2)])
    xt_r = Ring([sb("xf%d" % i, [128, D], F32) for i in range(1)])
    st_r = Ring([sb("stf%d" % i, [128, 8], F32) for i in range(2)])
    cx.junk_f, cx.b_junk_f = sb("junk_f2", [128, D], BF16), Buf()
    b_out = Buf()
    cx.b_x1d_save = cx.b_x1d
    pg = Ring([ps("pg%d" % i, [128, 512], F32) for i in range(2)])
    pu = Ring([ps("pu%d" % i, [128, 512], F32) for i in range(2)])
    py = Ring([ps("py%d" % i, [128, 512], F32) for i in range(3)])
    tog = [0]

    def load_mat(src_flat, off, dst, dstb, view):
        for half in range(2):
            wt, wtb = wst.next()
            o = off + half * 128 * 4096
            fw.dma("sp", lambda e, wt=wt, o=o: e.dma_start(out=wt[:], in_=src_flat[o:o + 128 * 4096].rearrange("(p n) -> p n", p=128)), writes=[wtb])
            eng = "pool" if tog[0] % 2 == 0 else "dve"
            tog[0] += 1
            fw.op(eng, lambda e, wt=wt, half=half: e.tensor_copy(out=view(dst, half), in_=view_st(wt, view)), reads=[wtb], writes=[dstb], partial=True)

    def view_st(wt, view):
        return wt[:].rearrange("p (k n) -> p k n", k=KC) if view is v_gu else wt[:].rearrange("p (k n) -> p k n", k=4)

    def v_gu(dst, half):
        return dst[:, :, half * 256:(half + 1) * 256]

    def v_dn(dst, half):
        return dst[:, :, half * 1024:(half + 1) * 1024]

    def expert(grp, e, first):
        wg, wgb = wgu.next()
        load_mat(cx.w_eg_p, e * D * 512, wg, wgb, v_gu)
        wu, wub = wgu.next()
        load_mat(cx.w_eu_p, e * D * 512, wu, wub, v_gu)
        wd, wdb = wdr.next()
        load_mat(cx.w_ed_p, e * 512 * D, wd, wdb, v_dn)
        hT, hTb = hT_r.next()
        for fc in range(4):
            pgt, pgb = pg.next()
            put, pub = pu.next()
            for (pt, ptb, w, wb_) in ((pgt, pgb, wg, wgb), (put, pub, wu, wub)):
                for kc in range(KC):
                    fw.op("pe", lambda e_, pt=pt, w=w, kc=kc, fc=fc: e_.matmul(pt[:, :], lhsT=w[:, kc, fc * 128:(fc + 1) * 128], rhs=h2g[:, kc, :],
                          start=(kc == 0), stop=(kc == KC - 1)), reads=[wb_, b_h2g], writes=[ptb])
            sg, sgb = sg_r.next()
            fw.op("act", lambda e_, sg=sg, pgt=pgt: e_.activation(out=sg[:], in_=pgt[:, :], func=AF.Silu), reads=[pgb], writes=[sgb])
            fw.op("dve", lambda e_, sg=sg, put=put, hT=hT, fc=fc: e_.tensor_tensor(out=hT[:, fc, :], in0=put[:, :], in1=sg[:], op=ALU.mult),
                  reads=[pub, sgb], writes=[hTb], partial=(fc > 0))
        for ti in range(4):
            for n in range(4):
                pyt, pyb = py.next()
                for fc in range(4):
                    fw.op("pe", lambda e_, pyt=pyt, hT=hT, wd=wd, fc=fc, ti=ti, n=n: e_.matmul(pyt[:, :], lhsT=hT[:, fc, ti * 128:(ti + 1) * 128],
                          rhs=wd[:, fc, n * 512:(n + 1) * 512], start=(fc == 0), stop=(fc == 3)), reads=[hTb, wdb], writes=[pyb])
                wcol = Wr[:, grp * 4 + ti, e:e + 1]
                if first:
                    fw.op("dve", lambda e_, pyt=pyt, ti=ti, n=n, wcol=wcol: e_.tensor_scalar(out=yacc[:, ti, n * 512:(n + 1) * 512], in0=pyt[:, :], scalar1=wcol, scalar2=None, op0=ALU.mult),
                          reads=[pyb, b_Wr], writes=[b_y[ti]], partial=True)
                else:
                    fw.op("dve", lambda e_, pyt=pyt, ti=ti, n=n, wcol=wcol: e_.scalar_tensor_tensor(out=yacc[:, ti, n * 512:(n + 1) * 512], in0=pyt[:, :], scalar=wcol,
                          in1=yacc[:, ti, n * 512:(n + 1) * 512], op0=ALU.mult, op1=ALU.add), reads=[pyb, b_Wr, b_y[ti]], writes=[b_y[ti]], partial=True)

    class YT:
        pass
    for grp in groups:
        fw.dma("sp", lambda e, grp=grp: e.dma_start(out=h2g[:], in_=cx.h2T_d.rearrange("p (k t) -> p k t", k=KC)[:, :, grp * 512:(grp + 1) * 512]),
               reads=[cx.b_h2Td], writes=[b_h2g])
        elist = list(range(n_exp)) if n_exp == NE else list(range(n_exp - 1)) + [NE - 1]
        for k, e in enumerate(elist):
            expert(grp, e, k == 0)
        for ti in range(4):
            i = grp * 4 + ti
            cx.b_x1d = b_out
            post_norm_residual(fw, cx, i, yacc[:, ti, :], b_y[ti], xt_r, st_r, cx.x1_d, Grow[1], b_Grow[1], cx.out, x_dep=[cx.b_x1d_save])
    cx.b_out = b_out


from contextlib import ExitStack
from concourse.bass_utils import run_bass_kernel_spmd


def build_program(nc):
    cx = Ctx()
    declare_io_a(nc, cx); declare_io_b(nc, cx); declare_io_c(nc, cx); declare_io_d(nc, cx)
    fw = FW(nc)
    with ExitStack() as gs:
        cx.Wr = gs.enter_context(nc.sbuf_tensor("Wr", [128, 16, NE], F32))
        cx.b_Wr, cx.b_h2Td, cx.b_x1d = Buf(), Buf(), Buf()
        with ExitStack() as es:
            stage_abc(nc, fw, cx, es, gs=gs)
        fw.barrier()
        with ExitStack() as es:
            stage_nsa(nc, fw, cx, es)
        fw.barrier()
        with ExitStack() as es:
            stage_mlstm(nc, fw, cx, es)
        fw.barrier()
        with ExitStack() as es:
            stage_merge(nc, fw, cx, es)
        fw.barrier()
        with ExitStack() as es:
            stage_router(nc, fw, cx, es)
        fw.barrier()
        with ExitStack() as es:
            stage_moe(nc, fw, cx, es)
        fw.barrier()
    fw.finish([cx.b_out])
    return cx, fw


def shared_inputs(inp):
    P = {k: np.asarray(v)[0] for k, v in inp.items() if k not in ("x", "c")}
    col = lambda v: np.ascontiguousarray(v.reshape(16, 128).T)
    m = {}
    m["w_ada_p"] = pack_cols(P["w_ada"], [(c, 512) for c in range(0, 12288, 512)])[0]
    m["b_ada"] = P["b_ada"].reshape(1, -1)
    m["g4"] = np.concatenate([col(P["g_pre_mix"]), col(P["g_pre_ffn"]), np.zeros((128, 32), np.float32)], axis=1)
    m["gpost"] = np.stack([P["g_post_mix"], P["g_post_ffn"]])
    m["w_in_p"] = pack_cols(P["w_in"], win_chunks())[0]
    m["ident"] = np.eye(128, dtype=np.float32)
    m.update(nsa_consts())
    m["cmp_w1"] = P["cmp_w1"]
    m["cmp_w2"] = P["cmp_w2"]
    m["peT"] = np.ascontiguousarray(P["cmp_pe"].transpose(0, 2, 1))
    m.update(mlstm_consts())
    cw = np.concatenate([P["conv_w"], P["conv_b"][None, :]], 0)
    m["convp"] = np.ascontiguousarray(cw.reshape(5, 8, 128).transpose(2, 1, 0))
    m["bgate"] = np.ascontiguousarray(P["b_gates_m"].reshape(2, 4).T)
    m["mhg"] = P["mh_norm_g"].reshape(1, -1)
    ch = [(c, 512) for c in range(0, 2048, 512)]
    m["w_up_p"] = np.concatenate([pack_cols(P["w_up_nsa"], ch)[0], pack_cols(P["w_up_mlstm"], ch)[0]])
    m["w_out_p"] = pack_cols(P["w_out"], [(c, 128) for c in range(0, 2048, 128)])[0]
    m["w_router"] = P["w_router"]
    m["b_router"] = P["b_router"].reshape(1, 64)
    m["w_eg_p"] = np.concatenate([pack_gu(P["w_e_gate"][e]) for e in range(64)] + [pack_gu(P["w_sh_gate"])])
    m["w_eu_p"] = np.concatenate([pack_gu(P["w_e_up"][e]) for e in range(64)] + [pack_gu(P["w_sh_up"])])
    m["w_ed_p"] = np.concatenate([pack_dn(P["w_e_down"][e]) for e in range(64)] + [pack_dn(P["w_sh_down"])])
    return m


def kernel(**inputs):
    inp = {k: np.asarray(v) for k, v in inputs.items()}
    nc = bass.Bass("TRN2", target_bir_lowering=False)
    build_program(nc)
    sh = shared_inputs(inp)
    in_maps = []
    for b in range(8):
        m = dict(sh)
        m["x"] = np.ascontiguousarray(inp["x"][b])
        m["cT"] = np.ascontiguousarray(inp["c"][b].reshape(16, 128).T)
        in_maps.append(m)
    res = run_bass_kernel_spmd(nc, in_maps, core_ids=list(range(8)))
    return np.stack([np.asarray(r["out"]) for r in res.results], axis=0).astype(np.float32)
```

```python
import numpy as np
import concourse.bass as bass
import concourse.mybir as mybir

F32 = mybir.dt.float32
BF16 = mybir.dt.bfloat16
U32 = mybir.dt.uint32
I32 = mybir.dt.int32
AF = mybir.ActivationFunctionType
ALU = mybir.AluOpType
AX = mybir.AxisListType

ENGS = ("pe", "act", "dve", "pool", "sp")


class Buf:
    __slots__ = ("name", "w", "r")

    def __init__(self, name=""):
        self.name = name
        self.w = {}
        self.r = {}


class FW:
    def __init__(self, nc, dma_ring=8):
        self.nc = nc
        self.prog = {e: [] for e in ENGS}
        self.sem = {e: nc.alloc_semaphore("c_" + e) for e in ENGS}
        self.cnt = {e: 0 for e in ENGS}
        self.known = {e: {} for e in ENGS}
        self.R = dma_ring
        self.dsem = {q: [nc.alloc_semaphore("d_%s%d" % (q, i)) for i in range(dma_ring)] for q in ("sp", "pool", "act")}
        self.dn = {q: 0 for q in ("sp", "pool", "act")}

    def _need(self, e, tickets):
        need = {}
        for (s, v) in tickets:
            if v > need.get(s, 0):
                need[s] = v
        out = []
        kn = self.known[e]
        own = self.sem[e] if e == "pe" else None
        for s, v in need.items():
            if s is own:
                continue
            if kn.get(s, 0) < v:
                kn[s] = v
                out.append((s, v))
        return out

    def _deps(self, reads, writes, partial=False):
        t = []
        for b in reads:
            t.extend(b.w.items())
        for b in writes:
            if not partial:
                t.extend(b.w.items())
            t.extend(b.r.items())
        return t

    def _commit(self, ticket, reads, writes, partial=False):
        s, v = ticket
        for b in reads:
            if b.r.get(s, 0) < v:
                b.r[s] = v
        for b in writes:
            if partial:
                if b.w.get(s, 0) < v:
                    b.w[s] = v
            else:
                b.w = {s: v}
                b.r = {}

    @staticmethod
    def _compact(ts):
        m = {}
        so = {}
        for (s, v) in ts:
            k = id(s)
            so[k] = s
            if v > m.get(k, 0):
                m[k] = v
        return [(so[k], v) for k, v in m.items()]

    def op(self, e, fn, reads=(), writes=(), partial=False):
        deps = self._deps(reads, writes, partial)
        waits = self._need(e, deps)
        self.cnt[e] += 1
        tk = (self.sem[e], self.cnt[e])
        sem = self.sem[e]

        def emit(eng, waits=waits, fn=fn, sem=sem):
            for (s, v) in waits:
                eng.wait_ge(s, v)
            fn(eng).then_inc(sem, 1)
        self.prog[e].append(emit)
        self._commit(tk, reads, writes, partial)
        return tk

    def barrier(self):
        ts = [(self.sem[e], self.cnt[e]) for e in ENGS if self.cnt[e] > 0]
        for q in self.dsem:
            n = self.dn[q]
            for i, s in enumerate(self.dsem[q]):
                k = (n - 1 - i) // self.R + 1 if n > i else 0
                if k > 0:
                    ts.append((s, 16 * k))
        for e in ENGS:
            waits = self._need(e, ts)

            def emit(eng, waits=waits):
                for (s, v) in waits:
                    eng.wait_ge(s, v)
            self.prog[e].append(emit)

    def dma(self, q, fn, reads=(), writes=(), partial=False):
        n = self.dn[q]
        self.dn[q] += 1
        s = self.dsem[q][n % self.R]
        prev = 16 * (n // self.R)
        deps = self._deps(reads, writes, partial)
        if prev > 0:
            deps = deps + [(s, prev)]
        waits = self._need(q, deps)
        tk = (s, prev + 16)

        def emit(eng, waits=waits, fn=fn, s=s):
            for (ss, v) in waits:
                eng.wait_ge(ss, v)
            fn(eng).then_inc(s, 16)
        self.prog[q].append(emit)
        self._commit(tk, reads, writes, partial)
        return tk

    def flush(self):
        nc = self.nc
        prog = self.prog
        self.prog = {e: [] for e in ENGS}
        self._regs = {}
        if not any(prog[e] for e in ENGS):
            return
        with nc.Block() as block:
            @block.tensor
            def _(e):
                for f in prog["pe"]:
                    f(e)

            @block.scalar
            def _(e):
                for f in prog["act"]:
                    f(e)

            @block.vector
            def _(e):
                for f in prog["dve"]:
                    f(e)

            @block.gpsimd
            def _(e):
                for f in prog["pool"]:
                    f(e)

            @block.sync
            def _(e):
                for f in prog["sp"]:
                    f(e)

    def reg(self, eng, value):
        k = (id(eng), value)
        if k not in self._regs:
            self._regs[k] = eng.to_reg(value)
        return self._regs[k]

    def finish(self, final_bufs):
        deps = []
        for b in final_bufs:
            deps.extend(b.w.items())
        waits = self._need("sp", deps)

        def emit(eng, waits=waits):
            for (s, v) in waits:
                eng.wait_ge(s, v)
        self.prog["sp"].append(emit)
        self.flush()


T = 2048
D = 2048
KC = 16
D_IN = 9784
C_Q, C_KV, C_GA, C_QK, C_VM, C_IF, C_OM, C_GTA, C_GTB = 0, 1024, 2560, 2608, 3632, 4656, 4664, 5688, 7736
EPS = 1e-6


class Ctx:
    pass


def win_chunks():
    groups = [(C_Q, 1024), (C_KV, 1536), (C_KV + 768, 256), (C_KV + 1280, 256), (C_GA, 48), (C_QK, 1024), (C_VM, 1024),
              (C_IF, 8), (C_OM, 1024), (C_GTA, 2048), (C_GTB, 2048)]
    out = []
    for c0, n in groups:
        for cc in range(0, n, 512):
            out.append((c0 + cc, min(512, n - cc)))
    return out


def pack_cols(w, chunks):
    import numpy as np
    K = w.shape[0] // 128
    parts, offs, off = [], {}, 0
    for (c0, n) in chunks:
        offs[(c0, n)] = off
        for h0 in range(0, n, 256):
            nn = min(256, n - h0)
            blk = w[:, c0 + h0:c0 + h0 + nn].reshape(K, 128, nn).transpose(1, 0, 2)
            parts.append(np.ascontiguousarray(blk).reshape(-1))
            off += 128 * K * nn
    return np.concatenate(parts), offs, off


def declare_io_a(nc, cx, dbg=False):
    cx.dbg = dbg
    ch = win_chunks()
    off = 0
    cx.win_off = {}
    for (c0, n) in ch:
        cx.win_off[(c0, n)] = off
        off += 128 * KC * n
    cx.win_total = off
    def din(name, shape, dt=F32):
        t = nc.dram_tensor(name, list(shape), dt, kind="ExternalInput").ap()
        setattr(cx, name, t)
        return t

    def dscr(name, shape, dt):
        t = nc.dram_tensor(name, list(shape), dt, kind="ExternalOutput" if dbg else "Internal").ap()
        setattr(cx, name, t)
        return t
    din("x", [T, D])
    din("cT", [128, KC])
    din("w_ada_p", [D * 6 * D])
    din("b_ada", [1, 6 * D])
    din("g4", [128, 4 * KC])
    din("gpost", [3, D])
    din("w_in_p", [cx.win_total])
    din("ident", [128, 128])
    if dbg:
        dscr("dbg_modc", [128, 4 * KC], F32)
        dscr("dbg_G", [128, D], F32)
    dscr("qT_d", [1024, T], BF16)
    dscr("kvT_d", [1536, T], BF16)
    dscr("qkT_d", [1024, T], F32)
    dscr("ifT_d", [8, T], F32)
    dscr("gaT_d", [D, T], BF16)
    dscr("gbT_d", [D, T], BF16)
    dscr("vs_d", [T, 256], BF16)
    dscr("vw_d", [T, 256], BF16)
    dscr("ga_d", [T, 48], F32)
    dscr("vm_d", [T, 1024], BF16)
    dscr("om_d", [T, 1024], BF16)
    dscr("if_d", [T, 8], F32)


class Ring:
    def __init__(self, tiles):
        self.tiles = tiles
        self.bufs = [Buf() for _ in tiles]
        self.i = 0

    def next(self):
        k = self.i % len(self.tiles)
        self.i += 1
        return self.tiles[k], self.bufs[k]


def stage_abc(nc, fw, cx, es, stop=None, gs=None):
    def sb(name, shape, dt):
        return es.enter_context(nc.sbuf_tensor(name, list(shape), dt))

    def ps(name, shape, dt):
        return es.enter_context(nc.psum_tensor(name, list(shape), dt))

    def gsb(name, shape, dt):
        return (gs or es).enter_context(nc.sbuf_tensor(name, list(shape), dt))

    ident_f = gsb("ident_f", [128, 128], F32)
    ident_b = gsb("ident_b", [128, 128], BF16)
    modc = gsb("modc", [128, 4 * KC], F32)
    Grow = [gsb("Grow%d" % i, [128, D], F32) for i in range(2)]
    cT = sb("cT_s", [128, KC], F32)
    sc = sb("sc_s", [128, KC], F32)
    S_b = sb("S_b", [128, KC, 128], BF16)
    g4 = sb("g4_s", [128, 4 * KC], F32)
    modraw = sb("modraw", [128, 4 * KC], F32)
    b_Grow = [Buf(), Buf()]
    b_id, b_idb, b_cT, b_sc, b_Sb, b_g4, b_modraw, b_modc = (Buf() for _ in range(8))
    fw.dma("sp", lambda e: e.dma_start(out=ident_f[:], in_=cx.ident[:, :]), writes=[b_id])
    fw.dma("sp", lambda e: e.dma_start(out=cT[:], in_=cx.cT[:, :]), writes=[b_cT])
    fw.dma("sp", lambda e: e.dma_start(out=g4[:], in_=cx.g4[:, :]), writes=[b_g4])
    fw.op("dve", lambda e: e.tensor_copy(out=ident_b[:], in_=ident_f[:]), reads=[b_id], writes=[b_idb])
    fw.op("act", lambda e: e.activation(out=sc[:], in_=cT[:], func=AF.Silu), reads=[b_cT], writes=[b_sc])
    for kc in range(KC):
        fw.op("dve", lambda e, kc=kc: e.tensor_copy(out=S_b[:, kc, :], in_=sc[:, kc:kc + 1].to_broadcast([128, 128])),
              reads=[b_sc], writes=[b_Sb], partial=True)

    wf = Ring([sb("wf%d" % i, [128, KC, 256], F32) for i in range(2)])
    wb = Ring([sb("wb%d" % i, [128, KC, 512], BF16) for i in range(2)])
    pacc = Ring([ps("pacc%d" % i, [128, 512], F32) for i in range(4)])
    ptr = Ring([ps("ptr%d" % i, [128, 1024], BF16) for i in range(2)])
    bada_r = Ring([sb("bada%d" % i, [128, 512], F32) for i in range(2)])
    mrow_r = Ring([sb("mrow%d" % i, [128, 512], F32) for i in range(2)])
    arow_r = Ring([sb("arow%d" % i, [1, 512], F32) for i in range(2)])
    cx.b_rowAB = Buf()
    junkf = sb("junkf", [128, 128], F32)
    b_junkf = Buf()
    cast_tog = [0]

    def load_w(src_flat, off, ncols):
        wbt, wbb = wb.next()
        for h0 in range(0, ncols, 256):
            n = min(256, ncols - h0)
            wt, wtb = wf.next()
            o = off + h0 * 128 * KC
            fw.dma("sp", lambda e, wt=wt, o=o, n=n: e.dma_start(
                out=wt[:, :, 0:n], in_=src_flat[o:o + 128 * KC * n].rearrange("(p k n) -> p k n", p=128, k=KC)), writes=[wtb])
            eng = "pool" if cast_tog[0] % 2 == 0 else "dve"
            cast_tog[0] += 1
            fw.op(eng, lambda e, wt=wt, wbt=wbt, h0=h0, n=n: e.tensor_copy(out=wbt[:, :, h0:h0 + n], in_=wt[:, :, 0:n]),
                  reads=[wtb], writes=[wbb], partial=True)
        return wbt, wbb

    for ch in range(24):
        mi, q = ch // 4, ch % 4
        pt, pb = pacc.next()
        bt, btb = bada_r.next()
        fw.dma("sp", lambda e, bt=bt, ch=ch: e.dma_start(out=bt[:], in_=cx.b_ada[0, ch * 512:(ch + 1) * 512].partition_broadcast(128)), writes=[btb])
        wbt, wbb = load_w(cx.w_ada_p, ch * 512 * 128 * KC, 512)
        for kc in range(KC):
            fw.op("pe", lambda e, pt=pt, wbt=wbt, kc=kc: e.matmul(pt[:, :], lhsT=S_b[:, kc, :], rhs=wbt[:, kc, :],
                  start=(kc == 0), stop=(kc == KC - 1)), reads=[b_Sb, wbb], writes=[pb])
        mr, mrb = mrow_r.next()
        fw.op("dve", lambda e, pt=pt, mr=mr, bt=bt: e.tensor_tensor(out=mr[:], in0=pt[:, :], in1=bt[:], op=ALU.add),
              reads=[pb, btb], writes=[mrb])
        if mi in (2, 5):
            gi = 0 if mi == 2 else 1
            gt, gtb = bada_r.next()
            fw.dma("sp", lambda e, gt=gt, gi=gi, q=q: e.dma_start(out=gt[:], in_=cx.gpost[gi, q * 512:(q + 1) * 512].partition_broadcast(128)), writes=[gtb])
            fw.op("pool", lambda e, mr=mr, gt=gt, gi=gi, q=q: e.tensor_tensor(out=Grow[gi][:, q * 512:(q + 1) * 512], in0=mr[:], in1=gt[:], op=ALU.mult),
                  reads=[mrb, gtb], writes=[b_Grow[gi]], partial=True)
        else:
            vi = {1: 0, 0: 1, 4: 2, 3: 3}[mi]
            if mi == 3 and hasattr(cx, "rowAB_d"):
                fw.dma("sp", lambda e, mr=mr, q=q: e.dma_start(out=cx.rowAB_d[1:2, q * 512:(q + 1) * 512], in_=mr[0:1, :]), reads=[mrb], writes=[cx.b_rowAB], partial=True)
            if mi == 4 and hasattr(cx, "rowAB_d"):
                gt, gtb = bada_r.next()
                fw.dma("sp", lambda e, gt=gt, q=q: e.dma_start(out=gt[:], in_=cx.gpost[2, q * 512:(q + 1) * 512].partition_broadcast(128)), writes=[gtb])
                ar, arb = arow_r.next()
                fw.op("dve", lambda e, mr=mr, gt=gt, ar=ar: e.scalar_tensor_tensor(out=ar[:], in0=mr[0:1, :], scalar=1.0, in1=gt[0:1, :], op0=ALU.add, op1=ALU.mult),
                      reads=[mrb, gtb], writes=[arb])
                fw.dma("sp", lambda e, ar=ar, q=q: e.dma_start(out=cx.rowAB_d[0:1, q * 512:(q + 1) * 512], in_=ar[:]), reads=[arb], writes=[cx.b_rowAB], partial=True)
            for jj in range(4):
                j = q * 4 + jj
                fw.op("dve", lambda e, mr=mr, jj=jj, vi=vi, j=j: e.scalar_tensor_tensor(
                    out=junkf[:], in0=mr[:, jj * 128:(jj + 1) * 128], scalar=1.0, in1=ident_f[:],
                    op0=ALU.mult, op1=ALU.mult, accum_out=modraw[:, vi * KC + j:vi * KC + j + 1]),
                    reads=[mrb, b_id], writes=[b_junkf, b_modraw])
    for a_i, g_i in ((0, 0), (2, 1)):
        fw.op("dve", lambda e, a_i=a_i, g_i=g_i: e.scalar_tensor_tensor(
            out=modc[:, a_i * KC:(a_i + 1) * KC], in0=modraw[:, a_i * KC:(a_i + 1) * KC], scalar=1.0,
            in1=g4[:, g_i * KC:(g_i + 1) * KC], op0=ALU.add, op1=ALU.mult), reads=[b_modraw, b_g4], writes=[b_modc], partial=True)
        fw.op("dve", lambda e, a_i=a_i: e.tensor_copy(
            out=modc[:, (a_i + 1) * KC:(a_i + 2) * KC], in_=modraw[:, (a_i + 1) * KC:(a_i + 2) * KC]),
            reads=[b_modraw], writes=[b_modc], partial=True)
    cx.modc, cx.b_modc, cx.Grow, cx.b_Grow = modc, b_modc, Grow, b_Grow
    cx.ident_b, cx.b_idb, cx.ident_f, cx.b_id = ident_b, b_idb, ident_f, b_id
    if cx.dbg:
        fw.dma("sp", lambda e: e.dma_start(out=cx.dbg_modc[:, :], in_=modc[:]), reads=[b_modc], writes=[Buf()])
        fw.dma("sp", lambda e: e.dma_start(out=cx.dbg_G[:, :], in_=Grow[0][:]), reads=[b_Grow[0]], writes=[Buf()])
    if stop == 'A':
        return
    hT = sb("hT", [128, KC, T], BF16)
    b_hT = [Buf() for _ in range(16)]
    xt_r = Ring([sb("xt%d" % i, [128, D], F32) for i in range(2)])
    xn_r = Ring([sb("xn%d" % i, [128, D], BF16) for i in range(2)])
    junk = sb("junk", [128, D], BF16)
    b_junk = Buf()
    st_r = Ring([sb("stat%d" % i, [128, 4], F32) for i in range(2)])
    norm_pre(nc, fw, cx, cx.x, hT, b_hT, xt_r, xn_r, junk, b_junk, st_r, ptr, 0)
    cx.hT, cx.b_hT = hT, b_hT

    if stop == 'B':
        return
    stg_b = Ring([sb("stgb%d" % i, [128, 512], BF16) for i in range(4)])
    stg_f = Ring([sb("stgf%d" % i, [128, 512], F32) for i in range(2)])
    def evac(pt, pb, rows, ncols, func, dt, dst_ap):
        st, stb = (stg_b if dt == BF16 else stg_f).next()
        fw.op("act", lambda e: e.activation(out=st[0:rows, 0:ncols], in_=pt[0:rows, 0:ncols], func=func),
              reads=[pb], writes=[stb])
        fw.dma("sp", lambda e: e.dma_start(out=dst_ap, in_=st[0:rows, 0:ncols]), reads=[stb], writes=[Buf()])

    def proj_F(c0, ncols, func, dt, dst):
        for cc in range(0, ncols, 512):
            ncc = min(512, ncols - cc)
            wbt, wbb = load_w(cx.w_in_p, cx.win_off[(c0 + cc, ncc)], ncc)
            for m0 in range(0, ncc, 128):
                m = min(128, ncc - m0)
                for tch in range(4):
                    pt, pb = pacc.next()
                    for kc in range(KC):
                        fw.op("pe", lambda e, pt=pt, wbt=wbt, kc=kc, m0=m0, m=m, tch=tch: e.matmul(
                            pt[0:m, :], lhsT=wbt[:, kc, m0:m0 + m], rhs=hT[:, kc, tch * 512:(tch + 1) * 512],
                            start=(kc == 0), stop=(kc == KC - 1)), reads=[wbb] + b_hT[tch * 4:tch * 4 + 4], writes=[pb])
                    evac(pt, pb, m, 512, func, dt, dst[cc + m0:cc + m0 + m, tch * 512:(tch + 1) * 512])

    def proj_T(c0, ncols, func, dt, dst):
        for cc in range(0, ncols, 512):
            ncc = min(512, ncols - cc)
            wbt, wbb = load_w(cx.w_in_p, cx.win_off[(c0 + cc, ncc)], ncc)
            for ti in range(16):
                pt, pb = pacc.next()
                for kc in range(KC):
                    fw.op("pe", lambda e, pt=pt, wbt=wbt, kc=kc, ti=ti, ncc=ncc: e.matmul(
                        pt[:, 0:ncc], lhsT=hT[:, kc, ti * 128:(ti + 1) * 128], rhs=wbt[:, kc, 0:ncc],
                        start=(kc == 0), stop=(kc == KC - 1)), reads=[wbb, b_hT[ti]], writes=[pb])
                evac(pt, pb, 128, ncc, func, dt, dst[ti * 128:(ti + 1) * 128, cc:cc + ncc])

    ID = AF.Identity
    proj_F(C_Q, 1024, ID, BF16, cx.qT_d)
    proj_F(C_KV, 1536, ID, BF16, cx.kvT_d)
    proj_T(C_KV + 3 * 256, 256, ID, BF16, cx.vs_d)
    proj_T(C_KV + 5 * 256, 256, ID, BF16, cx.vw_d)
    proj_T(C_GA, 48, AF.Sigmoid, F32, cx.ga_d)
    proj_F(C_QK, 1024, ID, F32, cx.qkT_d)
    proj_T(C_VM, 1024, ID, BF16, cx.vm_d)
    proj_F(C_IF, 8, ID, F32, cx.ifT_d)
    proj_T(C_IF, 8, ID, F32, cx.if_d)
    proj_T(C_OM, 1024, AF.Sigmoid, BF16, cx.om_d)
    proj_F(C_GTA, 2048, AF.Sigmoid, BF16, cx.gaT_d)
    proj_F(C_GTB, 2048, AF.Sigmoid, BF16, cx.gbT_d)


def norm_pre(nc, fw, cx, x_ap, hT, b_hT, xt_r, xn_r, junk, b_junk, st_r, ptr, which, ntiles=16, x_dep=None, tok_hook=None):
    modc, b_modc = cx.modc, cx.b_modc
    a_off = which * 2 * KC
    for i in range(ntiles):
        xt, xtb = xt_r.next()
        fw.dma("sp", lambda e, xt=xt, i=i: e.dma_start(out=xt[:], in_=x_ap[i * 128:(i + 1) * 128, :]), writes=[xtb])
        st, stb = st_r.next()
        fw.op("dve", lambda e, xt=xt, st=st: e.scalar_tensor_tensor(out=junk[:], in0=xt[:], scalar=1.0 / D, in1=xt[:],
              op0=ALU.mult, op1=ALU.mult, accum_out=st[:, 0:1]), reads=[xtb], writes=[b_junk, stb])
        fw.op("dve", lambda e, st=st: e.tensor_scalar(out=st[:, 1:2], in0=st[:, 0:1], scalar1=EPS, scalar2=None, op0=ALU.add),
              reads=[stb], writes=[stb])
        fw.op("act", lambda e, st=st: e.activation(out=st[:, 3:4], in_=st[:, 1:2], func=AF.Sqrt), reads=[stb], writes=[stb])
        fw.op("dve", lambda e, st=st: e.reciprocal(out=st[:, 2:3], in_=st[:, 3:4]), reads=[stb], writes=[stb])
        xn, xnb = xn_r.next()
        fw.op("dve", lambda e, xn=xn, xt=xt, st=st: e.tensor_scalar(out=xn[:], in0=xt[:], scalar1=st[:, 2:3], scalar2=None, op0=ALU.mult),
              reads=[xtb, stb], writes=[xnb])
        if tok_hook is not None:
            tok_hook(i, xn, xnb)
        LV = 4
        if LV <= 2:
            fw.op("dve", lambda e, xn=xn, i=i: e.tensor_copy(out=hT[:, :, i * 128:(i + 1) * 128], in_=xn[:].rearrange("p (k n) -> p k n", k=16)), reads=[xnb], writes=[b_hT[i]])
            continue
        for half in range(2):
            pt, pb = ptr.next()
            for k8 in range(8):
                kc = half * 8 + k8
                fw.op("pe", lambda e, pt=pt, xn=xn, kc=kc, k8=k8: e.transpose(
                    out=pt[:, k8 * 128:(k8 + 1) * 128], in_=xn[:, kc * 128:(kc + 1) * 128], identity=cx.ident_b[:]),
                    reads=[xnb, cx.b_idb], writes=[pb])
            for k8 in range(8):
                kc = half * 8 + k8
                if LV == 3:
                    fw.op("dve", lambda e, pt=pt, kc=kc, k8=k8, i=i: e.tensor_copy(out=hT[:, kc, i * 128:(i + 1) * 128], in_=pt[:, k8 * 128:(k8 + 1) * 128]), reads=[pb], writes=[b_hT[i]], partial=True)
                elif k8 % 2 == 0 or LV == 4:
                    fw.op("dve", lambda e, pt=pt, kc=kc, k8=k8, i=i: e.tensor_scalar(
                        out=hT[:, kc, i * 128:(i + 1) * 128], in0=pt[:, k8 * 128:(k8 + 1) * 128],
                        scalar1=modc[:, a_off + kc:a_off + kc + 1], scalar2=modc[:, a_off + KC + kc:a_off + KC + kc + 1],
                        op0=ALU.mult, op1=ALU.add), reads=[pb, b_modc], writes=[b_hT[i]], partial=True)
                else:
                    fw.op("act", lambda e, pt=pt, kc=kc, k8=k8, i=i: e.activation(
                        out=hT[:, kc, i * 128:(i + 1) * 128], in_=pt[:, k8 * 128:(k8 + 1) * 128], func=AF.Identity,
                        scale=modc[:, a_off + kc:a_off + kc + 1], bias=modc[:, a_off + KC + kc:a_off + KC + kc + 1]),
                        reads=[pb, b_modc], writes=[b_hT[i]], partial=True)


SCALE = 0.125


def nsa_consts():
    import numpy as np
    import ml_dtypes
    bf = ml_dtypes.bfloat16
    t = np.arange(T)
    n = np.arange(127)
    cmaskT = ((16 * n[:, None] + 31) <= t[None, :]).astype(bf)
    ci = n[:, None]
    sj = np.arange(32)[None, :]
    ov = np.minimum(ci * 16 + 32, (sj + 1) * 64) - np.maximum(ci * 16, sj * 64)
    c2s = (np.clip(ov, 0, None).astype(np.float32) / 32).astype(bf)
    cur = (t // 64)[:, None]
    forced = (sj == 0) | (sj == cur) | (sj == cur - 1)
    addc = np.where(sj <= cur, np.where(forced, 1e3, 0.0), -1e9).astype(np.float32)
    addc = np.ascontiguousarray(addc.reshape(16, 128, 32).transpose(1, 0, 2))
    Xall = (np.arange(T)[None, :] // 64 == np.arange(32)[:, None]).astype(bf)
    a = np.arange(128)
    tri = (a[:, None] <= a[None, :]).astype(bf)
    tri2 = (a[:, None] > a[None, :]).astype(bf)
    return {"cmaskT": cmaskT, "c2s": c2s, "addc": addc, "Xall": Xall, "tri": tri, "tri2": tri2}


def declare_io_b(nc, cx, dbg=False):
    def din(name, shape, dt=F32):
        setattr(cx, name, nc.dram_tensor(name, list(shape), dt, kind="ExternalInput").ap())

    def dscr(name, shape, dt):
        setattr(cx, name, nc.dram_tensor(name, list(shape), dt, kind="ExternalOutput" if dbg else "Internal").ap())
    din("cmaskT", [127, T], BF16)
    din("c2s", [127, 32], BF16)
    din("addc", [128, 16, 32], F32)
    din("Xall", [32, T], BF16)
    din("tri", [128, 128], BF16)
    din("tri2", [128, 128], BF16)
    din("cmp_w1", [2, 2048, 256])
    din("cmp_w2", [2, 256, 64])
    din("peT", [2, 64, 32])
    dscr("onT_d", [1024, T], BF16)
    if dbg:
        dscr("dbg_kcc", [64, 4, 127], BF16)
        dscr("dbg_vcc", [127, 4, 97], BF16)
        dscr("dbg_sel", [T, 32], BF16)
        dscr("dbg_sm", [T, 256], F32)


def stage_nsa(nc, fw, cx, es):
    def sb(name, shape, dt):
        return es.enter_context(nc.sbuf_tensor(name, list(shape), dt))

    def ps(name, shape, dt):
        return es.enter_context(nc.psum_tensor(name, list(shape), dt))

    def load(name, shape, dt, src, q="sp", n_split=1):
        t = sb(name, shape, dt)
        b = Buf()
        if n_split == 1:
            fw.dma(q, lambda e: e.dma_start(out=t[:], in_=src), writes=[b])
        return t, b

    ident_b, b_idb = cx.ident_b, cx.b_idb
    cmaskT, b_cm = load("cmaskT_s", [127, T], BF16, cx.cmaskT[:, :])
    addc, b_addc = load("addc_s", [128, 16, 32], F32, cx.addc[:, :, :])
    Xall, b_X = load("Xall_s", [32, T], BF16, cx.Xall[:, :])
    tri, b_tri = load("tri_s", [128, 128], BF16, cx.tri[:, :])
    tri2, b_tri2 = load("tri2_s", [128, 128], BF16, cx.tri2[:, :])
    kT = {}
    for nm, r0 in (("ks", 512), ("kw", 1024)):
        kT[nm] = load("kT_" + nm, [64, 4, T], BF16, cx.kvT_d[r0:r0 + 256, :].rearrange("(g d) t -> d g t", d=64))
    v1 = {}
    for nm, src in (("vs", cx.vs_d), ("vw", cx.vw_d)):
        t_ = sb("v1_" + nm, [128, 16, 4, 65], BF16)
        b_ = Buf()
        fw.op("pool", lambda e, t_=t_: e.memset(t_[:], 1.0), writes=[b_])
        for i in range(16):
            fw.dma("sp", lambda e, t_=t_, i=i, src=src: e.dma_start(
                out=t_[:, i, :, 0:64], in_=src[i * 128:(i + 1) * 128, :].rearrange("p (g d) -> p g d", g=4)),
                reads=[b_], writes=[b_], partial=True)
        v1[nm] = (t_, b_)
    ga, b_ga = sb("ga_s", [128, 16, 48], F32), Buf()
    for i4 in range(4):
        fw.dma("sp", lambda e, i4=i4: e.dma_start(
            out=ga[:, i4 * 4:(i4 + 1) * 4, :], in_=cx.ga_d[i4 * 512:(i4 + 1) * 512, :].rearrange("(i p) c -> p i c", p=128)),
            writes=[b_ga], partial=True)

    pS = Ring([ps("pS%d" % i, [128, 512], F32) for i in range(2)])
    pM = Ring([ps("pM%d" % i, [128, 512], F32) for i in range(1)])
    pc_t, b_pc = ps("pc", [128, 512], F32)[:, 0:388].rearrange("p (h c) -> p h c", h=4), Buf()
    pss_t, b_pss = ps("pss", [128, 512], F32)[:, 0:260].rearrange("p (h c) -> p h c", h=4), Buf()
    psw_t, b_psw = ps("psw", [128, 512], F32)[:, 0:260].rearrange("p (h c) -> p h c", h=4), Buf()
    pT = Ring([ps("pT%d" % i, [128, 1024], BF16) for i in range(2)])

    kccT, b_kcc = sb("kccT", [64, 4, 127], BF16), Buf()
    vcc, b_vcc = sb("vcc", [127, 4, 97], BF16), Buf()
    from contextlib import ExitStack
    es_outer = es
    es = ExitStack()
    es.__enter__()
    w1f, b_w1f = sb("w1f", [64, 16, 256], F32), Buf()
    w1b, b_w1b = sb("w1b", [64, 32, 256], BF16), Buf()
    w2f, b_w2f = sb("w2f", [128, 2, 64], F32), Buf()
    w2b, b_w2b = sb("w2b", [128, 2, 64], BF16), Buf()
    pef, b_pef = sb("pef", [64, 32], F32), Buf()
    peb, b_peb = sb("peb", [64, 32], BF16), Buf()
    pbias, b_pbias = sb("pbias", [128, 2], F32), Buf()
    hidT, b_hid = sb("hidT", [128, 2, 508], BF16), Buf()
    gz = [sb("gz%d" % i, [128, 508], F32) for i in range(3)]
    b_gz = [Buf() for _ in range(3)]
    fw.op("pool", lambda e: e.memset(vcc[:], 1.0), writes=[b_vcc])
    for g in range(4):
        fw.dma("sp", lambda e, g=g: e.dma_start(out=vcc[:, g, 65:97], in_=cx.c2s[:, :]), reads=[b_vcc], writes=[b_vcc], partial=True)
    xT, b_xT = sb("xT_c", [64, 4, T], BF16), Buf()
    for ci, nm in ((0, "kc"), (1, "vc")):
        fw.dma("sp", lambda e, ci=ci: e.dma_start(out=xT[:], in_=cx.kvT_d[ci * 256:(ci + 1) * 256, :].rearrange("(g d) t -> d g t", d=64)), writes=[b_xT])
        for lh in range(2):
            fw.dma("sp", lambda e, ci=ci, lh=lh: e.dma_start(out=w1f[:], in_=cx.cmp_w1[ci, lh * 1024:(lh + 1) * 1024, :].rearrange("(l d) c -> d l c", d=64)), writes=[b_w1f])
            fw.op("pool", lambda e, lh=lh: e.tensor_copy(out=w1b[:, lh * 16:(lh + 1) * 16, :], in_=w1f[:]), reads=[b_w1f], writes=[b_w1b], partial=(lh == 1))
        fw.dma("sp", lambda e, ci=ci: e.dma_start(out=w2f[:], in_=cx.cmp_w2[ci].rearrange("(cc p) d -> p cc d", p=128)), writes=[b_w2f])
        fw.dma("sp", lambda e, ci=ci: e.dma_start(out=pef[:], in_=cx.peT[ci]), writes=[b_pef])
        fw.op("dve", lambda e: e.tensor_copy(out=w2b[:], in_=w2f[:]), reads=[b_w2f], writes=[b_w2b])
        fw.op("dve", lambda e: e.tensor_copy(out=peb[:], in_=pef[:]), reads=[b_pef], writes=[b_peb])
        for cc in range(2):
            pm, pmb = pM.next()
            for l in range(32):
                fw.op("pe", lambda e, pm=pm, l=l, cc=cc: e.matmul(pm[:, 0:1], lhsT=w1b[:, l, cc * 128:(cc + 1) * 128], rhs=peb[:, l:l + 1],
                      start=(l == 0), stop=(l == 31)), reads=[b_w1b, b_peb], writes=[pmb])
            fw.op("dve", lambda e, pm=pm, cc=cc: e.tensor_copy(out=pbias[:, cc:cc + 1], in_=pm[:, 0:1]), reads=[pmb], writes=[b_pbias])
            ph, phb = pS.next()
            for l in range(32):
                fw.op("pe", lambda e, ph=ph, l=l, cc=cc, xT=xT: e.matmul(
                    ph[:, 0:508].rearrange("p (g n) -> p g n", g=4), lhsT=w1b[:, l, cc * 128:(cc + 1) * 128],
                    rhs=xT[:, :, l:l + 2017:16], start=(l == 0), stop=(l == 31)), reads=[b_w1b, b_xT], writes=[phb])
            fw.op("dve", lambda e, ph=ph, cc=cc: e.tensor_scalar(out=gz[0][:], in0=ph[:, 0:508], scalar1=pbias[:, cc:cc + 1], scalar2=None, op0=ALU.add),
                  reads=[phb, b_pbias], writes=[b_gz[0]])
            fw.op("dve", lambda e: e.tensor_tensor(out=gz[1][:], in0=gz[0][:], in1=gz[0][:], op=ALU.mult), reads=[b_gz[0]], writes=[b_gz[1]])
            fw.op("dve", lambda e: e.tensor_scalar(out=gz[1][:], in0=gz[1][:], scalar1=0.044715, scalar2=1.0, op0=ALU.mult, op1=ALU.add),
                  reads=[b_gz[1]], writes=[b_gz[1]])
            fw.op("dve", lambda e: e.tensor_tensor(out=gz[1][:], in0=gz[1][:], in1=gz[0][:], op=ALU.mult), reads=[b_gz[0], b_gz[1]], writes=[b_gz[1]])
            fw.op("act", lambda e: e.activation(out=gz[2][:], in_=gz[1][:], func=AF.Tanh, scale=0.7978845608), reads=[b_gz[1]], writes=[b_gz[2]])
            fw.op("dve", lambda e: e.scalar_tensor_tensor(out=gz[2][:], in0=gz[2][:], scalar=1.0, in1=gz[0][:], op0=ALU.add, op1=ALU.mult),
                  reads=[b_gz[2], b_gz[0]], writes=[b_gz[2]])
            fw.op("dve", lambda e, cc=cc: e.tensor_scalar(out=hidT[:, cc, :], in0=gz[2][:], scalar1=0.5, scalar2=None, op0=ALU.mult),
                  reads=[b_gz[2]], writes=[b_hid], partial=(cc == 1))
        if ci == 0:
            pm, pmb = pM.next()
            for cc in range(2):
                fw.op("pe", lambda e, pm=pm, cc=cc: e.matmul(pm[0:64, 0:508], lhsT=w2b[:, cc, :], rhs=hidT[:, cc, :], start=(cc == 0), stop=(cc == 1)),
                      reads=[b_w2b, b_hid], writes=[pmb])
            fw.op("dve", lambda e, pm=pm: e.tensor_copy(out=kccT[:].rearrange("p g n -> p (g n)"), in_=pm[0:64, 0:508]), reads=[pmb], writes=[b_kcc])
        else:
            for g in range(4):
                pm, pmb = pM.next()
                for cc in range(2):
                    fw.op("pe", lambda e, pm=pm, cc=cc, g=g: e.matmul(pm[0:127, 0:64], lhsT=hidT[:, cc, g * 127:(g + 1) * 127], rhs=w2b[:, cc, :],
                          start=(cc == 0), stop=(cc == 1)), reads=[b_w2b, b_hid], writes=[pmb])
                fw.op("dve", lambda e, pm=pm, g=g: e.tensor_copy(out=vcc[:, g, 0:64], in_=pm[0:127, 0:64]), reads=[pmb, b_vcc], writes=[b_vcc], partial=True)
    if cx.dbg:
        fw.dma("sp", lambda e: e.dma_start(out=cx.dbg_kcc[:, :, :], in_=kccT[:]), reads=[b_kcc], writes=[Buf()])
        fw.dma("sp", lambda e: e.dma_start(out=cx.dbg_vcc[:, :, :], in_=vcc[:]), reads=[b_vcc], writes=[Buf()])

    fw.barrier()
    fw.flush()
    es.__exit__(None, None, None)
    es = es_outer
    qT, b_qT = sb("qT_s", [64, 16, T], BF16), Buf()
    for h4 in range(4):
        fw.dma("sp", lambda e, h4=h4: e.dma_start(
            out=qT[:, h4 * 4:(h4 + 1) * 4, :], in_=cx.qT_d[h4 * 256:(h4 + 1) * 256, :].rearrange("(h d) t -> d h t", d=64)),
            writes=[b_qT], partial=True)
    E_r = Ring([sb("E%d" % i, [128, 4, 128], BF16) for i in range(3)])
    Em_r = Ring([sb("Em%d" % i, [128, 4, 128], BF16) for i in range(3)])
    Mb_r = Ring([sb("Mb%d" % i, [128, 128], BF16) for i in range(2)])
    sm = Ring([sb("sm%d" % i, [128, 256], F32) for i in range(2)])
    selb_r = Ring([sb("selb%d" % i, [128, 32], BF16) for i in range(2)])
    selT_r = Ring([sb("selT%d" % i, [32, 128], BF16) for i in range(2)])
    o_r = Ring([sb("onsa%d" % i, [128, 1024], BF16) for i in range(2)])
    oT_r = Ring([sb("onsaT%d" % i, [128, 8, 128], BF16) for i in range(2)])
    acc_r = Ring([sb("oacc%d" % i, [128, 64], F32) for i in range(2)])
    kccT_, vs1, vw1 = kccT, v1["vs"], v1["vw"]
    ksT, b_ks = kT["ks"]
    kwT, b_kw = kT["kw"]

    def attn_branch(i, g, kts, kTt, b_k, v1t, pacc, b_pacc, mask_for):
        first = True
        for kt in kts:
            pst, psb = pS.next()
            fw.op("pe", lambda e, pst=pst, kt=kt: e.matmul(pst[:, :].rearrange("p (h t) -> p h t", h=4), lhsT=kTt[:, g, kt * 128:(kt + 1) * 128],
                  rhs=qT[:, 4 * g:4 * g + 4, i * 128:(i + 1) * 128], start=True, stop=True), reads=[b_k, b_qT], writes=[psb])
            Et, Eb = E_r.next()
            fw.op("act", lambda e, pst=pst, Et=Et: e.activation(out=Et[:].rearrange("p h t -> p (h t)"), in_=pst[:, :], func=AF.Exp, scale=SCALE),
                  reads=[psb], writes=[Eb])
            mk = mask_for(kt)
            if mk is None:
                lt, lb = Et, Eb
            else:
                mt, mb = mk
                lt, lb = Em_r.next()
                fw.op("dve", lambda e, lt=lt, Et=Et, mt=mt: e.tensor_tensor(out=lt[:], in0=Et[:], in1=mt[:].unsqueeze(1).to_broadcast([128, 4, 128]), op=ALU.mult),
                      reads=[Eb, mb], writes=[lb])
            for h in range(4):
                fw.op("pe", lambda e, lt=lt, h=h, kt=kt, first=first: e.matmul(pacc[:, h, :], lhsT=lt[:, h, :], rhs=v1t[0][:, kt, g, :],
                      start=(first and h == 0), stop=False, skip_group_check=True), reads=[lb, v1t[1]], writes=[b_pacc], partial=not (first and h == 0))
            first = False

    def do_group(i, g, ot, ob):
        pst, psb = pS.next()
        fw.op("pe", lambda e, pst=pst: e.matmul(pst[0:127, :].rearrange("p (h t) -> p h t", h=4), lhsT=kccT_[:, g, :],
              rhs=qT[:, 4 * g:4 * g + 4, i * 128:(i + 1) * 128], start=True, stop=True), reads=[b_kcc, b_qT], writes=[psb])
        Et, Eb = E_r.next()
        fw.op("act", lambda e, pst=pst, Et=Et: e.activation(out=Et[0:127].rearrange("p h t -> p (h t)"), in_=pst[0:127, :], func=AF.Exp, scale=SCALE),
              reads=[psb], writes=[Eb])
        Emt, Emb = Em_r.next()
        fw.op("dve", lambda e, Emt=Emt, Et=Et: e.tensor_tensor(out=Emt[0:127], in0=Et[0:127],
              in1=cmaskT[:, i * 128:(i + 1) * 128].unsqueeze(1).to_broadcast([127, 4, 128]), op=ALU.mult), reads=[Eb, b_cm], writes=[Emb])
        for h in range(4):
            fw.op("pe", lambda e, Emt=Emt, h=h: e.matmul(pc_t[:, h, :], lhsT=Emt[0:127, h, :], rhs=vcc[:, g, :], start=True, stop=True,
                  skip_group_check=True), reads=[Emb, b_vcc], writes=[b_pc], partial=(h > 0))
        s_, sb_ = sm.next()
        fw.op("dve", lambda e, s_=s_: e.tensor_scalar(out=s_[:, 0:4], in0=pc_t[:, :, 64], scalar1=1e-30, scalar2=None, op0=ALU.max), reads=[b_pc], writes=[sb_])
        fw.op("dve", lambda e, s_=s_: e.reciprocal(out=s_[:, 0:4], in_=s_[:, 0:4]), reads=[sb_], writes=[sb_])
        fw.op("dve", lambda e, s_=s_: e.scalar_tensor_tensor(out=s_[:, 32:64], in0=pc_t[:, 0, 65:97], scalar=s_[:, 0:1], in1=addc[:, i, :], op0=ALU.mult, op1=ALU.add),
              reads=[b_pc, sb_, b_addc], writes=[sb_])
        for h in range(1, 4):
            fw.op("dve", lambda e, s_=s_, h=h: e.scalar_tensor_tensor(out=s_[:, 32:64], in0=pc_t[:, h, 65:97], scalar=s_[:, h:h + 1], in1=s_[:, 32:64], op0=ALU.mult, op1=ALU.add),
                  reads=[b_pc, sb_], writes=[sb_])
        fw.op("dve", lambda e, s_=s_: e.max(out=s_[:, 96:104], in_=s_[:, 32:64]), reads=[sb_], writes=[sb_])
        fw.op("dve", lambda e, s_=s_: e.match_replace(out=s_[:, 64:96], in_to_replace=s_[:, 96:104], in_values=s_[:, 32:64], imm_value=-3.0e38), reads=[sb_], writes=[sb_])
        fw.op("dve", lambda e, s_=s_: e.max(out=s_[:, 104:112], in_=s_[:, 64:96]), reads=[sb_], writes=[sb_])
        fw.op("dve", lambda e, s_=s_: e.tensor_scalar(out=s_[:, 112:113], in0=s_[:, 111:112], scalar1=-1.0e8, scalar2=None, op0=ALU.max), reads=[sb_], writes=[sb_])
        selb, selbb = selb_r.next()
        fw.op("dve", lambda e, s_=s_, selb=selb: e.tensor_scalar(out=selb[:], in0=s_[:, 32:64], scalar1=s_[:, 112:113], scalar2=None, op0=ALU.is_ge), reads=[sb_], writes=[selbb])
        ptt, ptb = pT.next()
        fw.op("pe", lambda e, ptt=ptt, selb=selb: e.transpose(out=ptt[0:32, 0:128], in_=selb[:], identity=ident_b[:]), reads=[selbb, b_idb], writes=[ptb])
        selT, selTb = selT_r.next()
        fw.op("dve", lambda e, ptt=ptt, selT=selT: e.tensor_copy(out=selT[:], in_=ptt[0:32, 0:128]), reads=[ptb], writes=[selTb])
        if cx.dbg and g == 0:
            fw.dma("sp", lambda e, selb=selb: e.dma_start(out=cx.dbg_sel[i * 128:(i + 1) * 128, :], in_=selb[:]), reads=[selbb], writes=[Buf()])
            fw.dma("sp", lambda e, s_=s_: e.dma_start(out=cx.dbg_sm[i * 128:(i + 1) * 128, :], in_=s_[:]), reads=[sb_], writes=[Buf()])

        def sel_mask(kt, selT=selT, selTb=selTb):
            pm, pmb = pM.next()
            fw.op("pe", lambda e, pm=pm: e.matmul(pm[:, 0:128], lhsT=Xall[:, kt * 128:(kt + 1) * 128], rhs=selT[:], start=True, stop=True),
                  reads=[b_X, selTb], writes=[pmb])
            mt, mb = Mb_r.next()
            if kt == i:
                fw.op("dve", lambda e, pm=pm, mt=mt: e.tensor_tensor(out=mt[:], in0=pm[:, 0:128], in1=tri[:], op=ALU.mult), reads=[pmb, b_tri], writes=[mb])
            else:
                fw.op("dve", lambda e, pm=pm, mt=mt: e.tensor_copy(out=mt[:], in_=pm[:, 0:128]), reads=[pmb], writes=[mb])
            return mt, mb
        attn_branch(i, g, list(range(0, i + 1)), ksT, b_ks, vs1, pss_t, b_pss, sel_mask)

        def win_mask(kt):
            if kt == i:
                return tri, b_tri
            if kt == i - 4:
                return tri2, b_tri2
            return None
        attn_branch(i, g, list(range(max(0, i - 4), i + 1)), kwT, b_kw, vw1, psw_t, b_psw, win_mask)

        fw.op("dve", lambda e, s_=s_: e.reciprocal(out=s_[:, 4:8], in_=pss_t[:, :, 64]), reads=[b_pss, sb_], writes=[sb_])
        fw.op("dve", lambda e, s_=s_: e.reciprocal(out=s_[:, 8:12], in_=psw_t[:, :, 64]), reads=[b_psw, sb_], writes=[sb_])
        gav = ga[:, i, g * 12:(g + 1) * 12].rearrange("p (h b) -> p b h", b=3)
        fw.op("dve", lambda e, s_=s_, gav=gav: e.tensor_tensor(out=s_[:, 12:24].rearrange("p (b h) -> p b h", b=3), in0=gav,
              in1=s_[:, 0:12].rearrange("p (b h) -> p b h", b=3), op=ALU.mult), reads=[sb_, b_ga], writes=[sb_])
        for h in range(4):
            at, ab = acc_r.next()
            fw.op("dve", lambda e, s_=s_, at=at, h=h: e.tensor_scalar(out=at[:], in0=pc_t[:, h, 0:64], scalar1=s_[:, 12 + h:13 + h], scalar2=None, op0=ALU.mult),
                  reads=[b_pc, sb_], writes=[ab])
            fw.op("dve", lambda e, s_=s_, at=at, h=h: e.scalar_tensor_tensor(out=at[:], in0=pss_t[:, h, 0:64], scalar=s_[:, 16 + h:17 + h], in1=at[:], op0=ALU.mult, op1=ALU.add),
                  reads=[b_pss, sb_, ab], writes=[ab])
            c0 = (4 * g + h) * 64
            fw.op("dve", lambda e, s_=s_, at=at, h=h, ot=ot, c0=c0: e.scalar_tensor_tensor(out=ot[:, c0:c0 + 64], in0=psw_t[:, h, 0:64], scalar=s_[:, 20 + h:21 + h], in1=at[:], op0=ALU.mult, op1=ALU.add),
                  reads=[b_psw, sb_, ab], writes=[ob], partial=True)

    def finish_tile(i, ot, ob):
        ptt, ptb = pT.next()
        for j in range(8):
            fw.op("pe", lambda e, ptt=ptt, ot=ot, j=j: e.transpose(out=ptt[:, j * 128:(j + 1) * 128], in_=ot[:, j * 128:(j + 1) * 128], identity=ident_b[:]),
                  reads=[ob, b_idb], writes=[ptb], partial=(j > 0))
        oT, oTb = oT_r.next()
        fw.op("dve", lambda e, ptt=ptt, oT=oT: e.tensor_copy(out=oT[:].rearrange("p j t -> p (j t)"), in_=ptt[:, :]), reads=[ptb], writes=[oTb])
        fw.dma("sp", lambda e, oT=oT, i=i: e.dma_start(out=cx.onT_d[:, i * 128:(i + 1) * 128].rearrange("(j p) t -> p j t", p=128), in_=oT[:]),
               reads=[oTb], writes=[Buf()])

    for i in range(16):
        ot, ob = o_r.next()
        for g in range(4):
            do_group(i, g, ot, ob)
        finish_tile(i, ot, ob)


def mlstm_consts():
    import numpy as np
    import ml_dtypes
    a = np.arange(128)
    tri = (a[:, None] <= a[None, :])
    ms = np.zeros((4, 128, 512), np.float32)
    for r in range(4):
        for j in range(4):
            if j == r:
                ms[r][:, j * 128:(j + 1) * 128] = tri
            elif j > r:
                ms[r][:, j * 128:(j + 1) * 128] = 1.0
    return {"mmask": ms.astype(ml_dtypes.bfloat16)}


def declare_io_c(nc, cx, dbg=False):
    def din(name, shape, dt=F32):
        setattr(cx, name, nc.dram_tensor(name, list(shape), dt, kind="ExternalInput").ap())

    def dscr(name, shape, dt, force_out=False):
        setattr(cx, name, nc.dram_tensor(name, list(shape), dt, kind="ExternalOutput" if (dbg or force_out) else "Internal").ap())
    din("mmask", [4, 128, 512], BF16)
    din("convp", [128, 8, 5])
    din("bgate", [4, 2])
    din("mhg", [1, 1024])
    din("w_up_p", [2 * 1024 * D])
    din("w_out_p", [D * D])
    dscr("omT_d", [1024, T], BF16)
    dscr("ac_d", [8, T], F32)
    dscr("x1_d", [T, D], F32)


def stage_mlstm(nc, fw, cx, es):
    def sb(name, shape, dt):
        return es.enter_context(nc.sbuf_tensor(name, list(shape), dt))

    def ps(name, shape, dt):
        return es.enter_context(nc.psum_tensor(name, list(shape), dt))
    ident_b, b_idb, ident_f, b_id = cx.ident_b, cx.b_idb, cx.ident_f, cx.b_id

    mmask, b_mm = sb("mmask_s", [128, 4, 512], BF16), Buf()
    fw.dma("sp", lambda e: e.dma_start(out=mmask[:], in_=cx.mmask.rearrange("r p t -> p r t")), writes=[b_mm])
    convp, b_cp = sb("convp_s", [128, 8, 5], F32), Buf()
    fw.dma("sp", lambda e: e.dma_start(out=convp[:], in_=cx.convp[:, :, :]), writes=[b_cp])
    bg, b_bg = sb("bg_s", [4, 2], F32), Buf()
    fw.dma("sp", lambda e: e.dma_start(out=bg[:], in_=cx.bgate[:, :]), writes=[b_bg])
    mhg, b_mhg = sb("mhg_s", [128, 1024], F32), Buf()
    fw.dma("sp", lambda e: e.dma_start(out=mhg[:], in_=cx.mhg[0, :].partition_broadcast(128)), writes=[b_mhg])
    vm1, b_vm = sb("vm1", [128, 16, 4, 257], BF16), Buf()
    fw.op("pool", lambda e: e.memset(vm1[:], 1.0), writes=[b_vm])
    for i in range(16):
        fw.dma("sp", lambda e, i=i: e.dma_start(out=vm1[:, i, :, 0:256], in_=cx.vm_d[i * 128:(i + 1) * 128, :].rearrange("p (h d) -> p h d", h=4)),
               reads=[b_vm], writes=[b_vm], partial=True)

    qkb, b_qkb = sb("qkb", [128, 8, T], BF16), [Buf() for _ in range(8)]
    xin = Ring([sb("cx%d" % i, [128, T + 3], F32) for i in range(2)])
    yv = Ring([sb("cy%d" % i, [128, T], F32) for i in range(2)])
    for c in range(8):
        xt, xb = xin.next()
        fw.op("pool", lambda e, xt=xt: e.memset(xt[:, 0:3], 0.0), writes=[xb])
        fw.dma("sp", lambda e, xt=xt, c=c: e.dma_start(out=xt[:, 3:T + 3], in_=cx.qkT_d[c * 128:(c + 1) * 128, :]), reads=[xb], writes=[xb], partial=True)
        yt, yb = yv.next()
        fw.op("dve", lambda e, xt=xt, yt=yt, c=c: e.tensor_scalar(out=yt[:], in0=xt[:, 0:T], scalar1=convp[:, c, 0:1], scalar2=convp[:, c, 4:5], op0=ALU.mult, op1=ALU.add),
              reads=[xb, b_cp], writes=[yb])
        for k in range(1, 4):
            fw.op("dve", lambda e, xt=xt, yt=yt, c=c, k=k: e.scalar_tensor_tensor(out=yt[:], in0=xt[:, k:k + T], scalar=convp[:, c, k:k + 1], in1=yt[:], op0=ALU.mult, op1=ALU.add),
                  reads=[xb, b_cp, yb], writes=[yb])
        if c < 4:
            fw.op("act", lambda e, yt=yt, c=c: e.activation(out=qkb[:, c, :], in_=yt[:], func=AF.Silu), reads=[yb], writes=[b_qkb[c]])
        else:
            fw.op("act", lambda e, yt=yt: e.activation(out=yt[:], in_=yt[:], func=AF.Silu), reads=[yb], writes=[yb])
            fw.op("dve", lambda e, yt=yt, c=c: e.tensor_scalar(out=qkb[:, c, :], in0=yt[:], scalar1=128 ** -0.5, scalar2=None, op0=ALU.mult), reads=[yb], writes=[b_qkb[c]])

    gi, b_gi = sb("gi", [4, T], F32), Buf()
    gf, b_gf = sb("gf", [4, T], F32), Buf()
    ga_, b_ga = sb("ga_", [4, T], F32), Buf()
    ones4, b_o4 = sb("ones4", [4, T], F32), Buf()
    fw.dma("sp", lambda e: e.dma_start(out=gi[:], in_=cx.ifT_d[0:4, :]), writes=[b_gi])
    fw.dma("sp", lambda e: e.dma_start(out=gf[:], in_=cx.ifT_d[4:8, :]), writes=[b_gf])
    fw.op("pool", lambda e: e.memset(ones4[:], 1.0), writes=[b_o4])
    fw.op("dve", lambda e: e.tensor_scalar(out=gf[:], in0=gf[:], scalar1=bg[:, 1:2], scalar2=None, op0=ALU.add), reads=[b_gf, b_bg], writes=[b_gf])
    fw.op("act", lambda e: e.activation(out=gf[:], in_=gf[:], func=AF.Exp, scale=-1.0), reads=[b_gf], writes=[b_gf])
    fw.op("dve", lambda e: e.tensor_scalar(out=gf[:], in0=gf[:], scalar1=1.0, scalar2=None, op0=ALU.add), reads=[b_gf], writes=[b_gf])
    fw.op("act", lambda e: e.activation(out=gf[:], in_=gf[:], func=AF.Ln), reads=[b_gf], writes=[b_gf])
    fw.op("dve", lambda e: e.tensor_scalar(out=gf[:], in0=gf[:], scalar1=-1.0, scalar2=None, op0=ALU.mult), reads=[b_gf], writes=[b_gf])
    fw.op("dve", lambda e: e.tensor_tensor_scan(out=ga_[:], data0=ones4[:], data1=gf[:], initial=0.0, op0=ALU.mult, op1=ALU.add),
          reads=[b_gf, b_o4], writes=[b_ga])
    fw.op("dve", lambda e: e.scalar_tensor_tensor(out=gi[:], in0=gi[:], scalar=bg[:, 0:1], in1=ga_[:], op0=ALU.add, op1=ALU.subtract),
          reads=[b_gi, b_bg, b_ga], writes=[b_gi])
    b_acd = Buf()
    fw.dma("sp", lambda e: e.dma_start(out=cx.ac_d[0:4, :], in_=ga_[:]), reads=[b_ga], writes=[b_acd])
    pmisc, b_pmisc = ps("pmisc_m", [128, 512], F32), Buf()
    for i in range(16):
        fw.op("pe", lambda e, i=i: e.transpose(out=pmisc[:, i * 4:(i + 1) * 4], in_=gi[:, i * 128:(i + 1) * 128], identity=ident_f[0:4, 0:4]),
              reads=[b_gi, b_id], writes=[b_pmisc], partial=(i > 0))
    cT, b_cT = sb("cT_m", [128, 16, 4], F32), Buf()
    fw.op("dve", lambda e: e.tensor_copy(out=cT[:].rearrange("p i h -> p (i h)"), in_=pmisc[:, 0:64]), reads=[b_pmisc], writes=[b_cT])

    pS = Ring([ps("pSm%d" % i, [128, 512], F32) for i in range(2)])
    pacc = [ps("paccm%d" % i, [128, 512], F32) for i in range(4)]
    b_pacc = [Buf() for _ in range(4)]
    pT = Ring([ps("pTm%d" % i, [128, 1024], BF16) for i in range(1)])
    Abc_r = Ring([sb("Abc%d" % i, [128, T], F32) for i in range(2)])
    D_r = Ring([sb("Dm%d" % i, [128, 512], F32) for i in range(3)])
    W_r = Ring([sb("Wm%d" % i, [128, 512], BF16) for i in range(3)])
    om_r = Ring([sb("omt%d" % i, [128, 256], BF16) for i in range(2)])
    hc_r = Ring([sb("hc%d" % i, [128, 256], F32) for i in range(2)])
    hj_r = Ring([sb("hj%d" % i, [128, 256], F32) for i in range(2)])
    ho_r = Ring([sb("ho%d" % i, [128, 256], BF16) for i in range(2)])
    hT_r = Ring([sb("hoT%d" % i, [128, 2, 128], BF16) for i in range(2)])
    st_r = Ring([sb("stm%d" % i, [128, 8], F32) for i in range(2)])

    def head_chunk(h, c, Abc, Abcb):
        for kt in range(4 * c + 4):
            pst, psb = pS.next()
            fw.op("pe", lambda e, pst=pst, kt=kt: e.matmul(pst[:, :], lhsT=qkb[:, 4 + h, kt * 128:(kt + 1) * 128], rhs=qkb[:, h, c * 512:(c + 1) * 512],
                  start=True, stop=True), reads=[b_qkb[4 + h], b_qkb[h]], writes=[psb])
            Dt, Db = D_r.next()
            fw.op("act", lambda e, Dt=Dt, kt=kt: e.activation(out=Dt[:], in_=Abc[:, c * 512:(c + 1) * 512], func=AF.Exp, bias=cT[:, kt, h:h + 1]),
                  reads=[Abcb, b_cT], writes=[Db])
            Wt, Wb = W_r.next()
            if kt // 4 == c:
                fw.op("dve", lambda e, Dt=Dt, kt=kt: e.tensor_tensor(out=Dt[:], in0=Dt[:], in1=mmask[:, kt % 4, :], op=ALU.mult), reads=[Db, b_mm], writes=[Db])
            fw.op("dve", lambda e, Dt=Dt, Wt=Wt, pst=pst: e.tensor_tensor(out=Wt[:], in0=pst[:, :], in1=Dt[:], op=ALU.mult), reads=[psb, Db], writes=[Wb])
            for j in range(4):
                tj = 4 * c + j
                if tj < kt:
                    continue
                fw.op("pe", lambda e, Wt=Wt, j=j, kt=kt, tj=tj: e.matmul(pacc[j][:, 0:257], lhsT=Wt[:, j * 128:(j + 1) * 128], rhs=vm1[:, kt, h, :],
                      start=(kt == 0), stop=(kt == tj)), reads=[Wb, b_vm], writes=[b_pacc[j]])
        for j in range(4):
            tj = 4 * c + j
            st, stb = st_r.next()
            fw.op("dve", lambda e, st=st, j=j: e.tensor_scalar(out=st[:, 6:7], in0=pacc[j][:, 256:257], scalar1=-1.0, scalar2=None, op0=ALU.mult),
                  reads=[b_pacc[j]], writes=[stb])
            fw.op("dve", lambda e, st=st, j=j: e.tensor_tensor(out=st[:, 7:8], in0=st[:, 6:7], in1=pacc[j][:, 256:257], op=ALU.max),
                  reads=[b_pacc[j], stb], writes=[stb])
            fw.op("dve", lambda e, st=st, j=j: e.tensor_scalar(out=st[:, 0:1], in0=st[:, 7:8], scalar1=1.0, scalar2=None, op0=ALU.max),
                  reads=[stb], writes=[stb])
            fw.op("dve", lambda e, st=st: e.reciprocal(out=st[:, 1:2], in_=st[:, 0:1]), reads=[stb], writes=[stb])
            hc, hcb = hc_r.next()
            fw.op("dve", lambda e, st=st, hc=hc, j=j: e.tensor_scalar(out=hc[:], in0=pacc[j][:, 0:256], scalar1=st[:, 1:2], scalar2=None, op0=ALU.mult),
                  reads=[b_pacc[j], stb], writes=[hcb])
            hj, hjb = hj_r.next()
            fw.op("dve", lambda e, st=st, hc=hc, hj=hj: e.scalar_tensor_tensor(out=hj[:], in0=hc[:], scalar=1.0 / 256, in1=hc[:], op0=ALU.mult, op1=ALU.mult, accum_out=st[:, 2:3]),
                  reads=[hcb], writes=[hjb, stb])
            fw.op("dve", lambda e, st=st: e.tensor_scalar(out=st[:, 3:4], in0=st[:, 2:3], scalar1=EPS, scalar2=None, op0=ALU.add), reads=[stb], writes=[stb])
            fw.op("act", lambda e, st=st: e.activation(out=st[:, 4:5], in_=st[:, 3:4], func=AF.Sqrt), reads=[stb], writes=[stb])
            fw.op("dve", lambda e, st=st: e.reciprocal(out=st[:, 5:6], in_=st[:, 4:5]), reads=[stb], writes=[stb])
            omt, omb = om_r.next()
            fw.dma("sp", lambda e, omt=omt, tj=tj: e.dma_start(out=omt[:], in_=cx.om_d[tj * 128:(tj + 1) * 128, h * 256:(h + 1) * 256]), writes=[omb])
            fw.op("dve", lambda e, st=st, hc=hc, hj=hj: e.scalar_tensor_tensor(out=hj[:], in0=hc[:], scalar=st[:, 5:6], in1=mhg[:, h * 256:(h + 1) * 256], op0=ALU.mult, op1=ALU.mult),
                  reads=[hcb, stb, b_mhg], writes=[hjb])
            ho, hob = ho_r.next()
            fw.op("dve", lambda e, hj=hj, ho=ho, omt=omt: e.tensor_tensor(out=ho[:], in0=hj[:], in1=omt[:], op=ALU.mult), reads=[hjb, omb], writes=[hob])
            ptt, ptb = pT.next()
            for bk in range(2):
                fw.op("pe", lambda e, ptt=ptt, ho=ho, bk=bk: e.transpose(out=ptt[:, bk * 128:(bk + 1) * 128], in_=ho[:, bk * 128:(bk + 1) * 128], identity=ident_b[:]),
                      reads=[hob, b_idb], writes=[ptb], partial=(bk > 0))
            hT, hTb = hT_r.next()
            fw.op("dve", lambda e, ptt=ptt, hT=hT: e.tensor_copy(out=hT[:].rearrange("p b t -> p (b t)"), in_=ptt[:, 0:256]), reads=[ptb], writes=[hTb])
            fw.dma("sp", lambda e, hT=hT, tj=tj: e.dma_start(out=cx.omT_d[h * 256:(h + 1) * 256, tj * 128:(tj + 1) * 128].rearrange("(b p) t -> p b t", p=128), in_=hT[:]),
                   reads=[hTb], writes=[Buf()])

    for h in range(4):
        Abc, Abcb = Abc_r.next()
        fw.dma("sp", lambda e, Abc=Abc, h=h: e.dma_start(out=Abc[:], in_=cx.ac_d[h, :].partition_broadcast(128)), reads=[b_acd], writes=[Abcb])
        for c in range(4):
            head_chunk(h, c, Abc, Abcb)


def stage_merge(nc, fw, cx, es):
    from contextlib import ExitStack
    stack = [es]

    def sb(name, shape, dt):
        return stack[-1].enter_context(nc.sbuf_tensor(name, list(shape), dt))

    def ps(name, shape, dt):
        return stack[-1].enter_context(nc.psum_tensor(name, list(shape), dt))
    Grow, b_Grow = cx.Grow, cx.b_Grow
    mT = sb("mT", [128, KC, T], BF16)
    b_mT = [Buf() for _ in range(4)]
    pA = Ring([ps("pA%d" % i, [128, 512], F32) for i in range(2)])
    pB = Ring([ps("pB%d" % i, [128, 512], F32) for i in range(2)])
    pY = [ps("pY%d" % i, [128, 512], F32) for i in range(4)]
    b_pY = [Buf() for _ in range(4)]
    cast_tog = [0]

    stack.append(ExitStack())
    oT = [sb("oTa", [128, 8, T], BF16), sb("oTb", [128, 8, T], BF16)]
    b_oT = [Buf(), Buf()]
    for k, src in enumerate((cx.onT_d, cx.omT_d)):
        for j2 in range(2):
            fw.dma("sp", lambda e, k=k, src=src, j2=j2: e.dma_start(out=oT[k][:, j2 * 4:(j2 + 1) * 4, :],
                   in_=src[j2 * 512:(j2 + 1) * 512, :].rearrange("(j p) t -> p j t", p=128)), writes=[b_oT[k]], partial=True)
    wf = Ring([sb("wuf%d" % i, [128, 8, 256], F32) for i in range(2)])
    wb = Ring([sb("wub%d" % i, [128, 8, 512], BF16) for i in range(4)])
    gt_r = Ring([sb("gtile%d" % i, [128, 512], BF16) for i in range(4)])
    m1_r = Ring([sb("m1_%d" % i, [128, 512], F32) for i in range(2)])

    def load_up(which, cc):
        wbt, wbb = wb.next()
        for h0 in range(2):
            wt, wtb = wf.next()
            o = which * 1024 * D + (cc * 512 + h0 * 256) * 128 * 8
            fw.dma("sp", lambda e, wt=wt, o=o: e.dma_start(out=wt[:], in_=cx.w_up_p[o:o + 128 * 8 * 256].rearrange("(p k n) -> p k n", p=128, k=8)), writes=[wtb])
            eng = "pool" if cast_tog[0] % 2 == 0 else "dve"
            cast_tog[0] += 1
            fw.op(eng, lambda e, wt=wt, wbt=wbt, h0=h0: e.tensor_copy(out=wbt[:, :, h0 * 256:(h0 + 1) * 256], in_=wt[:]), reads=[wtb], writes=[wbb], partial=True)
        return wbt, wbb

    for cc in range(4):
        wa, wab = load_up(0, cc)
        wbm, wbmb = load_up(1, cc)
        for m in range(4):
            fc = cc * 4 + m
            for tc in range(4):
                pa, pab = pA.next()
                pb_, pbb = pB.next()
                for (pt, ptb, w, wbuf, k) in ((pa, pab, wa, wab, 0), (pb_, pbb, wbm, wbmb, 1)):
                    for kc in range(8):
                        fw.op("pe", lambda e, pt=pt, w=w, kc=kc, m=m, tc=tc, k=k: e.matmul(pt[:, :], lhsT=w[:, kc, m * 128:(m + 1) * 128],
                              rhs=oT[k][:, kc, tc * 512:(tc + 1) * 512], start=(kc == 0), stop=(kc == 7)), reads=[wbuf, b_oT[k]], writes=[ptb])
                g1, g1b = gt_r.next()
                g2, g2b = gt_r.next()
                fw.dma("sp", lambda e, g1=g1, fc=fc, tc=tc: e.dma_start(out=g1[:], in_=cx.gaT_d[fc * 128:(fc + 1) * 128, tc * 512:(tc + 1) * 512]), writes=[g1b])
                fw.dma("sp", lambda e, g2=g2, fc=fc, tc=tc: e.dma_start(out=g2[:], in_=cx.gbT_d[fc * 128:(fc + 1) * 128, tc * 512:(tc + 1) * 512]), writes=[g2b])
                m1, m1b = m1_r.next()
                m2, m2b = m1_r.next()
                fw.op("dve", lambda e, m1=m1, pa=pa, g1=g1: e.tensor_tensor(out=m1[:], in0=pa[:, :], in1=g1[:], op=ALU.mult), reads=[pab, g1b], writes=[m1b])
                fw.op("dve", lambda e, m2=m2, pb_=pb_, g2=g2: e.tensor_tensor(out=m2[:], in0=pb_[:, :], in1=g2[:], op=ALU.mult), reads=[pbb, g2b], writes=[m2b])
                fw.op("pool", lambda e, m1=m1, m2=m2, fc=fc, tc=tc: e.tensor_tensor(out=mT[:, fc, tc * 512:(tc + 1) * 512], in0=m1[:], in1=m2[:], op=ALU.add),
                      reads=[m1b, m2b], writes=[b_mT[tc]], partial=True)
    fw.barrier()
    fw.flush()
    stack.pop().close()

    stack.append(ExitStack())
    wo = sb("wo_b", [128, KC, D], BF16)
    b_wo = [Buf() for _ in range(4)]
    wof = Ring([sb("wof%d" % i, [128, KC, 128], F32) for i in range(2)])
    cx.junk_f, cx.b_junk_f, cx.b_x1d = sb("junk_f", [128, D], BF16), Buf(), Buf()
    for c16 in range(16):
        wt, wtb = wof.next()
        o = c16 * 128 * 128 * KC
        fw.dma("sp", lambda e, wt=wt, o=o: e.dma_start(out=wt[:], in_=cx.w_out_p[o:o + 128 * KC * 128].rearrange("(p k n) -> p k n", p=128, k=KC)), writes=[wtb])
        eng = "pool" if c16 % 2 == 0 else "dve"
        fw.op(eng, lambda e, wt=wt, c16=c16: e.tensor_copy(out=wo[:, :, c16 * 128:(c16 + 1) * 128], in_=wt[:]), reads=[wtb], writes=[b_wo[c16 // 4]], partial=True)
    xt_r = Ring([sb("xm%d" % i, [128, D], F32) for i in range(2)])
    y_r = Ring([sb("ym%d" % i, [128, D], F32) for i in range(2)])
    st_r = Ring([sb("stz%d" % i, [128, 8], F32) for i in range(2)])
    for i in range(16):
        for n in range(4):
            for fc in range(KC):
                fw.op("pe", lambda e, i=i, n=n, fc=fc: e.matmul(pY[n][:, :], lhsT=mT[:, fc, i * 128:(i + 1) * 128], rhs=wo[:, fc, n * 512:(n + 1) * 512],
                      start=(fc == 0), stop=(fc == KC - 1)), reads=[b_mT[i // 4], b_wo[n]], writes=[b_pY[n]])
        yt, yb = y_r.next()
        for n in range(4):
            if n % 2 == 0:
                fw.op("act", lambda e, yt=yt, n=n: e.activation(out=yt[:, n * 512:(n + 1) * 512], in_=pY[n][:, :], func=AF.Identity), reads=[b_pY[n]], writes=[yb], partial=True)
            else:
                fw.op("dve", lambda e, yt=yt, n=n: e.tensor_copy(out=yt[:, n * 512:(n + 1) * 512], in_=pY[n][:, :]), reads=[b_pY[n]], writes=[yb], partial=True)
        post_norm_residual(fw, cx, i, yt, yb, xt_r, st_r, cx.x, Grow[0], b_Grow[0], cx.x1_d)
    fw.barrier()
    fw.flush()
    stack.pop().close()


def post_norm_residual(fw, cx, i, yt, yb, xt_r, st_r, x_src, G, b_G, dst, x_dep=None):
    xt, xb = xt_r.next()
    fw.dma("sp", lambda e: e.dma_start(out=xt[:], in_=x_src[i * 128:(i + 1) * 128, :]), writes=[xb])
    st, stb = st_r.next()
    fw.op("dve", lambda e: e.scalar_tensor_tensor(out=cx.junk_f[:], in0=yt[:], scalar=1.0 / D, in1=yt[:], op0=ALU.mult, op1=ALU.mult, accum_out=st[:, 0:1]),
          reads=[yb], writes=[cx.b_junk_f, stb])
    fw.op("dve", lambda e: e.tensor_scalar(out=st[:, 1:2], in0=st[:, 0:1], scalar1=EPS, scalar2=None, op0=ALU.add), reads=[stb], writes=[stb])
    fw.op("act", lambda e: e.activation(out=st[:, 2:3], in_=st[:, 1:2], func=AF.Sqrt), reads=[stb], writes=[stb])
    fw.op("dve", lambda e: e.reciprocal(out=st[:, 3:4], in_=st[:, 2:3]), reads=[stb], writes=[stb])
    fw.op("dve", lambda e: e.scalar_tensor_tensor(out=yt[:], in0=yt[:], scalar=st[:, 3:4], in1=G[:], op0=ALU.mult, op1=ALU.mult), reads=[yb, stb, b_G], writes=[yb])
    fw.op("pool", lambda e: e.tensor_tensor(out=xt[:], in0=xt[:], in1=yt[:], op=ALU.add), reads=[xb, yb], writes=[xb])
    fw.dma("sp", lambda e: e.dma_start(out=dst[i * 128:(i + 1) * 128, :], in_=xt[:]), reads=[xb], writes=[cx.b_x1d], partial=True)


NE = 65


def pack_gu(w):
    return pack_cols(w, [(0, 512)])[0]


def pack_dn(w):
    import numpy as np
    return np.concatenate([np.ascontiguousarray(w[:, h * 1024:(h + 1) * 1024].reshape(4, 128, 1024).transpose(1, 0, 2)).reshape(-1) for h in range(2)])


def declare_io_d(nc, cx, dbg=False):
    def din(name, shape, dt=F32):
        setattr(cx, name, nc.dram_tensor(name, list(shape), dt, kind="ExternalInput").ap())

    def dscr(name, shape, dt, out=False):
        setattr(cx, name, nc.dram_tensor(name, list(shape), dt, kind="ExternalOutput" if (dbg or out) else "Internal").ap())
    din("w_router", [D, 64])
    din("b_router", [1, 64])
    din("w_eg_p", [NE * D * 512])
    din("w_eu_p", [NE * D * 512])
    din("w_ed_p", [NE * 512 * D])
    dscr("h2T_d", [128, KC * T], BF16)
    dscr("out", [T, D], F32, out=True)
    if dbg:
        dscr("dbg_wr", [128, 16, NE], F32)


def stage_router(nc, fw, cx, es):
    def sb(name, shape, dt):
        return es.enter_context(nc.sbuf_tensor(name, list(shape), dt))

    def ps(name, shape, dt):
        return es.enter_context(nc.psum_tensor(name, list(shape), dt))
    Wr, b_Wr = cx.Wr, cx.b_Wr
    h2T = sb("h2T", [128, KC, T], BF16)
    b_h2T = [Buf() for _ in range(16)]
    xt_r = Ring([sb("xt2_%d" % i, [128, D], F32) for i in range(2)])
    xn_r = Ring([sb("xn2_%d" % i, [128, D], BF16) for i in range(2)])
    junk = sb("junk2", [128, D], BF16)
    st_r = Ring([sb("stat2_%d" % i, [128, 4], F32) for i in range(2)])
    ptr = Ring([ps("ptr2_%d" % i, [128, 1024], BF16) for i in range(2)])
    tok_hook = None
    if hasattr(cx, "h2_d"):
        Arow, b_Ar = sb("Arow", [128, D], F32), Buf()
        Brow, b_Br = sb("Brow", [128, D], F32), Buf()
        fw.dma("sp", lambda e: e.dma_start(out=Arow[:], in_=cx.rowAB_d[0, :].partition_broadcast(128)), reads=[cx.b_rowAB], writes=[b_Ar])
        fw.dma("sp", lambda e: e.dma_start(out=Brow[:], in_=cx.rowAB_d[1, :].partition_broadcast(128)), reads=[cx.b_rowAB], writes=[b_Br])
        t1_r = Ring([sb("h2t1_%d" % i, [128, D], F32) for i in range(1)])
        h2t_r = Ring([sb("h2tok%d" % i, [128, D], BF16) for i in range(2)])
        cx.b_h2d = Buf()

        def tok_hook(i, xn, xnb):
            t1, t1b = t1_r.next()
            ht, htb = h2t_r.next()
            fw.op("pool", lambda e: e.tensor_tensor(out=t1[:], in0=xn[:], in1=Arow[:], op=ALU.mult), reads=[xnb, b_Ar], writes=[t1b])
            fw.op("pool", lambda e: e.tensor_tensor(out=ht[:], in0=t1[:], in1=Brow[:], op=ALU.add), reads=[t1b, b_Br], writes=[htb])
            fw.dma("sp", lambda e: e.dma_start(out=cx.h2_d[i * 128:(i + 1) * 128, :], in_=ht[:]), reads=[htb], writes=[cx.b_h2d], partial=True)
    norm_pre(nc, fw, cx, cx.x1_d, h2T, b_h2T, xt_r, xn_r, junk, Buf(), st_r, ptr, 1, x_dep=[cx.b_x1d], tok_hook=tok_hook)
    for i4 in range(4):
        fw.dma("sp", lambda e, i4=i4: e.dma_start(out=cx.h2T_d.rearrange("p (k t) -> p k t", k=KC)[:, :, i4 * 512:(i4 + 1) * 512], in_=h2T[:, :, i4 * 512:(i4 + 1) * 512]),
               reads=b_h2T[i4 * 4:(i4 + 1) * 4], writes=[cx.b_h2Td], partial=True)
    wrf, b_wrf = sb("wrf", [128, KC, 64], F32), Buf()
    wrb, b_wrb = sb("wrb", [128, KC, 64], BF16), Buf()
    brt, b_brt = sb("brt", [128, 64], F32), Buf()
    fw.dma("sp", lambda e: e.dma_start(out=wrf[:], in_=cx.w_router.rearrange("(k p) n -> p k n", p=128)), writes=[b_wrf])
    fw.dma("sp", lambda e: e.dma_start(out=brt[:], in_=cx.b_router[0, :].partition_broadcast(128)), writes=[b_brt])
    fw.op("dve", lambda e: e.tensor_copy(out=wrb[:], in_=wrf[:]), reads=[b_wrf], writes=[b_wrb])
    fw.op("pool", lambda e: e.memset(Wr[:], 1.0), writes=[b_Wr])
    pl = Ring([ps("plog%d" % i, [128, 512], F32) for i in range(2)])
    r_r = Ring([sb("rt%d" % i, [128, 512], F32) for i in range(2)])

    def route_tile(i):
        pt, pb = pl.next()
        for kc in range(KC):
            fw.op("pe", lambda e, kc=kc: e.matmul(pt[:, 0:64], lhsT=h2T[:, kc, i * 128:(i + 1) * 128], rhs=wrb[:, kc, :], start=(kc == 0), stop=(kc == KC - 1)),
                  reads=[b_h2T[i], b_wrb], writes=[pb])
        r, rb = r_r.next()
        S, SB, M8, GS, M, GM, T1, SBM, M8B, SEL = (r[:, 0:64], r[:, 64:128], r[:, 128:192], r[:, 192:200], r[:, 200:208], r[:, 208:216],
                                                  r[:, 216:224], r[:, 224:288], r[:, 288:296], r[:, 296:360])
        ops = []
        fw.op("act", lambda e: e.activation(out=S, in_=pt[:, 0:64], func=AF.Sigmoid), reads=[pb], writes=[rb])
        dv = lambda f, extra=(): fw.op("dve", f, reads=[rb] + list(extra), writes=[rb])
        dv(lambda e: e.tensor_tensor(out=SB, in0=S, in1=brt[:], op=ALU.add), [b_brt])
        for g in range(8):
            dv(lambda e, g=g: e.max(out=r[:, 128 + g * 8:136 + g * 8], in_=r[:, 64 + g * 8:72 + g * 8]))
        m8v = M8.rearrange("p (g k) -> p g k", k=8)
        dv(lambda e: e.tensor_tensor(out=GS, in0=m8v[:, :, 0], in1=m8v[:, :, 1], op=ALU.add))
        dv(lambda e: e.max(out=M, in_=GS))
        dv(lambda e: e.tensor_scalar(out=GM, in0=GS, scalar1=r[:, 203:204], scalar2=None, op0=ALU.is_ge))
        dv(lambda e: e.tensor_scalar(out=T1, in0=GM, scalar1=1.0e9, scalar2=-1.0e9, op0=ALU.mult, op1=ALU.add))
        dv(lambda e: e.tensor_tensor(out=SBM.rearrange("p (g k) -> p g k", k=8), in0=SB.rearrange("p (g k) -> p g k", k=8),
                                     in1=GM.unsqueeze(2).to_broadcast([128, 8, 8]), op=ALU.mult))
        dv(lambda e: e.tensor_tensor(out=SBM.rearrange("p (g k) -> p g k", k=8), in0=SBM.rearrange("p (g k) -> p g k", k=8),
                                     in1=T1.unsqueeze(2).to_broadcast([128, 8, 8]), op=ALU.add))
        dv(lambda e: e.max(out=M8B, in_=SBM))
        dv(lambda e: e.tensor_scalar(out=SEL, in0=SBM, scalar1=r[:, 295:296], scalar2=None, op0=ALU.is_ge))
        dv(lambda e: e.tensor_tensor(out=SEL, in0=SEL, in1=S, op=ALU.mult))
        dv(lambda e: e.tensor_reduce(out=r[:, 360:361], in_=SEL, axis=AX.X, op=ALU.add))
        dv(lambda e: e.reciprocal(out=r[:, 361:362], in_=r[:, 360:361]))
        fw.op("dve", lambda e: e.tensor_scalar(out=Wr[:, i, 0:64], in0=SEL, scalar1=r[:, 361:362], scalar2=2.5, op0=ALU.mult, op1=ALU.mult),
              reads=[rb, b_Wr], writes=[b_Wr], partial=True)

    for i in range(16):
        route_tile(i)
    if cx.dbg:
        fw.dma("sp", lambda e: e.dma_start(out=cx.dbg_wr[:, :, :], in_=Wr[:]), reads=[b_Wr], writes=[Buf()])


def stage_moe(nc, fw, cx, es, n_exp=NE, groups=(0, 1, 2, 3)):
    def sb(name, shape, dt):
        return es.enter_context(nc.sbuf_tensor(name, list(shape), dt))

    def ps(name, shape, dt):
        return es.enter_context(nc.psum_tensor(name, list(shape), dt))
    Wr, b_Wr = cx.Wr, cx.b_Wr
    Grow, b_Grow = cx.Grow, cx.b_Grow
    yacc = sb("yacc", [128, 4, D], F32)
    b_y = [Buf() for _ in range(4)]
    h2g, b_h2g = sb("h2g", [128, KC, 512], BF16), Buf()
    wgu = Ring([sb("wgu%d" % i, [128, KC, 512], BF16) for i in range(3)])
    wdr = Ring([sb("wdn%d" % i, [128, 4, D], BF16) for i in range(2)])
    wst = Ring([sb("wst%d" % i, [128, 4096], F32) for i in range(2)])
    sg_r = Ring([sb("sg%d" % i, [128, 512], F32) for i in range(2)])
    hT_r = Ring([sb("hTe%d" % i, [128, 4, 512], BF16) for i in range(2)])
    xt_r = Ring([sb("xf%d" % i, [128, D], F32) for i in range(1)])
    st_r = Ring([sb("stf%d" % i, [128, 8], F32) for i in range(2)])
    cx.junk_f, cx.b_junk_f = sb("junk_f2", [128, D], BF16), Buf()
    b_out = Buf()
    cx.b_x1d_save = cx.b_x1d
    pg = Ring([ps("pg%d" % i, [128, 512], F32) for i in range(2)])
    pu = Ring([ps("pu%d" % i, [128, 512], F32) for i in range(2)])
    py = Ring([ps("py%d" % i, [128, 512], F32) for i in range(3)])
    tog = [0]

    def load_mat(src_flat, off, dst, dstb, view):
        for half in range(2):
            wt, wtb = wst.next()
            o = off + half * 128 * 4096
            fw.dma("sp", lambda e, wt=wt, o=o: e.dma_start(out=wt[:], in_=src_flat[o:o + 128 * 4096].rearrange("(p n) -> p n", p=128)), writes=[wtb])
            eng = "pool" if tog[0] % 2 == 0 else "dve"
            tog[0] += 1
            fw.op(eng, lambda e, wt=wt, half=half: e.tensor_copy(out=view(dst, half), in_=view_st(wt, view)), reads=[wtb], writes=[dstb], partial=True)

    def view_st(wt, view):
        return wt[:].rearrange("p (k n) -> p k n", k=KC) if view is v_gu else wt[:].rearrange("p (k n) -> p k n", k=4)

    def v_gu(dst, half):
        return dst[:, :, half * 256:(half + 1) * 256]

    def v_dn(dst, half):
        return dst[:, :, half * 1024:(half + 1) * 1024]

    def expert(grp, e, first):
        wg, wgb = wgu.next()
        load_mat(cx.w_eg_p, e * D * 512, wg, wgb, v_gu)
        wu, wub = wgu.next()
        load_mat(cx.w_eu_p, e * D * 512, wu, wub, v_gu)
        wd, wdb = wdr.next()
        load_mat(cx.w_ed_p, e * 512 * D, wd, wdb, v_dn)
        hT, hTb = hT_r.next()
        for fc in range(4):
            pgt, pgb = pg.next()
            put, pub = pu.next()
            for (pt, ptb, w, wb_) in ((pgt, pgb, wg, wgb), (put, pub, wu, wub)):
                for kc in range(KC):
                    fw.op("pe", lambda e_, pt=pt, w=w, kc=kc, fc=fc: e_.matmul(pt[:, :], lhsT=w[:, kc, fc * 128:(fc + 1) * 128], rhs=h2g[:, kc, :],
                          start=(kc == 0), stop=(kc == KC - 1)), reads=[wb_, b_h2g], writes=[ptb])
            sg, sgb = sg_r.next()
            fw.op("act", lambda e_, sg=sg, pgt=pgt: e_.activation(out=sg[:], in_=pgt[:, :], func=AF.Silu), reads=[pgb], writes=[sgb])
            fw.op("dve", lambda e_, sg=sg, put=put, hT=hT, fc=fc: e_.tensor_tensor(out=hT[:, fc, :], in0=put[:, :], in1=sg[:], op=ALU.mult),
                  reads=[pub, sgb], writes=[hTb], partial=(fc > 0))
        for ti in range(4):
            for n in range(4):
                pyt, pyb = py.next()
                for fc in range(4):
                    fw.op("pe", lambda e_, pyt=pyt, hT=hT, wd=wd, fc=fc, ti=ti, n=n: e_.matmul(pyt[:, :], lhsT=hT[:, fc, ti * 128:(ti + 1) * 128],
                          rhs=wd[:, fc, n * 512:(n + 1) * 512], start=(fc == 0), stop=(fc == 3)), reads=[hTb, wdb], writes=[pyb])
                wcol = Wr[:, grp * 4 + ti, e:e + 1]
                if first:
                    fw.op("dve", lambda e_, pyt=pyt, ti=ti, n=n, wcol=wcol: e_.tensor_scalar(out=yacc[:, ti, n * 512:(n + 1) * 512], in0=pyt[:, :], scalar1=wcol, scalar2=None, op0=ALU.mult),
                          reads=[pyb, b_Wr], writes=[b_y[ti]], partial=True)
                else:
                    fw.op("dve", lambda e_, pyt=pyt, ti=ti, n=n, wcol=wcol: e_.scalar_tensor_tensor(out=yacc[:, ti, n * 512:(n + 1) * 512], in0=pyt[:, :], scalar=wcol,
                          in1=yacc[:, ti, n * 512:(n + 1) * 512], op0=ALU.mult, op1=ALU.add), reads=[pyb, b_Wr, b_y[ti]], writes=[b_y[ti]], partial=True)

    class YT:
        pass
    for grp in groups:
        fw.dma("sp", lambda e, grp=grp: e.dma_start(out=h2g[:], in_=cx.h2T_d.rearrange("p (k t) -> p k t", k=KC)[:, :, grp * 512:(grp + 1) * 512]),
               reads=[cx.b_h2Td], writes=[b_h2g])
        elist = list(range(n_exp)) if n_exp == NE else list(range(n_exp - 1)) + [NE - 1]
        for k, e in enumerate(elist):
            expert(grp, e, k == 0)
        for ti in range(4):
            i = grp * 4 + ti
            cx.b_x1d = b_out
            post_norm_residual(fw, cx, i, yacc[:, ti, :], b_y[ti], xt_r, st_r, cx.x1_d, Grow[1], b_Grow[1], cx.out, x_dep=[cx.b_x1d_save])
    cx.b_out = b_out


CAP = 768
NCJ = CAP // 128
BIG = 1.0e6


def pack_gu4(w):
    import numpy as np
    return np.concatenate([np.ascontiguousarray(w[:, q * 128:(q + 1) * 128].reshape(16, 128, 128).transpose(1, 0, 2)).reshape(-1) for q in range(4)])


def pack_dn4(w):
    import numpy as np
    return np.concatenate([np.ascontiguousarray(w[:, q * 512:(q + 1) * 512].reshape(4, 128, 512).transpose(1, 0, 2)).reshape(-1) for q in range(4)])


def moe_consts():
    import numpy as np
    import ml_dtypes
    bf = ml_dtypes.bfloat16
    a = np.arange(128)
    ltri = (a[:, None] < a[None, :]).astype(bf)
    iota_c = np.tile(np.arange(CAP, dtype=np.float32)[None, :], (128, 1))
    t = (np.arange(16)[None, :] * 128 + a[:, None])
    tconst = np.stack([t // 16, t % 16, np.ones_like(t)], -1).astype(bf)
    return {"ltri": ltri, "iota_c": iota_c, "tconst": tconst}


def declare_io_e(nc, cx, dbg=False):
    def din(name, shape, dt=F32):
        setattr(cx, name, nc.dram_tensor(name, list(shape), dt, kind="ExternalInput").ap())

    def dscr(name, shape, dt, out=False):
        setattr(cx, name, nc.dram_tensor(name, list(shape), dt, kind="ExternalOutput" if (dbg or out) else "Internal").ap())
    din("ltri", [128, 128], BF16)
    din("iota_c", [128, CAP], F32)
    din("tconst", [128, 16, 3], BF16)
    cx.h2_d = nc.dram_tensor("h2_d", [T, D], BF16, kind="Internal").ap()
    dscr("y_d", [T, D], F32)
    dscr("rowAB_d", [2, D], F32)


def stage_moe_sparse(nc, fw, cx, es, n_routed=64):
    from contextlib import ExitStack
    stack = [es]

    def sb(name, shape, dt):
        return stack[-1].enter_context(nc.sbuf_tensor(name, list(shape), dt))

    def ps(name, shape, dt):
        return stack[-1].enter_context(nc.psum_tensor(name, list(shape), dt))
    Wr, b_Wr = cx.Wr, cx.b_Wr
    Grow, b_Grow = cx.Grow, cx.b_Grow
    ident_b, b_idb = cx.ident_b, cx.b_idb
    b_yd = Buf()

    wring = Ring([sb("wq%d" % i, [128, 8192], BF16) for i in range(4)])
    wst = Ring([sb("wst%d" % i, [128, 2048], F32) for i in range(3)])
    tog = [0]

    def load_mat(src_flat, off):
        wt_, wb_ = wring.next()
        for q in range(4):
            st, stb = wst.next()
            o = off + q * 128 * 2048
            fw.dma("sp", lambda e, st=st, o=o: e.dma_start(out=st[:], in_=src_flat[o:o + 128 * 2048].rearrange("(p n) -> p n", p=128)), writes=[stb])
            k = tog[0] % 2
            tog[0] += 1
            dst = wt_[:, q * 2048:(q + 1) * 2048]
            if k == 0:
                fw.op("dve", lambda e, st=st, dst=dst: e.tensor_copy(out=dst, in_=st[:]), reads=[stb], writes=[wb_], partial=True)
            elif k == 1:
                fw.op("act", lambda e, st=st, dst=dst: e.activation(out=dst, in_=st[:], func=AF.Identity), reads=[stb], writes=[wb_], partial=True)
            else:
                fw.op("pool", lambda e, st=st, dst=dst: e.tensor_copy(out=dst, in_=st[:]), reads=[stb], writes=[wb_], partial=True)
        return wt_, wb_

    def gu_view(wt_):
        return wt_[:].rearrange("p (q k n) -> p q k n", q=4, k=KC)

    def dn_view(wt_):
        return wt_[:].rearrange("p (q k n) -> p q k n", q=4, k=4)

    pg = Ring([ps("pg%d" % i, [128, 512], F32) for i in range(2)])
    pu = Ring([ps("pu%d" % i, [128, 512], F32) for i in range(2)])
    py = Ring([ps("py%d" % i, [128, 512], F32) for i in range(2)])
    pT = Ring([ps("pTe%d" % i, [128, 1024], BF16) for i in range(1)])
    pslot = Ring([ps("pslot%d" % i, [128, 512], F32) for i in range(1)])
    sg_r = Ring([sb("sg%d" % i, [128, 512], F32) for i in range(2)])

    def ffn_hidden(wg, wgb, wu, wub, rhs_of, rhsb, ncol, hT, hTb):
        gv, uv = gu_view(wg), gu_view(wu)
        first = True
        for fc in range(4):
            for c0 in range(0, ncol, 512):
                cn = min(512, ncol - c0)
                pgt, pgb = pg.next()
                put, pub = pu.next()
                for (pt, ptb, v, vb) in ((pgt, pgb, gv, wgb), (put, pub, uv, wub)):
                    for kc in range(KC):
                        fw.op("pe", lambda e_, pt=pt, v=v, kc=kc, fc=fc, c0=c0, cn=cn: e_.matmul(pt[:, 0:cn], lhsT=v[:, fc, kc, :], rhs=rhs_of(kc, c0, cn),
                              start=(kc == 0), stop=(kc == KC - 1)), reads=[vb, rhsb], writes=[ptb])
                sg, sgb = sg_r.next()
                fw.op("act", lambda e_, sg=sg, pgt=pgt, cn=cn: e_.activation(out=sg[:, 0:cn], in_=pgt[:, 0:cn], func=AF.Silu), reads=[pgb], writes=[sgb])
                fw.op("dve", lambda e_, sg=sg, put=put, fc=fc, c0=c0, cn=cn: e_.tensor_tensor(out=hT[:, fc, c0:c0 + cn], in0=put[:, 0:cn], in1=sg[:, 0:cn], op=ALU.mult),
                      reads=[pub, sgb], writes=[hTb], partial=not first)
                first = False

    stack.append(ExitStack())
    h2g, b_h2g = sb("h2g", [128, KC, 512], BF16), Buf()
    hTs, b_hTs = sb("hTs", [128, 4, 512], BF16), Buf()
    ysh = Ring([sb("ysh%d" % i, [128, D], F32) for i in range(2)])
    wg, wgb = load_mat(cx.w_eg_p, 64 * D * 512)
    wu, wub = load_mat(cx.w_eu_p, 64 * D * 512)
    wd, wdb = load_mat(cx.w_ed_p, 64 * 512 * D)
    dv = dn_view(wd)

    def shared_group(grp):
        fw.dma("sp", lambda e: e.dma_start(out=h2g[:], in_=cx.h2T_d.rearrange("p (k t) -> p k t", k=KC)[:, :, grp * 512:(grp + 1) * 512]),
               reads=[cx.b_h2Td], writes=[b_h2g])
        ffn_hidden(wg, wgb, wu, wub, lambda kc, c0, cn: h2g[:, kc, c0:c0 + cn], b_h2g, 512, hTs, b_hTs)
        for ti in range(4):
            yt, ytb = ysh.next()
            for n in range(4):
                pyt, pyb = py.next()
                for fc in range(4):
                    fw.op("pe", lambda e_, pyt=pyt, fc=fc, ti=ti, n=n: e_.matmul(pyt[:, :], lhsT=hTs[:, fc, ti * 128:(ti + 1) * 128], rhs=dv[:, n, fc, :],
                          start=(fc == 0), stop=(fc == 3)), reads=[b_hTs, wdb], writes=[pyb])
                fw.op("dve", lambda e_, pyt=pyt, yt=yt, n=n: e_.tensor_copy(out=yt[:, n * 512:(n + 1) * 512], in_=pyt[:, :]), reads=[pyb], writes=[ytb], partial=(n > 0))
            i = grp * 4 + ti
            fw.dma("sp", lambda e, yt=yt, i=i: e.dma_start(out=cx.y_d[i * 128:(i + 1) * 128, :], in_=yt[:]), reads=[ytb], writes=[b_yd], partial=True)
    for grp in range(4):
        shared_group(grp)
    fw.barrier()
    fw.flush()
    stack.pop().close()

    stack.append(ExitStack())
    ltri, b_ltri = sb("ltri_s", [128, 128], BF16), Buf()
    ones_b, b_onesb = sb("ones_bb", [128, 128], BF16), Buf()
    iota_c, b_iota = sb("iota_s", [128, CAP], F32), Buf()
    TW, b_TW = sb("TW", [128, 16, 64, 5], BF16), Buf()
    tcs, b_tcs = sb("tconst_s", [128, 16, 3], BF16), Buf()
    posm, b_posm = sb("posm", [128, 16, 64], F32), Buf()
    selm, b_selm = sb("selm", [128, 16, 64], BF16), Buf()
    carry, b_carry = sb("carry", [128, 64], F32), Buf()
    wtmp, b_wtmp = posm, b_posm
    fw.dma("sp", lambda e: e.dma_start(out=ltri[:], in_=cx.ltri[:, :]), writes=[b_ltri])
    fw.dma("sp", lambda e: e.dma_start(out=iota_c[:], in_=cx.iota_c[:, :]), writes=[b_iota])
    fw.dma("sp", lambda e: e.dma_start(out=tcs[:], in_=cx.tconst[:, :, :]), writes=[b_tcs])
    fw.op("pool", lambda e: e.memset(ones_b[:], 1.0), writes=[b_onesb])
    fw.op("pool", lambda e: e.memset(carry[:], 0.0), writes=[b_carry])
    fw.op("dve", lambda e: e.tensor_scalar(out=selm[:], in0=Wr[:, :, 0:64], scalar1=0.0, scalar2=None, op0=ALU.is_gt), reads=[b_Wr], writes=[b_selm])
    for k, src_k in ((0, 0), (1, 1), (4, 2)):
        fw.op("dve", lambda e, k=k, src_k=src_k: e.tensor_copy(out=TW[:, :, :, k], in_=tcs[:, :, src_k:src_k + 1].to_broadcast([128, 16, 64])),
              reads=[b_tcs], writes=[b_TW], partial=True)
    fw.op("dve", lambda e: e.tensor_copy(out=TW[:, :, :, 2], in_=Wr[:, :, 0:64]), reads=[b_Wr], writes=[b_TW], partial=True)
    fw.op("dve", lambda e: e.tensor_tensor(out=wtmp[:], in0=Wr[:, :, 0:64], in1=TW[:, :, :, 2], op=ALU.subtract), reads=[b_Wr, b_TW], writes=[b_wtmp])
    fw.op("dve", lambda e: e.tensor_copy(out=TW[:, :, :, 3], in_=wtmp[:]), reads=[b_wtmp], writes=[b_TW], partial=True)

    def pos_tile(i):
        pp, ppb = pslot.next()
        fw.op("pe", lambda e: e.matmul(pp[:, 0:64], lhsT=ltri[:], rhs=selm[:, i, :], start=True, stop=True), reads=[b_ltri, b_selm], writes=[ppb])
        fw.op("pe", lambda e: e.matmul(pp[:, 64:128], lhsT=ones_b[:], rhs=selm[:, i, :], start=True, stop=True), reads=[b_onesb, b_selm], writes=[ppb], partial=True)
        fw.op("dve", lambda e: e.tensor_tensor(out=posm[:, i, :], in0=pp[:, 0:64], in1=carry[:], op=ALU.add), reads=[ppb, b_carry], writes=[b_posm], partial=True)
        fw.op("dve", lambda e: e.scalar_tensor_tensor(out=posm[:, i, :], in0=posm[:, i, :], scalar=1.0, in1=selm[:, i, :], op0=ALU.add, op1=ALU.mult),
              reads=[b_posm, b_selm], writes=[b_posm], partial=True)
        fw.op("dve", lambda e: e.tensor_scalar(out=posm[:, i, :], in0=posm[:, i, :], scalar1=-1.0, scalar2=None, op0=ALU.add), reads=[b_posm], writes=[b_posm], partial=True)
        fw.op("dve", lambda e: e.tensor_tensor(out=carry[:], in0=carry[:], in1=pp[:, 64:128], op=ALU.add), reads=[ppb, b_carry], writes=[b_carry])
    for i in range(16):
        pos_tile(i)

    oh_r = Ring([sb("oh%d" % i, [128, CAP], BF16) for i in range(2)])
    sl_r = Ring([sb("slot%d" % i, [128, 96], F32) for i in range(2)])
    sli_r = Ring([sb("sloti%d" % i, [128, 8], I32) for i in range(2)])
    xg_r = Ring([sb("xg%d" % i, [128, D], BF16) for i in range(NCJ)])
    xgT_r = Ring([sb("xgT%d" % i, [128, KC, CAP], BF16) for i in range(1)])
    hT_r = Ring([sb("hTe%d" % i, [128, 4, CAP], BF16) for i in range(1)])
    ye_r = Ring([sb("ye%d" % i, [128, D], F32) for i in range(2)])

    class St:
        pass

    def prep(e):
        s = St()
        pp, ppb = pslot.next()
        ppv = pp[:, 0:NCJ * 8].rearrange("p (j c) -> p j c", c=8)
        ohs = []
        for i in range(16):
            oh, ohb = oh_r.next()
            fw.op("dve", lambda e_, oh=oh, i=i: e_.tensor_scalar(out=oh[:], in0=iota_c[:], scalar1=posm[:, i, e:e + 1], scalar2=None, op0=ALU.is_equal),
                  reads=[b_iota, b_posm], writes=[ohb])
            for cj in range(NCJ):
                fw.op("pe", lambda e_, oh=oh, i=i, cj=cj: e_.matmul(ppv[:, cj, 0:5], lhsT=oh[:, cj * 128:(cj + 1) * 128], rhs=TW[:, i, e, :],
                      start=(i == 0 and cj == 0), stop=(i == 15), skip_group_check=True), reads=[ohb, b_TW], writes=[ppb], partial=not (i == 0 and cj == 0))
        sl, slb = sl_r.next()
        sli, slib = sli_r.next()
        fw.op("dve", lambda e_: e_.tensor_copy(out=sl[:, 32:32 + NCJ * 8], in_=pp[:, 0:NCJ * 8]), reads=[ppb], writes=[slb])
        rv = sl[:, 32:32 + NCJ * 8].rearrange("p (j c) -> p j c", c=8)
        fw.op("dve", lambda e_: e_.scalar_tensor_tensor(out=sl[:, 0:NCJ], in0=rv[:, :, 0], scalar=16.0, in1=rv[:, :, 1], op0=ALU.mult, op1=ALU.add), reads=[slb], writes=[slb])
        fw.op("dve", lambda e_: e_.tensor_scalar(out=sl[:, 16:16 + NCJ], in0=rv[:, :, 4], scalar1=-BIG, scalar2=BIG, op0=ALU.mult, op1=ALU.add), reads=[slb], writes=[slb])
        fw.op("dve", lambda e_: e_.tensor_tensor(out=sl[:, 0:NCJ], in0=sl[:, 0:NCJ], in1=sl[:, 16:16 + NCJ], op=ALU.add), reads=[slb], writes=[slb])
        fw.op("dve", lambda e_: e_.tensor_tensor(out=sl[:, 8:8 + NCJ], in0=rv[:, :, 2], in1=rv[:, :, 3], op=ALU.add), reads=[slb], writes=[slb])
        fw.op("dve", lambda e_: e_.tensor_copy(out=sli[:, 0:NCJ], in_=sl[:, 0:NCJ]), reads=[slb], writes=[slib])
        s.xgs = []
        for cj in range(NCJ):
            xg, xgb = xg_r.next()

            def _g(e_, xg=xg, cj=cj):
                return e_.indirect_dma_start(out=xg[:, :], out_offset=None, in_=cx.h2_d[:, :],
                                             in_offset=bass.IndirectOffsetOnAxis(ap=sli[:, cj:cj + 1], axis=0), bounds_check=fw.reg(e_, T - 1), oob_is_err=False)
            fw.dma("pool", _g, reads=[slib, cx.b_h2d], writes=[xgb])
            s.xgs.append((xg, xgb))
        s.sl, s.slb, s.sli, s.slib = sl, slb, sli, slib
        return s

    def prep_b(s):
        xgT, xgTb = xgT_r.next()
        for cj in range(NCJ):
            xg, xgb = s.xgs[cj]
            for half in range(2):
                ptt, ptb = pT.next()
                for k8 in range(8):
                    kc = half * 8 + k8
                    fw.op("pe", lambda e_, ptt=ptt, xg=xg, kc=kc, k8=k8: e_.transpose(out=ptt[:, k8 * 128:(k8 + 1) * 128], in_=xg[:, kc * 128:(kc + 1) * 128], identity=ident_b[:]),
                          reads=[xgb, b_idb], writes=[ptb], partial=(k8 > 0))
                fw.op("dve", lambda e_, ptt=ptt, half=half, cj=cj: e_.tensor_copy(out=xgT[:, half * 8:(half + 1) * 8, cj * 128:(cj + 1) * 128],
                      in_=ptt[:, :].rearrange("p (k c) -> p k c", k=8)), reads=[ptb], writes=[xgTb], partial=not (cj == 0 and half == 0))
        s.xgT, s.xgTb = xgT, xgTb

    def compute(e, s):
        wg, wgb = load_mat(cx.w_eg_p, e * D * 512)
        wu, wub = load_mat(cx.w_eu_p, e * D * 512)
        wd, wdb = load_mat(cx.w_ed_p, e * 512 * D)
        dvw = dn_view(wd)
        hT, hTb = hT_r.next()
        ffn_hidden(wg, wgb, wu, wub, lambda kc, c0, cn: s.xgT[:, kc, c0:c0 + cn], s.xgTb, CAP, hT, hTb)
        for cj in range(NCJ):
            ye, yeb = ye_r.next()
            for n in range(4):
                pyt, pyb = py.next()
                for fc in range(4):
                    fw.op("pe", lambda e_, pyt=pyt, fc=fc, cj=cj, n=n: e_.matmul(pyt[:, :], lhsT=hT[:, fc, cj * 128:(cj + 1) * 128], rhs=dvw[:, n, fc, :],
                          start=(fc == 0), stop=(fc == 3)), reads=[hTb, wdb], writes=[pyb])
                fw.op("dve", lambda e_, pyt=pyt, ye=ye, n=n, cj=cj: e_.tensor_scalar(out=ye[:, n * 512:(n + 1) * 512], in0=pyt[:, :], scalar1=s.sl[:, 8 + cj:9 + cj], scalar2=None, op0=ALU.mult),
                      reads=[pyb, s.slb], writes=[yeb], partial=(n > 0))
            fw.dma("pool", lambda e_, ye=ye, cj=cj: e_.indirect_dma_start(out=cx.y_d[:, :], out_offset=bass.IndirectOffsetOnAxis(ap=s.sli[:, cj:cj + 1], axis=0),
                   in_=ye[:, :], in_offset=None, bounds_check=fw.reg(e_, T - 1), oob_is_err=False, compute_op=ALU.add), reads=[yeb, s.slib, b_yd], writes=[b_yd])

    nxt = prep(0)
    for e in range(n_routed):
        cur = nxt
        prep_b(cur)
        if e + 1 < n_routed:
            nxt = prep(e + 1)
        compute(e, cur)
    fw.barrier()
    fw.flush()
    stack.pop().close()

    stack.append(ExitStack())
    yin = Ring([sb("yin%d" % i, [128, D], F32) for i in range(2)])
    xt_r = Ring([sb("xf%d" % i, [128, D], F32) for i in range(2)])
    st_r = Ring([sb("stf%d" % i, [128, 8], F32) for i in range(2)])
    cx.junk_f, cx.b_junk_f = sb("junk_f2", [128, D], BF16), Buf()
    b_out = Buf()
    cx.b_x1d = b_out

    def fin(i):
        yt, ytb = yin.next()
        fw.dma("sp", lambda e: e.dma_start(out=yt[:], in_=cx.y_d[i * 128:(i + 1) * 128, :]), reads=[b_yd], writes=[ytb])
        post_norm_residual(fw, cx, i, yt, ytb, xt_r, st_r, cx.x1_d, Grow[1], b_Grow[1], cx.out)
    for i in range(16):
        fin(i)
    cx.b_out = b_out
    fw.finish([cx.b_out])
    stack.pop().close()


from contextlib import ExitStack
from concourse.bass_utils import run_bass_kernel_spmd


def build_program(nc):
    cx = Ctx()
    declare_io_e(nc, cx); declare_io_a(nc, cx); declare_io_b(nc, cx); declare_io_c(nc, cx); declare_io_d(nc, cx)
    fw = FW(nc)
    with ExitStack() as gs:
        cx.Wr = gs.enter_context(nc.sbuf_tensor("Wr", [128, 16, NE], F32))
        cx.b_Wr, cx.b_h2Td, cx.b_x1d = Buf(), Buf(), Buf()
        stages = (lambda es: stage_abc(nc, fw, cx, es, gs=gs), lambda es: stage_nsa(nc, fw, cx, es), lambda es: stage_mlstm(nc, fw, cx, es),
                  lambda es: stage_merge(nc, fw, cx, es), lambda es: stage_router(nc, fw, cx, es), lambda es: stage_moe_sparse(nc, fw, cx, es))
        for fn in stages:
            with ExitStack() as es:
                fn(es)
                fw.barrier()
                fw.flush()
    return cx, fw


def shared_inputs(inp):
    P = {k: np.asarray(v)[0] for k, v in inp.items() if k not in ("x", "c")}
    col = lambda v: np.ascontiguousarray(v.reshape(16, 128).T)
    m = {}
    m["w_ada_p"] = pack_cols(P["w_ada"], [(c, 512) for c in range(0, 12288, 512)])[0]
    m["b_ada"] = P["b_ada"].reshape(1, -1)
    m["g4"] = np.concatenate([col(P["g_pre_mix"]), col(P["g_pre_ffn"]), np.zeros((128, 32), np.float32)], axis=1)
    m["gpost"] = np.stack([P["g_post_mix"], P["g_post_ffn"], P["g_pre_ffn"]])
    m["w_in_p"] = pack_cols(P["w_in"], win_chunks())[0]
    m["ident"] = np.eye(128, dtype=np.float32)
    m.update(nsa_consts())
    m["cmp_w1"] = P["cmp_w1"]
    m["cmp_w2"] = P["cmp_w2"]
    m["peT"] = np.ascontiguousarray(P["cmp_pe"].transpose(0, 2, 1))
    m.update(mlstm_consts())
    cw = np.concatenate([P["conv_w"], P["conv_b"][None, :]], 0)
    m["convp"] = np.ascontiguousarray(cw.reshape(5, 8, 128).transpose(2, 1, 0))
    m["bgate"] = np.ascontiguousarray(P["b_gates_m"].reshape(2, 4).T)
    m["mhg"] = P["mh_norm_g"].reshape(1, -1)
    ch = [(c, 512) for c in range(0, 2048, 512)]
    m["w_up_p"] = np.concatenate([pack_cols(P["w_up_nsa"], ch)[0], pack_cols(P["w_up_mlstm"], ch)[0]])
    m["w_out_p"] = pack_cols(P["w_out"], [(c, 128) for c in range(0, 2048, 128)])[0]
    m["w_router"] = P["w_router"]
    m["b_router"] = P["b_router"].reshape(1, 64)
    m.update(moe_consts())
    m["w_eg_p"] = np.concatenate([pack_gu4(P["w_e_gate"][e]) for e in range(64)] + [pack_gu4(P["w_sh_gate"])])
    m["w_eu_p"] = np.concatenate([pack_gu4(P["w_e_up"][e]) for e in range(64)] + [pack_gu4(P["w_sh_up"])])
    m["w_ed_p"] = np.concatenate([pack_dn4(P["w_e_down"][e]) for e in range(64)] + [pack_dn4(P["w_sh_down"])])
    return m


def kernel(**inputs):
    inp = {k: np.asarray(v) for k, v in inputs.items()}
    nc = bass.Bass("TRN2", target_bir_lowering=False)
    build_program(nc)
    sh = shared_inputs(inp)
    in_maps = []
    for b in range(8):
        m = dict(sh)
        m["x"] = np.ascontiguousarray(inp["x"][b])
        m["cT"] = np.ascontiguousarray(inp["c"][b].reshape(16, 128).T)
        in_maps.append(m)
    res = run_bass_kernel_spmd(nc, in_maps, core_ids=list(range(8)))
    return np.stack([np.asarray(r["out"]) for r in res.results], axis=0).astype(np.float32)
```

```python
import numpy as np
import concourse.bass as bass
import concourse.mybir as mybir

F32 = mybir.dt.float32
BF16 = mybir.dt.bfloat16
U32 = mybir.dt.uint32
I32 = mybir.dt.int32
AF = mybir.ActivationFunctionType
ALU = mybir.AluOpType
AX = mybir.AxisListType

ENGS = ("pe", "act", "dve", "pool", "sp")


class Buf:
    __slots__ = ("name", "w", "r")

    def __init__(self, name=""):
        self.name = name
        self.w = {}
        self.r = {}


class FW:
    def __init__(self, nc, dma_ring=8):
        self.nc = nc
        self.prog = {e: [] for e in ENGS}
        self.sem = {e: nc.alloc_semaphore("c_" + e) for e in ENGS}
        self.cnt = {e: 0 for e in ENGS}
        self.known = {e: {} for e in ENGS}
        self.R = dma_ring
        self.dsem = {q: [nc.alloc_semaphore("d_%s%d" % (q, i)) for i in range(dma_ring)] for q in ("sp", "pool", "act")}
        self.dn = {q: 0 for q in ("sp", "pool", "act")}

    def _need(self, e, tickets):
        need = {}
        for (s, v) in tickets:
            if v > need.get(s, 0):
                need[s] = v
        out = []
        kn = self.known[e]
        own = self.sem[e] if e == "pe" else None
        for s, v in need.items():
            if s is own:
                continue
            if kn.get(s, 0) < v:
                kn[s] = v
                out.append((s, v))
        return out

    def _deps(self, reads, writes, partial=False):
        t = []
        for b in reads:
            t.extend(b.w.items())
        for b in writes:
            if not partial:
                t.extend(b.w.items())
            t.extend(b.r.items())
        return t

    def _commit(self, ticket, reads, writes, partial=False):
        s, v = ticket
        for b in reads:
            if b.r.get(s, 0) < v:
                b.r[s] = v
        for b in writes:
            if partial:
                if b.w.get(s, 0) < v:
                    b.w[s] = v
            else:
                b.w = {s: v}
                b.r = {}

    @staticmethod
    def _compact(ts):
        m = {}
        so = {}
        for (s, v) in ts:
            k = id(s)
            so[k] = s
            if v > m.get(k, 0):
                m[k] = v
        return [(so[k], v) for k, v in m.items()]

    def op(self, e, fn, reads=(), writes=(), partial=False):
        deps = self._deps(reads, writes, partial)
        waits = self._need(e, deps)
        self.cnt[e] += 1
        tk = (self.sem[e], self.cnt[e])
        sem = self.sem[e]

        def emit(eng, waits=waits, fn=fn, sem=sem):
            for (s, v) in waits:
                eng.wait_ge(s, v)
            fn(eng).then_inc(sem, 1)
        self.prog[e].append(emit)
        self._commit(tk, reads, writes, partial)
        return tk

    def barrier(self):
        ts = [(self.sem[e], self.cnt[e]) for e in ENGS if self.cnt[e] > 0]
        for q in self.dsem:
            n = self.dn[q]
            for i, s in enumerate(self.dsem[q]):
                k = (n - 1 - i) // self.R + 1 if n > i else 0
                if k > 0:
                    ts.append((s, 16 * k))
        for e in ENGS:
            waits = self._need(e, ts)

            def emit(eng, waits=waits):
                for (s, v) in waits:
                    eng.wait_ge(s, v)
            self.prog[e].append(emit)

    def dma(self, q, fn, reads=(), writes=(), partial=False):
        n = self.dn[q]
        self.dn[q] += 1
        s = self.dsem[q][n % self.R]
        prev = 16 * (n // self.R)
        deps = self._deps(reads, writes, partial)
        if prev > 0:
            deps = deps + [(s, prev)]
        waits = self._need(q, deps)
        tk = (s, prev + 16)

        def emit(eng, waits=waits, fn=fn, s=s):
            for (ss, v) in waits:
                eng.wait_ge(ss, v)
            fn(eng).then_inc(s, 16)
        self.prog[q].append(emit)
        self._commit(tk, reads, writes, partial)
        return tk

    def flush(self):
        nc = self.nc
        prog = self.prog
        self.prog = {e: [] for e in ENGS}
        self._regs = {}
        if not any(prog[e] for e in ENGS):
            return
        with nc.Block() as block:
            @block.tensor
            def _(e):
                for f in prog["pe"]:
                    f(e)

            @block.scalar
            def _(e):
                for f in prog["act"]:
                    f(e)

            @block.vector
            def _(e):
                for f in prog["dve"]:
                    f(e)

            @block.gpsimd
            def _(e):
                for f in prog["pool"]:
                    f(e)

            @block.sync
            def _(e):
                for f in prog["sp"]:
                    f(e)

    def reg(self, eng, value):
        k = (id(eng), value)
        if k not in self._regs:
            self._regs[k] = eng.to_reg(value)
        return self._regs[k]

    def finish(self, final_bufs):
        deps = []
        for b in final_bufs:
            deps.extend(b.w.items())
        waits = self._need("sp", deps)

        def emit(eng, waits=waits):
            for (s, v) in waits:
                eng.wait_ge(s, v)
        self.prog["sp"].append(emit)
        self.flush()


T = 2048
D = 2048
KC = 16
D_IN = 9784
C_Q, C_KV, C_GA, C_QK, C_VM, C_IF, C_OM, C_GTA, C_GTB = 0, 1024, 2560, 2608, 3632, 4656, 4664, 5688, 7736
EPS = 1e-6


class Ctx:
    pass


def win_chunks():
    groups = [(C_Q, 1024), (C_KV, 1536), (C_KV + 768, 256), (C_KV + 1280, 256), (C_GA, 48), (C_QK, 1024), (C_VM, 1024),
              (C_IF, 8), (C_OM, 1024), (C_GTA, 2048), (C_GTB, 2048)]
    out = []
    for c0, n in groups:
        for cc in range(0, n, 512):
            out.append((c0 + cc, min(512, n - cc)))
    return out


def pack_cols(w, chunks):
    import numpy as np
    K = w.shape[0] // 128
    parts, offs, off = [], {}, 0
    for (c0, n) in chunks:
        offs[(c0, n)] = off
        for h0 in range(0, n, 256):
            nn = min(256, n - h0)
            blk = w[:, c0 + h0:c0 + h0 + nn].reshape(K, 128, nn).transpose(1, 0, 2)
            parts.append(np.ascontiguousarray(blk).reshape(-1))
            off += 128 * K * nn
    return np.concatenate(parts), offs, off


def declare_io_a(nc, cx, dbg=False):
    cx.dbg = dbg
    ch = win_chunks()
    off = 0
    cx.win_off = {}
    for (c0, n) in ch:
        cx.win_off[(c0, n)] = off
        off += 128 * KC * n
    cx.win_total = off
    def din(name, shape, dt=F32):
        t = nc.dram_tensor(name, list(shape), dt, kind="ExternalInput").ap()
        setattr(cx, name, t)
        return t

    def dscr(name, shape, dt):
        t = nc.dram_tensor(name, list(shape), dt, kind="ExternalOutput" if dbg else "Internal").ap()
        setattr(cx, name, t)
        return t
    din("x", [T, D])
    din("cT", [128, KC])
    din("w_ada_p", [D * 6 * D])
    din("b_ada", [1, 6 * D])
    din("g4", [128, 4 * KC])
    din("gpost", [3, D])
    din("w_in_p", [cx.win_total])
    din("ident", [128, 128])
    if dbg:
        dscr("dbg_modc", [128, 4 * KC], F32)
        dscr("dbg_G", [128, D], F32)
    dscr("qT_d", [1024, T], BF16)
    dscr("kvT_d", [1536, T], BF16)
    dscr("qkT_d", [1024, T], F32)
    dscr("ifT_d", [8, T], F32)
    dscr("gaT_d", [D, T], BF16)
    dscr("gbT_d", [D, T], BF16)
    dscr("vs_d", [T, 256], BF16)
    dscr("vw_d", [T, 256], BF16)
    dscr("ga_d", [T, 48], F32)
    dscr("vm_d", [T, 1024], BF16)
    dscr("om_d", [T, 1024], BF16)
    dscr("if_d", [T, 8], F32)


class Ring:
    def __init__(self, tiles):
        self.tiles = tiles
        self.bufs = [Buf() for _ in tiles]
        self.i = 0

    def next(self):
        k = self.i % len(self.tiles)
        self.i += 1
        return self.tiles[k], self.bufs[k]


def stage_abc(nc, fw, cx, es, stop=None, gs=None):
    def sb(name, shape, dt):
        return es.enter_context(nc.sbuf_tensor(name, list(shape), dt))

    def ps(name, shape, dt):
        return es.enter_context(nc.psum_tensor(name, list(shape), dt))

    def gsb(name, shape, dt):
        return (gs or es).enter_context(nc.sbuf_tensor(name, list(shape), dt))

    ident_f = gsb("ident_f", [128, 128], F32)
    ident_b = gsb("ident_b", [128, 128], BF16)
    modc = gsb("modc", [128, 4 * KC], F32)
    Grow = [gsb("Grow%d" % i, [128, D], F32) for i in range(2)]
    cT = sb("cT_s", [128, KC], F32)
    sc = sb("sc_s", [128, KC], F32)
    S_b = sb("S_b", [128, KC, 128], BF16)
    g4 = sb("g4_s", [128, 4 * KC], F32)
    modraw = sb("modraw", [128, 4 * KC], F32)
    b_Grow = [Buf(), Buf()]
    b_id, b_idb, b_cT, b_sc, b_Sb, b_g4, b_modraw, b_modc = (Buf() for _ in range(8))
    fw.dma("sp", lambda e: e.dma_start(out=ident_f[:], in_=cx.ident[:, :]), writes=[b_id])
    fw.dma("sp", lambda e: e.dma_start(out=cT[:], in_=cx.cT[:, :]), writes=[b_cT])
    fw.dma("sp", lambda e: e.dma_start(out=g4[:], in_=cx.g4[:, :]), writes=[b_g4])
    fw.op("dve", lambda e: e.tensor_copy(out=ident_b[:], in_=ident_f[:]), reads=[b_id], writes=[b_idb])
    fw.op("act", lambda e: e.activation(out=sc[:], in_=cT[:], func=AF.Silu), reads=[b_cT], writes=[b_sc])
    for kc in range(KC):
        fw.op("dve", lambda e, kc=kc: e.tensor_copy(out=S_b[:, kc, :], in_=sc[:, kc:kc + 1].to_broadcast([128, 128])),
              reads=[b_sc], writes=[b_Sb], partial=True)

    wb = Ring([sb("wb%d" % i, [128, KC, 512], BF16) for i in range(4)])
    pacc = Ring([ps("pacc%d" % i, [128, 512], F32) for i in range(4)])
    ptr = Ring([ps("ptr%d" % i, [128, 1024], BF16) for i in range(2)])
    bada_r = Ring([sb("bada%d" % i, [128, 512], F32) for i in range(2)])
    mrow_r = Ring([sb("mrow%d" % i, [128, 512], F32) for i in range(2)])
    arow_r = Ring([sb("arow%d" % i, [1, 512], F32) for i in range(2)])
    cx.b_rowAB = Buf()
    junkf = sb("junkf", [128, 128], F32)
    b_junkf = Buf()
    cast_tog = [0]

    def load_w(src_flat, off, ncols):
        wbt, wbb = wb.next()
        for h0 in range(0, ncols, 256):
            n = min(256, ncols - h0)
            o = off + h0 * 128 * KC
            fw.dma("pool", lambda e, wbt=wbt, o=o, n=n, h0=h0: e.dma_start(
                out=wbt[:, :, h0:h0 + n], in_=src_flat[o:o + 128 * KC * n].rearrange("(p k n) -> p k n", p=128, k=KC)), writes=[wbb], partial=True)
        return wbt, wbb

    for ch in range(24):
        mi, q = ch // 4, ch % 4
        pt, pb = pacc.next()
        bt, btb = bada_r.next()
        fw.dma("sp", lambda e, bt=bt, ch=ch: e.dma_start(out=bt[:], in_=cx.b_ada[0, ch * 512:(ch + 1) * 512].partition_broadcast(128)), writes=[btb])
        wbt, wbb = load_w(cx.w_ada_p, ch * 512 * 128 * KC, 512)
        for kc in range(KC):
            fw.op("pe", lambda e, pt=pt, wbt=wbt, kc=kc: e.matmul(pt[:, :], lhsT=S_b[:, kc, :], rhs=wbt[:, kc, :],
                  start=(kc == 0), stop=(kc == KC - 1)), reads=[b_Sb, wbb], writes=[pb])
        mr, mrb = mrow_r.next()
        fw.op("dve", lambda e, pt=pt, mr=mr, bt=bt: e.tensor_tensor(out=mr[:], in0=pt[:, :], in1=bt[:], op=ALU.add),
              reads=[pb, btb], writes=[mrb])
        if mi in (2, 5):
            gi = 0 if mi == 2 else 1
            gt, gtb = bada_r.next()
            fw.dma("sp", lambda e, gt=gt, gi=gi, q=q: e.dma_start(out=gt[:], in_=cx.gpost[gi, q * 512:(q + 1) * 512].partition_broadcast(128)), writes=[gtb])
            fw.op("pool", lambda e, mr=mr, gt=gt, gi=gi, q=q: e.tensor_tensor(out=Grow[gi][:, q * 512:(q + 1) * 512], in0=mr[:], in1=gt[:], op=ALU.mult),
                  reads=[mrb, gtb], writes=[b_Grow[gi]], partial=True)
        else:
            vi = {1: 0, 0: 1, 4: 2, 3: 3}[mi]
            if mi == 3 and hasattr(cx, "rowAB_d"):
                fw.dma("sp", lambda e, mr=mr, q=q: e.dma_start(out=cx.rowAB_d[1:2, q * 512:(q + 1) * 512], in_=mr[0:1, :]), reads=[mrb], writes=[cx.b_rowAB], partial=True)
            if mi == 4 and hasattr(cx, "rowAB_d"):
                gt, gtb = bada_r.next()
                fw.dma("sp", lambda e, gt=gt, q=q: e.dma_start(out=gt[:], in_=cx.gpost[2, q * 512:(q + 1) * 512].partition_broadcast(128)), writes=[gtb])
                ar, arb = arow_r.next()
                fw.op("dve", lambda e, mr=mr, gt=gt, ar=ar: e.scalar_tensor_tensor(out=ar[:], in0=mr[0:1, :], scalar=1.0, in1=gt[0:1, :], op0=ALU.add, op1=ALU.mult),
                      reads=[mrb, gtb], writes=[arb])
                fw.dma("sp", lambda e, ar=ar, q=q: e.dma_start(out=cx.rowAB_d[0:1, q * 512:(q + 1) * 512], in_=ar[:]), reads=[arb], writes=[cx.b_rowAB], partial=True)
            for jj in range(4):
                j = q * 4 + jj
                fw.op("dve", lambda e, mr=mr, jj=jj, vi=vi, j=j: e.scalar_tensor_tensor(
                    out=junkf[:], in0=mr[:, jj * 128:(jj + 1) * 128], scalar=1.0, in1=ident_f[:],
                    op0=ALU.mult, op1=ALU.mult, accum_out=modraw[:, vi * KC + j:vi * KC + j + 1]),
                    reads=[mrb, b_id], writes=[b_junkf, b_modraw])
    for a_i, g_i in ((0, 0), (2, 1)):
        fw.op("dve", lambda e, a_i=a_i, g_i=g_i: e.scalar_tensor_tensor(
            out=modc[:, a_i * KC:(a_i + 1) * KC], in0=modraw[:, a_i * KC:(a_i + 1) * KC], scalar=1.0,
            in1=g4[:, g_i * KC:(g_i + 1) * KC], op0=ALU.add, op1=ALU.mult), reads=[b_modraw, b_g4], writes=[b_modc], partial=True)
        fw.op("dve", lambda e, a_i=a_i: e.tensor_copy(
            out=modc[:, (a_i + 1) * KC:(a_i + 2) * KC], in_=modraw[:, (a_i + 1) * KC:(a_i + 2) * KC]),
            reads=[b_modraw], writes=[b_modc], partial=True)
    cx.modc, cx.b_modc, cx.Grow, cx.b_Grow = modc, b_modc, Grow, b_Grow
    cx.ident_b, cx.b_idb, cx.ident_f, cx.b_id = ident_b, b_idb, ident_f, b_id
    if cx.dbg:
        fw.dma("sp", lambda e: e.dma_start(out=cx.dbg_modc[:, :], in_=modc[:]), reads=[b_modc], writes=[Buf()])
        fw.dma("sp", lambda e: e.dma_start(out=cx.dbg_G[:, :], in_=Grow[0][:]), reads=[b_Grow[0]], writes=[Buf()])
    if stop == 'A':
        return
    hT = sb("hT", [128, KC, T], BF16)
    b_hT = [Buf() for _ in range(16)]
    xt_r = Ring([sb("xt%d" % i, [128, D], F32) for i in range(2)])
    xn_r = Ring([sb("xn%d" % i, [128, D], BF16) for i in range(2)])
    junk = sb("junk", [128, D], BF16)
    b_junk = Buf()
    st_r = Ring([sb("stat%d" % i, [128, 4], F32) for i in range(2)])
    norm_pre(nc, fw, cx, cx.x, hT, b_hT, xt_r, xn_r, junk, b_junk, st_r, ptr, 0)
    cx.hT, cx.b_hT = hT, b_hT

    if stop == 'B':
        return
    stg_b = Ring([sb("stgb%d" % i, [128, 512], BF16) for i in range(4)])
    stg_f = Ring([sb("stgf%d" % i, [128, 512], F32) for i in range(2)])
    def evac(pt, pb, rows, ncols, func, dt, dst_ap):
        st, stb = (stg_b if dt == BF16 else stg_f).next()
        fw.op("act", lambda e: e.activation(out=st[0:rows, 0:ncols], in_=pt[0:rows, 0:ncols], func=func),
              reads=[pb], writes=[stb])
        fw.dma("sp", lambda e: e.dma_start(out=dst_ap, in_=st[0:rows, 0:ncols]), reads=[stb], writes=[Buf()])

    def proj_F(c0, ncols, func, dt, dst):
        for cc in range(0, ncols, 512):
            ncc = min(512, ncols - cc)
            wbt, wbb = load_w(cx.w_in_p, cx.win_off[(c0 + cc, ncc)], ncc)
            for m0 in range(0, ncc, 128):
                m = min(128, ncc - m0)
                for tch in range(4):
                    pt, pb = pacc.next()
                    for kc in range(KC):
                        fw.op("pe", lambda e, pt=pt, wbt=wbt, kc=kc, m0=m0, m=m, tch=tch: e.matmul(
                            pt[0:m, :], lhsT=wbt[:, kc, m0:m0 + m], rhs=hT[:, kc, tch * 512:(tch + 1) * 512],
                            start=(kc == 0), stop=(kc == KC - 1)), reads=[wbb] + b_hT[tch * 4:tch * 4 + 4], writes=[pb])
                    evac(pt, pb, m, 512, func, dt, dst[cc + m0:cc + m0 + m, tch * 512:(tch + 1) * 512])

    def proj_T(c0, ncols, func, dt, dst):
        for cc in range(0, ncols, 512):
            ncc = min(512, ncols - cc)
            wbt, wbb = load_w(cx.w_in_p, cx.win_off[(c0 + cc, ncc)], ncc)
            for ti in range(16):
                pt, pb = pacc.next()
                for kc in range(KC):
                    fw.op("pe", lambda e, pt=pt, wbt=wbt, kc=kc, ti=ti, ncc=ncc: e.matmul(
                        pt[:, 0:ncc], lhsT=hT[:, kc, ti * 128:(ti + 1) * 128], rhs=wbt[:, kc, 0:ncc],
                        start=(kc == 0), stop=(kc == KC - 1)), reads=[wbb, b_hT[ti]], writes=[pb])
                evac(pt, pb, 128, ncc, func, dt, dst[ti * 128:(ti + 1) * 128, cc:cc + ncc])

    ID = AF.Identity
    proj_F(C_Q, 1024, ID, BF16, cx.qT_d)
    proj_F(C_KV, 1536, ID, BF16, cx.kvT_d)
    proj_T(C_KV + 3 * 256, 256, ID, BF16, cx.vs_d)
    proj_T(C_KV + 5 * 256, 256, ID, BF16, cx.vw_d)
    proj_T(C_GA, 48, AF.Sigmoid, F32, cx.ga_d)
    proj_F(C_QK, 1024, ID, F32, cx.qkT_d)
    proj_T(C_VM, 1024, ID, BF16, cx.vm_d)
    proj_F(C_IF, 8, ID, F32, cx.ifT_d)
    proj_T(C_IF, 8, ID, F32, cx.if_d)
    proj_T(C_OM, 1024, AF.Sigmoid, BF16, cx.om_d)
    proj_F(C_GTA, 2048, AF.Sigmoid, BF16, cx.gaT_d)
    proj_F(C_GTB, 2048, AF.Sigmoid, BF16, cx.gbT_d)


def norm_pre(nc, fw, cx, x_ap, hT, b_hT, xt_r, xn_r, junk, b_junk, st_r, ptr, which, ntiles=16, x_dep=None, tok_hook=None):
    modc, b_modc = cx.modc, cx.b_modc
    a_off = which * 2 * KC
    for i in range(ntiles):
        xt, xtb = xt_r.next()
        fw.dma("sp", lambda e, xt=xt, i=i: e.dma_start(out=xt[:], in_=x_ap[i * 128:(i + 1) * 128, :]), writes=[xtb])
        st, stb = st_r.next()
        fw.op("dve", lambda e, xt=xt, st=st: e.scalar_tensor_tensor(out=junk[:], in0=xt[:], scalar=1.0 / D, in1=xt[:],
              op0=ALU.mult, op1=ALU.mult, accum_out=st[:, 0:1]), reads=[xtb], writes=[b_junk, stb])
        fw.op("dve", lambda e, st=st: e.tensor_scalar(out=st[:, 1:2], in0=st[:, 0:1], scalar1=EPS, scalar2=None, op0=ALU.add),
              reads=[stb], writes=[stb])
        fw.op("act", lambda e, st=st: e.activation(out=st[:, 3:4], in_=st[:, 1:2], func=AF.Sqrt), reads=[stb], writes=[stb])
        fw.op("dve", lambda e, st=st: e.reciprocal(out=st[:, 2:3], in_=st[:, 3:4]), reads=[stb], writes=[stb])
        xn, xnb = xn_r.next()
        fw.op("dve", lambda e, xn=xn, xt=xt, st=st: e.tensor_scalar(out=xn[:], in0=xt[:], scalar1=st[:, 2:3], scalar2=None, op0=ALU.mult),
              reads=[xtb, stb], writes=[xnb])
        if tok_hook is not None:
            tok_hook(i, xn, xnb)
        LV = 4
        if LV <= 2:
            fw.op("dve", lambda e, xn=xn, i=i: e.tensor_copy(out=hT[:, :, i * 128:(i + 1) * 128], in_=xn[:].rearrange("p (k n) -> p k n", k=16)), reads=[xnb], writes=[b_hT[i]])
            continue
        for half in range(2):
            pt, pb = ptr.next()
            for k8 in range(8):
                kc = half * 8 + k8
                fw.op("pe", lambda e, pt=pt, xn=xn, kc=kc, k8=k8: e.transpose(
                    out=pt[:, k8 * 128:(k8 + 1) * 128], in_=xn[:, kc * 128:(kc + 1) * 128], identity=cx.ident_b[:]),
                    reads=[xnb, cx.b_idb], writes=[pb])
            for k8 in range(8):
                kc = half * 8 + k8
                if LV == 3:
                    fw.op("dve", lambda e, pt=pt, kc=kc, k8=k8, i=i: e.tensor_copy(out=hT[:, kc, i * 128:(i + 1) * 128], in_=pt[:, k8 * 128:(k8 + 1) * 128]), reads=[pb], writes=[b_hT[i]], partial=True)
                elif k8 % 2 == 0 or LV == 4:
                    fw.op("dve", lambda e, pt=pt, kc=kc, k8=k8, i=i: e.tensor_scalar(
                        out=hT[:, kc, i * 128:(i + 1) * 128], in0=pt[:, k8 * 128:(k8 + 1) * 128],
                        scalar1=modc[:, a_off + kc:a_off + kc + 1], scalar2=modc[:, a_off + KC + kc:a_off + KC + kc + 1],
                        op0=ALU.mult, op1=ALU.add), reads=[pb, b_modc], writes=[b_hT[i]], partial=True)
                else:
                    fw.op("act", lambda e, pt=pt, kc=kc, k8=k8, i=i: e.activation(
                        out=hT[:, kc, i * 128:(i + 1) * 128], in_=pt[:, k8 * 128:(k8 + 1) * 128], func=AF.Identity,
                        scale=modc[:, a_off + kc:a_off + kc + 1], bias=modc[:, a_off + KC + kc:a_off + KC + kc + 1]),
                        reads=[pb, b_modc], writes=[b_hT[i]], partial=True)


SCALE = 0.125


def nsa_consts():
    import numpy as np
    import ml_dtypes
    bf = ml_dtypes.bfloat16
    t = np.arange(T)
    n = np.arange(127)
    cmaskT = ((16 * n[:, None] + 31) <= t[None, :]).astype(bf)
    ci = n[:, None]
    sj = np.arange(32)[None, :]
    ov = np.minimum(ci * 16 + 32, (sj + 1) * 64) - np.maximum(ci * 16, sj * 64)
    c2s = (np.clip(ov, 0, None).astype(np.float32) / 32).astype(bf)
    cur = (t // 64)[:, None]
    forced = (sj == 0) | (sj == cur) | (sj == cur - 1)
    addc = np.where(sj <= cur, np.where(forced, 1e3, 0.0), -1e9).astype(np.float32)
    addc = np.ascontiguousarray(addc.reshape(16, 128, 32).transpose(1, 0, 2))
    Xall = (np.arange(T)[None, :] // 64 == np.arange(32)[:, None]).astype(bf)
    a = np.arange(128)
    tri = (a[:, None] <= a[None, :]).astype(bf)
    tri2 = (a[:, None] > a[None, :]).astype(bf)
    return {"cmaskT": cmaskT, "c2s": c2s, "addc": addc, "Xall": Xall, "tri": tri, "tri2": tri2}


def declare_io_b(nc, cx, dbg=False):
    def din(name, shape, dt=F32):
        setattr(cx, name, nc.dram_tensor(name, list(shape), dt, kind="ExternalInput").ap())

    def dscr(name, shape, dt):
        setattr(cx, name, nc.dram_tensor(name, list(shape), dt, kind="ExternalOutput" if dbg else "Internal").ap())
    din("cmaskT", [127, T], BF16)
    din("c2s", [127, 32], BF16)
    din("addc", [128, 16, 32], F32)
    din("Xall", [32, T], BF16)
    din("tri", [128, 128], BF16)
    din("tri2", [128, 128], BF16)
    din("cmp_w1", [2, 2048, 256])
    din("cmp_w2", [2, 256, 64])
    din("peT", [2, 64, 32])
    dscr("onT_d", [1024, T], BF16)
    if dbg:
        dscr("dbg_kcc", [64, 4, 127], BF16)
        dscr("dbg_vcc", [127, 4, 97], BF16)
        dscr("dbg_sel", [T, 32], BF16)
        dscr("dbg_sm", [T, 256], F32)


def stage_nsa(nc, fw, cx, es):
    def sb(name, shape, dt):
        return es.enter_context(nc.sbuf_tensor(name, list(shape), dt))

    def ps(name, shape, dt):
        return es.enter_context(nc.psum_tensor(name, list(shape), dt))

    def load(name, shape, dt, src, q="sp", n_split=1):
        t = sb(name, shape, dt)
        b = Buf()
        if n_split == 1:
            fw.dma(q, lambda e: e.dma_start(out=t[:], in_=src), writes=[b])
        return t, b

    ident_b, b_idb = cx.ident_b, cx.b_idb
    cmaskT, b_cm = load("cmaskT_s", [127, T], BF16, cx.cmaskT[:, :])
    addc, b_addc = load("addc_s", [128, 16, 32], F32, cx.addc[:, :, :])
    Xall, b_X = load("Xall_s", [32, T], BF16, cx.Xall[:, :])
    tri, b_tri = load("tri_s", [128, 128], BF16, cx.tri[:, :])
    tri2, b_tri2 = load("tri2_s", [128, 128], BF16, cx.tri2[:, :])
    kT = {}
    for nm, r0 in (("ks", 512), ("kw", 1024)):
        kT[nm] = load("kT_" + nm, [64, 4, T], BF16, cx.kvT_d[r0:r0 + 256, :].rearrange("(g d) t -> d g t", d=64))
    v1 = {}
    for nm, src in (("vs", cx.vs_d), ("vw", cx.vw_d)):
        t_ = sb("v1_" + nm, [128, 16, 4, 65], BF16)
        b_ = Buf()
        fw.op("pool", lambda e, t_=t_: e.memset(t_[:], 1.0), writes=[b_])
        for i in range(16):
            fw.dma("sp", lambda e, t_=t_, i=i, src=src: e.dma_start(
                out=t_[:, i, :, 0:64], in_=src[i * 128:(i + 1) * 128, :].rearrange("p (g d) -> p g d", g=4)),
                reads=[b_], writes=[b_], partial=True)
        v1[nm] = (t_, b_)
    ga, b_ga = sb("ga_s", [128, 16, 48], F32), Buf()
    for i4 in range(4):
        fw.dma("sp", lambda e, i4=i4: e.dma_start(
            out=ga[:, i4 * 4:(i4 + 1) * 4, :], in_=cx.ga_d[i4 * 512:(i4 + 1) * 512, :].rearrange("(i p) c -> p i c", p=128)),
            writes=[b_ga], partial=True)

    pS = Ring([ps("pS%d" % i, [128, 512], F32) for i in range(2)])
    pM = Ring([ps("pM%d" % i, [128, 512], F32) for i in range(1)])
    pc_t, b_pc = ps("pc", [128, 512], F32)[:, 0:388].rearrange("p (h c) -> p h c", h=4), Buf()
    pss_t, b_pss = ps("pss", [128, 512], F32)[:, 0:260].rearrange("p (h c) -> p h c", h=4), Buf()
    psw_t, b_psw = ps("psw", [128, 512], F32)[:, 0:260].rearrange("p (h c) -> p h c", h=4), Buf()
    pT = Ring([ps("pT%d" % i, [128, 1024], BF16) for i in range(2)])

    kccT, b_kcc = sb("kccT", [64, 4, 127], BF16), Buf()
    vcc, b_vcc = sb("vcc", [127, 4, 97], BF16), Buf()
    from contextlib import ExitStack
    es_outer = es
    es = ExitStack()
    es.__enter__()
    w1f, b_w1f = sb("w1f", [64, 16, 256], F32), Buf()
    w1b, b_w1b = sb("w1b", [64, 32, 256], BF16), Buf()
    w2f, b_w2f = sb("w2f", [128, 2, 64], F32), Buf()
    w2b, b_w2b = sb("w2b", [128, 2, 64], BF16), Buf()
    pef, b_pef = sb("pef", [64, 32], F32), Buf()
    peb, b_peb = sb("peb", [64, 32], BF16), Buf()
    pbias, b_pbias = sb("pbias", [128, 2], F32), Buf()
    hidT, b_hid = sb("hidT", [128, 2, 508], BF16), Buf()
    gz = [sb("gz%d" % i, [128, 508], F32) for i in range(3)]
    b_gz = [Buf() for _ in range(3)]
    fw.op("pool", lambda e: e.memset(vcc[:], 1.0), writes=[b_vcc])
    for g in range(4):
        fw.dma("sp", lambda e, g=g: e.dma_start(out=vcc[:, g, 65:97], in_=cx.c2s[:, :]), reads=[b_vcc], writes=[b_vcc], partial=True)
    xT, b_xT = sb("xT_c", [64, 4, T], BF16), Buf()
    for ci, nm in ((0, "kc"), (1, "vc")):
        fw.dma("sp", lambda e, ci=ci: e.dma_start(out=xT[:], in_=cx.kvT_d[ci * 256:(ci + 1) * 256, :].rearrange("(g d) t -> d g t", d=64)), writes=[b_xT])
        for lh in range(2):
            fw.dma("sp", lambda e, ci=ci, lh=lh: e.dma_start(out=w1f[:], in_=cx.cmp_w1[ci, lh * 1024:(lh + 1) * 1024, :].rearrange("(l d) c -> d l c", d=64)), writes=[b_w1f])
            fw.op("pool", lambda e, lh=lh: e.tensor_copy(out=w1b[:, lh * 16:(lh + 1) * 16, :], in_=w1f[:]), reads=[b_w1f], writes=[b_w1b], partial=(lh == 1))
        fw.dma("sp", lambda e, ci=ci: e.dma_start(out=w2f[:], in_=cx.cmp_w2[ci].rearrange("(cc p) d -> p cc d", p=128)), writes=[b_w2f])
        fw.dma("sp", lambda e, ci=ci: e.dma_start(out=pef[:], in_=cx.peT[ci]), writes=[b_pef])
        fw.op("dve", lambda e: e.tensor_copy(out=w2b[:], in_=w2f[:]), reads=[b_w2f], writes=[b_w2b])
        fw.op("dve", lambda e: e.tensor_copy(out=peb[:], in_=pef[:]), reads=[b_pef], writes=[b_peb])
        for cc in range(2):
            pm, pmb = pM.next()
            for l in range(32):
                fw.op("pe", lambda e, pm=pm, l=l, cc=cc: e.matmul(pm[:, 0:1], lhsT=w1b[:, l, cc * 128:(cc + 1) * 128], rhs=peb[:, l:l + 1],
                      start=(l == 0), stop=(l == 31)), reads=[b_w1b, b_peb], writes=[pmb])
            fw.op("dve", lambda e, pm=pm, cc=cc: e.tensor_copy(out=pbias[:, cc:cc + 1], in_=pm[:, 0:1]), reads=[pmb], writes=[b_pbias])
            ph, phb = pS.next()
            for l in range(32):
                fw.op("pe", lambda e, ph=ph, l=l, cc=cc, xT=xT: e.matmul(
                    ph[:, 0:508].rearrange("p (g n) -> p g n", g=4), lhsT=w1b[:, l, cc * 128:(cc + 1) * 128],
                    rhs=xT[:, :, l:l + 2017:16], start=(l == 0), stop=(l == 31)), reads=[b_w1b, b_xT], writes=[phb])
            fw.op("dve", lambda e, ph=ph, cc=cc: e.tensor_scalar(out=gz[0][:], in0=ph[:, 0:508], scalar1=pbias[:, cc:cc + 1], scalar2=None, op0=ALU.add),
                  reads=[phb, b_pbias], writes=[b_gz[0]])
            fw.op("dve", lambda e: e.tensor_tensor(out=gz[1][:], in0=gz[0][:], in1=gz[0][:], op=ALU.mult), reads=[b_gz[0]], writes=[b_gz[1]])
            fw.op("dve", lambda e: e.tensor_scalar(out=gz[1][:], in0=gz[1][:], scalar1=0.044715, scalar2=1.0, op0=ALU.mult, op1=ALU.add),
                  reads=[b_gz[1]], writes=[b_gz[1]])
            fw.op("dve", lambda e: e.tensor_tensor(out=gz[1][:], in0=gz[1][:], in1=gz[0][:], op=ALU.mult), reads=[b_gz[0], b_gz[1]], writes=[b_gz[1]])
            fw.op("act", lambda e: e.activation(out=gz[2][:], in_=gz[1][:], func=AF.Tanh, scale=0.7978845608), reads=[b_gz[1]], writes=[b_gz[2]])
            fw.op("dve", lambda e: e.scalar_tensor_tensor(out=gz[2][:], in0=gz[2][:], scalar=1.0, in1=gz[0][:], op0=ALU.add, op1=ALU.mult),
                  reads=[b_gz[2], b_gz[0]], writes=[b_gz[2]])
            fw.op("dve", lambda e, cc=cc: e.tensor_scalar(out=hidT[:, cc, :], in0=gz[2][:], scalar1=0.5, scalar2=None, op0=ALU.mult),
                  reads=[b_gz[2]], writes=[b_hid], partial=(cc == 1))
        if ci == 0:
            pm, pmb = pM.next()
            for cc in range(2):
                fw.op("pe", lambda e, pm=pm, cc=cc: e.matmul(pm[0:64, 0:508], lhsT=w2b[:, cc, :], rhs=hidT[:, cc, :], start=(cc == 0), stop=(cc == 1)),
                      reads=[b_w2b, b_hid], writes=[pmb])
            fw.op("dve", lambda e, pm=pm: e.tensor_copy(out=kccT[:].rearrange("p g n -> p (g n)"), in_=pm[0:64, 0:508]), reads=[pmb], writes=[b_kcc])
        else:
            for g in range(4):
                pm, pmb = pM.next()
                for cc in range(2):
                    fw.op("pe", lambda e, pm=pm, cc=cc, g=g: e.matmul(pm[0:127, 0:64], lhsT=hidT[:, cc, g * 127:(g + 1) * 127], rhs=w2b[:, cc, :],
                          start=(cc == 0), stop=(cc == 1)), reads=[b_w2b, b_hid], writes=[pmb])
                fw.op("dve", lambda e, pm=pm, g=g: e.tensor_copy(out=vcc[:, g, 0:64], in_=pm[0:127, 0:64]), reads=[pmb, b_vcc], writes=[b_vcc], partial=True)
    if cx.dbg:
        fw.dma("sp", lambda e: e.dma_start(out=cx.dbg_kcc[:, :, :], in_=kccT[:]), reads=[b_kcc], writes=[Buf()])
        fw.dma("sp", lambda e: e.dma_start(out=cx.dbg_vcc[:, :, :], in_=vcc[:]), reads=[b_vcc], writes=[Buf()])

    fw.barrier()
    fw.flush()
    es.__exit__(None, None, None)
    es = es_outer
    qT, b_qT = sb("qT_s", [64, 16, T], BF16), Buf()
    for h4 in range(4):
        fw.dma("sp", lambda e, h4=h4: e.dma_start(
            out=qT[:, h4 * 4:(h4 + 1) * 4, :], in_=cx.qT_d[h4 * 256:(h4 + 1) * 256, :].rearrange("(h d) t -> d h t", d=64)),
            writes=[b_qT], partial=True)
    E_r = Ring([sb("E%d" % i, [128, 4, 128], BF16) for i in range(3)])
    Em_r = Ring([sb("Em%d" % i, [128, 4, 128], BF16) for i in range(3)])
    Mb_r = Ring([sb("Mb%d" % i, [128, 128], BF16) for i in range(2)])
    sm = Ring([sb("sm%d" % i, [128, 256], F32) for i in range(2)])
    selb_r = Ring([sb("selb%d" % i, [128, 32], BF16) for i in range(2)])
    selT_r = Ring([sb("selT%d" % i, [32, 128], BF16) for i in range(2)])
    o_r = Ring([sb("onsa%d" % i, [128, 1024], BF16) for i in range(2)])
    oT_r = Ring([sb("onsaT%d" % i, [128, 8, 128], BF16) for i in range(2)])
    acc_r = Ring([sb("oacc%d" % i, [128, 64], F32) for i in range(2)])
    kccT_, vs1, vw1 = kccT, v1["vs"], v1["vw"]
    ksT, b_ks = kT["ks"]
    kwT, b_kw = kT["kw"]

    def attn_branch(i, g, kts, kTt, b_k, v1t, pacc, b_pacc, mask_for):
        first = True
        for kt in kts:
            pst, psb = pS.next()
            fw.op("pe", lambda e, pst=pst, kt=kt: e.matmul(pst[:, :].rearrange("p (h t) -> p h t", h=4), lhsT=kTt[:, g, kt * 128:(kt + 1) * 128],
                  rhs=qT[:, 4 * g:4 * g + 4, i * 128:(i + 1) * 128], start=True, stop=True), reads=[b_k, b_qT], writes=[psb])
            Et, Eb = E_r.next()
            fw.op("act", lambda e, pst=pst, Et=Et: e.activation(out=Et[:].rearrange("p h t -> p (h t)"), in_=pst[:, :], func=AF.Exp, scale=SCALE),
                  reads=[psb], writes=[Eb])
            mk = mask_for(kt)
            if mk is None:
                lt, lb = Et, Eb
            else:
                mt, mb = mk
                lt, lb = Em_r.next()
                fw.op("dve", lambda e, lt=lt, Et=Et, mt=mt: e.tensor_tensor(out=lt[:], in0=Et[:], in1=mt[:].unsqueeze(1).to_broadcast([128, 4, 128]), op=ALU.mult),
                      reads=[Eb, mb], writes=[lb])
            for h in range(4):
                fw.op("pe", lambda e, lt=lt, h=h, kt=kt, first=first: e.matmul(pacc[:, h, :], lhsT=lt[:, h, :], rhs=v1t[0][:, kt, g, :],
                      start=(first and h == 0), stop=False, skip_group_check=True), reads=[lb, v1t[1]], writes=[b_pacc], partial=not (first and h == 0))
            first = False

    def do_group(i, g, ot, ob):
        pst, psb = pS.next()
        fw.op("pe", lambda e, pst=pst: e.matmul(pst[0:127, :].rearrange("p (h t) -> p h t", h=4), lhsT=kccT_[:, g, :],
              rhs=qT[:, 4 * g:4 * g + 4, i * 128:(i + 1) * 128], start=True, stop=True), reads=[b_kcc, b_qT], writes=[psb])
        Et, Eb = E_r.next()
        fw.op("act", lambda e, pst=pst, Et=Et: e.activation(out=Et[0:127].rearrange("p h t -> p (h t)"), in_=pst[0:127, :], func=AF.Exp, scale=SCALE),
              reads=[psb], writes=[Eb])
        Emt, Emb = Em_r.next()
        fw.op("dve", lambda e, Emt=Emt, Et=Et: e.tensor_tensor(out=Emt[0:127], in0=Et[0:127],
              in1=cmaskT[:, i * 128:(i + 1) * 128].unsqueeze(1).to_broadcast([127, 4, 128]), op=ALU.mult), reads=[Eb, b_cm], writes=[Emb])
        for h in range(4):
            fw.op("pe", lambda e, Emt=Emt, h=h: e.matmul(pc_t[:, h, :], lhsT=Emt[0:127, h, :], rhs=vcc[:, g, :], start=True, stop=True,
                  skip_group_check=True), reads=[Emb, b_vcc], writes=[b_pc], partial=(h > 0))
        s_, sb_ = sm.next()
        fw.op("dve", lambda e, s_=s_: e.tensor_scalar(out=s_[:, 0:4], in0=pc_t[:, :, 64], scalar1=1e-30, scalar2=None, op0=ALU.max), reads=[b_pc], writes=[sb_])
        fw.op("dve", lambda e, s_=s_: e.reciprocal(out=s_[:, 0:4], in_=s_[:, 0:4]), reads=[sb_], writes=[sb_])
        fw.op("dve", lambda e, s_=s_: e.scalar_tensor_tensor(out=s_[:, 32:64], in0=pc_t[:, 0, 65:97], scalar=s_[:, 0:1], in1=addc[:, i, :], op0=ALU.mult, op1=ALU.add),
              reads=[b_pc, sb_, b_addc], writes=[sb_])
        for h in range(1, 4):
            fw.op("dve", lambda e, s_=s_, h=h: e.scalar_tensor_tensor(out=s_[:, 32:64], in0=pc_t[:, h, 65:97], scalar=s_[:, h:h + 1], in1=s_[:, 32:64], op0=ALU.mult, op1=ALU.add),
                  reads=[b_pc, sb_], writes=[sb_])
        fw.op("dve", lambda e, s_=s_: e.max(out=s_[:, 96:104], in_=s_[:, 32:64]), reads=[sb_], writes=[sb_])
        fw.op("dve", lambda e, s_=s_: e.match_replace(out=s_[:, 64:96], in_to_replace=s_[:, 96:104], in_values=s_[:, 32:64], imm_value=-3.0e38), reads=[sb_], writes=[sb_])
        fw.op("dve", lambda e, s_=s_: e.max(out=s_[:, 104:112], in_=s_[:, 64:96]), reads=[sb_], writes=[sb_])
        fw.op("dve", lambda e, s_=s_: e.tensor_scalar(out=s_[:, 112:113], in0=s_[:, 111:112], scalar1=-1.0e8, scalar2=None, op0=ALU.max), reads=[sb_], writes=[sb_])
        selb, selbb = selb_r.next()
        fw.op("dve", lambda e, s_=s_, selb=selb: e.tensor_scalar(out=selb[:], in0=s_[:, 32:64], scalar1=s_[:, 112:113], scalar2=None, op0=ALU.is_ge), reads=[sb_], writes=[selbb])
        ptt, ptb = pT.next()
        fw.op("pe", lambda e, ptt=ptt, selb=selb: e.transpose(out=ptt[0:32, 0:128], in_=selb[:], identity=ident_b[:]), reads=[selbb, b_idb], writes=[ptb])
        selT, selTb = selT_r.next()
        fw.op("dve", lambda e, ptt=ptt, selT=selT: e.tensor_copy(out=selT[:], in_=ptt[0:32, 0:128]), reads=[ptb], writes=[selTb])
        if cx.dbg and g == 0:
            fw.dma("sp", lambda e, selb=selb: e.dma_start(out=cx.dbg_sel[i * 128:(i + 1) * 128, :], in_=selb[:]), reads=[selbb], writes=[Buf()])
            fw.dma("sp", lambda e, s_=s_: e.dma_start(out=cx.dbg_sm[i * 128:(i + 1) * 128, :], in_=s_[:]), reads=[sb_], writes=[Buf()])

        def sel_mask(kt, selT=selT, selTb=selTb):
            pm, pmb = pM.next()
            fw.op("pe", lambda e, pm=pm: e.matmul(pm[:, 0:128], lhsT=Xall[:, kt * 128:(kt + 1) * 128], rhs=selT[:], start=True, stop=True),
                  reads=[b_X, selTb], writes=[pmb])
            mt, mb = Mb_r.next()
            if kt == i:
                fw.op("dve", lambda e, pm=pm, mt=mt: e.tensor_tensor(out=mt[:], in0=pm[:, 0:128], in1=tri[:], op=ALU.mult), reads=[pmb, b_tri], writes=[mb])
            else:
                fw.op("dve", lambda e, pm=pm, mt=mt: e.tensor_copy(out=mt[:], in_=pm[:, 0:128]), reads=[pmb], writes=[mb])
            return mt, mb
        attn_branch(i, g, list(range(0, i + 1)), ksT, b_ks, vs1, pss_t, b_pss, sel_mask)

        def win_mask(kt):
            if kt == i:
                return tri, b_tri
            if kt == i - 4:
                return tri2, b_tri2
            return None
        attn_branch(i, g, list(range(max(0, i - 4), i + 1)), kwT, b_kw, vw1, psw_t, b_psw, win_mask)

        fw.op("dve", lambda e, s_=s_: e.reciprocal(out=s_[:, 4:8], in_=pss_t[:, :, 64]), reads=[b_pss, sb_], writes=[sb_])
        fw.op("dve", lambda e, s_=s_: e.reciprocal(out=s_[:, 8:12], in_=psw_t[:, :, 64]), reads=[b_psw, sb_], writes=[sb_])
        gav = ga[:, i, g * 12:(g + 1) * 12].rearrange("p (h b) -> p b h", b=3)
        fw.op("dve", lambda e, s_=s_, gav=gav: e.tensor_tensor(out=s_[:, 12:24].rearrange("p (b h) -> p b h", b=3), in0=gav,
              in1=s_[:, 0:12].rearrange("p (b h) -> p b h", b=3), op=ALU.mult), reads=[sb_, b_ga], writes=[sb_])
        for h in range(4):
            at, ab = acc_r.next()
            fw.op("dve", lambda e, s_=s_, at=at, h=h: e.tensor_scalar(out=at[:], in0=pc_t[:, h, 0:64], scalar1=s_[:, 12 + h:13 + h], scalar2=None, op0=ALU.mult),
                  reads=[b_pc, sb_], writes=[ab])
            fw.op("dve", lambda e, s_=s_, at=at, h=h: e.scalar_tensor_tensor(out=at[:], in0=pss_t[:, h, 0:64], scalar=s_[:, 16 + h:17 + h], in1=at[:], op0=ALU.mult, op1=ALU.add),
                  reads=[b_pss, sb_, ab], writes=[ab])
            c0 = (4 * g + h) * 64
            fw.op("dve", lambda e, s_=s_, at=at, h=h, ot=ot, c0=c0: e.scalar_tensor_tensor(out=ot[:, c0:c0 + 64], in0=psw_t[:, h, 0:64], scalar=s_[:, 20 + h:21 + h], in1=at[:], op0=ALU.mult, op1=ALU.add),
                  reads=[b_psw, sb_, ab], writes=[ob], partial=True)

    def finish_tile(i, ot, ob):
        ptt, ptb = pT.next()
        for j in range(8):
            fw.op("pe", lambda e, ptt=ptt, ot=ot, j=j: e.transpose(out=ptt[:, j * 128:(j + 1) * 128], in_=ot[:, j * 128:(j + 1) * 128], identity=ident_b[:]),
                  reads=[ob, b_idb], writes=[ptb], partial=(j > 0))
        oT, oTb = oT_r.next()
        fw.op("dve", lambda e, ptt=ptt, oT=oT: e.tensor_copy(out=oT[:].rearrange("p j t -> p (j t)"), in_=ptt[:, :]), reads=[ptb], writes=[oTb])
        fw.dma("sp", lambda e, oT=oT, i=i: e.dma_start(out=cx.onT_d[:, i * 128:(i + 1) * 128].rearrange("(j p) t -> p j t", p=128), in_=oT[:]),
               reads=[oTb], writes=[Buf()])

    for i in range(16):
        ot, ob = o_r.next()
        for g in range(4):
            do_group(i, g, ot, ob)
        finish_tile(i, ot, ob)


def mlstm_consts():
    import numpy as np
    import ml_dtypes
    a = np.arange(128)
    tri = (a[:, None] <= a[None, :])
    ms = np.zeros((4, 128, 512), np.float32)
    for r in range(4):
        for j in range(4):
            if j == r:
                ms[r][:, j * 128:(j + 1) * 128] = tri
            elif j > r:
                ms[r][:, j * 128:(j + 1) * 128] = 1.0
    return {"mmask": ms.astype(ml_dtypes.bfloat16)}


def declare_io_c(nc, cx, dbg=False):
    def din(name, shape, dt=F32):
        setattr(cx, name, nc.dram_tensor(name, list(shape), dt, kind="ExternalInput").ap())

    def dscr(name, shape, dt, force_out=False):
        setattr(cx, name, nc.dram_tensor(name, list(shape), dt, kind="ExternalOutput" if (dbg or force_out) else "Internal").ap())
    din("mmask", [4, 128, 512], BF16)
    din("convp", [128, 8, 5])
    din("bgate", [4, 2])
    din("mhg", [1, 1024])
    din("w_up_p", [2 * 1024 * D])
    din("w_out_p", [D * D])
    dscr("omT_d", [1024, T], BF16)
    dscr("ac_d", [8, T], F32)
    dscr("x1_d", [T, D], F32)


def stage_mlstm(nc, fw, cx, es):
    def sb(name, shape, dt):
        return es.enter_context(nc.sbuf_tensor(name, list(shape), dt))

    def ps(name, shape, dt):
        return es.enter_context(nc.psum_tensor(name, list(shape), dt))
    ident_b, b_idb, ident_f, b_id = cx.ident_b, cx.b_idb, cx.ident_f, cx.b_id

    mmask, b_mm = sb("mmask_s", [128, 4, 512], BF16), Buf()
    fw.dma("sp", lambda e: e.dma_start(out=mmask[:], in_=cx.mmask.rearrange("r p t -> p r t")), writes=[b_mm])
    convp, b_cp = sb("convp_s", [128, 8, 5], F32), Buf()
    fw.dma("sp", lambda e: e.dma_start(out=convp[:], in_=cx.convp[:, :, :]), writes=[b_cp])
    bg, b_bg = sb("bg_s", [4, 2], F32), Buf()
    fw.dma("sp", lambda e: e.dma_start(out=bg[:], in_=cx.bgate[:, :]), writes=[b_bg])
    mhg, b_mhg = sb("mhg_s", [128, 1024], F32), Buf()
    fw.dma("sp", lambda e: e.dma_start(out=mhg[:], in_=cx.mhg[0, :].partition_broadcast(128)), writes=[b_mhg])
    vm1, b_vm = sb("vm1", [128, 16, 4, 257], BF16), Buf()
    fw.op("pool", lambda e: e.memset(vm1[:], 1.0), writes=[b_vm])
    for i in range(16):
        fw.dma("sp", lambda e, i=i: e.dma_start(out=vm1[:, i, :, 0:256], in_=cx.vm_d[i * 128:(i + 1) * 128, :].rearrange("p (h d) -> p h d", h=4)),
               reads=[b_vm], writes=[b_vm], partial=True)

    qkb, b_qkb = sb("qkb", [128, 8, T], BF16), [Buf() for _ in range(8)]
    xin = Ring([sb("cx%d" % i, [128, T + 3], F32) for i in range(2)])
    yv = Ring([sb("cy%d" % i, [128, T], F32) for i in range(2)])
    for c in range(8):
        xt, xb = xin.next()
        fw.op("pool", lambda e, xt=xt: e.memset(xt[:, 0:3], 0.0), writes=[xb])
        fw.dma("sp", lambda e, xt=xt, c=c: e.dma_start(out=xt[:, 3:T + 3], in_=cx.qkT_d[c * 128:(c + 1) * 128, :]), reads=[xb], writes=[xb], partial=True)
        yt, yb = yv.next()
        fw.op("dve", lambda e, xt=xt, yt=yt, c=c: e.tensor_scalar(out=yt[:], in0=xt[:, 0:T], scalar1=convp[:, c, 0:1], scalar2=convp[:, c, 4:5], op0=ALU.mult, op1=ALU.add),
              reads=[xb, b_cp], writes=[yb])
        for k in range(1, 4):
            fw.op("dve", lambda e, xt=xt, yt=yt, c=c, k=k: e.scalar_tensor_tensor(out=yt[:], in0=xt[:, k:k + T], scalar=convp[:, c, k:k + 1], in1=yt[:], op0=ALU.mult, op1=ALU.add),
                  reads=[xb, b_cp, yb], writes=[yb])
        if c < 4:
            fw.op("act", lambda e, yt=yt, c=c: e.activation(out=qkb[:, c, :], in_=yt[:], func=AF.Silu), reads=[yb], writes=[b_qkb[c]])
        else:
            fw.op("act", lambda e, yt=yt: e.activation(out=yt[:], in_=yt[:], func=AF.Silu), reads=[yb], writes=[yb])
            fw.op("dve", lambda e, yt=yt, c=c: e.tensor_scalar(out=qkb[:, c, :], in0=yt[:], scalar1=128 ** -0.5, scalar2=None, op0=ALU.mult), reads=[yb], writes=[b_qkb[c]])

    gi, b_gi = sb("gi", [4, T], F32), Buf()
    gf, b_gf = sb("gf", [4, T], F32), Buf()
    ga_, b_ga = sb("ga_", [4, T], F32), Buf()
    ones4, b_o4 = sb("ones4", [4, T], F32), Buf()
    fw.dma("sp", lambda e: e.dma_start(out=gi[:], in_=cx.ifT_d[0:4, :]), writes=[b_gi])
    fw.dma("sp", lambda e: e.dma_start(out=gf[:], in_=cx.ifT_d[4:8, :]), writes=[b_gf])
    fw.op("pool", lambda e: e.memset(ones4[:], 1.0), writes=[b_o4])
    fw.op("dve", lambda e: e.tensor_scalar(out=gf[:], in0=gf[:], scalar1=bg[:, 1:2], scalar2=None, op0=ALU.add), reads=[b_gf, b_bg], writes=[b_gf])
    fw.op("act", lambda e: e.activation(out=gf[:], in_=gf[:], func=AF.Exp, scale=-1.0), reads=[b_gf], writes=[b_gf])
    fw.op("dve", lambda e: e.tensor_scalar(out=gf[:], in0=gf[:], scalar1=1.0, scalar2=None, op0=ALU.add), reads=[b_gf], writes=[b_gf])
    fw.op("act", lambda e: e.activation(out=gf[:], in_=gf[:], func=AF.Ln), reads=[b_gf], writes=[b_gf])
    fw.op("dve", lambda e: e.tensor_scalar(out=gf[:], in0=gf[:], scalar1=-1.0, scalar2=None, op0=ALU.mult), reads=[b_gf], writes=[b_gf])
    fw.op("dve", lambda e: e.tensor_tensor_scan(out=ga_[:], data0=ones4[:], data1=gf[:], initial=0.0, op0=ALU.mult, op1=ALU.add),
          reads=[b_gf, b_o4], writes=[b_ga])
    fw.op("dve", lambda e: e.scalar_tensor_tensor(out=gi[:], in0=gi[:], scalar=bg[:, 0:1], in1=ga_[:], op0=ALU.add, op1=ALU.subtract),
          reads=[b_gi, b_bg, b_ga], writes=[b_gi])
    b_acd = Buf()
    fw.dma("sp", lambda e: e.dma_start(out=cx.ac_d[0:4, :], in_=ga_[:]), reads=[b_ga], writes=[b_acd])
    pmisc, b_pmisc = ps("pmisc_m", [128, 512], F32), Buf()
    for i in range(16):
        fw.op("pe", lambda e, i=i: e.transpose(out=pmisc[:, i * 4:(i + 1) * 4], in_=gi[:, i * 128:(i + 1) * 128], identity=ident_f[0:4, 0:4]),
              reads=[b_gi, b_id], writes=[b_pmisc], partial=(i > 0))
    cT, b_cT = sb("cT_m", [128, 16, 4], F32), Buf()
    fw.op("dve", lambda e: e.tensor_copy(out=cT[:].rearrange("p i h -> p (i h)"), in_=pmisc[:, 0:64]), reads=[b_pmisc], writes=[b_cT])

    pS = Ring([ps("pSm%d" % i, [128, 512], F32) for i in range(2)])
    pacc = [ps("paccm%d" % i, [128, 512], F32) for i in range(4)]
    b_pacc = [Buf() for _ in range(4)]
    pT = Ring([ps("pTm%d" % i, [128, 1024], BF16) for i in range(1)])
    Abc_r = Ring([sb("Abc%d" % i, [128, T], F32) for i in range(2)])
    D_r = Ring([sb("Dm%d" % i, [128, 512], F32) for i in range(3)])
    W_r = Ring([sb("Wm%d" % i, [128, 512], BF16) for i in range(3)])
    om_r = Ring([sb("omt%d" % i, [128, 256], BF16) for i in range(2)])
    hc_r = Ring([sb("hc%d" % i, [128, 256], F32) for i in range(2)])
    hj_r = Ring([sb("hj%d" % i, [128, 256], F32) for i in range(2)])
    ho_r = Ring([sb("ho%d" % i, [128, 256], BF16) for i in range(2)])
    hT_r = Ring([sb("hoT%d" % i, [128, 2, 128], BF16) for i in range(2)])
    st_r = Ring([sb("stm%d" % i, [128, 8], F32) for i in range(2)])

    def head_chunk(h, c, Abc, Abcb):
        for kt in range(4 * c + 4):
            pst, psb = pS.next()
            fw.op("pe", lambda e, pst=pst, kt=kt: e.matmul(pst[:, :], lhsT=qkb[:, 4 + h, kt * 128:(kt + 1) * 128], rhs=qkb[:, h, c * 512:(c + 1) * 512],
                  start=True, stop=True), reads=[b_qkb[4 + h], b_qkb[h]], writes=[psb])
            Dt, Db = D_r.next()
            fw.op("act", lambda e, Dt=Dt, kt=kt: e.activation(out=Dt[:], in_=Abc[:, c * 512:(c + 1) * 512], func=AF.Exp, bias=cT[:, kt, h:h + 1]),
                  reads=[Abcb, b_cT], writes=[Db])
            Wt, Wb = W_r.next()
            if kt // 4 == c:
                fw.op("dve", lambda e, Dt=Dt, kt=kt: e.tensor_tensor(out=Dt[:], in0=Dt[:], in1=mmask[:, kt % 4, :], op=ALU.mult), reads=[Db, b_mm], writes=[Db])
            fw.op("dve", lambda e, Dt=Dt, Wt=Wt, pst=pst: e.tensor_tensor(out=Wt[:], in0=pst[:, :], in1=Dt[:], op=ALU.mult), reads=[psb, Db], writes=[Wb])
            for j in range(4):
                tj = 4 * c + j
                if tj < kt:
                    continue
                fw.op("pe", lambda e, Wt=Wt, j=j, kt=kt, tj=tj: e.matmul(pacc[j][:, 0:257], lhsT=Wt[:, j * 128:(j + 1) * 128], rhs=vm1[:, kt, h, :],
                      start=(kt == 0), stop=(kt == tj)), reads=[Wb, b_vm], writes=[b_pacc[j]])
        for j in range(4):
            tj = 4 * c + j
            st, stb = st_r.next()
            fw.op("dve", lambda e, st=st, j=j: e.tensor_scalar(out=st[:, 6:7], in0=pacc[j][:, 256:257], scalar1=-1.0, scalar2=None, op0=ALU.mult),
                  reads=[b_pacc[j]], writes=[stb])
            fw.op("dve", lambda e, st=st, j=j: e.tensor_tensor(out=st[:, 7:8], in0=st[:, 6:7], in1=pacc[j][:, 256:257], op=ALU.max),
                  reads=[b_pacc[j], stb], writes=[stb])
            fw.op("dve", lambda e, st=st, j=j: e.tensor_scalar(out=st[:, 0:1], in0=st[:, 7:8], scalar1=1.0, scalar2=None, op0=ALU.max),
                  reads=[stb], writes=[stb])
            fw.op("dve", lambda e, st=st: e.reciprocal(out=st[:, 1:2], in_=st[:, 0:1]), reads=[stb], writes=[stb])
            hc, hcb = hc_r.next()
            fw.op("dve", lambda e, st=st, hc=hc, j=j: e.tensor_scalar(out=hc[:], in0=pacc[j][:, 0:256], scalar1=st[:, 1:2], scalar2=None, op0=ALU.mult),
                  reads=[b_pacc[j], stb], writes=[hcb])
            hj, hjb = hj_r.next()
            fw.op("dve", lambda e, st=st, hc=hc, hj=hj: e.scalar_tensor_tensor(out=hj[:], in0=hc[:], scalar=1.0 / 256, in1=hc[:], op0=ALU.mult, op1=ALU.mult, accum_out=st[:, 2:3]),
                  reads=[hcb], writes=[hjb, stb])
            fw.op("dve", lambda e, st=st: e.tensor_scalar(out=st[:, 3:4], in0=st[:, 2:3], scalar1=EPS, scalar2=None, op0=ALU.add), reads=[stb], writes=[stb])
            fw.op("act", lambda e, st=st: e.activation(out=st[:, 4:5], in_=st[:, 3:4], func=AF.Sqrt), reads=[stb], writes=[stb])
            fw.op("dve", lambda e, st=st: e.reciprocal(out=st[:, 5:6], in_=st[:, 4:5]), reads=[stb], writes=[stb])
            omt, omb = om_r.next()
            fw.dma("sp", lambda e, omt=omt, tj=tj: e.dma_start(out=omt[:], in_=cx.om_d[tj * 128:(tj + 1) * 128, h * 256:(h + 1) * 256]), writes=[omb])
            fw.op("dve", lambda e, st=st, hc=hc, hj=hj: e.scalar_tensor_tensor(out=hj[:], in0=hc[:], scalar=st[:, 5:6], in1=mhg[:, h * 256:(h + 1) * 256], op0=ALU.mult, op1=ALU.mult),
                  reads=[hcb, stb, b_mhg], writes=[hjb])
            ho, hob = ho_r.next()
            fw.op("dve", lambda e, hj=hj, ho=ho, omt=omt: e.tensor_tensor(out=ho[:], in0=hj[:], in1=omt[:], op=ALU.mult), reads=[hjb, omb], writes=[hob])
            ptt, ptb = pT.next()
            for bk in range(2):
                fw.op("pe", lambda e, ptt=ptt, ho=ho, bk=bk: e.transpose(out=ptt[:, bk * 128:(bk + 1) * 128], in_=ho[:, bk * 128:(bk + 1) * 128], identity=ident_b[:]),
                      reads=[hob, b_idb], writes=[ptb], partial=(bk > 0))
            hT, hTb = hT_r.next()
            fw.op("dve", lambda e, ptt=ptt, hT=hT: e.tensor_copy(out=hT[:].rearrange("p b t -> p (b t)"), in_=ptt[:, 0:256]), reads=[ptb], writes=[hTb])
            fw.dma("sp", lambda e, hT=hT, tj=tj: e.dma_start(out=cx.omT_d[h * 256:(h + 1) * 256, tj * 128:(tj + 1) * 128].rearrange("(b p) t -> p b t", p=128), in_=hT[:]),
                   reads=[hTb], writes=[Buf()])

    for h in range(4):
        Abc, Abcb = Abc_r.next()
        fw.dma("sp", lambda e, Abc=Abc, h=h: e.dma_start(out=Abc[:], in_=cx.ac_d[h, :].partition_broadcast(128)), reads=[b_acd], writes=[Abcb])
        for c in range(4):
            head_chunk(h, c, Abc, Abcb)


def stage_merge(nc, fw, cx, es):
    from contextlib import ExitStack
    stack = [es]

    def sb(name, shape, dt):
        return stack[-1].enter_context(nc.sbuf_tensor(name, list(shape), dt))

    def ps(name, shape, dt):
        return stack[-1].enter_context(nc.psum_tensor(name, list(shape), dt))
    Grow, b_Grow = cx.Grow, cx.b_Grow
    mT = sb("mT", [128, KC, T], BF16)
    b_mT = [Buf() for _ in range(4)]
    pA = Ring([ps("pA%d" % i, [128, 512], F32) for i in range(2)])
    pB = Ring([ps("pB%d" % i, [128, 512], F32) for i in range(2)])
    pY = [ps("pY%d" % i, [128, 512], F32) for i in range(4)]
    b_pY = [Buf() for _ in range(4)]
    cast_tog = [0]

    stack.append(ExitStack())
    oT = [sb("oTa", [128, 8, T], BF16), sb("oTb", [128, 8, T], BF16)]
    b_oT = [Buf(), Buf()]
    for k, src in enumerate((cx.onT_d, cx.omT_d)):
        for j2 in range(2):
            fw.dma("sp", lambda e, k=k, src=src, j2=j2: e.dma_start(out=oT[k][:, j2 * 4:(j2 + 1) * 4, :],
                   in_=src[j2 * 512:(j2 + 1) * 512, :].rearrange("(j p) t -> p j t", p=128)), writes=[b_oT[k]], partial=True)
    wb = Ring([sb("wub%d" % i, [128, 8, 512], BF16) for i in range(4)])
    gt_r = Ring([sb("gtile%d" % i, [128, 512], BF16) for i in range(4)])
    m1_r = Ring([sb("m1_%d" % i, [128, 512], F32) for i in range(2)])

    def load_up(which, cc):
        wbt, wbb = wb.next()
        for h0 in range(2):
            o = which * 1024 * D + (cc * 512 + h0 * 256) * 128 * 8
            fw.dma("pool", lambda e, wbt=wbt, o=o, h0=h0: e.dma_start(out=wbt[:, :, h0 * 256:(h0 + 1) * 256],
                   in_=cx.w_up_p[o:o + 128 * 8 * 256].rearrange("(p k n) -> p k n", p=128, k=8)), writes=[wbb], partial=True)
        return wbt, wbb

    for cc in range(4):
        wa, wab = load_up(0, cc)
        wbm, wbmb = load_up(1, cc)
        for m in range(4):
            fc = cc * 4 + m
            for tc in range(4):
                pa, pab = pA.next()
                pb_, pbb = pB.next()
                for (pt, ptb, w, wbuf, k) in ((pa, pab, wa, wab, 0), (pb_, pbb, wbm, wbmb, 1)):
                    for kc in range(8):
                        fw.op("pe", lambda e, pt=pt, w=w, kc=kc, m=m, tc=tc, k=k: e.matmul(pt[:, :], lhsT=w[:, kc, m * 128:(m + 1) * 128],
                              rhs=oT[k][:, kc, tc * 512:(tc + 1) * 512], start=(kc == 0), stop=(kc == 7)), reads=[wbuf, b_oT[k]], writes=[ptb])
                g1, g1b = gt_r.next()
                g2, g2b = gt_r.next()
                fw.dma("sp", lambda e, g1=g1, fc=fc, tc=tc: e.dma_start(out=g1[:], in_=cx.gaT_d[fc * 128:(fc + 1) * 128, tc * 512:(tc + 1) * 512]), writes=[g1b])
                fw.dma("sp", lambda e, g2=g2, fc=fc, tc=tc: e.dma_start(out=g2[:], in_=cx.gbT_d[fc * 128:(fc + 1) * 128, tc * 512:(tc + 1) * 512]), writes=[g2b])
                m1, m1b = m1_r.next()
                m2, m2b = m1_r.next()
                fw.op("dve", lambda e, m1=m1, pa=pa, g1=g1: e.tensor_tensor(out=m1[:], in0=pa[:, :], in1=g1[:], op=ALU.mult), reads=[pab, g1b], writes=[m1b])
                fw.op("dve", lambda e, m2=m2, pb_=pb_, g2=g2: e.tensor_tensor(out=m2[:], in0=pb_[:, :], in1=g2[:], op=ALU.mult), reads=[pbb, g2b], writes=[m2b])
                fw.op("pool", lambda e, m1=m1, m2=m2, fc=fc, tc=tc: e.tensor_tensor(out=mT[:, fc, tc * 512:(tc + 1) * 512], in0=m1[:], in1=m2[:], op=ALU.add),
                      reads=[m1b, m2b], writes=[b_mT[tc]], partial=True)
    fw.barrier()
    fw.flush()
    stack.pop().close()

    stack.append(ExitStack())
    wo = sb("wo_b", [128, KC, D], BF16)
    b_wo = [Buf() for _ in range(4)]
    cx.junk_f, cx.b_junk_f, cx.b_x1d = sb("junk_f", [128, D], BF16), Buf(), Buf()
    for c16 in range(16):
        o = c16 * 128 * 128 * KC
        fw.dma("pool", lambda e, o=o, c16=c16: e.dma_start(out=wo[:, :, c16 * 128:(c16 + 1) * 128],
               in_=cx.w_out_p[o:o + 128 * KC * 128].rearrange("(p k n) -> p k n", p=128, k=KC)), writes=[b_wo[c16 // 4]], partial=True)
    xt_r = Ring([sb("xm%d" % i, [128, D], F32) for i in range(2)])
    y_r = Ring([sb("ym%d" % i, [128, D], F32) for i in range(2)])
    st_r = Ring([sb("stz%d" % i, [128, 8], F32) for i in range(2)])
    for i in range(16):
        for n in range(4):
            for fc in range(KC):
                fw.op("pe", lambda e, i=i, n=n, fc=fc: e.matmul(pY[n][:, :], lhsT=mT[:, fc, i * 128:(i + 1) * 128], rhs=wo[:, fc, n * 512:(n + 1) * 512],
                      start=(fc == 0), stop=(fc == KC - 1)), reads=[b_mT[i // 4], b_wo[n]], writes=[b_pY[n]])
        yt, yb = y_r.next()
        for n in range(4):
            if n % 2 == 0:
                fw.op("act", lambda e, yt=yt, n=n: e.activation(out=yt[:, n * 512:(n + 1) * 512], in_=pY[n][:, :], func=AF.Identity), reads=[b_pY[n]], writes=[yb], partial=True)
            else:
                fw.op("dve", lambda e, yt=yt, n=n: e.tensor_copy(out=yt[:, n * 512:(n + 1) * 512], in_=pY[n][:, :]), reads=[b_pY[n]], writes=[yb], partial=True)
        post_norm_residual(fw, cx, i, yt, yb, xt_r, st_r, cx.x, Grow[0], b_Grow[0], cx.x1_d)
    fw.barrier()
    fw.flush()
    stack.pop().close()


def post_norm_residual(fw, cx, i, yt, yb, xt_r, st_r, x_src, G, b_G, dst, x_dep=None):
    xt, xb = xt_r.next()
    fw.dma("sp", lambda e: e.dma_start(out=xt[:], in_=x_src[i * 128:(i + 1) * 128, :]), writes=[xb])
    st, stb = st_r.next()
    fw.op("dve", lambda e: e.scalar_tensor_tensor(out=cx.junk_f[:], in0=yt[:], scalar=1.0 / D, in1=yt[:], op0=ALU.mult, op1=ALU.mult, accum_out=st[:, 0:1]),
          reads=[yb], writes=[cx.b_junk_f, stb])
    fw.op("dve", lambda e: e.tensor_scalar(out=st[:, 1:2], in0=st[:, 0:1], scalar1=EPS, scalar2=None, op0=ALU.add), reads=[stb], writes=[stb])
    fw.op("act", lambda e: e.activation(out=st[:, 2:3], in_=st[:, 1:2], func=AF.Sqrt), reads=[stb], writes=[stb])
    fw.op("dve", lambda e: e.reciprocal(out=st[:, 3:4], in_=st[:, 2:3]), reads=[stb], writes=[stb])
    fw.op("dve", lambda e: e.scalar_tensor_tensor(out=yt[:], in0=yt[:], scalar=st[:, 3:4], in1=G[:], op0=ALU.mult, op1=ALU.mult), reads=[yb, stb, b_G], writes=[yb])
    fw.op("pool", lambda e: e.tensor_tensor(out=xt[:], in0=xt[:], in1=yt[:], op=ALU.add), reads=[xb, yb], writes=[xb])
    fw.dma("sp", lambda e: e.dma_start(out=dst[i * 128:(i + 1) * 128, :], in_=xt[:]), reads=[xb], writes=[cx.b_x1d], partial=True)


NE = 65


def pack_gu(w):
    return pack_cols(w, [(0, 512)])[0]


def pack_dn(w):
    import numpy as np
    return np.concatenate([np.ascontiguousarray(w[:, h * 1024:(h + 1) * 1024].reshape(4, 128, 1024).transpose(1, 0, 2)).reshape(-1) for h in range(2)])


def declare_io_d(nc, cx, dbg=False):
    def din(name, shape, dt=F32):
        setattr(cx, name, nc.dram_tensor(name, list(shape), dt, kind="ExternalInput").ap())

    def dscr(name, shape, dt, out=False):
        setattr(cx, name, nc.dram_tensor(name, list(shape), dt, kind="ExternalOutput" if (dbg or out) else "Internal").ap())
    din("w_router", [D, 64])
    din("b_router", [1, 64])
    din("w_eg_p", [NE * D * 512])
    din("w_eu_p", [NE * D * 512])
    din("w_ed_p", [NE * 512 * D])
    dscr("h2T_d", [128, KC * T], BF16)
    dscr("out", [T, D], F32, out=True)
    if dbg:
        dscr("dbg_wr", [128, 16, NE], F32)


def stage_router(nc, fw, cx, es):
    def sb(name, shape, dt):
        return es.enter_context(nc.sbuf_tensor(name, list(shape), dt))

    def ps(name, shape, dt):
        return es.enter_context(nc.psum_tensor(name, list(shape), dt))
    Wr, b_Wr = cx.Wr, cx.b_Wr
    h2T = sb("h2T", [128, KC, T], BF16)
    b_h2T = [Buf() for _ in range(16)]
    xt_r = Ring([sb("xt2_%d" % i, [128, D], F32) for i in range(2)])
    xn_r = Ring([sb("xn2_%d" % i, [128, D], BF16) for i in range(2)])
    junk = sb("junk2", [128, D], BF16)
    st_r = Ring([sb("stat2_%d" % i, [128, 4], F32) for i in range(2)])
    ptr = Ring([ps("ptr2_%d" % i, [128, 1024], BF16) for i in range(2)])
    tok_hook = None
    if hasattr(cx, "h2_d"):
        Arow, b_Ar = sb("Arow", [128, D], F32), Buf()
        Brow, b_Br = sb("Brow", [128, D], F32), Buf()
        fw.dma("sp", lambda e: e.dma_start(out=Arow[:], in_=cx.rowAB_d[0, :].partition_broadcast(128)), reads=[cx.b_rowAB], writes=[b_Ar])
        fw.dma("sp", lambda e: e.dma_start(out=Brow[:], in_=cx.rowAB_d[1, :].partition_broadcast(128)), reads=[cx.b_rowAB], writes=[b_Br])
        t1_r = Ring([sb("h2t1_%d" % i, [128, D], F32) for i in range(1)])
        h2t_r = Ring([sb("h2tok%d" % i, [128, D], BF16) for i in range(2)])
        cx.b_h2d = Buf()

        def tok_hook(i, xn, xnb):
            t1, t1b = t1_r.next()
            ht, htb = h2t_r.next()
            fw.op("pool", lambda e: e.tensor_tensor(out=t1[:], in0=xn[:], in1=Arow[:], op=ALU.mult), reads=[xnb, b_Ar], writes=[t1b])
            fw.op("pool", lambda e: e.tensor_tensor(out=ht[:], in0=t1[:], in1=Brow[:], op=ALU.add), reads=[t1b, b_Br], writes=[htb])
            fw.dma("sp", lambda e: e.dma_start(out=cx.h2_d[i * 128:(i + 1) * 128, :], in_=ht[:]), reads=[htb], writes=[cx.b_h2d], partial=True)
    norm_pre(nc, fw, cx, cx.x1_d, h2T, b_h2T, xt_r, xn_r, junk, Buf(), st_r, ptr, 1, x_dep=[cx.b_x1d], tok_hook=tok_hook)
    for i4 in range(4):
        fw.dma("sp", lambda e, i4=i4: e.dma_start(out=cx.h2T_d.rearrange("p (k t) -> p k t", k=KC)[:, :, i4 * 512:(i4 + 1) * 512], in_=h2T[:, :, i4 * 512:(i4 + 1) * 512]),
               reads=b_h2T[i4 * 4:(i4 + 1) * 4], writes=[cx.b_h2Td], partial=True)
    wrf, b_wrf = sb("wrf", [128, KC, 64], F32), Buf()
    wrb, b_wrb = sb("wrb", [128, KC, 64], BF16), Buf()
    brt, b_brt = sb("brt", [128, 64], F32), Buf()
    fw.dma("sp", lambda e: e.dma_start(out=wrf[:], in_=cx.w_router.rearrange("(k p) n -> p k n", p=128)), writes=[b_wrf])
    fw.dma("sp", lambda e: e.dma_start(out=brt[:], in_=cx.b_router[0, :].partition_broadcast(128)), writes=[b_brt])
    fw.op("dve", lambda e: e.tensor_copy(out=wrb[:], in_=wrf[:]), reads=[b_wrf], writes=[b_wrb])
    fw.op("pool", lambda e: e.memset(Wr[:], 1.0), writes=[b_Wr])
    pl = Ring([ps("plog%d" % i, [128, 512], F32) for i in range(2)])
    r_r = Ring([sb("rt%d" % i, [128, 512], F32) for i in range(2)])

    def route_tile(i):
        pt, pb = pl.next()
        for kc in range(KC):
            fw.op("pe", lambda e, kc=kc: e.matmul(pt[:, 0:64], lhsT=h2T[:, kc, i * 128:(i + 1) * 128], rhs=wrb[:, kc, :], start=(kc == 0), stop=(kc == KC - 1)),
                  reads=[b_h2T[i], b_wrb], writes=[pb])
        r, rb = r_r.next()
        S, SB, M8, GS, M, GM, T1, SBM, M8B, SEL = (r[:, 0:64], r[:, 64:128], r[:, 128:192], r[:, 192:200], r[:, 200:208], r[:, 208:216],
                                                  r[:, 216:224], r[:, 224:288], r[:, 288:296], r[:, 296:360])
        ops = []
        fw.op("act", lambda e: e.activation(out=S, in_=pt[:, 0:64], func=AF.Sigmoid), reads=[pb], writes=[rb])
        dv = lambda f, extra=(): fw.op("dve", f, reads=[rb] + list(extra), writes=[rb])
        dv(lambda e: e.tensor_tensor(out=SB, in0=S, in1=brt[:], op=ALU.add), [b_brt])
        for g in range(8):
            dv(lambda e, g=g: e.max(out=r[:, 128 + g * 8:136 + g * 8], in_=r[:, 64 + g * 8:72 + g * 8]))
        m8v = M8.rearrange("p (g k) -> p g k", k=8)
        dv(lambda e: e.tensor_tensor(out=GS, in0=m8v[:, :, 0], in1=m8v[:, :, 1], op=ALU.add))
        dv(lambda e: e.max(out=M, in_=GS))
        dv(lambda e: e.tensor_scalar(out=GM, in0=GS, scalar1=r[:, 203:204], scalar2=None, op0=ALU.is_ge))
        dv(lambda e: e.tensor_scalar(out=T1, in0=GM, scalar1=1.0e9, scalar2=-1.0e9, op0=ALU.mult, op1=ALU.add))
        dv(lambda e: e.tensor_tensor(out=SBM.rearrange("p (g k) -> p g k", k=8), in0=SB.rearrange("p (g k) -> p g k", k=8),
                                     in1=GM.unsqueeze(2).to_broadcast([128, 8, 8]), op=ALU.mult))
        dv(lambda e: e.tensor_tensor(out=SBM.rearrange("p (g k) -> p g k", k=8), in0=SBM.rearrange("p (g k) -> p g k", k=8),
                                     in1=T1.unsqueeze(2).to_broadcast([128, 8, 8]), op=ALU.add))
        dv(lambda e: e.max(out=M8B, in_=SBM))
        dv(lambda e: e.tensor_scalar(out=SEL, in0=SBM, scalar1=r[:, 295:296], scalar2=None, op0=ALU.is_ge))
        dv(lambda e: e.tensor_tensor(out=SEL, in0=SEL, in1=S, op=ALU.mult))
        dv(lambda e: e.tensor_reduce(out=r[:, 360:361], in_=SEL, axis=AX.X, op=ALU.add))
        dv(lambda e: e.reciprocal(out=r[:, 361:362], in_=r[:, 360:361]))
        fw.op("dve", lambda e: e.tensor_scalar(out=Wr[:, i, 0:64], in0=SEL, scalar1=r[:, 361:362], scalar2=2.5, op0=ALU.mult, op1=ALU.mult),
              reads=[rb, b_Wr], writes=[b_Wr], partial=True)

    for i in range(16):
        route_tile(i)
    if cx.dbg:
        fw.dma("sp", lambda e: e.dma_start(out=cx.dbg_wr[:, :, :], in_=Wr[:]), reads=[b_Wr], writes=[Buf()])


def stage_moe(nc, fw, cx, es, n_exp=NE, groups=(0, 1, 2, 3)):
    def sb(name, shape, dt):
        return es.enter_context(nc.sbuf_tensor(name, list(shape), dt))

    def ps(name, shape, dt):
        return es.enter_context(nc.psum_tensor(name, list(shape), dt))
    Wr, b_Wr = cx.Wr, cx.b_Wr
    Grow, b_Grow = cx.Grow, cx.b_Grow
    yacc = sb("yacc", [128, 4, D], F32)
    b_y = [Buf() for _ in range(4)]
    h2g, b_h2g = sb("h2g", [128, KC, 512], BF16), Buf()
    wgu = Ring([sb("wgu%d" % i, [128, KC, 512], BF16) for i in range(3)])
    wdr = Ring([sb("wdn%d" % i, [128, 4, D], BF16) for i in range(2)])
    wst = Ring([sb("wst%d" % i, [128, 4096], F32) for i in range(2)])
    sg_r = Ring([sb("sg%d" % i, [128, 512], F32) for i in range(2)])
    hT_r = Ring([sb("hTe%d" % i, [128, 4, 512], BF16) for i in range(2)])
    xt_r = Ring([sb("xf%d" % i, [128, D], F32) for i in range(1)])
    st_r = Ring([sb("stf%d" % i, [128, 8], F32) for i in range(2)])
    cx.junk_f, cx.b_junk_f = sb("junk_f2", [128, D], BF16), Buf()
    b_out = Buf()
    cx.b_x1d_save = cx.b_x1d
    pg = Ring([ps("pg%d" % i, [128, 512], F32) for i in range(2)])
    pu = Ring([ps("pu%d" % i, [128, 512], F32) for i in range(2)])
    py = Ring([ps("py%d" % i, [128, 512], F32) for i in range(3)])
    tog = [0]

    def load_mat(src_flat, off, dst, dstb, view):
        for half in range(2):
            wt, wtb = wst.next()
            o = off + half * 128 * 4096
            fw.dma("sp", lambda e, wt=wt, o=o: e.dma_start(out=wt[:], in_=src_flat[o:o + 128 * 4096].rearrange("(p n) -> p n", p=128)), writes=[wtb])
            eng = "pool" if tog[0] % 2 == 0 else "dve"
            tog[0] += 1
            fw.op(eng, lambda e, wt=wt, half=half: e.tensor_copy(out=view(dst, half), in_=view_st(wt, view)), reads=[wtb], writes=[dstb], partial=True)

    def view_st(wt, view):
        return wt[:].rearrange("p (k n) -> p k n", k=KC) if view is v_gu else wt[:].rearrange("p (k n) -> p k n", k=4)

    def v_gu(dst, half):
        return dst[:, :, half * 256:(half + 1) * 256]

    def v_dn(dst, half):
        return dst[:, :, half * 1024:(half + 1) * 1024]

    def expert(grp, e, first):
        wg, wgb = wgu.next()
        load_mat(cx.w_eg_p, e * D * 512, wg, wgb, v_gu)
        wu, wub = wgu.next()
        load_mat(cx.w_eu_p, e * D * 512, wu, wub, v_gu)
        wd, wdb = wdr.next()
        load_mat(cx.w_ed_p, e * 512 * D, wd, wdb, v_dn)
        hT, hTb = hT_r.next()
        for fc in range(4):
            pgt, pgb = pg.next()
            put, pub = pu.next()
            for (pt, ptb, w, wb_) in ((pgt, pgb, wg, wgb), (put, pub, wu, wub)):
                for kc in range(KC):
                    fw.op("pe", lambda e_, pt=pt, w=w, kc=kc, fc=fc: e_.matmul(pt[:, :], lhsT=w[:, kc, fc * 128:(fc + 1) * 128], rhs=h2g[:, kc, :],
                          start=(kc == 0), stop=(kc == KC - 1)), reads=[wb_, b_h2g], writes=[ptb])
            sg, sgb = sg_r.next()
            fw.op("act", lambda e_, sg=sg, pgt=pgt: e_.activation(out=sg[:], in_=pgt[:, :], func=AF.Silu), reads=[pgb], writes=[sgb])
            fw.op("dve", lambda e_, sg=sg, put=put, hT=hT, fc=fc: e_.tensor_tensor(out=hT[:, fc, :], in0=put[:, :], in1=sg[:], op=ALU.mult),
                  reads=[pub, sgb], writes=[hTb], partial=(fc > 0))
        for ti in range(4):
            for n in range(4):
                pyt, pyb = py.next()
                for fc in range(4):
                    fw.op("pe", lambda e_, pyt=pyt, hT=hT, wd=wd, fc=fc, ti=ti, n=n: e_.matmul(pyt[:, :], lhsT=hT[:, fc, ti * 128:(ti + 1) * 128],
                          rhs=wd[:, fc, n * 512:(n + 1) * 512], start=(fc == 0), stop=(fc == 3)), reads=[hTb, wdb], writes=[pyb])
                wcol = Wr[:, grp * 4 + ti, e:e + 1]
                if first:
                    fw.op("dve", lambda e_, pyt=pyt, ti=ti, n=n, wcol=wcol: e_.tensor_scalar(out=yacc[:, ti, n * 512:(n + 1) * 512], in0=pyt[:, :], scalar1=wcol, scalar2=None, op0=ALU.mult),
                          reads=[pyb, b_Wr], writes=[b_y[ti]], partial=True)
                else:
                    fw.op("dve", lambda e_, pyt=pyt, ti=ti, n=n, wcol=wcol: e_.scalar_tensor_tensor(out=yacc[:, ti, n * 512:(n + 1) * 512], in0=pyt[:, :], scalar=wcol,
                          in1=yacc[:, ti, n * 512:(n + 1) * 512], op0=ALU.mult, op1=ALU.add), reads=[pyb, b_Wr, b_y[ti]], writes=[b_y[ti]], partial=True)

    class YT:
        pass
    for grp in groups:
        fw.dma("sp", lambda e, grp=grp: e.dma_start(out=h2g[:], in_=cx.h2T_d.rearrange("p (k t) -> p k t", k=KC)[:, :, grp * 512:(grp + 1) * 512]),
               reads=[cx.b_h2Td], writes=[b_h2g])
        elist = list(range(n_exp)) if n_exp == NE else list(range(n_exp - 1)) + [NE - 1]
        for k, e in enumerate(elist):
            expert(grp, e, k == 0)
        for ti in range(4):
            i = grp * 4 + ti
            cx.b_x1d = b_out
            post_norm_residual(fw, cx, i, yacc[:, ti, :], b_y[ti], xt_r, st_r, cx.x1_d, Grow[1], b_Grow[1], cx.out, x_dep=[cx.b_x1d_save])
    cx.b_out = b_out


CAP = 768
NCJ = CAP // 128
BIG = 1.0e6


def pack_gu4(w):
    import numpy as np
    return np.concatenate([np.ascontiguousarray(w[:, q * 128:(q + 1) * 128].reshape(16, 128, 128).transpose(1, 0, 2)).reshape(-1) for q in range(4)])


def pack_dn4(w):
    import numpy as np
    return np.concatenate([np.ascontiguousarray(w[:, q * 512:(q + 1) * 512].reshape(4, 128, 512).transpose(1, 0, 2)).reshape(-1) for q in range(4)])


def moe_consts():
    import numpy as np
    import ml_dtypes
    bf = ml_dtypes.bfloat16
    a = np.arange(128)
    ltri = (a[:, None] < a[None, :]).astype(bf)
    iota_c = np.tile(np.arange(CAP, dtype=np.float32)[None, :], (128, 1))
    t = (np.arange(16)[None, :] * 128 + a[:, None])
    tconst = np.stack([t // 16, t % 16, np.ones_like(t)], -1).astype(bf)
    return {"ltri": ltri, "iota_c": iota_c, "tconst": tconst}


def declare_io_e(nc, cx, dbg=False):
    def din(name, shape, dt=F32):
        setattr(cx, name, nc.dram_tensor(name, list(shape), dt, kind="ExternalInput").ap())

    def dscr(name, shape, dt, out=False):
        setattr(cx, name, nc.dram_tensor(name, list(shape), dt, kind="ExternalOutput" if (dbg or out) else "Internal").ap())
    din("ltri", [128, 128], BF16)
    din("iota_c", [128, CAP], F32)
    din("tconst", [128, 16, 3], BF16)
    cx.h2_d = nc.dram_tensor("h2_d", [T, D], BF16, kind="Internal").ap()
    dscr("y_d", [T, D], F32)
    dscr("rowAB_d", [2, D], F32)


def stage_moe_sparse(nc, fw, cx, es, n_routed=64):
    from contextlib import ExitStack
    stack = [es]

    def sb(name, shape, dt):
        return stack[-1].enter_context(nc.sbuf_tensor(name, list(shape), dt))

    def ps(name, shape, dt):
        return stack[-1].enter_context(nc.psum_tensor(name, list(shape), dt))
    Wr, b_Wr = cx.Wr, cx.b_Wr
    Grow, b_Grow = cx.Grow, cx.b_Grow
    ident_b, b_idb = cx.ident_b, cx.b_idb
    b_yd = Buf()

    wring = Ring([sb("wq%d" % i, [128, 8192], BF16) for i in range(6)])

    def load_mat(src_flat, off):
        wt_, wb_ = wring.next()
        fw.dma("pool", lambda e, wt_=wt_: e.dma_start(out=wt_[:].rearrange("p (q n) -> p q n", q=4),
               in_=src_flat[off:off + 128 * 8192].rearrange("(q p n) -> p q n", q=4, p=128)), writes=[wb_])
        return wt_, wb_

    def load3(ex):
        return load_mat(cx.w_eg_p, ex * D * 512) + load_mat(cx.w_eu_p, ex * D * 512) + load_mat(cx.w_ed_p, ex * 512 * D)

    def gu_view(wt_):
        return wt_[:].rearrange("p (q k n) -> p q k n", q=4, k=KC)

    def dn_view(wt_):
        return wt_[:].rearrange("p (q k n) -> p q k n", q=4, k=4)

    pg = Ring([ps("pg%d" % i, [128, 512], F32) for i in range(2)])
    pu = Ring([ps("pu%d" % i, [128, 512], F32) for i in range(2)])
    py = Ring([ps("py%d" % i, [128, 512], F32) for i in range(2)])
    pT = Ring([ps("pTe%d" % i, [128, 1024], BF16) for i in range(1)])
    pslot = Ring([ps("pslot%d" % i, [128, 512], F32) for i in range(1)])
    sg_r = Ring([sb("sg%d" % i, [128, 512], F32) for i in range(2)])

    def ffn_hidden(wg, wgb, wu, wub, rhs_of, rhsb, ncol, hT, hTb):
        gv, uv = gu_view(wg), gu_view(wu)
        first = True
        for fc in range(4):
            for c0 in range(0, ncol, 512):
                cn = min(512, ncol - c0)
                pgt, pgb = pg.next()
                put, pub = pu.next()
                for (pt, ptb, v, vb) in ((pgt, pgb, gv, wgb), (put, pub, uv, wub)):
                    for kc in range(KC):
                        fw.op("pe", lambda e_, pt=pt, v=v, kc=kc, fc=fc, c0=c0, cn=cn: e_.matmul(pt[:, 0:cn], lhsT=v[:, fc, kc, :], rhs=rhs_of(kc, c0, cn),
                              start=(kc == 0), stop=(kc == KC - 1)), reads=[vb, rhsb], writes=[ptb])
                sg, sgb = sg_r.next()
                fw.op("act", lambda e_, sg=sg, pgt=pgt, cn=cn: e_.activation(out=sg[:, 0:cn], in_=pgt[:, 0:cn], func=AF.Silu), reads=[pgb], writes=[sgb])
                fw.op("dve", lambda e_, sg=sg, put=put, fc=fc, c0=c0, cn=cn: e_.tensor_tensor(out=hT[:, fc, c0:c0 + cn], in0=put[:, 0:cn], in1=sg[:, 0:cn], op=ALU.mult),
                      reads=[pub, sgb], writes=[hTb], partial=not first)
                first = False

    stack.append(ExitStack())
    h2g, b_h2g = sb("h2g", [128, KC, 512], BF16), Buf()
    hTs, b_hTs = sb("hTs", [128, 4, 512], BF16), Buf()
    ysh = Ring([sb("ysh%d" % i, [128, D], F32) for i in range(2)])
    wg, wgb, wu, wub, wd, wdb = load3(64)
    dv = dn_view(wd)

    def shared_group(grp):
        fw.dma("sp", lambda e: e.dma_start(out=h2g[:], in_=cx.h2T_d.rearrange("p (k t) -> p k t", k=KC)[:, :, grp * 512:(grp + 1) * 512]),
               reads=[cx.b_h2Td], writes=[b_h2g])
        ffn_hidden(wg, wgb, wu, wub, lambda kc, c0, cn: h2g[:, kc, c0:c0 + cn], b_h2g, 512, hTs, b_hTs)
        for ti in range(4):
            yt, ytb = ysh.next()
            for n in range(4):
                pyt, pyb = py.next()
                for fc in range(4):
                    fw.op("pe", lambda e_, pyt=pyt, fc=fc, ti=ti, n=n: e_.matmul(pyt[:, :], lhsT=hTs[:, fc, ti * 128:(ti + 1) * 128], rhs=dv[:, n, fc, :],
                          start=(fc == 0), stop=(fc == 3)), reads=[b_hTs, wdb], writes=[pyb])
                fw.op("dve", lambda e_, pyt=pyt, yt=yt, n=n: e_.tensor_copy(out=yt[:, n * 512:(n + 1) * 512], in_=pyt[:, :]), reads=[pyb], writes=[ytb], partial=(n > 0))
            i = grp * 4 + ti
            fw.dma("sp", lambda e, yt=yt, i=i: e.dma_start(out=cx.y_d[i * 128:(i + 1) * 128, :], in_=yt[:]), reads=[ytb], writes=[b_yd], partial=True)
    for grp in range(4):
        shared_group(grp)
    fw.barrier()
    fw.flush()
    stack.pop().close()

    stack.append(ExitStack())
    ltri, b_ltri = sb("ltri_s", [128, 128], BF16), Buf()
    ones_b, b_onesb = sb("ones_bb", [128, 128], BF16), Buf()
    iota_c, b_iota = sb("iota_s", [128, CAP], F32), Buf()
    TW, b_TW = sb("TW", [128, 16, 64, 5], BF16), Buf()
    tcs, b_tcs = sb("tconst_s", [128, 16, 3], BF16), Buf()
    posm, b_posm = sb("posm", [128, 16, 64], F32), Buf()
    selm, b_selm = sb("selm", [128, 16, 64], BF16), Buf()
    carry, b_carry = sb("carry", [128, 64], F32), Buf()
    wtmp, b_wtmp = posm, b_posm
    fw.dma("sp", lambda e: e.dma_start(out=ltri[:], in_=cx.ltri[:, :]), writes=[b_ltri])
    fw.dma("sp", lambda e: e.dma_start(out=iota_c[:], in_=cx.iota_c[:, :]), writes=[b_iota])
    fw.dma("sp", lambda e: e.dma_start(out=tcs[:], in_=cx.tconst[:, :, :]), writes=[b_tcs])
    fw.op("pool", lambda e: e.memset(ones_b[:], 1.0), writes=[b_onesb])
    fw.op("pool", lambda e: e.memset(carry[:], 0.0), writes=[b_carry])
    fw.op("dve", lambda e: e.tensor_scalar(out=selm[:], in0=Wr[:, :, 0:64], scalar1=0.0, scalar2=None, op0=ALU.is_gt), reads=[b_Wr], writes=[b_selm])
    for k, src_k in ((0, 0), (1, 1), (4, 2)):
        fw.op("dve", lambda e, k=k, src_k=src_k: e.tensor_copy(out=TW[:, :, :, k], in_=tcs[:, :, src_k:src_k + 1].to_broadcast([128, 16, 64])),
              reads=[b_tcs], writes=[b_TW], partial=True)
    fw.op("dve", lambda e: e.tensor_copy(out=TW[:, :, :, 2], in_=Wr[:, :, 0:64]), reads=[b_Wr], writes=[b_TW], partial=True)
    fw.op("dve", lambda e: e.tensor_tensor(out=wtmp[:], in0=Wr[:, :, 0:64], in1=TW[:, :, :, 2], op=ALU.subtract), reads=[b_Wr, b_TW], writes=[b_wtmp])
    fw.op("dve", lambda e: e.tensor_copy(out=TW[:, :, :, 3], in_=wtmp[:]), reads=[b_wtmp], writes=[b_TW], partial=True)

    def pos_tile(i):
        pp, ppb = pslot.next()
        fw.op("pe", lambda e: e.matmul(pp[:, 0:64], lhsT=ltri[:], rhs=selm[:, i, :], start=True, stop=True), reads=[b_ltri, b_selm], writes=[ppb])
        fw.op("pe", lambda e: e.matmul(pp[:, 64:128], lhsT=ones_b[:], rhs=selm[:, i, :], start=True, stop=True), reads=[b_onesb, b_selm], writes=[ppb], partial=True)
        fw.op("dve", lambda e: e.tensor_tensor(out=posm[:, i, :], in0=pp[:, 0:64], in1=carry[:], op=ALU.add), reads=[ppb, b_carry], writes=[b_posm], partial=True)
        fw.op("dve", lambda e: e.scalar_tensor_tensor(out=posm[:, i, :], in0=posm[:, i, :], scalar=1.0, in1=selm[:, i, :], op0=ALU.add, op1=ALU.mult),
              reads=[b_posm, b_selm], writes=[b_posm], partial=True)
        fw.op("dve", lambda e: e.tensor_scalar(out=posm[:, i, :], in0=posm[:, i, :], scalar1=-1.0, scalar2=None, op0=ALU.add), reads=[b_posm], writes=[b_posm], partial=True)
        fw.op("dve", lambda e: e.tensor_tensor(out=carry[:], in0=carry[:], in1=pp[:, 64:128], op=ALU.add), reads=[ppb, b_carry], writes=[b_carry])
    for i in range(16):
        pos_tile(i)

    oh_r = Ring([sb("oh%d" % i, [128, CAP], BF16) for i in range(2)])
    sl_r = Ring([sb("slot%d" % i, [128, 96], F32) for i in range(2)])
    sli_r = Ring([sb("sloti%d" % i, [128, 8], I32) for i in range(2)])
    xg_r = Ring([sb("xg%d" % i, [128, D], BF16) for i in range(NCJ)])
    xgT_r = Ring([sb("xgT%d" % i, [128, KC, CAP], BF16) for i in range(1)])
    hT_r = Ring([sb("hTe%d" % i, [128, 4, CAP], BF16) for i in range(1)])
    ye_r = Ring([sb("ye%d" % i, [128, D], F32) for i in range(1)])

    class St:
        pass

    def prep(e):
        s = St()
        pp, ppb = pslot.next()
        ppv = pp[:, 0:NCJ * 8].rearrange("p (j c) -> p j c", c=8)
        ohs = []
        for i in range(16):
            oh, ohb = oh_r.next()
            fw.op("dve", lambda e_, oh=oh, i=i: e_.tensor_scalar(out=oh[:], in0=iota_c[:], scalar1=posm[:, i, e:e + 1], scalar2=None, op0=ALU.is_equal),
                  reads=[b_iota, b_posm], writes=[ohb])
            for cj in range(NCJ):
                fw.op("pe", lambda e_, oh=oh, i=i, cj=cj: e_.matmul(ppv[:, cj, 0:5], lhsT=oh[:, cj * 128:(cj + 1) * 128], rhs=TW[:, i, e, :],
                      start=(i == 0 and cj == 0), stop=(i == 15), skip_group_check=True), reads=[ohb, b_TW], writes=[ppb], partial=not (i == 0 and cj == 0))
        sl, slb = sl_r.next()
        sli, slib = sli_r.next()
        fw.op("dve", lambda e_: e_.tensor_copy(out=sl[:, 32:32 + NCJ * 8], in_=pp[:, 0:NCJ * 8]), reads=[ppb], writes=[slb])
        rv = sl[:, 32:32 + NCJ * 8].rearrange("p (j c) -> p j c", c=8)
        fw.op("dve", lambda e_: e_.scalar_tensor_tensor(out=sl[:, 0:NCJ], in0=rv[:, :, 0], scalar=16.0, in1=rv[:, :, 1], op0=ALU.mult, op1=ALU.add), reads=[slb], writes=[slb])
        fw.op("dve", lambda e_: e_.tensor_scalar(out=sl[:, 16:16 + NCJ], in0=rv[:, :, 4], scalar1=-BIG, scalar2=BIG, op0=ALU.mult, op1=ALU.add), reads=[slb], writes=[slb])
        fw.op("dve", lambda e_: e_.tensor_tensor(out=sl[:, 0:NCJ], in0=sl[:, 0:NCJ], in1=sl[:, 16:16 + NCJ], op=ALU.add), reads=[slb], writes=[slb])
        fw.op("dve", lambda e_: e_.tensor_tensor(out=sl[:, 8:8 + NCJ], in0=rv[:, :, 2], in1=rv[:, :, 3], op=ALU.add), reads=[slb], writes=[slb])
        fw.op("dve", lambda e_: e_.tensor_copy(out=sli[:, 0:NCJ], in_=sl[:, 0:NCJ]), reads=[slb], writes=[slib])
        s.xgs = []
        for cj in range(NCJ):
            xg, xgb = xg_r.next()

            def _g(e_, xg=xg, cj=cj):
                return e_.indirect_dma_start(out=xg[:, :], out_offset=None, in_=cx.h2_d[:, :],
                                             in_offset=bass.IndirectOffsetOnAxis(ap=sli[:, cj:cj + 1], axis=0), bounds_check=fw.reg(e_, T - 1), oob_is_err=False)
            fw.dma("pool", _g, reads=[slib, cx.b_h2d], writes=[xgb])
            s.xgs.append((xg, xgb))
        s.sl, s.slb, s.sli, s.slib = sl, slb, sli, slib
        return s

    def prep_b(s):
        xgT, xgTb = xgT_r.next()
        for cj in range(NCJ):
            xg, xgb = s.xgs[cj]
            for half in range(2):
                ptt, ptb = pT.next()
                for k8 in range(8):
                    kc = half * 8 + k8
                    fw.op("pe", lambda e_, ptt=ptt, xg=xg, kc=kc, k8=k8: e_.transpose(out=ptt[:, k8 * 128:(k8 + 1) * 128], in_=xg[:, kc * 128:(kc + 1) * 128], identity=ident_b[:]),
                          reads=[xgb, b_idb], writes=[ptb], partial=(k8 > 0))
                fw.op("dve", lambda e_, ptt=ptt, half=half, cj=cj: e_.tensor_copy(out=xgT[:, half * 8:(half + 1) * 8, cj * 128:(cj + 1) * 128],
                      in_=ptt[:, :].rearrange("p (k c) -> p k c", k=8)), reads=[ptb], writes=[xgTb], partial=not (cj == 0 and half == 0))
        s.xgT, s.xgTb = xgT, xgTb

    def compute(e, s, W):
        wg, wgb, wu, wub, wd, wdb = W
        dvw = dn_view(wd)
        hT, hTb = hT_r.next()
        ffn_hidden(wg, wgb, wu, wub, lambda kc, c0, cn: s.xgT[:, kc, c0:c0 + cn], s.xgTb, CAP, hT, hTb)
        for cj in range(NCJ):
            ye, yeb = ye_r.next()
            for n in range(4):
                pyt, pyb = py.next()
                for fc in range(4):
                    fw.op("pe", lambda e_, pyt=pyt, fc=fc, cj=cj, n=n: e_.matmul(pyt[:, :], lhsT=hT[:, fc, cj * 128:(cj + 1) * 128], rhs=dvw[:, n, fc, :],
                          start=(fc == 0), stop=(fc == 3)), reads=[hTb, wdb], writes=[pyb])
                fw.op("dve", lambda e_, pyt=pyt, ye=ye, n=n, cj=cj: e_.tensor_scalar(out=ye[:, n * 512:(n + 1) * 512], in0=pyt[:, :], scalar1=s.sl[:, 8 + cj:9 + cj], scalar2=None, op0=ALU.mult),
                      reads=[pyb, s.slb], writes=[yeb], partial=(n > 0))
            fw.dma("pool", lambda e_, ye=ye, cj=cj: e_.indirect_dma_start(out=cx.y_d[:, :], out_offset=bass.IndirectOffsetOnAxis(ap=s.sli[:, cj:cj + 1], axis=0),
                   in_=ye[:, :], in_offset=None, bounds_check=fw.reg(e_, T - 1), oob_is_err=False, compute_op=ALU.add), reads=[yeb, s.slib, b_yd], writes=[b_yd])

    nxt = prep(0)
    Wn = load3(0)
    for e in range(n_routed):
        cur, W = nxt, Wn
        prep_b(cur)
        if e + 1 < n_routed:
            nxt = prep(e + 1)
            Wn = load3(e + 1)
        compute(e, cur, W)
    fw.barrier()
    fw.flush()
    stack.pop().close()

    stack.append(ExitStack())
    yin = Ring([sb("yin%d" % i, [128, D], F32) for i in range(2)])
    xt_r = Ring([sb("xf%d" % i, [128, D], F32) for i in range(2)])
    st_r = Ring([sb("stf%d" % i, [128, 8], F32) for i in range(2)])
    cx.junk_f, cx.b_junk_f = sb("junk_f2", [128, D], BF16), Buf()
    b_out = Buf()
    cx.b_x1d = b_out

    def fin(i):
        yt, ytb = yin.next()
        fw.dma("sp", lambda e: e.dma_start(out=yt[:], in_=cx.y_d[i * 128:(i + 1) * 128, :]), reads=[b_yd], writes=[ytb])
        post_norm_residual(fw, cx, i, yt, ytb, xt_r, st_r, cx.x1_d, Grow[1], b_Grow[1], cx.out)
    for i in range(16):
        fin(i)
    cx.b_out = b_out
    fw.finish([cx.b_out])
    stack.pop().close()


from contextlib import ExitStack
from concourse.bass_utils import run_bass_kernel_spmd


def build_program(nc):
    cx = Ctx()
    declare_io_e(nc, cx); declare_io_a(nc, cx); declare_io_b(nc, cx); declare_io_c(nc, cx); declare_io_d(nc, cx)
    fw = FW(nc)
    with ExitStack() as gs:
        cx.Wr = gs.enter_context(nc.sbuf_tensor("Wr", [128, 16, NE], F32))
        cx.b_Wr, cx.b_h2Td, cx.b_x1d = Buf(), Buf(), Buf()
        stages = (lambda es: stage_abc(nc, fw, cx, es, gs=gs), lambda es: stage_nsa(nc, fw, cx, es), lambda es: stage_mlstm(nc, fw, cx, es),
                  lambda es: stage_merge(nc, fw, cx, es), lambda es: stage_router(nc, fw, cx, es), lambda es: stage_moe_sparse(nc, fw, cx, es))
        for fn in stages:
            with ExitStack() as es:
                fn(es)
                fw.barrier()
                fw.flush()
    return cx, fw


def shared_inputs(inp):
    P = {k: np.asarray(v)[0] for k, v in inp.items() if k not in ("x", "c")}
    col = lambda v: np.ascontiguousarray(v.reshape(16, 128).T)
    m = {}
    m["w_ada_p"] = pack_cols(P["w_ada"], [(c, 512) for c in range(0, 12288, 512)])[0]
    m["b_ada"] = P["b_ada"].reshape(1, -1)
    m["g4"] = np.concatenate([col(P["g_pre_mix"]), col(P["g_pre_ffn"]), np.zeros((128, 32), np.float32)], axis=1)
    m["gpost"] = np.stack([P["g_post_mix"], P["g_post_ffn"], P["g_pre_ffn"]])
    m["w_in_p"] = pack_cols(P["w_in"], win_chunks())[0]
    m["ident"] = np.eye(128, dtype=np.float32)
    m.update(nsa_consts())
    m["cmp_w1"] = P["cmp_w1"]
    m["cmp_w2"] = P["cmp_w2"]
    m["peT"] = np.ascontiguousarray(P["cmp_pe"].transpose(0, 2, 1))
    m.update(mlstm_consts())
    cw = np.concatenate([P["conv_w"], P["conv_b"][None, :]], 0)
    m["convp"] = np.ascontiguousarray(cw.reshape(5, 8, 128).transpose(2, 1, 0))
    m["bgate"] = np.ascontiguousarray(P["b_gates_m"].reshape(2, 4).T)
    m["mhg"] = P["mh_norm_g"].reshape(1, -1)
    ch = [(c, 512) for c in range(0, 2048, 512)]
    m["w_up_p"] = np.concatenate([pack_cols(P["w_up_nsa"], ch)[0], pack_cols(P["w_up_mlstm"], ch)[0]])
    m["w_out_p"] = pack_cols(P["w_out"], [(c, 128) for c in range(0, 2048, 128)])[0]
    m["w_router"] = P["w_router"]
    m["b_router"] = P["b_router"].reshape(1, 64)
    m.update(moe_consts())
    m["w_eg_p"] = np.concatenate([pack_gu4(P["w_e_gate"][e]) for e in range(64)] + [pack_gu4(P["w_sh_gate"])])
    m["w_eu_p"] = np.concatenate([pack_gu4(P["w_e_up"][e]) for e in range(64)] + [pack_gu4(P["w_sh_up"])])
    m["w_ed_p"] = np.concatenate([pack_dn4(P["w_e_down"][e]) for e in range(64)] + [pack_dn4(P["w_sh_down"])])
    return m


def kernel(**inputs):
    inp = {k: np.asarray(v) for k, v in inputs.items()}
    nc = bass.Bass("TRN2", target_bir_lowering=False)
    build_program(nc)
    sh = shared_inputs(inp)
    in_maps = []
    for b in range(8):
        m = dict(sh)
        m["x"] = np.ascontiguousarray(inp["x"][b])
        m["cT"] = np.ascontiguousarray(inp["c"][b].reshape(16, 128).T)
        in_maps.append(m)
    res = run_bass_kernel_spmd(nc, in_maps, core_ids=list(range(8)))
    return np.stack([np.asarray(r["out"]) for r in res.results], axis=0).astype(np.float32)
```

```python
import numpy as np
import concourse.bass as bass
import concourse.mybir as mybir

F32 = mybir.dt.float32
BF16 = mybir.dt.bfloat16
U32 = mybir.dt.uint32
I32 = mybir.dt.int32
AF = mybir.ActivationFunctionType
ALU = mybir.AluOpType
AX = mybir.AxisListType

ENGS = ("pe", "act", "dve", "pool", "sp")


class Buf:
    __slots__ = ("name", "w", "r")

    def __init__(self, name=""):
        self.name = name
        self.w = {}
        self.r = {}


class FW:
    def __init__(self, nc, dma_ring=8):
        self.nc = nc
        self.prog = {e: [] for e in ENGS}
        self.sem = {e: nc.alloc_semaphore("c_" + e) for e in ENGS}
        self.cnt = {e: 0 for e in ENGS}
        self.known = {e: {} for e in ENGS}
        self.R = dma_ring
        self.dsem = {q: [nc.alloc_semaphore("d_%s%d" % (q, i)) for i in range(dma_ring)] for q in ("sp", "pool", "act")}
        self.dn = {q: 0 for q in ("sp", "pool", "act")}

    def _need(self, e, tickets):
        need = {}
        for (s, v) in tickets:
            if v > need.get(s, 0):
                need[s] = v
        out = []
        kn = self.known[e]
        own = self.sem[e] if e == "pe" else None
        for s, v in need.items():
            if s is own:
                continue
            if kn.get(s, 0) < v:
                kn[s] = v
                out.append((s, v))
        return out

    def _deps(self, reads, writes, partial=False):
        t = []
        for b in reads:
            t.extend(b.w.items())
        for b in writes:
            if not partial:
                t.extend(b.w.items())
            t.extend(b.r.items())
        return t

    def _commit(self, ticket, reads, writes, partial=False):
        s, v = ticket
        for b in reads:
            if b.r.get(s, 0) < v:
                b.r[s] = v
        for b in writes:
            if partial:
                if b.w.get(s, 0) < v:
                    b.w[s] = v
            else:
                b.w = {s: v}
                b.r = {}

    @staticmethod
    def _compact(ts):
        m = {}
        so = {}
        for (s, v) in ts:
            k = id(s)
            so[k] = s
            if v > m.get(k, 0):
                m[k] = v
        return [(so[k], v) for k, v in m.items()]

    def op(self, e, fn, reads=(), writes=(), partial=False):
        deps = self._deps(reads, writes, partial)
        waits = self._need(e, deps)
        self.cnt[e] += 1
        tk = (self.sem[e], self.cnt[e])
        sem = self.sem[e]

        def emit(eng, waits=waits, fn=fn, sem=sem):
            for (s, v) in waits:
                eng.wait_ge(s, v)
            fn(eng).then_inc(sem, 1)
        self.prog[e].append(emit)
        self._commit(tk, reads, writes, partial)
        return tk

    def barrier(self):
        ts = [(self.sem[e], self.cnt[e]) for e in ENGS if self.cnt[e] > 0]
        for q in self.dsem:
            n = self.dn[q]
            for i, s in enumerate(self.dsem[q]):
                k = (n - 1 - i) // self.R + 1 if n > i else 0
                if k > 0:
                    ts.append((s, 16 * k))
        for e in ENGS:
            waits = self._need(e, ts)

            def emit(eng, waits=waits):
                for (s, v) in waits:
                    eng.wait_ge(s, v)
            self.prog[e].append(emit)

    def dma(self, q, fn, reads=(), writes=(), partial=False, extra=()):
        n = self.dn[q]
        self.dn[q] += 1
        s = self.dsem[q][n % self.R]
        prev = 16 * (n // self.R)
        deps = self._deps(reads, writes, partial) + list(extra)
        if prev > 0:
            deps = deps + [(s, prev)]
        waits = self._need(q, deps)
        tk = (s, prev + 16)

        def emit(eng, waits=waits, fn=fn, s=s):
            for (ss, v) in waits:
                eng.wait_ge(ss, v)
            fn(eng).then_inc(s, 16)
        self.prog[q].append(emit)
        self._commit(tk, reads, writes, partial)
        return tk

    def flush(self):
        nc = self.nc
        prog = self.prog
        self.prog = {e: [] for e in ENGS}
        self._regs = {}
        if not any(prog[e] for e in ENGS):
            return
        with nc.Block() as block:
            @block.tensor
            def _(e):
                for f in prog["pe"]:
                    f(e)

            @block.scalar
            def _(e):
                for f in prog["act"]:
                    f(e)

            @block.vector
            def _(e):
                for f in prog["dve"]:
                    f(e)

            @block.gpsimd
            def _(e):
                for f in prog["pool"]:
                    f(e)

            @block.sync
            def _(e):
                for f in prog["sp"]:
                    f(e)

    def reg(self, eng, value):
        k = (id(eng), value)
        if k not in self._regs:
            self._regs[k] = eng.to_reg(value)
        return self._regs[k]

    def finish(self, final_bufs):
        deps = []
        for b in final_bufs:
            deps.extend(b.w.items())
        waits = self._need("sp", deps)

        def emit(eng, waits=waits):
            for (s, v) in waits:
                eng.wait_ge(s, v)
        self.prog["sp"].append(emit)
        self.flush()


T = 2048
D = 2048
KC = 16
D_IN = 9784
C_Q, C_KV, C_GA, C_QK, C_VM, C_IF, C_OM, C_GTA, C_GTB = 0, 1024, 2560, 2608, 3632, 4656, 4664, 5688, 7736
EPS = 1e-6


class Ctx:
    pass


def win_chunks():
    groups = [(C_Q, 1024), (C_KV, 1536), (C_KV + 768, 256), (C_KV + 1280, 256), (C_GA, 48), (C_QK, 1024), (C_VM, 1024),
              (C_IF, 8), (C_OM, 1024), (C_GTA, 2048), (C_GTB, 2048)]
    out = []
    for c0, n in groups:
        for cc in range(0, n, 512):
            out.append((c0 + cc, min(512, n - cc)))
    return out


def pack_cols(w, chunks):
    import numpy as np
    K = w.shape[0] // 128
    parts, offs, off = [], {}, 0
    for (c0, n) in chunks:
        offs[(c0, n)] = off
        for h0 in range(0, n, 256):
            nn = min(256, n - h0)
            blk = w[:, c0 + h0:c0 + h0 + nn].reshape(K, 128, nn).transpose(1, 0, 2)
            parts.append(np.ascontiguousarray(blk).reshape(-1))
            off += 128 * K * nn
    return np.concatenate(parts), offs, off


def declare_io_a(nc, cx, dbg=False):
    cx.dbg = dbg
    ch = win_chunks()
    off = 0
    cx.win_off = {}
    for (c0, n) in ch:
        cx.win_off[(c0, n)] = off
        off += 128 * KC * n
    cx.win_total = off
    def din(name, shape, dt=F32):
        t = nc.dram_tensor(name, list(shape), dt, kind="ExternalInput").ap()
        setattr(cx, name, t)
        return t

    def dscr(name, shape, dt):
        t = nc.dram_tensor(name, list(shape), dt, kind="ExternalOutput" if dbg else "Internal").ap()
        setattr(cx, name, t)
        return t
    din("x", [T, D])
    din("cT", [128, KC])
    din("w_ada_p", [D * 6 * D])
    din("b_ada", [1, 6 * D])
    din("g4", [128, 4 * KC])
    din("gpost", [3, D])
    din("w_in_p", [cx.win_total])
    din("ident", [128, 128])
    if dbg:
        dscr("dbg_modc", [128, 4 * KC], F32)
        dscr("dbg_G", [128, D], F32)
    dscr("qT_d", [1024, T], BF16)
    dscr("kvT_d", [1536, T], BF16)
    dscr("qkT_d", [1024, T], F32)
    dscr("ifT_d", [8, T], F32)
    dscr("gaT_d", [D, T], BF16)
    dscr("gbT_d", [D, T], BF16)
    dscr("vs_d", [T, 256], BF16)
    dscr("vw_d", [T, 256], BF16)
    dscr("ga_d", [T, 48], F32)
    dscr("vm_d", [T, 1024], BF16)
    dscr("om_d", [T, 1024], BF16)
    dscr("if_d", [T, 8], F32)


class Ring:
    def __init__(self, tiles):
        self.tiles = tiles
        self.bufs = [Buf() for _ in tiles]
        self.i = 0

    def next(self):
        k = self.i % len(self.tiles)
        self.i += 1
        return self.tiles[k], self.bufs[k]


def stage_abc(nc, fw, cx, es, stop=None, gs=None):
    def sb(name, shape, dt):
        return es.enter_context(nc.sbuf_tensor(name, list(shape), dt))

    def ps(name, shape, dt):
        return es.enter_context(nc.psum_tensor(name, list(shape), dt))

    def gsb(name, shape, dt):
        return (gs or es).enter_context(nc.sbuf_tensor(name, list(shape), dt))

    ident_f = gsb("ident_f", [128, 128], F32)
    ident_b = gsb("ident_b", [128, 128], BF16)
    modc = gsb("modc", [128, 4 * KC], F32)
    Grow = [gsb("Grow%d" % i, [128, D], F32) for i in range(2)]
    cT = sb("cT_s", [128, KC], F32)
    sc = sb("sc_s", [128, KC], F32)
    S_b = sb("S_b", [128, KC, 128], BF16)
    g4 = sb("g4_s", [128, 4 * KC], F32)
    modraw = sb("modraw", [128, 4 * KC], F32)
    b_Grow = [Buf(), Buf()]
    b_id, b_idb, b_cT, b_sc, b_Sb, b_g4, b_modraw, b_modc = (Buf() for _ in range(8))
    fw.dma("sp", lambda e: e.dma_start(out=ident_f[:], in_=cx.ident[:, :]), writes=[b_id])
    fw.dma("sp", lambda e: e.dma_start(out=cT[:], in_=cx.cT[:, :]), writes=[b_cT])
    fw.dma("sp", lambda e: e.dma_start(out=g4[:], in_=cx.g4[:, :]), writes=[b_g4])
    fw.op("dve", lambda e: e.tensor_copy(out=ident_b[:], in_=ident_f[:]), reads=[b_id], writes=[b_idb])
    fw.op("act", lambda e: e.activation(out=sc[:], in_=cT[:], func=AF.Silu), reads=[b_cT], writes=[b_sc])
    for kc in range(KC):
        fw.op("dve", lambda e, kc=kc: e.tensor_copy(out=S_b[:, kc, :], in_=sc[:, kc:kc + 1].to_broadcast([128, 128])),
              reads=[b_sc], writes=[b_Sb], partial=True)

    wb = Ring([sb("wb%d" % i, [128, KC, 512], BF16) for i in range(4)])
    pacc = Ring([ps("pacc%d" % i, [128, 512], F32) for i in range(4)])
    ptr = Ring([ps("ptr%d" % i, [128, 1024], BF16) for i in range(2)])
    bada_r = Ring([sb("bada%d" % i, [128, 512], F32) for i in range(2)])
    mrow_r = Ring([sb("mrow%d" % i, [128, 512], F32) for i in range(2)])
    arow_r = Ring([sb("arow%d" % i, [1, 512], F32) for i in range(2)])
    cx.b_rowAB = Buf()
    junkf = sb("junkf", [128, 128], F32)
    b_junkf = Buf()
    cast_tog = [0]

    def load_w(src_flat, off, ncols):
        wbt, wbb = wb.next()
        for h0 in range(0, ncols, 256):
            n = min(256, ncols - h0)
            o = off + h0 * 128 * KC
            fw.dma("pool", lambda e, wbt=wbt, o=o, n=n, h0=h0: e.dma_start(
                out=wbt[:, :, h0:h0 + n], in_=src_flat[o:o + 128 * KC * n].rearrange("(p k n) -> p k n", p=128, k=KC)), writes=[wbb], partial=True)
        return wbt, wbb

    for ch in range(24):
        mi, q = ch // 4, ch % 4
        pt, pb = pacc.next()
        bt, btb = bada_r.next()
        fw.dma("sp", lambda e, bt=bt, ch=ch: e.dma_start(out=bt[:], in_=cx.b_ada[0, ch * 512:(ch + 1) * 512].partition_broadcast(128)), writes=[btb])
        wbt, wbb = load_w(cx.w_ada_p, ch * 512 * 128 * KC, 512)
        for kc in range(KC):
            fw.op("pe", lambda e, pt=pt, wbt=wbt, kc=kc: e.matmul(pt[:, :], lhsT=S_b[:, kc, :], rhs=wbt[:, kc, :],
                  start=(kc == 0), stop=(kc == KC - 1)), reads=[b_Sb, wbb], writes=[pb])
        mr, mrb = mrow_r.next()
        fw.op("dve", lambda e, pt=pt, mr=mr, bt=bt: e.tensor_tensor(out=mr[:], in0=pt[:, :], in1=bt[:], op=ALU.add),
              reads=[pb, btb], writes=[mrb])
        if mi in (2, 5):
            gi = 0 if mi == 2 else 1
            gt, gtb = bada_r.next()
            fw.dma("sp", lambda e, gt=gt, gi=gi, q=q: e.dma_start(out=gt[:], in_=cx.gpost[gi, q * 512:(q + 1) * 512].partition_broadcast(128)), writes=[gtb])
            fw.op("pool", lambda e, mr=mr, gt=gt, gi=gi, q=q: e.tensor_tensor(out=Grow[gi][:, q * 512:(q + 1) * 512], in0=mr[:], in1=gt[:], op=ALU.mult),
                  reads=[mrb, gtb], writes=[b_Grow[gi]], partial=True)
        else:
            vi = {1: 0, 0: 1, 4: 2, 3: 3}[mi]
            if mi == 3 and hasattr(cx, "rowAB_d"):
                fw.dma("sp", lambda e, mr=mr, q=q: e.dma_start(out=cx.rowAB_d[1:2, q * 512:(q + 1) * 512], in_=mr[0:1, :]), reads=[mrb], writes=[cx.b_rowAB], partial=True)
            if mi == 4 and hasattr(cx, "rowAB_d"):
                gt, gtb = bada_r.next()
                fw.dma("sp", lambda e, gt=gt, q=q: e.dma_start(out=gt[:], in_=cx.gpost[2, q * 512:(q + 1) * 512].partition_broadcast(128)), writes=[gtb])
                ar, arb = arow_r.next()
                fw.op("dve", lambda e, mr=mr, gt=gt, ar=ar: e.scalar_tensor_tensor(out=ar[:], in0=mr[0:1, :], scalar=1.0, in1=gt[0:1, :], op0=ALU.add, op1=ALU.mult),
                      reads=[mrb, gtb], writes=[arb])
                fw.dma("sp", lambda e, ar=ar, q=q: e.dma_start(out=cx.rowAB_d[0:1, q * 512:(q + 1) * 512], in_=ar[:]), reads=[arb], writes=[cx.b_rowAB], partial=True)
            for jj in range(4):
                j = q * 4 + jj
                fw.op("dve", lambda e, mr=mr, jj=jj, vi=vi, j=j: e.scalar_tensor_tensor(
                    out=junkf[:], in0=mr[:, jj * 128:(jj + 1) * 128], scalar=1.0, in1=ident_f[:],
                    op0=ALU.mult, op1=ALU.mult, accum_out=modraw[:, vi * KC + j:vi * KC + j + 1]),
                    reads=[mrb, b_id], writes=[b_junkf, b_modraw])
    for a_i, g_i in ((0, 0), (2, 1)):
        fw.op("dve", lambda e, a_i=a_i, g_i=g_i: e.scalar_tensor_tensor(
            out=modc[:, a_i * KC:(a_i + 1) * KC], in0=modraw[:, a_i * KC:(a_i + 1) * KC], scalar=1.0,
            in1=g4[:, g_i * KC:(g_i + 1) * KC], op0=ALU.add, op1=ALU.mult), reads=[b_modraw, b_g4], writes=[b_modc], partial=True)
        fw.op("dve", lambda e, a_i=a_i: e.tensor_copy(
            out=modc[:, (a_i + 1) * KC:(a_i + 2) * KC], in_=modraw[:, (a_i + 1) * KC:(a_i + 2) * KC]),
            reads=[b_modraw], writes=[b_modc], partial=True)
    cx.modc, cx.b_modc, cx.Grow, cx.b_Grow = modc, b_modc, Grow, b_Grow
    cx.ident_b, cx.b_idb, cx.ident_f, cx.b_id = ident_b, b_idb, ident_f, b_id
    if cx.dbg:
        fw.dma("sp", lambda e: e.dma_start(out=cx.dbg_modc[:, :], in_=modc[:]), reads=[b_modc], writes=[Buf()])
        fw.dma("sp", lambda e: e.dma_start(out=cx.dbg_G[:, :], in_=Grow[0][:]), reads=[b_Grow[0]], writes=[Buf()])
    if stop == 'A':
        return
    hT = sb("hT", [128, KC, T], BF16)
    b_hT = [Buf() for _ in range(16)]
    xt_r = Ring([sb("xt%d" % i, [128, D], F32) for i in range(2)])
    xn_r = Ring([sb("xn%d" % i, [128, D], BF16) for i in range(2)])
    junk = sb("junk", [128, D], BF16)
    b_junk = Buf()
    st_r = Ring([sb("stat%d" % i, [128, 4], F32) for i in range(2)])
    norm_pre(nc, fw, cx, cx.x, hT, b_hT, xt_r, xn_r, junk, b_junk, st_r, ptr, 0)
    cx.hT, cx.b_hT = hT, b_hT

    if stop == 'B':
        return
    stg_b = Ring([sb("stgb%d" % i, [128, 512], BF16) for i in range(4)])
    stg_f = Ring([sb("stgf%d" % i, [128, 512], F32) for i in range(2)])
    def evac(pt, pb, rows, ncols, func, dt, dst_ap):
        st, stb = (stg_b if dt == BF16 else stg_f).next()
        fw.op("act", lambda e: e.activation(out=st[0:rows, 0:ncols], in_=pt[0:rows, 0:ncols], func=func),
              reads=[pb], writes=[stb])
        fw.dma("sp", lambda e: e.dma_start(out=dst_ap, in_=st[0:rows, 0:ncols]), reads=[stb], writes=[Buf()])

    def proj_F(c0, ncols, func, dt, dst):
        for cc in range(0, ncols, 512):
            ncc = min(512, ncols - cc)
            wbt, wbb = load_w(cx.w_in_p, cx.win_off[(c0 + cc, ncc)], ncc)
            for m0 in range(0, ncc, 128):
                m = min(128, ncc - m0)
                for tch in range(4):
                    pt, pb = pacc.next()
                    for kc in range(KC):
                        fw.op("pe", lambda e, pt=pt, wbt=wbt, kc=kc, m0=m0, m=m, tch=tch: e.matmul(
                            pt[0:m, :], lhsT=wbt[:, kc, m0:m0 + m], rhs=hT[:, kc, tch * 512:(tch + 1) * 512],
                            start=(kc == 0), stop=(kc == KC - 1)), reads=[wbb] + b_hT[tch * 4:tch * 4 + 4], writes=[pb])
                    evac(pt, pb, m, 512, func, dt, dst[cc + m0:cc + m0 + m, tch * 512:(tch + 1) * 512])

    def proj_T(c0, ncols, func, dt, dst):
        for cc in range(0, ncols, 512):
            ncc = min(512, ncols - cc)
            wbt, wbb = load_w(cx.w_in_p, cx.win_off[(c0 + cc, ncc)], ncc)
            for ti in range(16):
                pt, pb = pacc.next()
                for kc in range(KC):
                    fw.op("pe", lambda e, pt=pt, wbt=wbt, kc=kc, ti=ti, ncc=ncc: e.matmul(
                        pt[:, 0:ncc], lhsT=hT[:, kc, ti * 128:(ti + 1) * 128], rhs=wbt[:, kc, 0:ncc],
                        start=(kc == 0), stop=(kc == KC - 1)), reads=[wbb, b_hT[ti]], writes=[pb])
                evac(pt, pb, 128, ncc, func, dt, dst[ti * 128:(ti + 1) * 128, cc:cc + ncc])

    ID = AF.Identity
    proj_F(C_Q, 1024, ID, BF16, cx.qT_d)
    proj_F(C_KV, 1536, ID, BF16, cx.kvT_d)
    proj_T(C_KV + 3 * 256, 256, ID, BF16, cx.vs_d)
    proj_T(C_KV + 5 * 256, 256, ID, BF16, cx.vw_d)
    proj_T(C_GA, 48, AF.Sigmoid, F32, cx.ga_d)
    proj_F(C_QK, 1024, ID, F32, cx.qkT_d)
    proj_T(C_VM, 1024, ID, BF16, cx.vm_d)
    proj_F(C_IF, 8, ID, F32, cx.ifT_d)
    proj_T(C_IF, 8, ID, F32, cx.if_d)
    proj_T(C_OM, 1024, AF.Sigmoid, BF16, cx.om_d)
    proj_F(C_GTA, 2048, AF.Sigmoid, BF16, cx.gaT_d)
    proj_F(C_GTB, 2048, AF.Sigmoid, BF16, cx.gbT_d)


def norm_pre(nc, fw, cx, x_ap, hT, b_hT, xt_r, xn_r, junk, b_junk, st_r, ptr, which, ntiles=16, x_dep=None, tok_hook=None):
    modc, b_modc = cx.modc, cx.b_modc
    a_off = which * 2 * KC
    for i in range(ntiles):
        xt, xtb = xt_r.next()
        fw.dma("sp", lambda e, xt=xt, i=i: e.dma_start(out=xt[:], in_=x_ap[i * 128:(i + 1) * 128, :]), writes=[xtb])
        st, stb = st_r.next()
        fw.op("dve", lambda e, xt=xt, st=st: e.scalar_tensor_tensor(out=junk[:], in0=xt[:], scalar=1.0 / D, in1=xt[:],
              op0=ALU.mult, op1=ALU.mult, accum_out=st[:, 0:1]), reads=[xtb], writes=[b_junk, stb])
        fw.op("dve", lambda e, st=st: e.tensor_scalar(out=st[:, 1:2], in0=st[:, 0:1], scalar1=EPS, scalar2=None, op0=ALU.add),
              reads=[stb], writes=[stb])
        fw.op("act", lambda e, st=st: e.activation(out=st[:, 3:4], in_=st[:, 1:2], func=AF.Sqrt), reads=[stb], writes=[stb])
        fw.op("dve", lambda e, st=st: e.reciprocal(out=st[:, 2:3], in_=st[:, 3:4]), reads=[stb], writes=[stb])
        xn, xnb = xn_r.next()
        fw.op("dve", lambda e, xn=xn, xt=xt, st=st: e.tensor_scalar(out=xn[:], in0=xt[:], scalar1=st[:, 2:3], scalar2=None, op0=ALU.mult),
              reads=[xtb, stb], writes=[xnb])
        if tok_hook is not None:
            tok_hook(i, xn, xnb)
        LV = 4
        if LV <= 2:
            fw.op("dve", lambda e, xn=xn, i=i: e.tensor_copy(out=hT[:, :, i * 128:(i + 1) * 128], in_=xn[:].rearrange("p (k n) -> p k n", k=16)), reads=[xnb], writes=[b_hT[i]])
            continue
        for half in range(2):
            pt, pb = ptr.next()
            for k8 in range(8):
                kc = half * 8 + k8
                fw.op("pe", lambda e, pt=pt, xn=xn, kc=kc, k8=k8: e.transpose(
                    out=pt[:, k8 * 128:(k8 + 1) * 128], in_=xn[:, kc * 128:(kc + 1) * 128], identity=cx.ident_b[:]),
                    reads=[xnb, cx.b_idb], writes=[pb])
            for k8 in range(8):
                kc = half * 8 + k8
                if LV == 3:
                    fw.op("dve", lambda e, pt=pt, kc=kc, k8=k8, i=i: e.tensor_copy(out=hT[:, kc, i * 128:(i + 1) * 128], in_=pt[:, k8 * 128:(k8 + 1) * 128]), reads=[pb], writes=[b_hT[i]], partial=True)
                elif k8 % 2 == 0 or LV == 4:
                    fw.op("dve", lambda e, pt=pt, kc=kc, k8=k8, i=i: e.tensor_scalar(
                        out=hT[:, kc, i * 128:(i + 1) * 128], in0=pt[:, k8 * 128:(k8 + 1) * 128],
                        scalar1=modc[:, a_off + kc:a_off + kc + 1], scalar2=modc[:, a_off + KC + kc:a_off + KC + kc + 1],
                        op0=ALU.mult, op1=ALU.add), reads=[pb, b_modc], writes=[b_hT[i]], partial=True)
                else:
                    fw.op("act", lambda e, pt=pt, kc=kc, k8=k8, i=i: e.activation(
                        out=hT[:, kc, i * 128:(i + 1) * 128], in_=pt[:, k8 * 128:(k8 + 1) * 128], func=AF.Identity,
                        scale=modc[:, a_off + kc:a_off + kc + 1], bias=modc[:, a_off + KC + kc:a_off + KC + kc + 1]),
                        reads=[pb, b_modc], writes=[b_hT[i]], partial=True)


SCALE = 0.125


def nsa_consts():
    import numpy as np
    import ml_dtypes
    bf = ml_dtypes.bfloat16
    t = np.arange(T)
    n = np.arange(127)
    cmaskT = ((16 * n[:, None] + 31) <= t[None, :]).astype(bf)
    ci = n[:, None]
    sj = np.arange(32)[None, :]
    ov = np.minimum(ci * 16 + 32, (sj + 1) * 64) - np.maximum(ci * 16, sj * 64)
    c2s = (np.clip(ov, 0, None).astype(np.float32) / 32).astype(bf)
    cur = (t // 64)[:, None]
    forced = (sj == 0) | (sj == cur) | (sj == cur - 1)
    addc = np.where(sj <= cur, np.where(forced, 1e3, 0.0), -1e9).astype(np.float32)
    addc = np.ascontiguousarray(addc.reshape(16, 128, 32).transpose(1, 0, 2))
    Xall = (np.arange(T)[None, :] // 64 == np.arange(32)[:, None]).astype(bf)
    a = np.arange(128)
    tri = (a[:, None] <= a[None, :]).astype(bf)
    tri2 = (a[:, None] > a[None, :]).astype(bf)
    return {"cmaskT": cmaskT, "c2s": c2s, "addc": addc, "Xall": Xall, "tri": tri, "tri2": tri2}


def declare_io_b(nc, cx, dbg=False):
    def din(name, shape, dt=F32):
        setattr(cx, name, nc.dram_tensor(name, list(shape), dt, kind="ExternalInput").ap())

    def dscr(name, shape, dt):
        setattr(cx, name, nc.dram_tensor(name, list(shape), dt, kind="ExternalOutput" if dbg else "Internal").ap())
    din("cmaskT", [127, T], BF16)
    din("c2s", [127, 32], BF16)
    din("addc", [128, 16, 32], F32)
    din("Xall", [32, T], BF16)
    din("tri", [128, 128], BF16)
    din("tri2", [128, 128], BF16)
    din("cmp_w1", [2, 2048, 256])
    din("cmp_w2", [2, 256, 64])
    din("peT", [2, 64, 32])
    dscr("onT_d", [1024, T], BF16)
    if dbg:
        dscr("dbg_kcc", [64, 4, 127], BF16)
        dscr("dbg_vcc", [127, 4, 97], BF16)
        dscr("dbg_sel", [T, 32], BF16)
        dscr("dbg_sm", [T, 256], F32)


def stage_nsa(nc, fw, cx, es):
    def sb(name, shape, dt):
        return es.enter_context(nc.sbuf_tensor(name, list(shape), dt))

    def ps(name, shape, dt):
        return es.enter_context(nc.psum_tensor(name, list(shape), dt))

    def load(name, shape, dt, src, q="sp", n_split=1):
        t = sb(name, shape, dt)
        b = Buf()
        if n_split == 1:
            fw.dma(q, lambda e: e.dma_start(out=t[:], in_=src), writes=[b])
        return t, b

    ident_b, b_idb = cx.ident_b, cx.b_idb
    cmaskT, b_cm = load("cmaskT_s", [127, T], BF16, cx.cmaskT[:, :])
    addc, b_addc = load("addc_s", [128, 16, 32], F32, cx.addc[:, :, :])
    Xall, b_X = load("Xall_s", [32, T], BF16, cx.Xall[:, :])
    tri, b_tri = load("tri_s", [128, 128], BF16, cx.tri[:, :])
    tri2, b_tri2 = load("tri2_s", [128, 128], BF16, cx.tri2[:, :])
    kT = {}
    for nm, r0 in (("ks", 512), ("kw", 1024)):
        kT[nm] = load("kT_" + nm, [64, 4, T], BF16, cx.kvT_d[r0:r0 + 256, :].rearrange("(g d) t -> d g t", d=64))
    v1 = {}
    for nm, src in (("vs", cx.vs_d), ("vw", cx.vw_d)):
        t_ = sb("v1_" + nm, [128, 16, 4, 65], BF16)
        b_ = Buf()
        fw.op("pool", lambda e, t_=t_: e.memset(t_[:], 1.0), writes=[b_])
        for i in range(16):
            fw.dma("sp", lambda e, t_=t_, i=i, src=src: e.dma_start(
                out=t_[:, i, :, 0:64], in_=src[i * 128:(i + 1) * 128, :].rearrange("p (g d) -> p g d", g=4)),
                reads=[b_], writes=[b_], partial=True)
        v1[nm] = (t_, b_)
    ga, b_ga = sb("ga_s", [128, 16, 48], F32), Buf()
    for i4 in range(4):
        fw.dma("sp", lambda e, i4=i4: e.dma_start(
            out=ga[:, i4 * 4:(i4 + 1) * 4, :], in_=cx.ga_d[i4 * 512:(i4 + 1) * 512, :].rearrange("(i p) c -> p i c", p=128)),
            writes=[b_ga], partial=True)

    pS = Ring([ps("pS%d" % i, [128, 512], F32) for i in range(2)])
    pM = Ring([ps("pM%d" % i, [128, 512], F32) for i in range(1)])
    pc_t, b_pc = ps("pc", [128, 512], F32)[:, 0:388].rearrange("p (h c) -> p h c", h=4), Buf()
    pss_t, b_pss = ps("pss", [128, 512], F32)[:, 0:260].rearrange("p (h c) -> p h c", h=4), Buf()
    psw_t, b_psw = ps("psw", [128, 512], F32)[:, 0:260].rearrange("p (h c) -> p h c", h=4), Buf()
    pT = Ring([ps("pT%d" % i, [128, 1024], BF16) for i in range(2)])

    kccT, b_kcc = sb("kccT", [64, 4, 127], BF16), Buf()
    vcc, b_vcc = sb("vcc", [127, 4, 97], BF16), Buf()
    from contextlib import ExitStack
    es_outer = es
    es = ExitStack()
    es.__enter__()
    w1f, b_w1f = sb("w1f", [64, 16, 256], F32), Buf()
    w1b, b_w1b = sb("w1b", [64, 32, 256], BF16), Buf()
    w2f, b_w2f = sb("w2f", [128, 2, 64], F32), Buf()
    w2b, b_w2b = sb("w2b", [128, 2, 64], BF16), Buf()
    pef, b_pef = sb("pef", [64, 32], F32), Buf()
    peb, b_peb = sb("peb", [64, 32], BF16), Buf()
    pbias, b_pbias = sb("pbias", [128, 2], F32), Buf()
    hidT, b_hid = sb("hidT", [128, 2, 508], BF16), Buf()
    gz = [sb("gz%d" % i, [128, 508], F32) for i in range(3)]
    b_gz = [Buf() for _ in range(3)]
    fw.op("pool", lambda e: e.memset(vcc[:], 1.0), writes=[b_vcc])
    for g in range(4):
        fw.dma("sp", lambda e, g=g: e.dma_start(out=vcc[:, g, 65:97], in_=cx.c2s[:, :]), reads=[b_vcc], writes=[b_vcc], partial=True)
    xT, b_xT = sb("xT_c", [64, 4, T], BF16), Buf()
    for ci, nm in ((0, "kc"), (1, "vc")):
        fw.dma("sp", lambda e, ci=ci: e.dma_start(out=xT[:], in_=cx.kvT_d[ci * 256:(ci + 1) * 256, :].rearrange("(g d) t -> d g t", d=64)), writes=[b_xT])
        for lh in range(2):
            fw.dma("sp", lambda e, ci=ci, lh=lh: e.dma_start(out=w1f[:], in_=cx.cmp_w1[ci, lh * 1024:(lh + 1) * 1024, :].rearrange("(l d) c -> d l c", d=64)), writes=[b_w1f])
            fw.op("pool", lambda e, lh=lh: e.tensor_copy(out=w1b[:, lh * 16:(lh + 1) * 16, :], in_=w1f[:]), reads=[b_w1f], writes=[b_w1b], partial=(lh == 1))
        fw.dma("sp", lambda e, ci=ci: e.dma_start(out=w2f[:], in_=cx.cmp_w2[ci].rearrange("(cc p) d -> p cc d", p=128)), writes=[b_w2f])
        fw.dma("sp", lambda e, ci=ci: e.dma_start(out=pef[:], in_=cx.peT[ci]), writes=[b_pef])
        fw.op("dve", lambda e: e.tensor_copy(out=w2b[:], in_=w2f[:]), reads=[b_w2f], writes=[b_w2b])
        fw.op("dve", lambda e: e.tensor_copy(out=peb[:], in_=pef[:]), reads=[b_pef], writes=[b_peb])
        for cc in range(2):
            pm, pmb = pM.next()
            for l in range(32):
                fw.op("pe", lambda e, pm=pm, l=l, cc=cc: e.matmul(pm[:, 0:1], lhsT=w1b[:, l, cc * 128:(cc + 1) * 128], rhs=peb[:, l:l + 1],
                      start=(l == 0), stop=(l == 31)), reads=[b_w1b, b_peb], writes=[pmb])
            fw.op("dve", lambda e, pm=pm, cc=cc: e.tensor_copy(out=pbias[:, cc:cc + 1], in_=pm[:, 0:1]), reads=[pmb], writes=[b_pbias])
            ph, phb = pS.next()
            for l in range(32):
                fw.op("pe", lambda e, ph=ph, l=l, cc=cc, xT=xT: e.matmul(
                    ph[:, 0:508].rearrange("p (g n) -> p g n", g=4), lhsT=w1b[:, l, cc * 128:(cc + 1) * 128],
                    rhs=xT[:, :, l:l + 2017:16], start=(l == 0), stop=(l == 31)), reads=[b_w1b, b_xT], writes=[phb])
            fw.op("dve", lambda e, ph=ph, cc=cc: e.tensor_scalar(out=gz[0][:], in0=ph[:, 0:508], scalar1=pbias[:, cc:cc + 1], scalar2=None, op0=ALU.add),
                  reads=[phb, b_pbias], writes=[b_gz[0]])
            fw.op("dve", lambda e: e.tensor_tensor(out=gz[1][:], in0=gz[0][:], in1=gz[0][:], op=ALU.mult), reads=[b_gz[0]], writes=[b_gz[1]])
            fw.op("dve", lambda e: e.tensor_scalar(out=gz[1][:], in0=gz[1][:], scalar1=0.044715, scalar2=1.0, op0=ALU.mult, op1=ALU.add),
                  reads=[b_gz[1]], writes=[b_gz[1]])
            fw.op("dve", lambda e: e.tensor_tensor(out=gz[1][:], in0=gz[1][:], in1=gz[0][:], op=ALU.mult), reads=[b_gz[0], b_gz[1]], writes=[b_gz[1]])
            fw.op("act", lambda e: e.activation(out=gz[2][:], in_=gz[1][:], func=AF.Tanh, scale=0.7978845608), reads=[b_gz[1]], writes=[b_gz[2]])
            fw.op("dve", lambda e: e.scalar_tensor_tensor(out=gz[2][:], in0=gz[2][:], scalar=1.0, in1=gz[0][:], op0=ALU.add, op1=ALU.mult),
                  reads=[b_gz[2], b_gz[0]], writes=[b_gz[2]])
            fw.op("dve", lambda e, cc=cc: e.tensor_scalar(out=hidT[:, cc, :], in0=gz[2][:], scalar1=0.5, scalar2=None, op0=ALU.mult),
                  reads=[b_gz[2]], writes=[b_hid], partial=(cc == 1))
        if ci == 0:
            pm, pmb = pM.next()
            for cc in range(2):
                fw.op("pe", lambda e, pm=pm, cc=cc: e.matmul(pm[0:64, 0:508], lhsT=w2b[:, cc, :], rhs=hidT[:, cc, :], start=(cc == 0), stop=(cc == 1)),
                      reads=[b_w2b, b_hid], writes=[pmb])
            fw.op("dve", lambda e, pm=pm: e.tensor_copy(out=kccT[:].rearrange("p g n -> p (g n)"), in_=pm[0:64, 0:508]), reads=[pmb], writes=[b_kcc])
        else:
            for g in range(4):
                pm, pmb = pM.next()
                for cc in range(2):
                    fw.op("pe", lambda e, pm=pm, cc=cc, g=g: e.matmul(pm[0:127, 0:64], lhsT=hidT[:, cc, g * 127:(g + 1) * 127], rhs=w2b[:, cc, :],
                          start=(cc == 0), stop=(cc == 1)), reads=[b_w2b, b_hid], writes=[pmb])
                fw.op("dve", lambda e, pm=pm, g=g: e.tensor_copy(out=vcc[:, g, 0:64], in_=pm[0:127, 0:64]), reads=[pmb, b_vcc], writes=[b_vcc], partial=True)
    if cx.dbg:
        fw.dma("sp", lambda e: e.dma_start(out=cx.dbg_kcc[:, :, :], in_=kccT[:]), reads=[b_kcc], writes=[Buf()])
        fw.dma("sp", lambda e: e.dma_start(out=cx.dbg_vcc[:, :, :], in_=vcc[:]), reads=[b_vcc], writes=[Buf()])

    fw.barrier()
    fw.flush()
    es.__exit__(None, None, None)
    es = es_outer
    qT, b_qT = sb("qT_s", [64, 16, T], BF16), Buf()
    for h4 in range(4):
        fw.dma("sp", lambda e, h4=h4: e.dma_start(
            out=qT[:, h4 * 4:(h4 + 1) * 4, :], in_=cx.qT_d[h4 * 256:(h4 + 1) * 256, :].rearrange("(h d) t -> d h t", d=64)),
            writes=[b_qT], partial=True)
    E_r = Ring([sb("E%d" % i, [128, 4, 128], BF16) for i in range(3)])
    Em_r = Ring([sb("Em%d" % i, [128, 4, 128], BF16) for i in range(3)])
    Mb_r = Ring([sb("Mb%d" % i, [128, 128], BF16) for i in range(2)])
    sm = Ring([sb("sm%d" % i, [128, 256], F32) for i in range(2)])
    selb_r = Ring([sb("selb%d" % i, [128, 32], BF16) for i in range(2)])
    selT_r = Ring([sb("selT%d" % i, [32, 128], BF16) for i in range(2)])
    o_r = Ring([sb("onsa%d" % i, [128, 1024], BF16) for i in range(2)])
    oT_r = Ring([sb("onsaT%d" % i, [128, 8, 128], BF16) for i in range(2)])
    acc_r = Ring([sb("oacc%d" % i, [128, 64], F32) for i in range(2)])
    kccT_, vs1, vw1 = kccT, v1["vs"], v1["vw"]
    ksT, b_ks = kT["ks"]
    kwT, b_kw = kT["kw"]

    def attn_branch(i, g, kts, kTt, b_k, v1t, pacc, b_pacc, mask_for):
        first = True
        for kt in kts:
            pst, psb = pS.next()
            fw.op("pe", lambda e, pst=pst, kt=kt: e.matmul(pst[:, :].rearrange("p (h t) -> p h t", h=4), lhsT=kTt[:, g, kt * 128:(kt + 1) * 128],
                  rhs=qT[:, 4 * g:4 * g + 4, i * 128:(i + 1) * 128], start=True, stop=True), reads=[b_k, b_qT], writes=[psb])
            Et, Eb = E_r.next()
            fw.op("act", lambda e, pst=pst, Et=Et: e.activation(out=Et[:].rearrange("p h t -> p (h t)"), in_=pst[:, :], func=AF.Exp, scale=SCALE),
                  reads=[psb], writes=[Eb])
            mk = mask_for(kt)
            if mk is None:
                lt, lb = Et, Eb
            else:
                mt, mb = mk
                lt, lb = Em_r.next()
                fw.op("dve", lambda e, lt=lt, Et=Et, mt=mt: e.tensor_tensor(out=lt[:], in0=Et[:], in1=mt[:].unsqueeze(1).to_broadcast([128, 4, 128]), op=ALU.mult),
                      reads=[Eb, mb], writes=[lb])
            for h in range(4):
                fw.op("pe", lambda e, lt=lt, h=h, kt=kt, first=first: e.matmul(pacc[:, h, :], lhsT=lt[:, h, :], rhs=v1t[0][:, kt, g, :],
                      start=(first and h == 0), stop=False, skip_group_check=True), reads=[lb, v1t[1]], writes=[b_pacc], partial=not (first and h == 0))
            first = False

    def do_group(i, g, ot, ob):
        pst, psb = pS.next()
        fw.op("pe", lambda e, pst=pst: e.matmul(pst[0:127, :].rearrange("p (h t) -> p h t", h=4), lhsT=kccT_[:, g, :],
              rhs=qT[:, 4 * g:4 * g + 4, i * 128:(i + 1) * 128], start=True, stop=True), reads=[b_kcc, b_qT], writes=[psb])
        Et, Eb = E_r.next()
        fw.op("act", lambda e, pst=pst, Et=Et: e.activation(out=Et[0:127].rearrange("p h t -> p (h t)"), in_=pst[0:127, :], func=AF.Exp, scale=SCALE),
              reads=[psb], writes=[Eb])
        Emt, Emb = Em_r.next()
        fw.op("dve", lambda e, Emt=Emt, Et=Et: e.tensor_tensor(out=Emt[0:127], in0=Et[0:127],
              in1=cmaskT[:, i * 128:(i + 1) * 128].unsqueeze(1).to_broadcast([127, 4, 128]), op=ALU.mult), reads=[Eb, b_cm], writes=[Emb])
        for h in range(4):
            fw.op("pe", lambda e, Emt=Emt, h=h: e.matmul(pc_t[:, h, :], lhsT=Emt[0:127, h, :], rhs=vcc[:, g, :], start=True, stop=True,
                  skip_group_check=True), reads=[Emb, b_vcc], writes=[b_pc], partial=(h > 0))
        s_, sb_ = sm.next()
        fw.op("dve", lambda e, s_=s_: e.tensor_scalar(out=s_[:, 0:4], in0=pc_t[:, :, 64], scalar1=1e-30, scalar2=None, op0=ALU.max), reads=[b_pc], writes=[sb_])
        fw.op("dve", lambda e, s_=s_: e.reciprocal(out=s_[:, 0:4], in_=s_[:, 0:4]), reads=[sb_], writes=[sb_])
        fw.op("dve", lambda e, s_=s_: e.scalar_tensor_tensor(out=s_[:, 32:64], in0=pc_t[:, 0, 65:97], scalar=s_[:, 0:1], in1=addc[:, i, :], op0=ALU.mult, op1=ALU.add),
              reads=[b_pc, sb_, b_addc], writes=[sb_])
        for h in range(1, 4):
            fw.op("dve", lambda e, s_=s_, h=h: e.scalar_tensor_tensor(out=s_[:, 32:64], in0=pc_t[:, h, 65:97], scalar=s_[:, h:h + 1], in1=s_[:, 32:64], op0=ALU.mult, op1=ALU.add),
                  reads=[b_pc, sb_], writes=[sb_])
        fw.op("dve", lambda e, s_=s_: e.max(out=s_[:, 96:104], in_=s_[:, 32:64]), reads=[sb_], writes=[sb_])
        fw.op("dve", lambda e, s_=s_: e.match_replace(out=s_[:, 64:96], in_to_replace=s_[:, 96:104], in_values=s_[:, 32:64], imm_value=-3.0e38), reads=[sb_], writes=[sb_])
        fw.op("dve", lambda e, s_=s_: e.max(out=s_[:, 104:112], in_=s_[:, 64:96]), reads=[sb_], writes=[sb_])
        fw.op("dve", lambda e, s_=s_: e.tensor_scalar(out=s_[:, 112:113], in0=s_[:, 111:112], scalar1=-1.0e8, scalar2=None, op0=ALU.max), reads=[sb_], writes=[sb_])
        selb, selbb = selb_r.next()
        fw.op("dve", lambda e, s_=s_, selb=selb: e.tensor_scalar(out=selb[:], in0=s_[:, 32:64], scalar1=s_[:, 112:113], scalar2=None, op0=ALU.is_ge), reads=[sb_], writes=[selbb])
        ptt, ptb = pT.next()
        fw.op("pe", lambda e, ptt=ptt, selb=selb: e.transpose(out=ptt[0:32, 0:128], in_=selb[:], identity=ident_b[:]), reads=[selbb, b_idb], writes=[ptb])
        selT, selTb = selT_r.next()
        fw.op("dve", lambda e, ptt=ptt, selT=selT: e.tensor_copy(out=selT[:], in_=ptt[0:32, 0:128]), reads=[ptb], writes=[selTb])
        if cx.dbg and g == 0:
            fw.dma("sp", lambda e, selb=selb: e.dma_start(out=cx.dbg_sel[i * 128:(i + 1) * 128, :], in_=selb[:]), reads=[selbb], writes=[Buf()])
            fw.dma("sp", lambda e, s_=s_: e.dma_start(out=cx.dbg_sm[i * 128:(i + 1) * 128, :], in_=s_[:]), reads=[sb_], writes=[Buf()])

        def sel_mask(kt, selT=selT, selTb=selTb):
            pm, pmb = pM.next()
            fw.op("pe", lambda e, pm=pm: e.matmul(pm[:, 0:128], lhsT=Xall[:, kt * 128:(kt + 1) * 128], rhs=selT[:], start=True, stop=True),
                  reads=[b_X, selTb], writes=[pmb])
            mt, mb = Mb_r.next()
            if kt == i:
                fw.op("dve", lambda e, pm=pm, mt=mt: e.tensor_tensor(out=mt[:], in0=pm[:, 0:128], in1=tri[:], op=ALU.mult), reads=[pmb, b_tri], writes=[mb])
            else:
                fw.op("dve", lambda e, pm=pm, mt=mt: e.tensor_copy(out=mt[:], in_=pm[:, 0:128]), reads=[pmb], writes=[mb])
            return mt, mb
        attn_branch(i, g, list(range(0, i + 1)), ksT, b_ks, vs1, pss_t, b_pss, sel_mask)

        def win_mask(kt):
            if kt == i:
                return tri, b_tri
            if kt == i - 4:
                return tri2, b_tri2
            return None
        attn_branch(i, g, list(range(max(0, i - 4), i + 1)), kwT, b_kw, vw1, psw_t, b_psw, win_mask)

        fw.op("dve", lambda e, s_=s_: e.reciprocal(out=s_[:, 4:8], in_=pss_t[:, :, 64]), reads=[b_pss, sb_], writes=[sb_])
        fw.op("dve", lambda e, s_=s_: e.reciprocal(out=s_[:, 8:12], in_=psw_t[:, :, 64]), reads=[b_psw, sb_], writes=[sb_])
        gav = ga[:, i, g * 12:(g + 1) * 12].rearrange("p (h b) -> p b h", b=3)
        fw.op("dve", lambda e, s_=s_, gav=gav: e.tensor_tensor(out=s_[:, 12:24].rearrange("p (b h) -> p b h", b=3), in0=gav,
              in1=s_[:, 0:12].rearrange("p (b h) -> p b h", b=3), op=ALU.mult), reads=[sb_, b_ga], writes=[sb_])
        for h in range(4):
            at, ab = acc_r.next()
            fw.op("dve", lambda e, s_=s_, at=at, h=h: e.tensor_scalar(out=at[:], in0=pc_t[:, h, 0:64], scalar1=s_[:, 12 + h:13 + h], scalar2=None, op0=ALU.mult),
                  reads=[b_pc, sb_], writes=[ab])
            fw.op("dve", lambda e, s_=s_, at=at, h=h: e.scalar_tensor_tensor(out=at[:], in0=pss_t[:, h, 0:64], scalar=s_[:, 16 + h:17 + h], in1=at[:], op0=ALU.mult, op1=ALU.add),
                  reads=[b_pss, sb_, ab], writes=[ab])
            c0 = (4 * g + h) * 64
            fw.op("dve", lambda e, s_=s_, at=at, h=h, ot=ot, c0=c0: e.scalar_tensor_tensor(out=ot[:, c0:c0 + 64], in0=psw_t[:, h, 0:64], scalar=s_[:, 20 + h:21 + h], in1=at[:], op0=ALU.mult, op1=ALU.add),
                  reads=[b_psw, sb_, ab], writes=[ob], partial=True)

    def finish_tile(i, ot, ob):
        ptt, ptb = pT.next()
        for j in range(8):
            fw.op("pe", lambda e, ptt=ptt, ot=ot, j=j: e.transpose(out=ptt[:, j * 128:(j + 1) * 128], in_=ot[:, j * 128:(j + 1) * 128], identity=ident_b[:]),
                  reads=[ob, b_idb], writes=[ptb], partial=(j > 0))
        oT, oTb = oT_r.next()
        fw.op("dve", lambda e, ptt=ptt, oT=oT: e.tensor_copy(out=oT[:].rearrange("p j t -> p (j t)"), in_=ptt[:, :]), reads=[ptb], writes=[oTb])
        fw.dma("sp", lambda e, oT=oT, i=i: e.dma_start(out=cx.onT_d[:, i * 128:(i + 1) * 128].rearrange("(j p) t -> p j t", p=128), in_=oT[:]),
               reads=[oTb], writes=[Buf()])

    for i in range(16):
        ot, ob = o_r.next()
        for g in range(4):
            do_group(i, g, ot, ob)
        finish_tile(i, ot, ob)


def mlstm_consts():
    import numpy as np
    import ml_dtypes
    a = np.arange(128)
    tri = (a[:, None] <= a[None, :])
    ms = np.zeros((4, 128, 512), np.float32)
    for r in range(4):
        for j in range(4):
            if j == r:
                ms[r][:, j * 128:(j + 1) * 128] = tri
            elif j > r:
                ms[r][:, j * 128:(j + 1) * 128] = 1.0
    return {"mmask": ms.astype(ml_dtypes.bfloat16)}


def declare_io_c(nc, cx, dbg=False):
    def din(name, shape, dt=F32):
        setattr(cx, name, nc.dram_tensor(name, list(shape), dt, kind="ExternalInput").ap())

    def dscr(name, shape, dt, force_out=False):
        setattr(cx, name, nc.dram_tensor(name, list(shape), dt, kind="ExternalOutput" if (dbg or force_out) else "Internal").ap())
    din("mmask", [4, 128, 512], BF16)
    din("convp", [128, 8, 5])
    din("bgate", [4, 2])
    din("mhg", [1, 1024])
    din("w_up_p", [2 * 1024 * D])
    din("w_out_p", [D * D])
    dscr("omT_d", [1024, T], BF16)
    dscr("ac_d", [8, T], F32)
    dscr("x1_d", [T, D], F32)


def stage_mlstm(nc, fw, cx, es):
    def sb(name, shape, dt):
        return es.enter_context(nc.sbuf_tensor(name, list(shape), dt))

    def ps(name, shape, dt):
        return es.enter_context(nc.psum_tensor(name, list(shape), dt))
    ident_b, b_idb, ident_f, b_id = cx.ident_b, cx.b_idb, cx.ident_f, cx.b_id

    mmask, b_mm = sb("mmask_s", [128, 4, 512], BF16), Buf()
    fw.dma("sp", lambda e: e.dma_start(out=mmask[:], in_=cx.mmask.rearrange("r p t -> p r t")), writes=[b_mm])
    convp, b_cp = sb("convp_s", [128, 8, 5], F32), Buf()
    fw.dma("sp", lambda e: e.dma_start(out=convp[:], in_=cx.convp[:, :, :]), writes=[b_cp])
    bg, b_bg = sb("bg_s", [4, 2], F32), Buf()
    fw.dma("sp", lambda e: e.dma_start(out=bg[:], in_=cx.bgate[:, :]), writes=[b_bg])
    mhg, b_mhg = sb("mhg_s", [128, 1024], F32), Buf()
    fw.dma("sp", lambda e: e.dma_start(out=mhg[:], in_=cx.mhg[0, :].partition_broadcast(128)), writes=[b_mhg])
    vm1, b_vm = sb("vm1", [128, 16, 4, 257], BF16), Buf()
    fw.op("pool", lambda e: e.memset(vm1[:], 1.0), writes=[b_vm])
    for i in range(16):
        fw.dma("sp", lambda e, i=i: e.dma_start(out=vm1[:, i, :, 0:256], in_=cx.vm_d[i * 128:(i + 1) * 128, :].rearrange("p (h d) -> p h d", h=4)),
               reads=[b_vm], writes=[b_vm], partial=True)

    qkb, b_qkb = sb("qkb", [128, 8, T], BF16), [Buf() for _ in range(8)]
    xin = Ring([sb("cx%d" % i, [128, T + 3], F32) for i in range(2)])
    yv = Ring([sb("cy%d" % i, [128, T], F32) for i in range(2)])
    for c in range(8):
        xt, xb = xin.next()
        fw.op("pool", lambda e, xt=xt: e.memset(xt[:, 0:3], 0.0), writes=[xb])
        fw.dma("sp", lambda e, xt=xt, c=c: e.dma_start(out=xt[:, 3:T + 3], in_=cx.qkT_d[c * 128:(c + 1) * 128, :]), reads=[xb], writes=[xb], partial=True)
        yt, yb = yv.next()
        fw.op("dve", lambda e, xt=xt, yt=yt, c=c: e.tensor_scalar(out=yt[:], in0=xt[:, 0:T], scalar1=convp[:, c, 0:1], scalar2=convp[:, c, 4:5], op0=ALU.mult, op1=ALU.add),
              reads=[xb, b_cp], writes=[yb])
        for k in range(1, 4):
            fw.op("dve", lambda e, xt=xt, yt=yt, c=c, k=k: e.scalar_tensor_tensor(out=yt[:], in0=xt[:, k:k + T], scalar=convp[:, c, k:k + 1], in1=yt[:], op0=ALU.mult, op1=ALU.add),
                  reads=[xb, b_cp, yb], writes=[yb])
        if c < 4:
            fw.op("act", lambda e, yt=yt, c=c: e.activation(out=qkb[:, c, :], in_=yt[:], func=AF.Silu), reads=[yb], writes=[b_qkb[c]])
        else:
            fw.op("act", lambda e, yt=yt: e.activation(out=yt[:], in_=yt[:], func=AF.Silu), reads=[yb], writes=[yb])
            fw.op("dve", lambda e, yt=yt, c=c: e.tensor_scalar(out=qkb[:, c, :], in0=yt[:], scalar1=128 ** -0.5, scalar2=None, op0=ALU.mult), reads=[yb], writes=[b_qkb[c]])

    gi, b_gi = sb("gi", [4, T], F32), Buf()
    gf, b_gf = sb("gf", [4, T], F32), Buf()
    ga_, b_ga = sb("ga_", [4, T], F32), Buf()
    ones4, b_o4 = sb("ones4", [4, T], F32), Buf()
    fw.dma("sp", lambda e: e.dma_start(out=gi[:], in_=cx.ifT_d[0:4, :]), writes=[b_gi])
    fw.dma("sp", lambda e: e.dma_start(out=gf[:], in_=cx.ifT_d[4:8, :]), writes=[b_gf])
    fw.op("pool", lambda e: e.memset(ones4[:], 1.0), writes=[b_o4])
    fw.op("dve", lambda e: e.tensor_scalar(out=gf[:], in0=gf[:], scalar1=bg[:, 1:2], scalar2=None, op0=ALU.add), reads=[b_gf, b_bg], writes=[b_gf])
    fw.op("act", lambda e: e.activation(out=gf[:], in_=gf[:], func=AF.Exp, scale=-1.0), reads=[b_gf], writes=[b_gf])
    fw.op("dve", lambda e: e.tensor_scalar(out=gf[:], in0=gf[:], scalar1=1.0, scalar2=None, op0=ALU.add), reads=[b_gf], writes=[b_gf])
    fw.op("act", lambda e: e.activation(out=gf[:], in_=gf[:], func=AF.Ln), reads=[b_gf], writes=[b_gf])
    fw.op("dve", lambda e: e.tensor_scalar(out=gf[:], in0=gf[:], scalar1=-1.0, scalar2=None, op0=ALU.mult), reads=[b_gf], writes=[b_gf])
    fw.op("dve", lambda e: e.tensor_tensor_scan(out=ga_[:], data0=ones4[:], data1=gf[:], initial=0.0, op0=ALU.mult, op1=ALU.add),
          reads=[b_gf, b_o4], writes=[b_ga])
    fw.op("dve", lambda e: e.scalar_tensor_tensor(out=gi[:], in0=gi[:], scalar=bg[:, 0:1], in1=ga_[:], op0=ALU.add, op1=ALU.subtract),
          reads=[b_gi, b_bg, b_ga], writes=[b_gi])
    b_acd = Buf()
    fw.dma("sp", lambda e: e.dma_start(out=cx.ac_d[0:4, :], in_=ga_[:]), reads=[b_ga], writes=[b_acd])
    pmisc, b_pmisc = ps("pmisc_m", [128, 512], F32), Buf()
    for i in range(16):
        fw.op("pe", lambda e, i=i: e.transpose(out=pmisc[:, i * 4:(i + 1) * 4], in_=gi[:, i * 128:(i + 1) * 128], identity=ident_f[0:4, 0:4]),
              reads=[b_gi, b_id], writes=[b_pmisc], partial=(i > 0))
    cT, b_cT = sb("cT_m", [128, 16, 4], F32), Buf()
    fw.op("dve", lambda e: e.tensor_copy(out=cT[:].rearrange("p i h -> p (i h)"), in_=pmisc[:, 0:64]), reads=[b_pmisc], writes=[b_cT])

    pS = Ring([ps("pSm%d" % i, [128, 512], F32) for i in range(2)])
    pacc = [ps("paccm%d" % i, [128, 512], F32) for i in range(4)]
    b_pacc = [Buf() for _ in range(4)]
    pT = Ring([ps("pTm%d" % i, [128, 1024], BF16) for i in range(1)])
    Abc_r = Ring([sb("Abc%d" % i, [128, T], F32) for i in range(2)])
    D_r = Ring([sb("Dm%d" % i, [128, 512], F32) for i in range(3)])
    W_r = Ring([sb("Wm%d" % i, [128, 512], BF16) for i in range(3)])
    om_r = Ring([sb("omt%d" % i, [128, 256], BF16) for i in range(2)])
    hc_r = Ring([sb("hc%d" % i, [128, 256], F32) for i in range(2)])
    hj_r = Ring([sb("hj%d" % i, [128, 256], F32) for i in range(2)])
    ho_r = Ring([sb("ho%d" % i, [128, 256], BF16) for i in range(2)])
    hT_r = Ring([sb("hoT%d" % i, [128, 2, 128], BF16) for i in range(2)])
    st_r = Ring([sb("stm%d" % i, [128, 8], F32) for i in range(2)])

    def head_chunk(h, c, Abc, Abcb):
        for kt in range(4 * c + 4):
            pst, psb = pS.next()
            fw.op("pe", lambda e, pst=pst, kt=kt: e.matmul(pst[:, :], lhsT=qkb[:, 4 + h, kt * 128:(kt + 1) * 128], rhs=qkb[:, h, c * 512:(c + 1) * 512],
                  start=True, stop=True), reads=[b_qkb[4 + h], b_qkb[h]], writes=[psb])
            Dt, Db = D_r.next()
            fw.op("act", lambda e, Dt=Dt, kt=kt: e.activation(out=Dt[:], in_=Abc[:, c * 512:(c + 1) * 512], func=AF.Exp, bias=cT[:, kt, h:h + 1]),
                  reads=[Abcb, b_cT], writes=[Db])
            Wt, Wb = W_r.next()
            if kt // 4 == c:
                fw.op("dve", lambda e, Dt=Dt, kt=kt: e.tensor_tensor(out=Dt[:], in0=Dt[:], in1=mmask[:, kt % 4, :], op=ALU.mult), reads=[Db, b_mm], writes=[Db])
            fw.op("dve", lambda e, Dt=Dt, Wt=Wt, pst=pst: e.tensor_tensor(out=Wt[:], in0=pst[:, :], in1=Dt[:], op=ALU.mult), reads=[psb, Db], writes=[Wb])
            for j in range(4):
                tj = 4 * c + j
                if tj < kt:
                    continue
                fw.op("pe", lambda e, Wt=Wt, j=j, kt=kt, tj=tj: e.matmul(pacc[j][:, 0:257], lhsT=Wt[:, j * 128:(j + 1) * 128], rhs=vm1[:, kt, h, :],
                      start=(kt == 0), stop=(kt == tj)), reads=[Wb, b_vm], writes=[b_pacc[j]])
        for j in range(4):
            tj = 4 * c + j
            st, stb = st_r.next()
            fw.op("dve", lambda e, st=st, j=j: e.tensor_scalar(out=st[:, 6:7], in0=pacc[j][:, 256:257], scalar1=-1.0, scalar2=None, op0=ALU.mult),
                  reads=[b_pacc[j]], writes=[stb])
            fw.op("dve", lambda e, st=st, j=j: e.tensor_tensor(out=st[:, 7:8], in0=st[:, 6:7], in1=pacc[j][:, 256:257], op=ALU.max),
                  reads=[b_pacc[j], stb], writes=[stb])
            fw.op("dve", lambda e, st=st, j=j: e.tensor_scalar(out=st[:, 0:1], in0=st[:, 7:8], scalar1=1.0, scalar2=None, op0=ALU.max),
                  reads=[stb], writes=[stb])
            fw.op("dve", lambda e, st=st: e.reciprocal(out=st[:, 1:2], in_=st[:, 0:1]), reads=[stb], writes=[stb])
            hc, hcb = hc_r.next()
            fw.op("dve", lambda e, st=st, hc=hc, j=j: e.tensor_scalar(out=hc[:], in0=pacc[j][:, 0:256], scalar1=st[:, 1:2], scalar2=None, op0=ALU.mult),
                  reads=[b_pacc[j], stb], writes=[hcb])
            hj, hjb = hj_r.next()
            fw.op("dve", lambda e, st=st, hc=hc, hj=hj: e.scalar_tensor_tensor(out=hj[:], in0=hc[:], scalar=1.0 / 256, in1=hc[:], op0=ALU.mult, op1=ALU.mult, accum_out=st[:, 2:3]),
                  reads=[hcb], writes=[hjb, stb])
            fw.op("dve", lambda e, st=st: e.tensor_scalar(out=st[:, 3:4], in0=st[:, 2:3], scalar1=EPS, scalar2=None, op0=ALU.add), reads=[stb], writes=[stb])
            fw.op("act", lambda e, st=st: e.activation(out=st[:, 4:5], in_=st[:, 3:4], func=AF.Sqrt), reads=[stb], writes=[stb])
            fw.op("dve", lambda e, st=st: e.reciprocal(out=st[:, 5:6], in_=st[:, 4:5]), reads=[stb], writes=[stb])
            omt, omb = om_r.next()
            fw.dma("sp", lambda e, omt=omt, tj=tj: e.dma_start(out=omt[:], in_=cx.om_d[tj * 128:(tj + 1) * 128, h * 256:(h + 1) * 256]), writes=[omb])
            fw.op("dve", lambda e, st=st, hc=hc, hj=hj: e.scalar_tensor_tensor(out=hj[:], in0=hc[:], scalar=st[:, 5:6], in1=mhg[:, h * 256:(h + 1) * 256], op0=ALU.mult, op1=ALU.mult),
                  reads=[hcb, stb, b_mhg], writes=[hjb])
            ho, hob = ho_r.next()
            fw.op("dve", lambda e, hj=hj, ho=ho, omt=omt: e.tensor_tensor(out=ho[:], in0=hj[:], in1=omt[:], op=ALU.mult), reads=[hjb, omb], writes=[hob])
            ptt, ptb = pT.next()
            for bk in range(2):
                fw.op("pe", lambda e, ptt=ptt, ho=ho, bk=bk: e.transpose(out=ptt[:, bk * 128:(bk + 1) * 128], in_=ho[:, bk * 128:(bk + 1) * 128], identity=ident_b[:]),
                      reads=[hob, b_idb], writes=[ptb], partial=(bk > 0))
            hT, hTb = hT_r.next()
            fw.op("dve", lambda e, ptt=ptt, hT=hT: e.tensor_copy(out=hT[:].rearrange("p b t -> p (b t)"), in_=ptt[:, 0:256]), reads=[ptb], writes=[hTb])
            fw.dma("sp", lambda e, hT=hT, tj=tj: e.dma_start(out=cx.omT_d[h * 256:(h + 1) * 256, tj * 128:(tj + 1) * 128].rearrange("(b p) t -> p b t", p=128), in_=hT[:]),
                   reads=[hTb], writes=[Buf()])

    for h in range(4):
        Abc, Abcb = Abc_r.next()
        fw.dma("sp", lambda e, Abc=Abc, h=h: e.dma_start(out=Abc[:], in_=cx.ac_d[h, :].partition_broadcast(128)), reads=[b_acd], writes=[Abcb])
        for c in range(4):
            head_chunk(h, c, Abc, Abcb)


def stage_merge(nc, fw, cx, es):
    from contextlib import ExitStack
    stack = [es]

    def sb(name, shape, dt):
        return stack[-1].enter_context(nc.sbuf_tensor(name, list(shape), dt))

    def ps(name, shape, dt):
        return stack[-1].enter_context(nc.psum_tensor(name, list(shape), dt))
    Grow, b_Grow = cx.Grow, cx.b_Grow
    mT = sb("mT", [128, KC, T], BF16)
    b_mT = [Buf() for _ in range(4)]
    pA = Ring([ps("pA%d" % i, [128, 512], F32) for i in range(2)])
    pB = Ring([ps("pB%d" % i, [128, 512], F32) for i in range(2)])
    pY = [ps("pY%d" % i, [128, 512], F32) for i in range(4)]
    b_pY = [Buf() for _ in range(4)]
    cast_tog = [0]

    stack.append(ExitStack())
    oT = [sb("oTa", [128, 8, T], BF16), sb("oTb", [128, 8, T], BF16)]
    b_oT = [Buf(), Buf()]
    for k, src in enumerate((cx.onT_d, cx.omT_d)):
        for j2 in range(2):
            fw.dma("sp", lambda e, k=k, src=src, j2=j2: e.dma_start(out=oT[k][:, j2 * 4:(j2 + 1) * 4, :],
                   in_=src[j2 * 512:(j2 + 1) * 512, :].rearrange("(j p) t -> p j t", p=128)), writes=[b_oT[k]], partial=True)
    wb = Ring([sb("wub%d" % i, [128, 8, 512], BF16) for i in range(4)])
    gt_r = Ring([sb("gtile%d" % i, [128, 512], BF16) for i in range(4)])
    m1_r = Ring([sb("m1_%d" % i, [128, 512], F32) for i in range(2)])

    def load_up(which, cc):
        wbt, wbb = wb.next()
        for h0 in range(2):
            o = which * 1024 * D + (cc * 512 + h0 * 256) * 128 * 8
            fw.dma("pool", lambda e, wbt=wbt, o=o, h0=h0: e.dma_start(out=wbt[:, :, h0 * 256:(h0 + 1) * 256],
                   in_=cx.w_up_p[o:o + 128 * 8 * 256].rearrange("(p k n) -> p k n", p=128, k=8)), writes=[wbb], partial=True)
        return wbt, wbb

    for cc in range(4):
        wa, wab = load_up(0, cc)
        wbm, wbmb = load_up(1, cc)
        for m in range(4):
            fc = cc * 4 + m
            for tc in range(4):
                pa, pab = pA.next()
                pb_, pbb = pB.next()
                for (pt, ptb, w, wbuf, k) in ((pa, pab, wa, wab, 0), (pb_, pbb, wbm, wbmb, 1)):
                    for kc in range(8):
                        fw.op("pe", lambda e, pt=pt, w=w, kc=kc, m=m, tc=tc, k=k: e.matmul(pt[:, :], lhsT=w[:, kc, m * 128:(m + 1) * 128],
                              rhs=oT[k][:, kc, tc * 512:(tc + 1) * 512], start=(kc == 0), stop=(kc == 7)), reads=[wbuf, b_oT[k]], writes=[ptb])
                g1, g1b = gt_r.next()
                g2, g2b = gt_r.next()
                fw.dma("sp", lambda e, g1=g1, fc=fc, tc=tc: e.dma_start(out=g1[:], in_=cx.gaT_d[fc * 128:(fc + 1) * 128, tc * 512:(tc + 1) * 512]), writes=[g1b])
                fw.dma("sp", lambda e, g2=g2, fc=fc, tc=tc: e.dma_start(out=g2[:], in_=cx.gbT_d[fc * 128:(fc + 1) * 128, tc * 512:(tc + 1) * 512]), writes=[g2b])
                m1, m1b = m1_r.next()
                m2, m2b = m1_r.next()
                fw.op("dve", lambda e, m1=m1, pa=pa, g1=g1: e.tensor_tensor(out=m1[:], in0=pa[:, :], in1=g1[:], op=ALU.mult), reads=[pab, g1b], writes=[m1b])
                fw.op("dve", lambda e, m2=m2, pb_=pb_, g2=g2: e.tensor_tensor(out=m2[:], in0=pb_[:, :], in1=g2[:], op=ALU.mult), reads=[pbb, g2b], writes=[m2b])
                fw.op("pool", lambda e, m1=m1, m2=m2, fc=fc, tc=tc: e.tensor_tensor(out=mT[:, fc, tc * 512:(tc + 1) * 512], in0=m1[:], in1=m2[:], op=ALU.add),
                      reads=[m1b, m2b], writes=[b_mT[tc]], partial=True)
    fw.barrier()
    fw.flush()
    stack.pop().close()

    stack.append(ExitStack())
    wo = sb("wo_b", [128, KC, D], BF16)
    b_wo = [Buf() for _ in range(4)]
    cx.junk_f, cx.b_junk_f, cx.b_x1d = sb("junk_f", [128, D], BF16), Buf(), Buf()
    for c16 in range(16):
        o = c16 * 128 * 128 * KC
        fw.dma("pool", lambda e, o=o, c16=c16: e.dma_start(out=wo[:, :, c16 * 128:(c16 + 1) * 128],
               in_=cx.w_out_p[o:o + 128 * KC * 128].rearrange("(p k n) -> p k n", p=128, k=KC)), writes=[b_wo[c16 // 4]], partial=True)
    xt_r = Ring([sb("xm%d" % i, [128, D], F32) for i in range(2)])
    y_r = Ring([sb("ym%d" % i, [128, D], F32) for i in range(2)])
    st_r = Ring([sb("stz%d" % i, [128, 8], F32) for i in range(2)])
    for i in range(16):
        for n in range(4):
            for fc in range(KC):
                fw.op("pe", lambda e, i=i, n=n, fc=fc: e.matmul(pY[n][:, :], lhsT=mT[:, fc, i * 128:(i + 1) * 128], rhs=wo[:, fc, n * 512:(n + 1) * 512],
                      start=(fc == 0), stop=(fc == KC - 1)), reads=[b_mT[i // 4], b_wo[n]], writes=[b_pY[n]])
        yt, yb = y_r.next()
        for n in range(4):
            if n % 2 == 0:
                fw.op("act", lambda e, yt=yt, n=n: e.activation(out=yt[:, n * 512:(n + 1) * 512], in_=pY[n][:, :], func=AF.Identity), reads=[b_pY[n]], writes=[yb], partial=True)
            else:
                fw.op("dve", lambda e, yt=yt, n=n: e.tensor_copy(out=yt[:, n * 512:(n + 1) * 512], in_=pY[n][:, :]), reads=[b_pY[n]], writes=[yb], partial=True)
        post_norm_residual(fw, cx, i, yt, yb, xt_r, st_r, cx.x, Grow[0], b_Grow[0], cx.x1_d)
    fw.barrier()
    fw.flush()
    stack.pop().close()


def post_norm_residual(fw, cx, i, yt, yb, xt_r, st_r, x_src, G, b_G, dst, x_dep=None):
    xt, xb = xt_r.next()
    fw.dma("sp", lambda e: e.dma_start(out=xt[:], in_=x_src[i * 128:(i + 1) * 128, :]), writes=[xb])
    st, stb = st_r.next()
    fw.op("dve", lambda e: e.scalar_tensor_tensor(out=cx.junk_f[:], in0=yt[:], scalar=1.0 / D, in1=yt[:], op0=ALU.mult, op1=ALU.mult, accum_out=st[:, 0:1]),
          reads=[yb], writes=[cx.b_junk_f, stb])
    fw.op("dve", lambda e: e.tensor_scalar(out=st[:, 1:2], in0=st[:, 0:1], scalar1=EPS, scalar2=None, op0=ALU.add), reads=[stb], writes=[stb])
    fw.op("act", lambda e: e.activation(out=st[:, 2:3], in_=st[:, 1:2], func=AF.Sqrt), reads=[stb], writes=[stb])
    fw.op("dve", lambda e: e.reciprocal(out=st[:, 3:4], in_=st[:, 2:3]), reads=[stb], writes=[stb])
    fw.op("dve", lambda e: e.scalar_tensor_tensor(out=yt[:], in0=yt[:], scalar=st[:, 3:4], in1=G[:], op0=ALU.mult, op1=ALU.mult), reads=[yb, stb, b_G], writes=[yb])
    fw.op("pool", lambda e: e.tensor_tensor(out=xt[:], in0=xt[:], in1=yt[:], op=ALU.add), reads=[xb, yb], writes=[xb])
    fw.dma("sp", lambda e: e.dma_start(out=dst[i * 128:(i + 1) * 128, :], in_=xt[:]), reads=[xb], writes=[cx.b_x1d], partial=True)


NE = 65


def pack_gu(w):
    return pack_cols(w, [(0, 512)])[0]


def pack_dn(w):
    import numpy as np
    return np.concatenate([np.ascontiguousarray(w[:, h * 1024:(h + 1) * 1024].reshape(4, 128, 1024).transpose(1, 0, 2)).reshape(-1) for h in range(2)])


def declare_io_d(nc, cx, dbg=False):
    def din(name, shape, dt=F32):
        setattr(cx, name, nc.dram_tensor(name, list(shape), dt, kind="ExternalInput").ap())

    def dscr(name, shape, dt, out=False):
        setattr(cx, name, nc.dram_tensor(name, list(shape), dt, kind="ExternalOutput" if (dbg or out) else "Internal").ap())
    din("w_router", [D, 64])
    din("b_router", [1, 64])
    din("w_eg_p", [NE * D * 512])
    din("w_eu_p", [NE * D * 512])
    din("w_ed_p", [NE * 512 * D])
    dscr("h2T_d", [128, KC * T], BF16)
    dscr("out", [T, D], F32, out=True)
    if dbg:
        dscr("dbg_wr", [128, 16, NE], F32)


def stage_router(nc, fw, cx, es):
    def sb(name, shape, dt):
        return es.enter_context(nc.sbuf_tensor(name, list(shape), dt))

    def ps(name, shape, dt):
        return es.enter_context(nc.psum_tensor(name, list(shape), dt))
    Wr, b_Wr = cx.Wr, cx.b_Wr
    h2T = sb("h2T", [128, KC, T], BF16)
    b_h2T = [Buf() for _ in range(16)]
    xt_r = Ring([sb("xt2_%d" % i, [128, D], F32) for i in range(2)])
    xn_r = Ring([sb("xn2_%d" % i, [128, D], BF16) for i in range(2)])
    junk = sb("junk2", [128, D], BF16)
    st_r = Ring([sb("stat2_%d" % i, [128, 4], F32) for i in range(2)])
    ptr = Ring([ps("ptr2_%d" % i, [128, 1024], BF16) for i in range(2)])
    tok_hook = None
    if hasattr(cx, "h2_d"):
        Arow, b_Ar = sb("Arow", [128, D], F32), Buf()
        Brow, b_Br = sb("Brow", [128, D], F32), Buf()
        fw.dma("sp", lambda e: e.dma_start(out=Arow[:], in_=cx.rowAB_d[0, :].partition_broadcast(128)), reads=[cx.b_rowAB], writes=[b_Ar])
        fw.dma("sp", lambda e: e.dma_start(out=Brow[:], in_=cx.rowAB_d[1, :].partition_broadcast(128)), reads=[cx.b_rowAB], writes=[b_Br])
        t1_r = Ring([sb("h2t1_%d" % i, [128, D], F32) for i in range(1)])
        h2t_r = Ring([sb("h2tok%d" % i, [128, D], BF16) for i in range(2)])
        cx.b_h2d = Buf()

        def tok_hook(i, xn, xnb):
            t1, t1b = t1_r.next()
            ht, htb = h2t_r.next()
            fw.op("pool", lambda e: e.tensor_tensor(out=t1[:], in0=xn[:], in1=Arow[:], op=ALU.mult), reads=[xnb, b_Ar], writes=[t1b])
            fw.op("pool", lambda e: e.tensor_tensor(out=ht[:], in0=t1[:], in1=Brow[:], op=ALU.add), reads=[t1b, b_Br], writes=[htb])
            fw.dma("sp", lambda e: e.dma_start(out=cx.h2_d[i * 128:(i + 1) * 128, :], in_=ht[:]), reads=[htb], writes=[cx.b_h2d], partial=True)
    norm_pre(nc, fw, cx, cx.x1_d, h2T, b_h2T, xt_r, xn_r, junk, Buf(), st_r, ptr, 1, x_dep=[cx.b_x1d], tok_hook=tok_hook)
    for i4 in range(4):
        fw.dma("sp", lambda e, i4=i4: e.dma_start(out=cx.h2T_d.rearrange("p (k t) -> p k t", k=KC)[:, :, i4 * 512:(i4 + 1) * 512], in_=h2T[:, :, i4 * 512:(i4 + 1) * 512]),
               reads=b_h2T[i4 * 4:(i4 + 1) * 4], writes=[cx.b_h2Td], partial=True)
    wrf, b_wrf = sb("wrf", [128, KC, 64], F32), Buf()
    wrb, b_wrb = sb("wrb", [128, KC, 64], BF16), Buf()
    brt, b_brt = sb("brt", [128, 64], F32), Buf()
    fw.dma("sp", lambda e: e.dma_start(out=wrf[:], in_=cx.w_router.rearrange("(k p) n -> p k n", p=128)), writes=[b_wrf])
    fw.dma("sp", lambda e: e.dma_start(out=brt[:], in_=cx.b_router[0, :].partition_broadcast(128)), writes=[b_brt])
    fw.op("dve", lambda e: e.tensor_copy(out=wrb[:], in_=wrf[:]), reads=[b_wrf], writes=[b_wrb])
    fw.op("pool", lambda e: e.memset(Wr[:], 1.0), writes=[b_Wr])
    pl = Ring([ps("plog%d" % i, [128, 512], F32) for i in range(2)])
    r_r = Ring([sb("rt%d" % i, [128, 512], F32) for i in range(2)])

    def route_tile(i):
        pt, pb = pl.next()
        for kc in range(KC):
            fw.op("pe", lambda e, kc=kc: e.matmul(pt[:, 0:64], lhsT=h2T[:, kc, i * 128:(i + 1) * 128], rhs=wrb[:, kc, :], start=(kc == 0), stop=(kc == KC - 1)),
                  reads=[b_h2T[i], b_wrb], writes=[pb])
        r, rb = r_r.next()
        S, SB, M8, GS, M, GM, T1, SBM, M8B, SEL = (r[:, 0:64], r[:, 64:128], r[:, 128:192], r[:, 192:200], r[:, 200:208], r[:, 208:216],
                                                  r[:, 216:224], r[:, 224:288], r[:, 288:296], r[:, 296:360])
        ops = []
        fw.op("act", lambda e: e.activation(out=S, in_=pt[:, 0:64], func=AF.Sigmoid), reads=[pb], writes=[rb])
        dv = lambda f, extra=(): fw.op("dve", f, reads=[rb] + list(extra), writes=[rb])
        dv(lambda e: e.tensor_tensor(out=SB, in0=S, in1=brt[:], op=ALU.add), [b_brt])
        for g in range(8):
            dv(lambda e, g=g: e.max(out=r[:, 128 + g * 8:136 + g * 8], in_=r[:, 64 + g * 8:72 + g * 8]))
        m8v = M8.rearrange("p (g k) -> p g k", k=8)
        dv(lambda e: e.tensor_tensor(out=GS, in0=m8v[:, :, 0], in1=m8v[:, :, 1], op=ALU.add))
        dv(lambda e: e.max(out=M, in_=GS))
        dv(lambda e: e.tensor_scalar(out=GM, in0=GS, scalar1=r[:, 203:204], scalar2=None, op0=ALU.is_ge))
        dv(lambda e: e.tensor_scalar(out=T1, in0=GM, scalar1=1.0e9, scalar2=-1.0e9, op0=ALU.mult, op1=ALU.add))
        dv(lambda e: e.tensor_tensor(out=SBM.rearrange("p (g k) -> p g k", k=8), in0=SB.rearrange("p (g k) -> p g k", k=8),
                                     in1=GM.unsqueeze(2).to_broadcast([128, 8, 8]), op=ALU.mult))
        dv(lambda e: e.tensor_tensor(out=SBM.rearrange("p (g k) -> p g k", k=8), in0=SBM.rearrange("p (g k) -> p g k", k=8),
                                     in1=T1.unsqueeze(2).to_broadcast([128, 8, 8]), op=ALU.add))
        dv(lambda e: e.max(out=M8B, in_=SBM))
        dv(lambda e: e.tensor_scalar(out=SEL, in0=SBM, scalar1=r[:, 295:296], scalar2=None, op0=ALU.is_ge))
        dv(lambda e: e.tensor_tensor(out=SEL, in0=SEL, in1=S, op=ALU.mult))
        dv(lambda e: e.tensor_reduce(out=r[:, 360:361], in_=SEL, axis=AX.X, op=ALU.add))
        dv(lambda e: e.reciprocal(out=r[:, 361:362], in_=r[:, 360:361]))
        fw.op("dve", lambda e: e.tensor_scalar(out=Wr[:, i, 0:64], in0=SEL, scalar1=r[:, 361:362], scalar2=2.5, op0=ALU.mult, op1=ALU.mult),
              reads=[rb, b_Wr], writes=[b_Wr], partial=True)

    for i in range(16):
        route_tile(i)
    if cx.dbg:
        fw.dma("sp", lambda e: e.dma_start(out=cx.dbg_wr[:, :, :], in_=Wr[:]), reads=[b_Wr], writes=[Buf()])


def stage_moe(nc, fw, cx, es, n_exp=NE, groups=(0, 1, 2, 3)):
    def sb(name, shape, dt):
        return es.enter_context(nc.sbuf_tensor(name, list(shape), dt))

    def ps(name, shape, dt):
        return es.enter_context(nc.psum_tensor(name, list(shape), dt))
    Wr, b_Wr = cx.Wr, cx.b_Wr
    Grow, b_Grow = cx.Grow, cx.b_Grow
    yacc = sb("yacc", [128, 4, D], F32)
    b_y = [Buf() for _ in range(4)]
    h2g, b_h2g = sb("h2g", [128, KC, 512], BF16), Buf()
    wgu = Ring([sb("wgu%d" % i, [128, KC, 512], BF16) for i in range(3)])
    wdr = Ring([sb("wdn%d" % i, [128, 4, D], BF16) for i in range(2)])
    wst = Ring([sb("wst%d" % i, [128, 4096], F32) for i in range(2)])
    sg_r = Ring([sb("sg%d" % i, [128, 512], F32) for i in range(2)])
    hT_r = Ring([sb("hTe%d" % i, [128, 4, 512], BF16) for i in range(2)])
    xt_r = Ring([sb("xf%d" % i, [128, D], F32) for i in range(1)])
    st_r = Ring([sb("stf%d" % i, [128, 8], F32) for i in range(2)])
    cx.junk_f, cx.b_junk_f = sb("junk_f2", [128, D], BF16), Buf()
    b_out = Buf()
    cx.b_x1d_save = cx.b_x1d
    pg = Ring([ps("pg%d" % i, [128, 512], F32) for i in range(2)])
    pu = Ring([ps("pu%d" % i, [128, 512], F32) for i in range(2)])
    py = Ring([ps("py%d" % i, [128, 512], F32) for i in range(3)])
    tog = [0]

    def load_mat(src_flat, off, dst, dstb, view):
        for half in range(2):
            wt, wtb = wst.next()
            o = off + half * 128 * 4096
            fw.dma("sp", lambda e, wt=wt, o=o: e.dma_start(out=wt[:], in_=src_flat[o:o + 128 * 4096].rearrange("(p n) -> p n", p=128)), writes=[wtb])
            eng = "pool" if tog[0] % 2 == 0 else "dve"
            tog[0] += 1
            fw.op(eng, lambda e, wt=wt, half=half: e.tensor_copy(out=view(dst, half), in_=view_st(wt, view)), reads=[wtb], writes=[dstb], partial=True)

    def view_st(wt, view):
        return wt[:].rearrange("p (k n) -> p k n", k=KC) if view is v_gu else wt[:].rearrange("p (k n) -> p k n", k=4)

    def v_gu(dst, half):
        return dst[:, :, half * 256:(half + 1) * 256]

    def v_dn(dst, half):
        return dst[:, :, half * 1024:(half + 1) * 1024]

    def expert(grp, e, first):
        wg, wgb = wgu.next()
        load_mat(cx.w_eg_p, e * D * 512, wg, wgb, v_gu)
        wu, wub = wgu.next()
        load_mat(cx.w_eu_p, e * D * 512, wu, wub, v_gu)
        wd, wdb = wdr.next()
        load_mat(cx.w_ed_p, e * 512 * D, wd, wdb, v_dn)
        hT, hTb = hT_r.next()
        for fc in range(4):
            pgt, pgb = pg.next()
            put, pub = pu.next()
            for (pt, ptb, w, wb_) in ((pgt, pgb, wg, wgb), (put, pub, wu, wub)):
                for kc in range(KC):
                    fw.op("pe", lambda e_, pt=pt, w=w, kc=kc, fc=fc: e_.matmul(pt[:, :], lhsT=w[:, kc, fc * 128:(fc + 1) * 128], rhs=h2g[:, kc, :],
                          start=(kc == 0), stop=(kc == KC - 1)), reads=[wb_, b_h2g], writes=[ptb])
            sg, sgb = sg_r.next()
            fw.op("act", lambda e_, sg=sg, pgt=pgt: e_.activation(out=sg[:], in_=pgt[:, :], func=AF.Silu), reads=[pgb], writes=[sgb])
            fw.op("dve", lambda e_, sg=sg, put=put, hT=hT, fc=fc: e_.tensor_tensor(out=hT[:, fc, :], in0=put[:, :], in1=sg[:], op=ALU.mult),
                  reads=[pub, sgb], writes=[hTb], partial=(fc > 0))
        for ti in range(4):
            for n in range(4):
                pyt, pyb = py.next()
                for fc in range(4):
                    fw.op("pe", lambda e_, pyt=pyt, hT=hT, wd=wd, fc=fc, ti=ti, n=n: e_.matmul(pyt[:, :], lhsT=hT[:, fc, ti * 128:(ti + 1) * 128],
                          rhs=wd[:, fc, n * 512:(n + 1) * 512], start=(fc == 0), stop=(fc == 3)), reads=[hTb, wdb], writes=[pyb])
                wcol = Wr[:, grp * 4 + ti, e:e + 1]
                if first:
                    fw.op("dve", lambda e_, pyt=pyt, ti=ti, n=n, wcol=wcol: e_.tensor_scalar(out=yacc[:, ti, n * 512:(n + 1) * 512], in0=pyt[:, :], scalar1=wcol, scalar2=None, op0=ALU.mult),
                          reads=[pyb, b_Wr], writes=[b_y[ti]], partial=True)
                else:
                    fw.op("dve", lambda e_, pyt=pyt, ti=ti, n=n, wcol=wcol: e_.scalar_tensor_tensor(out=yacc[:, ti, n * 512:(n + 1) * 512], in0=pyt[:, :], scalar=wcol,
                          in1=yacc[:, ti, n * 512:(n + 1) * 512], op0=ALU.mult, op1=ALU.add), reads=[pyb, b_Wr, b_y[ti]], writes=[b_y[ti]], partial=True)

    class YT:
        pass
    for grp in groups:
        fw.dma("sp", lambda e, grp=grp: e.dma_start(out=h2g[:], in_=cx.h2T_d.rearrange("p (k t) -> p k t", k=KC)[:, :, grp * 512:(grp + 1) * 512]),
               reads=[cx.b_h2Td], writes=[b_h2g])
        elist = list(range(n_exp)) if n_exp == NE else list(range(n_exp - 1)) + [NE - 1]
        for k, e in enumerate(elist):
            expert(grp, e, k == 0)
        for ti in range(4):
            i = grp * 4 + ti
            cx.b_x1d = b_out
            post_norm_residual(fw, cx, i, yacc[:, ti, :], b_y[ti], xt_r, st_r, cx.x1_d, Grow[1], b_Grow[1], cx.out, x_dep=[cx.b_x1d_save])
    cx.b_out = b_out


CAP = 768
NCJ = CAP // 128
BIG = 1.0e6


def pack_gu4(w):
    import numpy as np
    return np.concatenate([np.ascontiguousarray(w[:, q * 128:(q + 1) * 128].reshape(16, 128, 128).transpose(1, 0, 2)).reshape(-1) for q in range(4)])


def pack_dn4(w):
    import numpy as np
    return np.concatenate([np.ascontiguousarray(w[:, q * 512:(q + 1) * 512].reshape(4, 128, 512).transpose(1, 0, 2)).reshape(-1) for q in range(4)])


def moe_consts():
    import numpy as np
    import ml_dtypes
    bf = ml_dtypes.bfloat16
    a = np.arange(128)
    ltri = (a[:, None] < a[None, :]).astype(bf)
    iota_c = np.tile(np.arange(CAP, dtype=np.float32)[None, :], (128, 1))
    t = (np.arange(16)[None, :] * 128 + a[:, None])
    tconst = np.stack([t // 16, t % 16, np.ones_like(t)], -1).astype(bf)
    return {"ltri": ltri, "iota_c": iota_c, "tconst": tconst}


def declare_io_e(nc, cx, dbg=False):
    def din(name, shape, dt=F32):
        setattr(cx, name, nc.dram_tensor(name, list(shape), dt, kind="ExternalInput").ap())

    def dscr(name, shape, dt, out=False):
        setattr(cx, name, nc.dram_tensor(name, list(shape), dt, kind="ExternalOutput" if (dbg or out) else "Internal").ap())
    din("ltri", [128, 128], BF16)
    din("iota_c", [128, CAP], F32)
    din("tconst", [128, 16, 3], BF16)
    cx.h2_d = nc.dram_tensor("h2_d", [T, D], BF16, kind="Internal").ap()
    dscr("y_d", [T, D], F32)
    dscr("rowAB_d", [2, D], F32)


def stage_moe_sparse(nc, fw, cx, es, n_routed=64):
    from contextlib import ExitStack
    stack = [es]

    def sb(name, shape, dt):
        return stack[-1].enter_context(nc.sbuf_tensor(name, list(shape), dt))

    def ps(name, shape, dt):
        return stack[-1].enter_context(nc.psum_tensor(name, list(shape), dt))
    Wr, b_Wr = cx.Wr, cx.b_Wr
    Grow, b_Grow = cx.Grow, cx.b_Grow
    ident_b, b_idb = cx.ident_b, cx.b_idb
    b_yd = Buf()

    wring = Ring([sb("wq%d" % i, [128, 8192], BF16) for i in range(6)])

    def load_mat(src_flat, off):
        wt_, wb_ = wring.next()
        fw.dma("pool", lambda e, wt_=wt_: e.dma_start(out=wt_[:].rearrange("p (q n) -> p q n", q=4),
               in_=src_flat[off:off + 128 * 8192].rearrange("(q p n) -> p q n", q=4, p=128)), writes=[wb_])
        return wt_, wb_

    def load3(ex):
        return load_mat(cx.w_eg_p, ex * D * 512) + load_mat(cx.w_eu_p, ex * D * 512) + load_mat(cx.w_ed_p, ex * 512 * D)

    def gu_view(wt_):
        return wt_[:].rearrange("p (q k n) -> p q k n", q=4, k=KC)

    def dn_view(wt_):
        return wt_[:].rearrange("p (q k n) -> p q k n", q=4, k=4)

    pg = Ring([ps("pg%d" % i, [128, 512], F32) for i in range(2)])
    pu = Ring([ps("pu%d" % i, [128, 512], F32) for i in range(2)])
    py = Ring([ps("py%d" % i, [128, 512], F32) for i in range(2)])
    pT = Ring([ps("pTe%d" % i, [128, 1024], BF16) for i in range(2)])
    pslot = py
    sg_r = Ring([sb("sg%d" % i, [128, 512], F32) for i in range(1)])

    def ffn_hidden(wg, wgb, wu, wub, rhs_of, rhsb, ncol, hT, hTb):
        gv, uv = gu_view(wg), gu_view(wu)
        first = True
        for fc in range(4):
            for c0 in range(0, ncol, 512):
                cn = min(512, ncol - c0)
                pgt, pgb = pg.next()
                put, pub = pu.next()
                for (pt, ptb, v, vb) in ((pgt, pgb, gv, wgb), (put, pub, uv, wub)):
                    for kc in range(KC):
                        fw.op("pe", lambda e_, pt=pt, v=v, kc=kc, fc=fc, c0=c0, cn=cn: e_.matmul(pt[:, 0:cn], lhsT=v[:, fc, kc, :], rhs=rhs_of(kc, c0, cn),
                              start=(kc == 0), stop=(kc == KC - 1)), reads=[vb, rhsb], writes=[ptb])
                sg, sgb = sg_r.next()
                fw.op("act", lambda e_, sg=sg, pgt=pgt, cn=cn: e_.activation(out=sg[:, 0:cn], in_=pgt[:, 0:cn], func=AF.Silu), reads=[pgb], writes=[sgb])
                fw.op("dve", lambda e_, sg=sg, put=put, fc=fc, c0=c0, cn=cn: e_.tensor_tensor(out=hT[:, fc, c0:c0 + cn], in0=put[:, 0:cn], in1=sg[:, 0:cn], op=ALU.mult),
                      reads=[pub, sgb], writes=[hTb], partial=not first)
                first = False

    stack.append(ExitStack())
    h2g, b_h2g = sb("h2g", [128, KC, 512], BF16), Buf()
    hTs, b_hTs = sb("hTs", [128, 4, 512], BF16), Buf()
    ysh = Ring([sb("ysh%d" % i, [128, D], F32) for i in range(2)])
    wg, wgb, wu, wub, wd, wdb = load3(64)
    dv = dn_view(wd)

    def shared_group(grp):
        fw.dma("sp", lambda e: e.dma_start(out=h2g[:], in_=cx.h2T_d.rearrange("p (k t) -> p k t", k=KC)[:, :, grp * 512:(grp + 1) * 512]),
               reads=[cx.b_h2Td], writes=[b_h2g])
        ffn_hidden(wg, wgb, wu, wub, lambda kc, c0, cn: h2g[:, kc, c0:c0 + cn], b_h2g, 512, hTs, b_hTs)
        for ti in range(4):
            yt, ytb = ysh.next()
            for n in range(4):
                pyt, pyb = py.next()
                for fc in range(4):
                    fw.op("pe", lambda e_, pyt=pyt, fc=fc, ti=ti, n=n: e_.matmul(pyt[:, :], lhsT=hTs[:, fc, ti * 128:(ti + 1) * 128], rhs=dv[:, n, fc, :],
                          start=(fc == 0), stop=(fc == 3)), reads=[b_hTs, wdb], writes=[pyb])
                fw.op("dve", lambda e_, pyt=pyt, yt=yt, n=n: e_.tensor_copy(out=yt[:, n * 512:(n + 1) * 512], in_=pyt[:, :]), reads=[pyb], writes=[ytb], partial=(n > 0))
            i = grp * 4 + ti
            fw.dma("sp", lambda e, yt=yt, i=i: e.dma_start(out=cx.y_d[i * 128:(i + 1) * 128, :], in_=yt[:]), reads=[ytb], writes=[b_yd], partial=True)
    for grp in range(4):
        shared_group(grp)
    fw.barrier()
    fw.flush()
    stack.pop().close()

    stack.append(ExitStack())
    iota_c, b_iota = sb("iota_s", [128, CAP], F32), Buf()
    TW, b_TW = sb("TW", [128, 16, 64, 4], BF16), Buf()
    tcs, b_tcs = sb("tconst_s", [128, 16, 3], BF16), Buf()
    posm, b_posm = sb("posm", [128, 16, 64], F32), Buf()
    stack.append(ExitStack())
    ltri, b_ltri = sb("ltri_s", [128, 128], BF16), Buf()
    ones_b, b_onesb = sb("ones_bb", [128, 128], BF16), Buf()
    selm, b_selm = sb("selm", [128, 16, 64], BF16), Buf()
    carry, b_carry = sb("carry", [128, 64], F32), Buf()
    wtmp, b_wtmp = posm, b_posm
    fw.dma("sp", lambda e: e.dma_start(out=ltri[:], in_=cx.ltri[:, :]), writes=[b_ltri])
    fw.dma("sp", lambda e: e.dma_start(out=iota_c[:], in_=cx.iota_c[:, :]), writes=[b_iota])
    fw.dma("sp", lambda e: e.dma_start(out=tcs[:], in_=cx.tconst[:, :, :]), writes=[b_tcs])
    fw.op("pool", lambda e: e.memset(ones_b[:], 1.0), writes=[b_onesb])
    fw.op("pool", lambda e: e.memset(carry[:], 0.0), writes=[b_carry])
    fw.op("dve", lambda e: e.tensor_scalar(out=selm[:], in0=Wr[:, :, 0:64], scalar1=0.0, scalar2=None, op0=ALU.is_gt), reads=[b_Wr], writes=[b_selm])
    for k, src_k in ((0, 0), (1, 1)):
        fw.op("dve", lambda e, k=k, src_k=src_k: e.tensor_copy(out=TW[:, :, :, k], in_=tcs[:, :, src_k:src_k + 1].to_broadcast([128, 16, 64])),
              reads=[b_tcs], writes=[b_TW], partial=True)
    fw.op("dve", lambda e: e.tensor_copy(out=TW[:, :, :, 2], in_=Wr[:, :, 0:64]), reads=[b_Wr], writes=[b_TW], partial=True)
    fw.op("dve", lambda e: e.tensor_tensor(out=wtmp[:], in0=Wr[:, :, 0:64], in1=TW[:, :, :, 2], op=ALU.subtract), reads=[b_Wr, b_TW], writes=[b_wtmp])
    fw.op("dve", lambda e: e.tensor_copy(out=TW[:, :, :, 3], in_=wtmp[:]), reads=[b_wtmp], writes=[b_TW], partial=True)

    def pos_tile(i):
        pp, ppb = pslot.next()
        fw.op("pe", lambda e: e.matmul(pp[:, 0:64], lhsT=ltri[:], rhs=selm[:, i, :], start=True, stop=True), reads=[b_ltri, b_selm], writes=[ppb])
        fw.op("pe", lambda e: e.matmul(pp[:, 64:128], lhsT=ones_b[:], rhs=selm[:, i, :], start=True, stop=True), reads=[b_onesb, b_selm], writes=[ppb], partial=True)
        fw.op("dve", lambda e: e.tensor_tensor(out=posm[:, i, :], in0=pp[:, 0:64], in1=carry[:], op=ALU.add), reads=[ppb, b_carry], writes=[b_posm], partial=True)
        fw.op("dve", lambda e: e.scalar_tensor_tensor(out=posm[:, i, :], in0=posm[:, i, :], scalar=1.0, in1=selm[:, i, :], op0=ALU.add, op1=ALU.mult),
              reads=[b_posm, b_selm], writes=[b_posm], partial=True)
        fw.op("dve", lambda e: e.tensor_scalar(out=posm[:, i, :], in0=posm[:, i, :], scalar1=-1.0, scalar2=None, op0=ALU.add), reads=[b_posm], writes=[b_posm], partial=True)
        fw.op("dve", lambda e: e.tensor_tensor(out=carry[:], in0=carry[:], in1=pp[:, 64:128], op=ALU.add), reads=[ppb, b_carry], writes=[b_carry])
    for i in range(16):
        pos_tile(i)
    fw.barrier()
    fw.flush()
    stack.pop().close()

    oh_r = Ring([sb("oh%d" % i, [128, CAP], BF16) for i in range(1)])
    sl_r = Ring([sb("slot%d" % i, [128, 96], F32) for i in range(2)])
    sli_r = Ring([sb("sloti%d" % i, [128, 8], I32) for i in range(2)])
    xg_r = Ring([sb("xg%d" % i, [128, D], BF16) for i in range(NCJ)])
    xgT_r = Ring([sb("xgT%d" % i, [128, KC, CAP], BF16) for i in range(1)])
    hT_r = Ring([sb("hTe%d" % i, [128, 4, CAP], BF16) for i in range(1)])
    ye_r = Ring([sb("ye%d" % i, [128, D], F32) for i in range(2)])

    class St:
        pass

    def prep(e):
        s = St()
        pp, ppb = pslot.next()
        ppv = pp[:, 0:NCJ * 8].rearrange("p (j c) -> p j c", c=8)
        ohs = []
        for i in range(16):
            oh, ohb = oh_r.next()
            fw.op("dve", lambda e_, oh=oh, i=i: e_.tensor_scalar(out=oh[:], in0=iota_c[:], scalar1=posm[:, i, e:e + 1], scalar2=None, op0=ALU.is_equal),
                  reads=[b_iota, b_posm], writes=[ohb])
            for cj in range(NCJ):
                fw.op("pe", lambda e_, oh=oh, i=i, cj=cj: e_.matmul(ppv[:, cj, 0:4], lhsT=oh[:, cj * 128:(cj + 1) * 128], rhs=TW[:, i, e, :],
                      start=(i == 0 and cj == 0), stop=(i == 15), skip_group_check=True), reads=[ohb, b_TW], writes=[ppb], partial=not (i == 0 and cj == 0))
        sl, slb = sl_r.next()
        sli, slib = sli_r.next()
        fw.op("dve", lambda e_: e_.tensor_copy(out=sl[:, 32:32 + NCJ * 8], in_=pp[:, 0:NCJ * 8]), reads=[ppb], writes=[slb])
        rv = sl[:, 32:32 + NCJ * 8].rearrange("p (j c) -> p j c", c=8)
        fw.op("dve", lambda e_: e_.scalar_tensor_tensor(out=sl[:, 0:NCJ], in0=rv[:, :, 0], scalar=16.0, in1=rv[:, :, 1], op0=ALU.mult, op1=ALU.add), reads=[slb], writes=[slb])
        fw.op("dve", lambda e_: e_.tensor_tensor(out=sl[:, 8:8 + NCJ], in0=rv[:, :, 2], in1=rv[:, :, 3], op=ALU.add), reads=[slb], writes=[slb])
        fw.op("dve", lambda e_: e_.tensor_scalar(out=sl[:, 16:16 + NCJ], in0=sl[:, 8:8 + NCJ], scalar1=0.0, scalar2=None, op0=ALU.is_gt), reads=[slb], writes=[slb])
        fw.op("dve", lambda e_: e_.tensor_scalar(out=sl[:, 16:16 + NCJ], in0=sl[:, 16:16 + NCJ], scalar1=-BIG, scalar2=BIG, op0=ALU.mult, op1=ALU.add), reads=[slb], writes=[slb])
        fw.op("dve", lambda e_: e_.tensor_tensor(out=sl[:, 0:NCJ], in0=sl[:, 0:NCJ], in1=sl[:, 16:16 + NCJ], op=ALU.add), reads=[slb], writes=[slb])
        fw.op("dve", lambda e_: e_.tensor_copy(out=sli[:, 0:NCJ], in_=sl[:, 0:NCJ]), reads=[slb], writes=[slib])
        s.xgs = []
        for cj in range(NCJ):
            xg, xgb = xg_r.next()

            def _g(e_, xg=xg, cj=cj):
                return e_.indirect_dma_start(out=xg[:, :], out_offset=None, in_=cx.h2_d[:, :],
                                             in_offset=bass.IndirectOffsetOnAxis(ap=sli[:, cj:cj + 1], axis=0), bounds_check=fw.reg(e_, T - 1), oob_is_err=False)
            fw.dma("pool", _g, reads=[slib, cx.b_h2d], writes=[xgb])
            s.xgs.append((xg, xgb))
        s.sl, s.slb, s.sli, s.slib = sl, slb, sli, slib
        return s

    def prep_b(s):
        xgT, xgTb = xgT_r.next()
        for cj in range(NCJ):
            xg, xgb = s.xgs[cj]
            for half in range(2):
                ptt, ptb = pT.next()
                for k8 in range(8):
                    kc = half * 8 + k8
                    fw.op("pe", lambda e_, ptt=ptt, xg=xg, kc=kc, k8=k8: e_.transpose(out=ptt[:, k8 * 128:(k8 + 1) * 128], in_=xg[:, kc * 128:(kc + 1) * 128], identity=ident_b[:]),
                          reads=[xgb, b_idb], writes=[ptb], partial=(k8 > 0))
                fw.op("dve", lambda e_, ptt=ptt, half=half, cj=cj: e_.tensor_copy(out=xgT[:, half * 8:(half + 1) * 8, cj * 128:(cj + 1) * 128],
                      in_=ptt[:, :].rearrange("p (k c) -> p k c", k=8)), reads=[ptb], writes=[xgTb], partial=not (cj == 0 and half == 0))
        s.xgT, s.xgTb = xgT, xgTb

    scat_prev = [list(b_yd.w.items())]
    scat_cur = []

    def compute(e, s, W):
        wg, wgb, wu, wub, wd, wdb = W
        dvw = dn_view(wd)
        hT, hTb = hT_r.next()
        ffn_hidden(wg, wgb, wu, wub, lambda kc, c0, cn: s.xgT[:, kc, c0:c0 + cn], s.xgTb, CAP, hT, hTb)
        for cj in range(NCJ):
            ye, yeb = ye_r.next()
            for n in range(4):
                pyt, pyb = py.next()
                for fc in range(4):
                    fw.op("pe", lambda e_, pyt=pyt, fc=fc, cj=cj, n=n: e_.matmul(pyt[:, :], lhsT=hT[:, fc, cj * 128:(cj + 1) * 128], rhs=dvw[:, n, fc, :],
                          start=(fc == 0), stop=(fc == 3)), reads=[hTb, wdb], writes=[pyb])
                fw.op("dve", lambda e_, pyt=pyt, ye=ye, n=n, cj=cj: e_.tensor_scalar(out=ye[:, n * 512:(n + 1) * 512], in0=pyt[:, :], scalar1=s.sl[:, 8 + cj:9 + cj], scalar2=None, op0=ALU.mult),
                      reads=[pyb, s.slb], writes=[yeb], partial=(n > 0))
            tk = fw.dma("pool", lambda e_, ye=ye, cj=cj: e_.indirect_dma_start(out=cx.y_d[:, :], out_offset=bass.IndirectOffsetOnAxis(ap=s.sli[:, cj:cj + 1], axis=0),
                        in_=ye[:, :], in_offset=None, bounds_check=fw.reg(e_, T - 1), oob_is_err=False, compute_op=ALU.add), reads=[yeb, s.slib, b_yd], writes=[b_yd])
            scat_cur.append(tk)
        scat_prev[0] = list(scat_cur)
        del scat_cur[:]

    nxt = prep(0)
    Wn = load3(0)
    for e in range(n_routed):
        cur, W = nxt, Wn
        prep_b(cur)
        if e + 1 < n_routed:
            nxt = prep(e + 1)
            Wn = load3(e + 1)
        compute(e, cur, W)
    fw.barrier()
    fw.flush()
    stack.pop().close()

    stack.append(ExitStack())
    yin = Ring([sb("yin%d" % i, [128, D], F32) for i in range(2)])
    xt_r = Ring([sb("xf%d" % i, [128, D], F32) for i in range(2)])
    st_r = Ring([sb("stf%d" % i, [128, 8], F32) for i in range(2)])
    cx.junk_f, cx.b_junk_f = sb("junk_f2", [128, D], BF16), Buf()
    b_out = Buf()
    cx.b_x1d = b_out

    def fin(i):
        yt, ytb = yin.next()
        fw.dma("sp", lambda e: e.dma_start(out=yt[:], in_=cx.y_d[i * 128:(i + 1) * 128, :]), reads=[b_yd], writes=[ytb])
        post_norm_residual(fw, cx, i, yt, ytb, xt_r, st_r, cx.x1_d, Grow[1], b_Grow[1], cx.out)
    for i in range(16):
        fin(i)
    cx.b_out = b_out
    fw.finish([cx.b_out])
    stack.pop().close()


from contextlib import ExitStack
from concourse.bass_utils import run_bass_kernel_spmd


def build_program(nc):
    cx = Ctx()
    declare_io_e(nc, cx); declare_io_a(nc, cx); declare_io_b(nc, cx); declare_io_c(nc, cx); declare_io_d(nc, cx)
    fw = FW(nc)
    with ExitStack() as gs:
        cx.Wr = gs.enter_context(nc.sbuf_tensor("Wr", [128, 16, NE], F32))
        cx.b_Wr, cx.b_h2Td, cx.b_x1d = Buf(), Buf(), Buf()
        stages = (lambda es: stage_abc(nc, fw, cx, es, gs=gs), lambda es: stage_nsa(nc, fw, cx, es), lambda es: stage_mlstm(nc, fw, cx, es),
                  lambda es: stage_merge(nc, fw, cx, es), lambda es: stage_router(nc, fw, cx, es), lambda es: stage_moe_sparse(nc, fw, cx, es))
        for fn in stages:
            with ExitStack() as es:
                fn(es)
                fw.barrier()
                fw.flush()
    return cx, fw


def shared_inputs(inp):
    P = {k: np.asarray(v)[0] for k, v in inp.items() if k not in ("x", "c")}
    col = lambda v: np.ascontiguousarray(v.reshape(16, 128).T)
    m = {}
    m["w_ada_p"] = pack_cols(P["w_ada"], [(c, 512) for c in range(0, 12288, 512)])[0]
    m["b_ada"] = P["b_ada"].reshape(1, -1)
    m["g4"] = np.concatenate([col(P["g_pre_mix"]), col(P["g_pre_ffn"]), np.zeros((128, 32), np.float32)], axis=1)
    m["gpost"] = np.stack([P["g_post_mix"], P["g_post_ffn"], P["g_pre_ffn"]])
    m["w_in_p"] = pack_cols(P["w_in"], win_chunks())[0]
    m["ident"] = np.eye(128, dtype=np.float32)
    m.update(nsa_consts())
    m["cmp_w1"] = P["cmp_w1"]
    m["cmp_w2"] = P["cmp_w2"]
    m["peT"] = np.ascontiguousarray(P["cmp_pe"].transpose(0, 2, 1))
    m.update(mlstm_consts())
    cw = np.concatenate([P["conv_w"], P["conv_b"][None, :]], 0)
    m["convp"] = np.ascontiguousarray(cw.reshape(5, 8, 128).transpose(2, 1, 0))
    m["bgate"] = np.ascontiguousarray(P["b_gates_m"].reshape(2, 4).T)
    m["mhg"] = P["mh_norm_g"].reshape(1, -1)
    ch = [(c, 512) for c in range(0, 2048, 512)]
    m["w_up_p"] = np.concatenate([pack_cols(P["w_up_nsa"], ch)[0], pack_cols(P["w_up_mlstm"], ch)[0]])
    m["w_out_p"] = pack_cols(P["w_out"], [(c, 128) for c in range(0, 2048, 128)])[0]
    m["w_router"] = P["w_router"]
    m["b_router"] = P["b_router"].reshape(1, 64)
    m.update(moe_consts())
    m["w_eg_p"] = np.concatenate([pack_gu4(P["w_e_gate"][e]) for e in range(64)] + [pack_gu4(P["w_sh_gate"])])
    m["w_eu_p"] = np.concatenate([pack_gu4(P["w_e_up"][e]) for e in range(64)] + [pack_gu4(P["w_sh_up"])])
    m["w_ed_p"] = np.concatenate([pack_dn4(P["w_e_down"][e]) for e in range(64)] + [pack_dn4(P["w_sh_down"])])
    return m


def kernel(**inputs):
    inp = {k: np.asarray(v) for k, v in inputs.items()}
    nc = bass.Bass("TRN2", target_bir_lowering=False)
    build_program(nc)
    sh = shared_inputs(inp)
    in_maps = []
    for b in range(8):
        m = dict(sh)
        m["x"] = np.ascontiguousarray(inp["x"][b])
        m["cT"] = np.ascontiguousarray(inp["c"][b].reshape(16, 128).T)
        in_maps.append(m)
    res = run_bass_kernel_spmd(nc, in_maps, core_ids=list(range(8)))
    return np.stack([np.asarray(r["out"]) for r in res.results], axis=0).astype(np.float32)
```
